# Optimizing a Trainium2 kernel written in Bass

```python
import math
import jax
import jax.numpy as jnp
from jax import lax
import numpy as np

D_MODEL = 1024
BATCH = 16
SEQ = 2048
DEPTH = 4
DEC_BATCH = 128
DEC_SEQ = 1
PAST_LEN = 8192
PAGE_SIZE = 128

N_MIXERS = 4
EXPAND = 2
D_BRANCH = EXPAND * D_MODEL
RMS_EPS = 1e-6
LN_EPS = 1e-5
CONV_W = 31
RWKV_HEAD = 64
RWKV_HEADS = D_BRANCH // RWKV_HEAD
DECAY_LORA = 64
ICLR_LORA = 64
GN_EPS = 64e-5
MLA_HEADS = 16
QK_NOPE = 128
QK_ROPE = 64
V_HEAD = D_BRANCH // MLA_HEADS
KV_LORA = D_MODEL // 4
Q_LORA = 3 * D_MODEL // 8
ROPE_THETA = 10000.0
MLA_SCALE = (QK_NOPE + QK_ROPE) ** -0.5
Q_BLOCK = 128
NEG_INF = -1e30
GROUP_CH = 16
N_GROUPS = D_BRANCH // GROUP_CH
SSM_STATE = 64
DT_MIN = 0.001
DT_MAX = 0.1

kernel_name = 'hybrid_conv_rwkv7_mla_s5_step'


def rmsnorm(x, g):
    xf = x.astype(jnp.float32)
    y = xf * lax.rsqrt(jnp.mean(xf * xf, axis=-1, keepdims=True) + RMS_EPS)
    return (y * g.astype(jnp.float32)).astype(x.dtype)


def layernorm(x, g, b):
    xf = x.astype(jnp.float32)
    mu = jnp.mean(xf, axis=-1, keepdims=True)
    var = jnp.mean(jnp.square(xf - mu), axis=-1, keepdims=True)
    y = (xf - mu) * lax.rsqrt(var + LN_EPS)
    return (y * g.astype(jnp.float32) + b.astype(jnp.float32)).astype(x.dtype)


def rope(x, pos):
    half = QK_ROPE // 2
    inv_freq = ROPE_THETA ** (-jnp.arange(half, dtype=jnp.float32) / half)
    ang = pos.astype(jnp.float32)[:, None] * inv_freq[None, :]
    cos = jnp.cos(ang)[:, None, :]
    sin = jnp.sin(ang)[:, None, :]
    x1 = x[..., :half].astype(jnp.float32)
    x2 = x[..., half:].astype(jnp.float32)
    return jnp.concatenate([x1 * cos - x2 * sin, x1 * sin + x2 * cos], axis=-1).astype(x.dtype)


def conv_branch(h, buf, w_in, b_in, conv_w, conv_b, ln_g, ln_b, w_out):
    ga, gb, z = jnp.split(h @ w_in + b_in, 3, axis=-1)
    u = ga * jax.nn.sigmoid(gb)
    if buf is None:
        buf = jnp.zeros((u.shape[0], CONV_W - 1, u.shape[-1]), u.dtype)
    ext = jnp.concatenate([buf.astype(u.dtype), u], axis=1)
    c = lax.conv_general_dilated(ext, conv_w[:, None, :], window_strides=(1,), padding='VALID',
                                 dimension_numbers=('NWC', 'WIO', 'NWC'),
                                 feature_group_count=u.shape[-1]) + conv_b
    c = jax.nn.silu(layernorm(c, ln_g, ln_b))
    out = (c * jax.nn.silu(z)) @ w_out
    return out, ext[:, -(CONV_W - 1):]


def rwkv7_branch(h, shift, wkv, mu, w_rkvz, w0, w1, w2, a0, a1, a2, k_k, k_a, r_k, ln_g, ln_b, w_out):
    bsz, t, _ = h.shape
    if shift is None:
        shift = jnp.zeros((bsz, h.shape[-1]), h.dtype)
    if wkv is None:
        wkv = jnp.zeros((bsz, RWKV_HEADS, RWKV_HEAD, RWKV_HEAD), jnp.float32)
    h_prev = jnp.concatenate([shift[:, None, :].astype(h.dtype), h[:, :-1]], axis=1)
    xs = h[:, :, None, :] + (h_prev - h)[:, :, None, :] * mu
    rkvz = jnp.einsum('btjd,jde->btje', xs[:, :, :4], w_rkvz)
    r, k, v, z = rkvz[:, :, 0], rkvz[:, :, 1], rkvz[:, :, 2], rkvz[:, :, 3]
    xw, xa = xs[:, :, 4], xs[:, :, 5]
    w_log = -jax.nn.softplus(-(w0 + jnp.tanh(xw @ w1) @ w2)) - 0.5
    decay = jnp.exp(-jnp.exp(w_log.astype(jnp.float32)))
    a = jax.nn.sigmoid(a0 + (xa @ a1) @ a2)

    def heads(y):
        return y.reshape(bsz, t, RWKV_HEADS, RWKV_HEAD)

    kkf = heads(k * k_k).astype(jnp.float32)
    kk = kkf / jnp.maximum(jnp.sqrt(jnp.sum(kkf * kkf, axis=-1, keepdims=True)), 1e-12)
    k = k * (1.0 + (a - 1.0) * k_a)
    rh = heads(r).astype(jnp.float32)
    kh = heads(k).astype(jnp.float32)
    vh = heads(v).astype(jnp.float32)
    bv = kk * heads(a).astype(jnp.float32)

    def step(S, inp):
        r_t, w_t, k_t, v_t, kk_t, b_t = inp
        sa = jnp.einsum('bhij,bhj->bhi', S, -kk_t)
        S = S * w_t[:, :, None, :] + sa[..., None] * b_t[:, :, None, :] + v_t[..., None] * k_t[:, :, None, :]
        return S, jnp.einsum('bhij,bhj->bhi', S, r_t)

    seq = tuple(jnp.moveaxis(y, 1, 0) for y in (rh, heads(decay), kh, vh, kk, bv))
    s_fin, y = lax.scan(step, wkv.astype(jnp.float32), seq)
    y = jnp.moveaxis(y, 0, 1)
    mu_y = jnp.mean(y, axis=-1, keepdims=True)
    var_y = jnp.mean(jnp.square(y - mu_y), axis=-1, keepdims=True)
    y = ((y - mu_y) * lax.rsqrt(var_y + GN_EPS)).reshape(bsz, t, D_BRANCH)
    y = y * ln_g.astype(jnp.float32) + ln_b.astype(jnp.float32)
    bonus = jnp.sum(rh * kh * r_k.astype(jnp.float32), axis=-1, keepdims=True) * vh
    y = (y + bonus.reshape(bsz, t, D_BRANCH)).astype(h.dtype)
    out = (y * jax.nn.silu(z)) @ w_out
    return out, h[:, -1], s_fin


def mla_attend(q_lat, q_rope, ckv, kr, q_pos, k_pos):
    s = jnp.einsum('bthc,blc->bhtl', q_lat, ckv, preferred_element_type=jnp.float32)
    s = s + jnp.einsum('bthr,blr->bhtl', q_rope, kr, preferred_element_type=jnp.float32)
    s = jnp.where(k_pos[None, :] <= q_pos[:, None], s * MLA_SCALE, NEG_INF)
    p = jax.nn.softmax(s, axis=-1)
    return jnp.einsum('bhtl,blc->bthc', p.astype(ckv.dtype), ckv)


def mla_branch(h, pos, past_ckv, past_kr, w_in, q_norm, kv_norm, w_uq, w_uk, w_uv, w_out):
    bsz, t, _ = h.shape
    cq, ckv, kr, z = jnp.split(h @ w_in, [Q_LORA, Q_LORA + KV_LORA, Q_LORA + KV_LORA + QK_ROPE], axis=-1)
    q = jnp.einsum('btq,qhd->bthd', rmsnorm(cq, q_norm), w_uq)
    q_lat = jnp.einsum('bthn,chn->bthc', q[..., :QK_NOPE], w_uk)
    q_rope = rope(q[..., QK_NOPE:], pos)
    ckv = rmsnorm(ckv, kv_norm)
    kr = rope(kr[:, :, None, :], pos)[:, :, 0]
    if past_ckv is None:
        nblk = t // Q_BLOCK

        def to_blocks(y):
            return jnp.moveaxis(y.reshape((bsz, nblk, Q_BLOCK) + y.shape[2:]), 1, 0)

        o_lat = lax.map(lambda blk: mla_attend(blk[0], blk[1], ckv, kr, blk[2], pos),
                        (to_blocks(q_lat), to_blocks(q_rope), pos.reshape(nblk, Q_BLOCK)))
        o_lat = jnp.moveaxis(o_lat, 0, 1).reshape(bsz, t, MLA_HEADS, KV_LORA)
    else:
        keys_c = jnp.concatenate([past_ckv.astype(ckv.dtype), ckv], axis=1)
        keys_r = jnp.concatenate([past_kr.astype(kr.dtype), kr], axis=1)
        k_pos = jnp.arange(keys_c.shape[1], dtype=jnp.int32)
        o_lat = mla_attend(q_lat, q_rope, keys_c, keys_r, pos, k_pos)
    o = jnp.einsum('bthc,chv->bthv', o_lat, w_uv).reshape(bsz, t, D_BRANCH)
    out = (o * jax.nn.silu(z)) @ w_out
    return out, ckv, kr


def s5_branch(h, s0_re, s0_im, w_in, lam_re, lam_im, log_dt, b_re, b_im, c_re, c_im, d_skip, w_glu, b_glu, w_out):
    bsz, t, _ = h.shape
    u, z = jnp.split(h @ w_in, 2, axis=-1)
    lr = lam_re.astype(jnp.float32)
    li = lam_im.astype(jnp.float32)
    dt = jnp.exp(log_dt.astype(jnp.float32))[:, None]
    mag = jnp.exp(lr * dt)
    ab_re = mag * jnp.cos(li * dt)
    ab_im = mag * jnp.sin(li * dt)
    den = lr * lr + li * li
    nr = ab_re - 1.0
    f_re = (nr * lr + ab_im * li) / den
    f_im = (ab_im * lr - nr * li) / den
    br = b_re.astype(jnp.float32)
    bi = b_im.astype(jnp.float32)
    bb_re = f_re[..., None] * br - f_im[..., None] * bi
    bb_im = f_re[..., None] * bi + f_im[..., None] * br
    uf = u.astype(jnp.float32)
    ug = uf.reshape(bsz, t, N_GROUPS, GROUP_CH)
    bu_re = jnp.einsum('btgc,gpc->btgp', ug, bb_re)
    bu_im = jnp.einsum('btgc,gpc->btgp', ug, bb_im)
    a_re = jnp.broadcast_to(ab_re, (1, t, N_GROUPS, SSM_STATE))
    a_im = jnp.broadcast_to(ab_im, (1, t, N_GROUPS, SSM_STATE))

    def combine(e1, e2):
        a1r, a1i, b1r, b1i = e1
        a2r, a2i, b2r, b2i = e2
        return (a2r * a1r - a2i * a1i, a2r * a1i + a2i * a1r,
                a2r * b1r - a2i * b1i + b2r, a2r * b1i + a2i * b1r + b2i)

    p_re, p_im, s_re, s_im = lax.associative_scan(combine, (a_re, a_im, bu_re, bu_im), axis=1)
    if s0_re is not None:
        h0r = s0_re.astype(jnp.float32)[:, None]
        h0i = s0_im.astype(jnp.float32)[:, None]
        s_re, s_im = s_re + p_re * h0r - p_im * h0i, s_im + p_re * h0i + p_im * h0r
    y = (jnp.einsum('btgp,gcp->btgc', s_re, c_re.astype(jnp.float32))
         - jnp.einsum('btgp,gcp->btgc', s_im, c_im.astype(jnp.float32)))
    y = y.reshape(bsz, t, D_BRANCH) + d_skip.astype(jnp.float32) * uf
    g = jax.nn.gelu(y).astype(h.dtype)
    y = g * jax.nn.sigmoid(g @ w_glu + b_glu)
    out = (y * jax.nn.silu(z)) @ w_out
    return out, s_re[:, -1], s_im[:, -1]


def setup_inputs(seed: int = 0) -> dict:
    key = jax.random.key(seed)
    ks = iter(jax.random.split(key, 80))
    f32 = jnp.float32
    E = D_BRANCH
    D = D_MODEL

    def nrm(shape, scale):
        return scale * jax.random.normal(next(ks), shape, f32)

    def gain(shape):
        return 1.0 + nrm(shape, 0.01)

    n_pages = PAST_LEN // PAGE_SIZE
    n_used = DEC_BATCH * n_pages
    n_pool = n_used + max(1, n_used // 4)
    page_table = jax.random.permutation(next(ks), n_pool)[:n_used].reshape(DEC_BATCH, n_pages).astype(jnp.int32)
    return {
        'x_prompt': nrm((BATCH, SEQ, D), 1.0),
        'x_sample': nrm((DEC_BATCH, DEC_SEQ, D), 1.0),
        'state_conv': nrm((DEC_BATCH, CONV_W - 1, E), 0.5),
        'state_shift': nrm((DEC_BATCH, D), 1.0),
        'state_wkv': nrm((DEC_BATCH, RWKV_HEADS, RWKV_HEAD, RWKV_HEAD), 0.3),
        'cache_ckv': nrm((n_pool, PAGE_SIZE, KV_LORA), 1.0),
        'cache_krope': nrm((n_pool, PAGE_SIZE, QK_ROPE), 1.0),
        'state_ssm_re': nrm((DEC_BATCH, N_GROUPS, SSM_STATE), 0.5),
        'state_ssm_im': nrm((DEC_BATCH, N_GROUPS, SSM_STATE), 0.5),
        'page_table': page_table,
        'norm_pre': gain((DEPTH, D)),
        'norm_post': gain((DEPTH, D)),
        'a_w_in': nrm((D, 3 * E), D ** -0.5),
        'a_b_in': nrm((3 * E,), 0.01),
        'a_conv_w': nrm((CONV_W, E), CONV_W ** -0.5),
        'a_conv_b': nrm((E,), 0.01),
        'a_ln_g': gain((E,)),
        'a_ln_b': nrm((E,), 0.01),
        'a_w_out': nrm((E, D), E ** -0.5),
        'b_mu': jax.random.uniform(next(ks), (6, D), f32),
        'b_w_rkvz': nrm((4, D, E), D ** -0.5),
        'b_w0': -1.0 + nrm((E,), 0.5),
        'b_w1': nrm((D, DECAY_LORA), D ** -0.5),
        'b_w2': nrm((DECAY_LORA, E), 0.1 * DECAY_LORA ** -0.5),
        'b_a0': nrm((E,), 0.1),
        'b_a1': nrm((D, ICLR_LORA), D ** -0.5),
        'b_a2': nrm((ICLR_LORA, E), 0.1 * ICLR_LORA ** -0.5),
        'b_k_k': 0.85 + nrm((E,), 0.05),
        'b_k_a': 1.0 + nrm((E,), 0.05),
        'b_r_k': nrm((RWKV_HEADS, RWKV_HEAD), 0.1),
        'b_ln_g': gain((E,)),
        'b_ln_b': nrm((E,), 0.01),
        'b_w_out': nrm((E, D), E ** -0.5),
        'c_w_in': nrm((D, Q_LORA + KV_LORA + QK_ROPE + E), D ** -0.5),
        'c_q_norm': gain((Q_LORA,)),
        'c_kv_norm': gain((KV_LORA,)),
        'c_w_uq': nrm((Q_LORA, MLA_HEADS, QK_NOPE + QK_ROPE), Q_LORA ** -0.5),
        'c_w_uk': nrm((KV_LORA, MLA_HEADS, QK_NOPE), KV_LORA ** -0.5),
        'c_w_uv': nrm((KV_LORA, MLA_HEADS, V_HEAD), KV_LORA ** -0.5),
        'c_w_out': nrm((E, D), E ** -0.5),
        'd_w_in': nrm((D, 2 * E), D ** -0.5),
        'd_lambda_re': -0.5 + nrm((N_GROUPS, SSM_STATE), 0.01),
        'd_lambda_im': math.pi * jnp.arange(SSM_STATE, dtype=f32)[None, :] + nrm((N_GROUPS, SSM_STATE), 0.01),
        'd_log_dt': jax.random.uniform(next(ks), (N_GROUPS,), f32, math.log(DT_MIN), math.log(DT_MAX)),
        'd_b_re': nrm((N_GROUPS, SSM_STATE, GROUP_CH), (2 * GROUP_CH) ** -0.5),
        'd_b_im': nrm((N_GROUPS, SSM_STATE, GROUP_CH), (2 * GROUP_CH) ** -0.5),
        'd_c_re': nrm((N_GROUPS, GROUP_CH, SSM_STATE), (2 * SSM_STATE) ** -0.5),
        'd_c_im': nrm((N_GROUPS, GROUP_CH, SSM_STATE), (2 * SSM_STATE) ** -0.5),
        'd_d': nrm((E,), 1.0),
        'd_w_glu': nrm((E, E), E ** -0.5),
        'd_b_glu': nrm((E,), 0.01),
        'd_w_out': nrm((E, D), E ** -0.5),
    }


def reference(x_prompt, x_sample, state_conv, state_shift, state_wkv, cache_ckv, cache_krope,
              state_ssm_re, state_ssm_im, page_table, norm_pre, norm_post,
              a_w_in, a_b_in, a_conv_w, a_conv_b, a_ln_g, a_ln_b, a_w_out,
              b_mu, b_w_rkvz, b_w0, b_w1, b_w2, b_a0, b_a1, b_a2, b_k_k, b_k_a, b_r_k, b_ln_g, b_ln_b, b_w_out,
              c_w_in, c_q_norm, c_kv_norm, c_w_uq, c_w_uk, c_w_uv, c_w_out,
              d_w_in, d_lambda_re, d_lambda_im, d_log_dt, d_b_re, d_b_im, d_c_re, d_c_im, d_d,
              d_w_glu, d_b_glu, d_w_out):
    dec_b = x_sample.shape[0]
    pos_p = jnp.arange(x_prompt.shape[1], dtype=jnp.int32)
    pos_s = PAST_LEN + jnp.arange(x_sample.shape[1], dtype=jnp.int32)
    xp, xs = x_prompt, x_sample
    for i in range(DEPTH):
        hp = rmsnorm(xp, norm_pre[i])
        hs = rmsnorm(xs, norm_pre[i])
        mixer = i % N_MIXERS
        if mixer == 0:
            wa = (a_w_in, a_b_in, a_conv_w, a_conv_b, a_ln_g, a_ln_b, a_w_out)
            op, conv_p = conv_branch(hp, None, *wa)
            osm, conv_s = conv_branch(hs, state_conv, *wa)
        elif mixer == 1:
            wb = (b_mu, b_w_rkvz, b_w0, b_w1, b_w2, b_a0, b_a1, b_a2, b_k_k, b_k_a, b_r_k, b_ln_g, b_ln_b, b_w_out)
            op, shift_p, wkv_p = rwkv7_branch(hp, None, None, *wb)
            osm, shift_s, wkv_s = rwkv7_branch(hs, state_shift, state_wkv, *wb)
        elif mixer == 2:
            wc = (c_w_in, c_q_norm, c_kv_norm, c_w_uq, c_w_uk, c_w_uv, c_w_out)
            past_ckv = cache_ckv[page_table].reshape(dec_b, PAST_LEN, KV_LORA)
            past_kr = cache_krope[page_table].reshape(dec_b, PAST_LEN, QK_ROPE)
            op, ckv_p, kr_p = mla_branch(hp, pos_p, None, None, *wc)
            osm, ckv_s, kr_s = mla_branch(hs, pos_s, past_ckv, past_kr, *wc)
        else:
            wd = (d_w_in, d_lambda_re, d_lambda_im, d_log_dt, d_b_re, d_b_im, d_c_re, d_c_im, d_d,
                  d_w_glu, d_b_glu, d_w_out)
            op, ssm_re_p, ssm_im_p = s5_branch(hp, None, None, *wd)
            osm, ssm_re_s, ssm_im_s = s5_branch(hs, state_ssm_re, state_ssm_im, *wd)
        xp = xp + rmsnorm(op, norm_post[i])
        xs = xs + rmsnorm(osm, norm_post[i])
    return (xp, xs, conv_p, conv_s, shift_p, shift_s, wkv_p, wkv_s, ckv_p, ckv_s, kr_p, kr_s,
            ssm_re_p, ssm_re_s, ssm_im_p, ssm_im_s)
```

```python
from concourse.bass_utils import run_bass_kernel_spmd
import numpy as np
import concourse.bass as bass
import concourse.mybir as mybir

F32 = mybir.dt.float32
BF16 = mybir.dt.bfloat16
I32 = mybir.dt.int32
AF = mybir.ActivationFunctionType
ALU = mybir.AluOpType
AX = mybir.AxisListType

NDS = 6
ENG = ['pe', 'act', 'dve', 'pool', 'sp']
BLK = {'pe': 'tensor', 'act': 'scalar', 'dve': 'vector', 'pool': 'gpsimd', 'sp': 'sync'}


class Buf:
    __slots__ = ('name', 'w', 'r', 'parent', 'kids')

    def __init__(s, name, parent=None):
        s.name = name
        s.w = None
        s.r = []
        s.parent = parent
        s.kids = {}

    def sub(s, key):
        k = s.kids.get(key)
        if k is None:
            k = Buf(f'{s.name}.{key}', s)
            s.kids[key] = k
        return k

    def family(s):
        out = [s]
        p = s.parent
        while p is not None:
            out.append(p)
            p = p.parent
        if s.kids:
            st = list(s.kids.values())
            while st:
                k = st.pop()
                out.append(k)
                if k.kids:
                    st.extend(k.kids.values())
        return out


class Op:
    __slots__ = ('eng', 'fn', 'waits', 'idx', 'inc', 'dma', 'semk', 'val', 'seq')


class Prog:
    def __init__(s):
        s.q = {e: [] for e in ENG}
        s.seen = {e: {} for e in ENG}
        s.rr = {e: 0 for e in ENG}
        s.dlast = {}
        s.dcount = {}
        s.alldma = []

    def op(s, eng, fn, reads=(), writes=(), dma=False):
        if getattr(s, "dead", False):
            return None
        o = Op()
        o.eng = eng
        o.fn = fn
        o.dma = dma
        o.inc = bool(dma)
        o.waits = []
        o.idx = len(s.q[eng])
        o.semk = None
        o.val = None
        o.seq = None
        deps = {}
        for b in reads:
            for f in b.family():
                if f.w is not None:
                    deps[id(f.w)] = f.w
        for b in writes:
            for f in b.family():
                if f.w is not None:
                    deps[id(f.w)] = f.w
                for r in f.r:
                    deps[id(r)] = r
        if dma:
            k = s.rr[eng] % NDS
            s.rr[eng] += 1
            o.semk = k
            prev = s.dlast.get((eng, k))
            if prev is not None:
                deps[id(prev)] = prev
            s.dlast[(eng, k)] = o
            o.seq = s.dcount.get((eng, k), 0) + 1
            s.dcount[(eng, k)] = o.seq
            s.alldma.append(o)
        seen = s.seen[eng]
        for d in deps.values():
            if d.dma:
                key = (d.eng, d.semk)
                v = d.seq
            else:
                if d.eng == eng and eng == 'pe':
                    continue
                key = d.eng
                v = d.idx
            if seen.get(key, -1) >= v:
                continue
            seen[key] = v
            o.waits.append(d)
            d.inc = True
        for b in reads:
            b.r.append(o)
        for b in writes:
            b.w = o
            b.r = []
        s.q[eng].append(o)
        return o

    def chk(s, name):
        import os
        if os.environ.get("KSTOP", "") == name:
            s.dead = True

    def barrier(s):
        if getattr(s, "dead", False):
            return
        lasts = []
        for e in ENG:
            for o in reversed(s.q[e]):
                if o.fn is not None and not o.dma:
                    lasts.append(o)
                    break
        dl = list(s.dlast.values())
        for e in ENG:
            o = Op()
            o.eng = e
            o.fn = None
            o.dma = False
            o.inc = False
            o.waits = []
            o.idx = len(s.q[e])
            o.semk = None
            o.val = None
            o.seq = None
            seen = s.seen[e]
            for d in lasts + dl:
                if d.dma:
                    key = (d.eng, d.semk)
                    v = d.seq
                else:
                    if d.eng == e:
                        continue
                    key = d.eng
                    v = d.idx
                if seen.get(key, -1) >= v:
                    continue
                seen[key] = v
                o.waits.append(d)
                d.inc = True
            s.q[e].append(o)

    def finish(s, eng='sp'):
        o = Op()
        o.eng = eng
        o.fn = None
        o.dma = False
        o.inc = False
        o.waits = list(s.dlast.values())
        o.idx = len(s.q[eng])
        s.q[eng].append(o)

    def emit(s, nc):
        sems = {}
        for e in ENG:
            sems[e] = nc.alloc_semaphore(f'S_{e}')
            for k in range(NDS):
                sems[(e, k)] = nc.alloc_semaphore(f'D_{e}_{k}')
        for e in ENG:
            c = 0
            for o in s.q[e]:
                if o.dma:
                    o.val = 16 * o.seq
                elif o.inc:
                    c += 1
                    o.val = c
        n_ins = {e: 0 for e in ENG}
        with nc.Block() as block:
            for e in ENG:
                def body(eng, e=e):
                    for o in s.q[e]:
                        for d in o.waits:
                            sm = sems[(d.eng, d.semk)] if d.dma else sems[d.eng]
                            eng.wait_ge(sm, d.val)
                            n_ins[e] += 1
                        if o.fn is None:
                            continue
                        ins = o.fn(eng)
                        n_ins[e] += 1
                        if o.dma:
                            ins.then_inc(sems[(e, o.semk)], 16)
                        elif o.inc:
                            ins.then_inc(sems[e], 1)
                getattr(block, BLK[e])(body)
        return n_ins

import math
NCORES = 8
T = 2048
D = 1024
E = 2048
NS = 16
TWA = T + NS
SB0 = 16640
SBMAX = 229376

OUT_SHAPES = [
    ("y_p", [2, T, D]), ("y_s", [NS, D]),
    ("conv_p", [2, 30, E]), ("conv_s", [NS, 30, E]),
    ("shift_p", [2, D]), ("shift_s", [NS, D]),
    ("wkv_p", [2, 32, 64, 64]), ("wkv_s", [NS, 32, 64, 64]),
    ("ckv_p", [2, T, 256]), ("ckv_s", [NS, 256]),
    ("kr_p", [2, T, 64]), ("kr_s", [NS, 64]),
    ("sre_p", [2, 128, 64]), ("sre_s", [NS, 128, 64]),
    ("sim_p", [2, 128, 64]), ("sim_s", [NS, 128, 64]),
]

IN_SHAPES = [
    ("xp", [2, T, D], F32), ("xs", [NS, D], F32),
    ("state_conv", [NS, 30, E], F32), ("state_shift", [NS, D], F32),
    ("norm_pre", [4, D], F32), ("norm_post", [4, D], F32),
    ("a_w_in", [D, 3 * E], F32), ("a_b_in", [3 * E], F32), ("a_conv_w", [31, E], F32),
    ("a_conv_b", [E], F32), ("a_ln_g", [E], F32), ("a_ln_b", [E], F32), ("a_w_out", [E, D], F32),
    ("state_wkv", [NS, 32, 64, 64], F32),
    ("b_mu", [6, D], F32), ("b_w_rkvz", [4, D, E], F32), ("b_w0", [E], F32), ("b_w1", [D, 64], F32), ("b_w2", [64, E], F32),
    ("b_a0", [E], F32), ("b_a1", [D, 64], F32), ("b_a2", [64, E], F32), ("b_k_k", [E], F32), ("b_k_a", [E], F32),
    ("b_r_k", [32, 64], F32), ("b_ln_g", [E], F32), ("b_ln_b", [E], F32), ("b_w_out", [E, D], F32),
    ("cache_ckv", [10240, 128, 256], F32), ("cache_krope", [10240, 128, 64], F32), ("page_table", [NS, 64], I32),
    ("c_w_in", [D, 2752], F32), ("c_q_norm", [384], F32), ("c_kv_norm", [256], F32), ("c_w_uq", [384, 16, 192], F32),
    ("c_w_uk", [256, 16, 128], F32), ("c_w_uv", [256, 16, 128], F32), ("c_w_out", [E, D], F32),
    ("state_ssm_re", [NS, 128, 64], F32), ("state_ssm_im", [NS, 128, 64], F32),
    ("d_w_in", [D, 2 * E], F32), ("d_lambda_re", [128, 64], F32), ("d_lambda_im", [128, 64], F32), ("d_log_dt", [128], F32),
    ("d_b_re", [128, 64, 16], F32), ("d_b_im", [128, 64, 16], F32), ("d_c_re", [128, 16, 64], F32), ("d_c_im", [128, 16, 64], F32),
    ("d_d", [E], F32), ("d_w_glu", [E, E], F32), ("d_b_glu", [E], F32), ("d_w_out", [E, D], F32),
]


N_IN_BY_LAYER = {1: 13, 2: 28, 3: 38, 4: 52}


class K:
    pass


def build(n_layers=1):
    nc = bass.Bass("TRN2", target_bir_lowering=False)
    P = Prog()
    k = K()
    k.nc = nc
    k.P = P
    dr = {}
    for name, shp, dt in IN_SHAPES[:N_IN_BY_LAYER[n_layers]]:
        dr[name] = nc.dram_tensor(name, shp, dt, kind="ExternalInput").ap()
    for name, shp in OUT_SHAPES:
        dr[name] = nc.dram_tensor(name, shp, F32, kind="ExternalOutput").ap()
    dr["xscr"] = nc.dram_tensor("xscr", [2, T, D], F32, kind="Internal").ap()
    dr["xscr_s"] = nc.dram_tensor("xscr_s", [NS, D], F32, kind="Internal").ap()
    dr["xscr2"] = nc.dram_tensor("xscr2", [2, T, D], F32, kind="Internal").ap()
    dr["xscr2_s"] = nc.dram_tensor("xscr2_s", [NS, D], F32, kind="Internal").ap()
    dr["gscr"] = nc.dram_tensor("gscr", [16, 128, TWA], BF16, kind="Internal").ap()
    k.dr = dr
    k.dbuf = {n: Buf("dram_" + n) for n in dr}

    off = [SB0]

    def sb(name, shape, dt, at=None):
        esz = 2 if dt == BF16 else 4
        nbytes = int(np.prod(shape[1:])) * esz
        if at is None:
            o = off[0]
            off[0] += (nbytes + 63) // 64 * 64
            assert off[0] <= SBMAX, (name, off[0])
        else:
            o = at
        t = nc.alloc_sbuf_tensor_at(name, shape, dt, offset=o)
        return t, Buf(name), o

    k.sb = sb
    k.ps = []
    for i in range(7):
        k.ps.append((nc.alloc_psum_tensor(f"ps{i}", [128, 512], F32), Buf(f"ps{i}")))
    k.psT = (nc.alloc_psum_tensor("psT", [128, 1024], BF16), Buf("psT"))
    k.ps_i = [0]

    def psn():
        i = k.ps_i[0] % 7
        k.ps_i[0] += 1
        return k.ps[i]
    k.psn = psn

    k.identF, k.identF_b, _ = sb("identF", [128, 128], F32)
    k.identB, k.identB_b, _ = sb("identB", [128, 128], BF16)
    k.onesB, k.onesB_b, _ = sb("onesB", [128, 128], BF16)
    k.epsr, k.epsr_b, _ = sb("epsr", [128, 4], F32)
    P.op('pool', lambda e: e.memset(k.identF[:], 0.0), writes=[k.identF_b])
    P.op('pool', lambda e: e.affine_select(out=k.identF[:], in_=k.identF[:], pattern=[[-1, 128]],
                                           compare_op=ALU.not_equal, fill=1.0, base=0, channel_multiplier=1),
         reads=[k.identF_b], writes=[k.identF_b])
    P.op('dve', lambda e: e.tensor_copy(out=k.identB[:], in_=k.identF[:]), reads=[k.identF_b], writes=[k.identB_b])
    P.op('dve', lambda e: e.memset(k.onesB[:], 1.0), writes=[k.onesB_b])
    P.op('dve', lambda e: e.memset(k.epsr[:, 0:1], 1e-6), writes=[k.epsr_b])
    P.op('dve', lambda e: e.memset(k.epsr[:, 1:2], 1e-5), writes=[k.epsr_b])
    P.op('dve', lambda e: e.memset(k.epsr[:, 2:3], 64e-5), writes=[k.epsr_b])
    P.op('dve', lambda e: e.memset(k.epsr[:, 3:4], 0.0), writes=[k.epsr_b])

    k.hT, k.hT_b, k.hT_off = sb("hT", [128, 8, TWA + 1], BF16)
    k.GT, k.GT_b, _ = sb("GT", [128, 16, TWA], BF16)
    k.wo = (nc.alloc_sbuf_tensor_at("wo", [128, 16, D], BF16, offset=k.hT_off), k.hT_b)
    k.layer_base = off[0]
    k.gpre, k.gpre_b, _ = sb("gpre", [128, D], F32)
    k.gpost, k.gpost_b = k.gpre, k.gpre_b
    k.xt = [sb(f"xt{i}", [128, D], F32) for i in range(2)]
    hn_par = Buf("hnpar")
    k.hn = []
    for i in range(2):
        t_, _, o_ = sb(f"hn{i}", [128, D], BF16)
        k.hn.append((t_, hn_par.sub(i), o_))
    k.of = nc.alloc_sbuf_tensor_at("of", [128, D], F32, offset=k.hn[0][2])
    k.of_b = hn_par
    k.sqf = lambda np_, h: k.of[0:np_, h * 512:(h + 1) * 512]
    k.sq = sb("sqjunk", [128, D], BF16)
    k.st = [sb(f"stat{i}", [128, 8], F32) for i in range(4)]
    k.st_i = [0]
    k.hf = sb("hf32", [128, D], F32)
    k.off = off

    x_src = (dr["xp"], dr["xs"], k.dbuf["xp"], k.dbuf["xs"])
    wouts = ["a_w_out", "b_w_out", "c_w_out", "d_w_out"]
    for l in range(n_layers):
        x_dst = (dr["y_p"], dr["y_s"], k.dbuf["y_p"], k.dbuf["y_s"]) if (l == n_layers - 1) else \
            ((dr["xscr"], dr["xscr_s"], k.dbuf["xscr"], k.dbuf["xscr_s"]) if l % 2 == 0 else
             (dr["xscr2"], dr["xscr2_s"], k.dbuf["xscr2"], k.dbuf["xscr2_s"]))
        for ps_ in range(2):
            stage1(k, l, ps_, x_src)
            P.barrier()
            off[0] = k.layer_base
            if l == 0:
                layer_conv(k, ps_)
            elif l == 1:
                layer_rwkv(k, ps_)
            elif l == 2:
                layer_mla(k, ps_)
            elif l == 3:
                layer_s5(k, ps_)
            P.barrier()
            stage3(k, l, ps_, x_src, x_dst, dr[wouts[l]], k.dbuf[wouts[l]])
            P.barrier()
        x_src = x_dst
    P.finish('sp')
    k.n_ins = P.emit(nc)
    return nc, k


def ntiles(ps_):
    tl = [(i * 512, 512) for i in range(4)]
    if ps_ == 0:
        tl.append((T, NS))
    return tl


def rstd_from_ss(k, ss_ap, ss_b, out_ap, out_b, npart, inv_n, eps_col):
    P = k.P
    P.op('act', lambda e: e.activation(out=out_ap, in_=ss_ap, func=AF.Sqrt, scale=inv_n,
                                       bias=k.epsr[0:npart, eps_col:eps_col + 1]),
         reads=[ss_b, k.epsr_b], writes=[out_b])
    P.op('dve', lambda e: e.reciprocal(out=out_ap, in_=out_ap), reads=[out_b], writes=[out_b])


def stage1(k, l, ps_, x_src):
    P, dr = k.P, k.dr
    xp, xs, xp_b, xs_b = x_src
    P.op('dve', lambda e: e.memset(k.hT[:, :, 0:1], 0.0), writes=[k.hT_b])
    P.op('sp', lambda e: e.dma_start(out=k.gpre[:], in_=dr["norm_pre"][l:l + 1, :].partition_broadcast(128)),
         writes=[k.gpre_b], dma=True)
    tiles = [(xp[ps_, tt * 128:(tt + 1) * 128, :], 128, tt * 128, xp_b) for tt in range(16)]
    if ps_ == 0:
        tiles.append((xs[:, :], NS, T, xs_b))
    for i, (src, np_, c0, src_b) in enumerate(tiles):
        xt, xt_b, _ = k.xt[i % 2]
        hn, hn_b, _ = k.hn[i % 2]
        sq, sq_b, _ = k.sq
        st, st_b, _ = k.st[k.st_i[0] % 4]
        k.st_i[0] += 1
        P.op('sp', lambda e, xt=xt, src=src, np_=np_: e.dma_start(out=xt[0:np_, :], in_=src),
             reads=[src_b], writes=[xt_b], dma=True)
        P.op('act', lambda e, xt=xt, sq=sq, st=st, np_=np_: e.activation(out=sq[0:np_, :], in_=xt[0:np_, :], func=AF.Square,
                                                                         accum_out=st[0:np_, 0:1]),
             reads=[xt_b], writes=[sq_b, st_b])
        rstd_from_ss(k, st[0:np_, 0:1], st_b, st[0:np_, 1:2], st_b, np_, 1.0 / D, 0)
        P.op('dve', lambda e, hn=hn, xt=xt, st=st, np_=np_: e.scalar_tensor_tensor(
            out=hn[0:np_, :], in0=xt[0:np_, :], scalar=st[0:np_, 1:2], in1=k.gpre[0:np_, :], op0=ALU.mult, op1=ALU.mult),
            reads=[xt_b, st_b, k.gpre_b], writes=[hn_b])
        psT, psT_b = k.psT
        for kk in range(8):
            P.op('pe', lambda e, hn=hn, kk=kk, np_=np_: e.transpose(out=psT[:, kk * 128:kk * 128 + np_],
                                                                    in_=hn[0:np_, kk * 128:(kk + 1) * 128],
                                                                    identity=k.identB[0:np_, 0:np_]),
                 reads=[hn_b, k.identB_b], writes=[psT_b])
        P.op('act', lambda e, c0=c0, np_=np_: e.activation(
            out=k.hT[:, :, c0 + 1:c0 + 1 + np_], in_=psT[:].rearrange("p (k t) -> p k t", k=8)[:, :, 0:np_], func=AF.Copy),
            reads=[psT_b], writes=[k.hT_b.sub(c0 // 128)])
        if l == 1 and (i == 15 or np_ == NS):
            hf, hf_b, _ = k.hf
            P.op('dve', lambda e, hf=hf, xt=xt, st=st, np_=np_: e.scalar_tensor_tensor(
                out=hf[0:np_, :], in0=xt[0:np_, :], scalar=st[0:np_, 1:2], in1=k.gpre[0:np_, :], op0=ALU.mult, op1=ALU.mult),
                reads=[xt_b, st_b, k.gpre_b], writes=[hf_b])
            if np_ == NS:
                P.op('sp', lambda e, hf=hf: e.dma_start(out=dr["shift_s"][:, :], in_=hf[0:NS, :]), reads=[hf_b],
                     writes=[k.dbuf["shift_s"]], dma=True)
            else:
                P.op('sp', lambda e, hf=hf: e.dma_start(out=dr["shift_p"][ps_:ps_ + 1, :], in_=hf[127:128, :]), reads=[hf_b],
                     writes=[k.dbuf["shift_p"]], dma=True)


def hT_reads(k, c0, w):
    return [k.hT_b.sub(c) for c in range(c0 // 128, (c0 + w + 127) // 128)]


def load_w_cols(k, wt, wt_b, w_dram, w_dram_b, col0, ncols=128, nk=8):
    src = w_dram[:, col0:col0 + ncols].rearrange("(k p) e -> p k e", p=128)
    k.P.op('pool', lambda e: e.dma_start(out=wt[:, 0:nk, 0:ncols], in_=src), reads=[w_dram_b], writes=[wt_b], dma=True)


def layer_conv(k, ps_):
    P, dr, nc, sb = k.P, k.dr, k.nc, k.sb
    TW = TWA if ps_ == 0 else T
    if not hasattr(k, "cv"):
        cv = K()
        k.cv = cv
        cv.wt = [[sb(f"cvw{j}_{i}", [128, 8, 128], BF16) for i in range(2)] for j in range(2)]
        cv.wt.append(cv.wt[0])
        cv.cw = sb("cv_cw", [128, 16, 31], F32)
        cv.bin = sb("cv_bin", [128, 48], F32)
        cv.cb = sb("cv_cb", [128, 16], F32)
        cv.lg = sb("cv_lg", [128, 16], F32)
        cv.lb = sb("cv_lb", [128, 16], F32)
        cv.dg = sb("cv_dg", [128, 31, 128], BF16)
        cv.u = sb("cv_u", [128, 30 + T], BF16)
        cv.us = sb("cv_us", [128, NS], F32)
        cv.uf = [sb(f"cv_uf{i}", [128, 512], F32) for i in range(2)]
        cv.sg = [sb(f"cv_sg{i}", [128, 512], F32) for i in range(2)]
        cv.mean = sb("cv_mean", [128, TWA], F32)
        cv.rstd = sb("cv_rstd", [128, TWA], F32)
        cv.tmp = [cv.uf[0], cv.sg[0], cv.uf[1]]
        cv.csq = [(nc.alloc_sbuf_tensor_at(f"cv_csq{i}", [128, 512], BF16, offset=cv.sg[1][2] + i * 1024), cv.sg[1][1].sub(i), 0)
                  for i in range(2)]
        cv.cpt = (nc.alloc_sbuf_tensor_at("cv_cpt", [32, E], F32, offset=cv.mean[2]), cv.mean[1], 0)
        cv.ust = (nc.alloc_sbuf_tensor_at("cv_ust", [16, E], F32, offset=cv.rstd[2]), cv.rstd[1], 0)
        cv.strow = [sb(f"cv_strow{i}", [128, 128], F32) for i in range(4)]
        cv.stT = sb("cv_stT", [128, 480], F32)
        cv.prod = sb("cv_prod", [128, 480], F32)
        cv.cs = sb("cv_cs", [128, NS], F32)
    cv = k.cv
    if True:
        for c_ in range(16):
            P.op('sp', lambda e, c_=c_: e.dma_start(out=cv.cw[0][:, c_, :],
                                                    in_=dr["a_conv_w"][:, c_ * 128:(c_ + 1) * 128].rearrange("k p -> p k"),
                                                    allow_slow_non_contiguous=True), writes=[cv.cw[1]], dma=True)
        P.op('sp', lambda e: e.dma_start(out=cv.bin[0][:], in_=dr["a_b_in"].rearrange("(c p) -> p c", p=128),
                                         allow_slow_non_contiguous=True), writes=[cv.bin[1]], dma=True)
        for t_, nm in ((cv.cb, "a_conv_b"), (cv.lg, "a_ln_g"), (cv.lb, "a_ln_b")):
            P.op('sp', lambda e, t_=t_, nm=nm: e.dma_start(out=t_[0][:], in_=dr[nm].rearrange("(c p) -> p c", p=128),
                                                           allow_slow_non_contiguous=True), writes=[t_[1]], dma=True)
        P.op('dve', lambda e: e.memset(cv.u[0][:, 0:30], 0.0), writes=[cv.u[1].sub('pad')])
    cv = k.cv
    tiles = ntiles(ps_)
    u, u_b, _ = cv.u
    a_w_in, a_w_in_b = dr["a_w_in"], k.dbuf["a_w_in"]
    for ec in range(16):
        wa, wa_b, _ = cv.wt[0][ec % 2]
        wb, wb_b, _ = cv.wt[1][ec % 2]
        load_w_cols(k, wa, wa_b, a_w_in, a_w_in_b, ec * 128)
        load_w_cols(k, wb, wb_b, a_w_in, a_w_in_b, E + ec * 128)
        dg, dg_b, _ = cv.dg
        for kk in range(31):
            P.op('dve', lambda e, kk=kk, ec=ec: e.tensor_scalar(out=dg[:, kk, :], in0=k.identF[:], scalar1=cv.cw[0][:, ec, kk:kk + 1],
                                                                scalar2=None, op0=ALU.mult),
                 reads=[k.identF_b, cv.cw[1]], writes=[dg_b.sub(kk)])
        for ti, (c0, w) in enumerate(tiles):
            pa, pa_b = k.psn()
            pb, pb_b = k.psn()
            for kk in range(8):
                P.op('pe', lambda e, kk=kk, c0=c0, w=w, pa=pa, wa=wa: e.matmul(pa[:, 0:w], lhsT=wa[:, kk, :], rhs=k.hT[:, kk, c0 + 1:c0 + 1 + w],
                                                                              start=(kk == 0), stop=(kk == 7)),
                     reads=[wa_b] + hT_reads(k, c0, w), writes=[pa_b])
            for kk in range(8):
                P.op('pe', lambda e, kk=kk, c0=c0, w=w, pb=pb, wb=wb: e.matmul(pb[:, 0:w], lhsT=wb[:, kk, :], rhs=k.hT[:, kk, c0 + 1:c0 + 1 + w],
                                                                              start=(kk == 0), stop=(kk == 7)),
                     reads=[wb_b] + hT_reads(k, c0, w), writes=[pb_b])
            sg, sg_b, _ = cv.sg[ti % 2]
            uf, uf_b, _ = cv.uf[ti % 2]
            P.op('act', lambda e, sg=sg, pb=pb, w=w, ec=ec: e.activation(out=sg[:, 0:w], in_=pb[:, 0:w], func=AF.Sigmoid,
                                                                        bias=cv.bin[0][:, 16 + ec:17 + ec]),
                 reads=[pb_b, cv.bin[1]], writes=[sg_b])
            if w == 512:
                P.op('dve', lambda e, uf=uf, pa=pa, sg=sg, ec=ec: e.scalar_tensor_tensor(
                    out=uf[:], in0=pa[:], scalar=cv.bin[0][:, ec:ec + 1], in1=sg[:], op0=ALU.add, op1=ALU.mult),
                    reads=[pa_b, sg_b, cv.bin[1]], writes=[uf_b])
                P.op('act', lambda e, uf=uf, c0=c0: e.activation(out=u[:, 30 + c0:30 + c0 + 512], in_=uf[:], func=AF.Copy),
                     reads=[uf_b], writes=[u_b.sub(ti)])
                if ti == 3:
                    pt, pt_b = k.psn()
                    P.op('pe', lambda e, uf=uf, pt=pt: e.transpose(out=pt[0:30, 0:128], in_=uf[:, 482:512], identity=k.identF[:]),
                         reads=[uf_b, k.identF_b], writes=[pt_b])
                    P.op('dve', lambda e, pt=pt, ec=ec: e.tensor_copy(out=cv.cpt[0][0:30, ec * 128:(ec + 1) * 128], in_=pt[0:30, 0:128]),
                         reads=[pt_b], writes=[cv.cpt[1].sub(ec)])
            else:
                us, us_b, _ = cv.us
                P.op('dve', lambda e, pa=pa, sg=sg, ec=ec: e.scalar_tensor_tensor(
                    out=us[:, :], in0=pa[:, 0:NS], scalar=cv.bin[0][:, ec:ec + 1], in1=sg[:, 0:NS], op0=ALU.add, op1=ALU.mult),
                    reads=[pa_b, sg_b, cv.bin[1]], writes=[us_b])
        for ti in range(4):
            c0 = ti * 512
            pc, pc_b = k.psn()
            rd = [u_b.sub(ti), u_b.sub('pad')] + ([u_b.sub(ti - 1)] if ti > 0 else [])
            for kk in range(31):
                P.op('pe', lambda e, kk=kk, c0=c0, pc=pc: e.matmul(pc[:, :], lhsT=dg[:, kk, :], rhs=u[:, c0 + kk:c0 + kk + 512],
                                                                   start=(kk == 0), stop=(kk == 30)),
                     reads=[dg_b.sub(kk)] + rd, writes=[pc_b])
            P.op('act', lambda e, pc=pc, c0=c0, ec=ec: e.activation(out=k.GT[:, ec, c0:c0 + 512], in_=pc[:, :], func=AF.Identity,
                                                                   bias=cv.cb[0][:, ec:ec + 1]),
                 reads=[pc_b, cv.cb[1]], writes=[k.GT_b.sub(ec).sub(ti)])
        if ps_ == 0:
            conv_sample(k, ec)
    P.op('sp', lambda e: e.dma_start(out=dr["conv_p"][ps_, :, :], in_=cv.cpt[0][0:30, :]), reads=[cv.cpt[1]],
         writes=[k.dbuf["conv_p"]], dma=True)
    if ps_ == 0:
        P.op('sp', lambda e: e.dma_start(out=dr["conv_s"][:, 29, :], in_=cv.ust[0][:, :]), reads=[cv.ust[1]],
             writes=[k.dbuf["conv_s"]], dma=True)
        P.op('sp', lambda e: e.dma_start(out=dr["conv_s"][:, 0:29, :], in_=dr["state_conv"][:, 1:30, :]),
             reads=[k.dbuf["state_conv"]], writes=[k.dbuf["conv_s"]], dma=True)
    mean, mean_b, _ = cv.mean
    rstd, rstd_b, _ = cv.rstd
    for ti, (c0, w) in enumerate(tiles):
        p1, p1_b = k.psn()
        p2, p2_b = k.psn()
        for ec in range(16):
            P.op('pe', lambda e, ec=ec, c0=c0, w=w, p1=p1: e.matmul(p1[:, 0:w], lhsT=k.onesB[:], rhs=k.GT[:, ec, c0:c0 + w],
                                                                   start=(ec == 0), stop=(ec == 15)),
                 reads=[k.onesB_b, k.GT_b.sub(ec).sub(ti)], writes=[p1_b])
        for ec in range(16):
            cq, cq_b, _ = cv.csq[ec % 2]
            P.op('act', lambda e, ec=ec, c0=c0, w=w, cq=cq: e.activation(out=cq[:, 0:w], in_=k.GT[:, ec, c0:c0 + w], func=AF.Square),
                 reads=[k.GT_b.sub(ec).sub(ti)], writes=[cq_b])
            P.op('pe', lambda e, ec=ec, w=w, p2=p2, cq=cq: e.matmul(p2[:, 0:w], lhsT=k.onesB[:], rhs=cq[:, 0:w],
                                                                   start=(ec == 0), stop=(ec == 15)),
                 reads=[k.onesB_b, cq_b], writes=[p2_b])
        tm, tm_b, _ = cv.tmp[0]
        P.op('act', lambda e, c0=c0, w=w, p1=p1: e.activation(out=mean[:, c0:c0 + w], in_=p1[:, 0:w], func=AF.Copy, scale=1.0 / E),
             reads=[p1_b], writes=[mean_b.sub(ti)])
        P.op('act', lambda e, w=w, p1=p1: e.activation(out=tm[:, 0:w], in_=p1[:, 0:w], func=AF.Square, scale=1.0 / E),
             reads=[p1_b], writes=[tm_b])
        P.op('dve', lambda e, c0=c0, w=w, p2=p2: e.scalar_tensor_tensor(out=rstd[:, c0:c0 + w], in0=p2[:, 0:w], scalar=1.0 / E,
                                                                        in1=tm[:, 0:w], op0=ALU.mult, op1=ALU.subtract),
             reads=[p2_b, tm_b], writes=[rstd_b.sub(ti)])
        P.op('act', lambda e, c0=c0, w=w: e.activation(out=rstd[:, c0:c0 + w], in_=rstd[:, c0:c0 + w], func=AF.Sqrt,
                                                       bias=k.epsr[:, 1:2]),
             reads=[rstd_b.sub(ti), k.epsr_b], writes=[rstd_b.sub(ti)])
        P.op('dve', lambda e, c0=c0, w=w: e.reciprocal(out=rstd[:, c0:c0 + w], in_=rstd[:, c0:c0 + w]),
             reads=[rstd_b.sub(ti)], writes=[rstd_b.sub(ti)])
    for ec in range(16):
        wz, wz_b, _ = cv.wt[2][ec % 2]
        load_w_cols(k, wz, wz_b, a_w_in, a_w_in_b, 2 * E + ec * 128)
        for ti, (c0, w) in enumerate(tiles):
            pz, pz_b = k.psn()
            for kk in range(8):
                P.op('pe', lambda e, kk=kk, c0=c0, w=w, pz=pz, wz=wz: e.matmul(pz[:, 0:w], lhsT=wz[:, kk, :], rhs=k.hT[:, kk, c0 + 1:c0 + 1 + w],
                                                                              start=(kk == 0), stop=(kk == 7)),
                     reads=[wz_b] + hT_reads(k, c0, w), writes=[pz_b])
            t0, t0_b, _ = cv.tmp[1]
            t1, t1_b, _ = cv.tmp[2]
            gb = k.GT_b.sub(ec).sub(ti)
            P.op('dve', lambda e, ec=ec, c0=c0, w=w: e.tensor_tensor(out=t0[:, 0:w], in0=k.GT[:, ec, c0:c0 + w], in1=mean[:, c0:c0 + w],
                                                                    op=ALU.subtract),
                 reads=[gb, mean_b.sub(ti)], writes=[t0_b])
            P.op('dve', lambda e, c0=c0, w=w: e.tensor_tensor(out=t0[:, 0:w], in0=t0[:, 0:w], in1=rstd[:, c0:c0 + w], op=ALU.mult),
                 reads=[t0_b, rstd_b.sub(ti)], writes=[t0_b])
            P.op('act', lambda e, ec=ec, w=w: e.activation(out=t0[:, 0:w], in_=t0[:, 0:w], func=AF.Silu,
                                                           scale=cv.lg[0][:, ec:ec + 1], bias=cv.lb[0][:, ec:ec + 1]),
                 reads=[t0_b, cv.lg[1], cv.lb[1]], writes=[t0_b])
            P.op('act', lambda e, ec=ec, w=w, pz=pz: e.activation(out=t1[:, 0:w], in_=pz[:, 0:w], func=AF.Silu,
                                                                  bias=cv.bin[0][:, 32 + ec:33 + ec]),
                 reads=[pz_b, cv.bin[1]], writes=[t1_b])
            P.op('dve', lambda e, ec=ec, c0=c0, w=w: e.tensor_tensor(out=k.GT[:, ec, c0:c0 + w], in0=t0[:, 0:w], in1=t1[:, 0:w], op=ALU.mult),
                 reads=[t0_b, t1_b], writes=[gb])


def conv_sample(k, ec):
    P, dr, cv = k.P, k.dr, k.cv
    us, us_b, _ = cv.us
    stT, stT_b, _ = cv.stT
    pt, pt_b = k.psn()
    for r in range(4):
        nr = 128 if r < 3 else 96
        sr, sr_b, _ = cv.strow[r]
        src = dr["state_conv"].rearrange("s k e -> (s k) e")[r * 128:r * 128 + nr, ec * 128:(ec + 1) * 128]
        P.op('sp', lambda e, sr=sr, src=src, nr=nr: e.dma_start(out=sr[0:nr, :], in_=src), reads=[k.dbuf["state_conv"]],
             writes=[sr_b], dma=True)
        P.op('pe', lambda e, sr=sr, nr=nr, r=r, pt=pt: e.transpose(out=pt[:, r * 128:r * 128 + nr], in_=sr[0:nr, :],
                                                                  identity=k.identF[0:nr, 0:nr]),
             reads=[sr_b, k.identF_b], writes=[pt_b])
    P.op('dve', lambda e, pt=pt: e.tensor_copy(out=stT[:, :], in_=pt[:, 0:480]), reads=[pt_b], writes=[stT_b])
    prod, prod_b, _ = cv.prod
    cs, cs_b, _ = cv.cs
    P.op('dve', lambda e, ec=ec: e.tensor_tensor(out=prod[:, :].rearrange("p (s k) -> p s k", k=30),
                                                 in0=stT[:, :].rearrange("p (s k) -> p s k", k=30),
                                                 in1=cv.cw[0][:, ec:ec + 1, 0:30].to_broadcast([128, NS, 30]), op=ALU.mult),
         reads=[stT_b, cv.cw[1]], writes=[prod_b])
    P.op('dve', lambda e: e.tensor_reduce(out=cs[:, :], in_=prod[:, :].rearrange("p (s k) -> p s k", k=30), axis=AX.X, op=ALU.add),
         reads=[prod_b], writes=[cs_b])
    P.op('dve', lambda e, ec=ec: e.scalar_tensor_tensor(out=cs[:, :], in0=us[:, :], scalar=cv.cw[0][:, ec, 30:31], in1=cs[:, :],
                                                        op0=ALU.mult, op1=ALU.add),
         reads=[us_b, cs_b, cv.cw[1]], writes=[cs_b])
    P.op('act', lambda e, ec=ec: e.activation(out=k.GT[:, ec, T:T + NS], in_=cs[:, :], func=AF.Identity, bias=cv.cb[0][:, ec:ec + 1]),
         reads=[cs_b, cv.cb[1]], writes=[k.GT_b.sub(ec).sub(4)])
    p2, p2_b = k.psn()
    P.op('pe', lambda e, p2=p2: e.transpose(out=p2[0:NS, 0:128], in_=us[:, :], identity=k.identF[:]),
         reads=[us_b, k.identF_b], writes=[p2_b])
    P.op('dve', lambda e, p2=p2, ec=ec: e.tensor_copy(out=cv.ust[0][:, ec * 128:(ec + 1) * 128], in_=p2[0:NS, 0:128]),
         reads=[p2_b], writes=[cv.ust[1].sub(ec)])


def stage3(k, l, ps_, x_src, x_dst, w_out, w_out_b):
    P, dr = k.P, k.dr
    xp, xs, xp_b, xs_b = x_src
    yp, ys, yp_b, ys_b = x_dst
    wo, wo_b = k.wo
    P.op('sp', lambda e: e.dma_start(out=k.gpost[:], in_=dr["norm_post"][l:l + 1, :].partition_broadcast(128)),
         writes=[k.gpost_b], dma=True)
    for ec in range(16):
        P.op('pool', lambda e, ec=ec: e.dma_start(out=wo[:, ec, :], in_=w_out[ec * 128:(ec + 1) * 128, :]),
             reads=[w_out_b], writes=[wo_b], dma=True)
    tiles = [(xp[ps_, tt * 128:(tt + 1) * 128, :], yp[ps_, tt * 128:(tt + 1) * 128, :], 128, tt * 128, xp_b, yp_b) for tt in range(16)]
    if ps_ == 0:
        tiles.append((xs[:, :], ys[:, :], NS, T, xs_b, ys_b))
    for i, (src, dst, np_, c0, src_b, dst_b) in enumerate(tiles):
        xt, xt_b, _ = k.xt[i % 2]
        hn, hn_b, _ = k.hn[i % 2]
        sq, sq_b, _ = k.sq
        st, st_b, _ = k.st[k.st_i[0] % 4]
        k.st_i[0] += 1
        P.op('sp', lambda e, xt=xt, src=src, np_=np_: e.dma_start(out=xt[0:np_, :], in_=src),
             reads=[src_b], writes=[xt_b], dma=True)
        pp = [k.psn(), k.psn()]
        gtr = [k.GT_b.sub(ec).sub(min(c0 // 512, 4)) for ec in range(16)]
        for h in range(2):
            ph, ph_b = pp[h]
            for ec in range(16):
                P.op('pe', lambda e, ec=ec, h=h, ph=ph, c0=c0, np_=np_: e.matmul(ph[0:np_, :], lhsT=k.GT[:, ec, c0:c0 + np_],
                                                                                rhs=wo[:, ec, h * 512:(h + 1) * 512],
                                                                                start=(ec == 0), stop=(ec == 15)),
                     reads=[wo_b, gtr[ec]], writes=[ph_b])
            P.op('act', lambda e, h=h, ph=ph, st=st, np_=np_: e.activation(out=sq[0:np_, 0:512], in_=ph[0:np_, :], func=AF.Square,
                                                                          accum_out=st[0:np_, 2 + h:3 + h]),
                 reads=[ph_b], writes=[sq_b, st_b])
        P.op('dve', lambda e, st=st, np_=np_: e.tensor_tensor(out=st[0:np_, 4:5], in0=st[0:np_, 2:3], in1=st[0:np_, 3:4], op=ALU.add),
             reads=[st_b], writes=[st_b])
        rstd_from_ss(k, st[0:np_, 4:5], st_b, st[0:np_, 5:6], st_b, np_, 1.0 / D, 0)
        for h in range(2):
            ph, ph_b = pp[h]
            P.op('dve', lambda e, h=h, ph=ph, st=st, np_=np_, hn=hn: e.scalar_tensor_tensor(
                out=k.sqf(np_, h), in0=ph[0:np_, :], scalar=st[0:np_, 5:6], in1=k.gpost[0:np_, h * 512:(h + 1) * 512],
                op0=ALU.mult, op1=ALU.mult),
                reads=[ph_b, st_b, k.gpost_b], writes=[k.of_b])
        P.op('dve', lambda e, xt=xt, np_=np_: e.tensor_tensor(out=xt[0:np_, :], in0=xt[0:np_, :], in1=k.of[0:np_, :], op=ALU.add),
             reads=[xt_b, k.of_b], writes=[xt_b])
        P.op('sp', lambda e, xt=xt, dst=dst, np_=np_: e.dma_start(out=dst, in_=xt[0:np_, :]),
             reads=[xt_b], writes=[dst_b], dma=True)

def op_tt(k, eng, out, in0, in1, op, reads, writes):
    return k.P.op(eng, lambda e: e.tensor_tensor(out=out, in0=in0, in1=in1, op=op), reads, writes)


def op_ts(k, eng, out, in0, s1, s2, op0, op1, reads, writes):
    if s2 is None:
        return k.P.op(eng, lambda e: e.tensor_scalar(out=out, in0=in0, scalar1=s1, scalar2=None, op0=op0), reads, writes)
    return k.P.op(eng, lambda e: e.tensor_scalar(out=out, in0=in0, scalar1=s1, scalar2=s2, op0=op0, op1=op1), reads, writes)


def op_stt(k, eng, out, in0, scalar, in1, op0, op1, reads, writes):
    return k.P.op(eng, lambda e: e.scalar_tensor_tensor(out=out, in0=in0, scalar=scalar, in1=in1, op0=op0, op1=op1), reads, writes)


def op_act(k, out, in_, func, reads, writes, scale=None, bias=None):
    kw = {}
    if scale is not None:
        kw['scale'] = scale
    if bias is not None:
        kw['bias'] = bias
    return k.P.op('act', lambda e: e.activation(out=out, in_=in_, func=func, **kw), reads, writes)


def op_mm(k, out, lhsT, rhs, start, stop, reads, writes):
    return k.P.op('pe', lambda e: e.matmul(out, lhsT=lhsT, rhs=rhs, start=start, stop=stop), reads, writes)


def op_tr(k, out, in_, ident, reads, writes):
    return k.P.op('pe', lambda e: e.transpose(out=out, in_=in_, identity=ident), reads, writes)


def op_dma(k, eng, out, in_, reads, writes, slow=False):
    if slow:
        return k.P.op(eng, lambda e: e.dma_start(out=out, in_=in_, allow_slow_non_contiguous=True), reads, writes, dma=True)
    return k.P.op(eng, lambda e: e.dma_start(out=out, in_=in_), reads, writes, dma=True)


def hT_reads_prev(k, c0, w):
    out = []
    if c0 == 0:
        out.append(k.hT_b.sub('pad'))
    lo = max(c0 - 1, 0) // 128
    hi = (c0 + w - 2) // 128
    for c in range(lo, hi + 1):
        out.append(k.hT_b.sub(c))
    return out


DECAY_C = 0.6065306597126334


def rwkv_setup(k):
    P, dr, nc, sb = k.P, k.dr, k.nc, k.sb
    rw = K()
    k.rw = rw
    rw.W = [[sb(f"rwW{j}{ab}", [128, 8, 128], BF16) for ab in range(2)] for j in range(4)]
    rw.wraw = sb("rw_wraw", [128, 8, 128], BF16)
    rw.mu = sb("rw_mu", [128, 6, 8], F32)
    rw.omu = sb("rw_omu", [128, 6, 8], F32)
    rw.par = {n: sb("rw_p_" + n, [128, 16], F32) for n in ["w0", "a0", "k_k", "k_a", "omk_a", "r_k", "ln_g", "ln_b"]}
    rw.lw = [sb(f"rw_lw{i}", [128, 8, 128], BF16) for i in range(2)]
    rw.w2a2 = sb("rw_w2a2", [128, E], BF16)
    rw.lora = sb("rw_lora", [128, TWA], BF16)
    rw.mscan = sb("rw_mscan", [128, 512], F32)
    rw.mk = {n: sb("rw_mk_" + n, [128, 512], BF16) for n in ["nUs", "nLs", "Us", "Ui"]}
    rw.irep = sb("rw_irep", [128, 64], F32)
    rw.boB = sb("rw_boB", [128, 128], BF16)
    rw.boF = sb("rw_boF", [128, 128], F32)
    rw.f = [sb(f"rw_f{i}", [128, 512], F32) for i in range(13)]
    rw.h16 = {n: sb("rw_h_" + n, [128, 512], BF16) for n in ["KT", "RT", "BT", "KKT", "VT", "ZS", "YB", "YQ"]}
    rw.tok = {n: sb("rw_tok_" + n, [128, 8, 64], BF16) for n in ["B", "K", "V"]}
    rw.g = {n: sb("rw_g_" + n, [128, 512], BF16) for n in ["Inv", "AkT", "BrT", "KrT"]}
    rw.Pp = [sb(f"rw_P{i}", [128, 512], BF16) for i in range(2)]
    rw.Qp = [sb(f"rw_Q{i}", [128, 512], BF16) for i in range(2)]
    rw.S = sb("rw_S", [128, 512], F32)
    rw.Sbf = sb("rw_Sbf", [128, 512], BF16)
    rw.Xsb = sb("rw_Xsb", [128, 64], BF16)
    rw.Usb = sb("rw_Usb", [128, 64], BF16)
    rw.H = sb("rw_H", [128, 64], F32)
    rw.Hbf = sb("rw_Hbf", [128, 64], BF16)
    rw.TH = sb("rw_TH", [128, 64], F32)
    rw.shT = sb("rw_shT", [128, 8, NS], BF16)
    rw.shrow = sb("rw_shrow", [NS, D], F32)
    rw.small = sb("rw_small", [128, 64], F32)


def rwkv_load(k):
    P, dr, nc, rw = k.P, k.dr, k.nc, k.rw
    for j in range(6):
        op_dma(k, 'sp', rw.mu[0][:, j, :], dr["b_mu"][j].rearrange("(k p) -> p k", p=128), [k.dbuf["b_mu"]], [rw.mu[1]], slow=True)
    op_ts(k, 'dve', rw.omu[0][:], rw.mu[0][:], -1.0, 1.0, ALU.mult, ALU.add, [rw.mu[1]], [rw.omu[1]])
    for n, src in [("w0", "b_w0"), ("a0", "b_a0"), ("k_k", "b_k_k"), ("k_a", "b_k_a"), ("ln_g", "b_ln_g"), ("ln_b", "b_ln_b")]:
        op_dma(k, 'sp', rw.par[n][0][:], dr[src].rearrange("(c p) -> p c", p=128), [k.dbuf[src]], [rw.par[n][1]], slow=True)
    op_dma(k, 'sp', rw.par["r_k"][0][:], dr["b_r_k"].rearrange("(c h2) j -> (h2 j) c", h2=2), [k.dbuf["b_r_k"]],
           [rw.par["r_k"][1]], slow=True)
    op_ts(k, 'dve', rw.par["omk_a"][0][:], rw.par["k_a"][0][:], -1.0, 1.0, ALU.mult, ALU.add, [rw.par["k_a"][1]], [rw.par["omk_a"][1]])
    wr, wr_b, _ = rw.wraw
    op_dma(k, 'pool', wr[:, :, 0:64], dr["b_w1"].rearrange("(k p) e -> p k e", p=128), [k.dbuf["b_w1"]], [wr_b])
    op_dma(k, 'pool', wr[:, :, 64:128], dr["b_a1"].rearrange("(k p) e -> p k e", p=128), [k.dbuf["b_a1"]], [wr_b])
    for half, j in ((0, 4), (1, 5)):
        cs_ = slice(half * 64, half * 64 + 64)
        op_tt(k, 'dve', rw.lw[0][0][:, :, cs_], wr[:, :, cs_], rw.omu[0][:, j, :].unsqueeze(2).to_broadcast([128, 8, 64]), ALU.mult,
              [wr_b, rw.omu[1]], [rw.lw[0][1]])
        op_tt(k, 'dve', rw.lw[1][0][:, :, cs_], wr[:, :, cs_], rw.mu[0][:, j, :].unsqueeze(2).to_broadcast([128, 8, 64]), ALU.mult,
              [wr_b, rw.mu[1]], [rw.lw[1][1]])
    op_dma(k, 'pool', rw.w2a2[0][0:64, :], dr["b_w2"], [k.dbuf["b_w2"]], [rw.w2a2[1]])
    op_dma(k, 'pool', rw.w2a2[0][64:128, :], dr["b_a2"], [k.dbuf["b_a2"]], [rw.w2a2[1]])
    ms, ms_b, _ = rw.mscan
    P.op('dve', lambda e: e.memset(ms[:], 1.0), writes=[ms_b])
    P.op('dve', lambda e: e.memset(ms[:].rearrange("p (c t) -> p c t", t=64)[:, :, 0:1], 0.0), writes=[ms_b])
    for n, val, cm, pat, cmp_ in [("nUs", -1.0, -1, 1, ALU.is_gt), ("Us", 1.0, -1, 1, ALU.is_gt), ("Ui", 1.0, -1, 1, ALU.is_ge),
                                  ("nLs", -1.0, 1, -1, ALU.is_gt)]:
        t_, b_, _ = rw.mk[n]
        tf, tf_b, _ = rw.f[0]
        P.op('pool', lambda e, tf=tf, val=val: e.memset(tf[0:64, :], val), writes=[tf_b])
        P.op('pool', lambda e, tf=tf, cm=cm, pat=pat, cmp_=cmp_: e.affine_select(
            out=tf[0:64, :].rearrange("p (c t) -> p c t", t=64), in_=tf[0:64, :].rearrange("p (c t) -> p c t", t=64),
            pattern=[[0, 8], [pat, 64]], compare_op=cmp_, fill=0.0, base=0, channel_multiplier=cm),
            reads=[tf_b], writes=[tf_b])
        P.op('dve', lambda e, t_=t_, tf=tf: e.tensor_copy(out=t_[0:64, :], in_=tf[0:64, :]), reads=[tf_b], writes=[b_])
        op_dma(k, 'sp', t_[64:128, :], t_[0:64, :], [b_], [b_])
    op_tt(k, 'dve', rw.irep[0][:], k.identF[:, 0:64], k.identF[:, 64:128], ALU.add, [k.identF_b], [rw.irep[1]])
    for t_, b_, _ in (rw.boB, rw.boF):
        P.op('dve', lambda e, t_=t_: e.memset(t_[:], 0.0), writes=[b_])
        P.op('dve', lambda e, t_=t_: e.memset(t_[0:64, 0:64], 1.0), writes=[b_])
        P.op('dve', lambda e, t_=t_: e.memset(t_[64:128, 64:128], 1.0), writes=[b_])


def layer_rwkv(k, ps_):
    P, dr, nc = k.P, k.dr, k.nc
    if not hasattr(k, "rw"):
        rwkv_setup(k)
    rwkv_load(k)
    rw = k.rw
    P.chk("setup")
    tiles = ntiles(ps_)
    lora, lora_b, _ = rw.lora
    if ps_ == 0:
        sr, sr_b, _ = rw.shrow
        op_dma(k, 'sp', sr[:, :], dr["state_shift"], [k.dbuf["state_shift"]], [sr_b])
        pt, pt_b = k.psn()
        for kk in range(8):
            op_tr(k, pt[:, kk * NS:(kk + 1) * NS], sr[0:NS, kk * 128:(kk + 1) * 128], k.identF[0:NS, 0:NS], [sr_b, k.identF_b], [pt_b])
        op_act(k, rw.shT[0][:, :, :], pt[:, 0:8 * NS].rearrange("p (k s) -> p k s", s=NS), AF.Copy, [pt_b], [rw.shT[1]])

    def prev_rhs(kk, c0, w):
        if c0 >= T:
            return rw.shT[0][:, kk, :], [rw.shT[1]]
        return k.hT[:, kk, c0:c0 + w], hT_reads_prev(k, c0, w)

    for ti, (c0, w) in enumerate(tiles):
        pl, pl_b = k.psn()
        for kk in range(8):
            op_mm(k, pl[:, 0:w], rw.lw[0][0][:, kk, :], k.hT[:, kk, c0 + 1:c0 + 1 + w], kk == 0, False,
                  [rw.lw[0][1]] + hT_reads(k, c0, w), [pl_b])
        for kk in range(8):
            r_, rb_ = prev_rhs(kk, c0, w)
            op_mm(k, pl[:, 0:w], rw.lw[1][0][:, kk, :], r_, False, kk == 7, [rw.lw[1][1]] + rb_, [pl_b])
        op_act(k, lora[0:64, c0:c0 + w], pl[0:64, 0:w], AF.Tanh, [pl_b], [lora_b.sub(ti)])
        op_act(k, lora[64:128, c0:c0 + w], pl[64:128, 0:w], AF.Copy, [pl_b], [lora_b.sub(ti)])

    P.chk("lora")
    F = rw.f
    for hp in range(16):
        hc = slice(hp * 128, (hp + 1) * 128)
        wr, wr_b, _ = rw.wraw
        for j in range(4):
            op_dma(k, 'pool', wr[:, :, :], dr["b_w_rkvz"][j][:, hc].rearrange("(k p) e -> p k e", p=128), [k.dbuf["b_w_rkvz"]], [wr_b])
            op_tt(k, 'dve', rw.W[j][0][0][:], wr[:], rw.omu[0][:, j, :].unsqueeze(2).to_broadcast([128, 8, 128]), ALU.mult,
                  [wr_b, rw.omu[1]], [rw.W[j][0][1]])
            op_tt(k, 'pool', rw.W[j][1][0][:], wr[:], rw.mu[0][:, j, :].unsqueeze(2).to_broadcast([128, 8, 128]), ALU.mult,
                  [wr_b, rw.mu[1]], [rw.W[j][1][1]])
        P.chk("w")
        H, H_b, _ = rw.H
        Hbf, Hbf_b, _ = rw.Hbf
        P.op('dve', lambda e: e.memset(H[:], 0.0), writes=[H_b])
        P.op('dve', lambda e: e.memset(Hbf[:], 0.0), writes=[Hbf_b])
        par = lambda n: rw.par[n][0][:, hp:hp + 1]
        parb = lambda n: rw.par[n][1]
        for ti, (c0, w) in enumerate(tiles):
            sample = c0 >= T
            pj = []
            for j in range(4):
                pp, pp_b = k.psn()
                for kk in range(8):
                    op_mm(k, pp[:, 0:w], rw.W[j][0][0][:, kk, :], k.hT[:, kk, c0 + 1:c0 + 1 + w], kk == 0, False,
                          [rw.W[j][0][1]] + hT_reads(k, c0, w), [pp_b])
                for kk in range(8):
                    r_, rb_ = prev_rhs(kk, c0, w)
                    op_mm(k, pp[:, 0:w], rw.W[j][1][0][:, kk, :], r_, False, kk == 7, [rw.W[j][1][1]] + rb_, [pp_b])
                pj.append((pp, pp_b))
            (pr, pr_b), (pk, pk_b), (pv, pv_b), (pz, pz_b) = pj
            P.chk("proj")
            pw, pw_b = k.psn()
            op_mm(k, pw[:, 0:w], rw.w2a2[0][0:64, hc], lora[0:64, c0:c0 + w], True, True, [rw.w2a2[1], lora_b.sub(ti)], [pw_b])
            pa, pa_b = k.psn()
            op_mm(k, pa[:, 0:w], rw.w2a2[0][64:128, hc], lora[64:128, c0:c0 + w], True, True, [rw.w2a2[1], lora_b.sub(ti)], [pa_b])
            P.chk("pwpa")
            W_ = slice(0, w)
            A, SG, TMP, KP, KKF, SQ, Bt, R, V, RK, BON, PP, YN = [F[i] for i in range(13)]
            op_act(k, A[0][:, W_], pa[:, W_], AF.Sigmoid, [pa_b, parb("a0")], [A[1]], bias=par("a0"))
            op_act(k, SG[0][:, W_], pw[:, W_], AF.Sigmoid, [pw_b, parb("w0")], [SG[1]], bias=par("w0"))
            P.chk("e0a")
            op_act(k, TMP[0][:, W_], A[0][:, W_], AF.Identity, [A[1], parb("k_a"), parb("omk_a")], [TMP[1]], scale=par("k_a"), bias=par("omk_a"))
            P.chk("e0b")
            op_tt(k, 'dve', KP[0][:, W_], pk[:, W_], TMP[0][:, W_], ALU.mult, [pk_b, TMP[1]], [KP[1]])
            P.chk("e0c")
            op_ts(k, 'dve', KKF[0][:, W_], pk[:, W_], par("k_k"), None, ALU.mult, None, [pk_b, parb("k_k")], [KKF[1]])
            P.chk("e0d")
            op_act(k, SQ[0][:, W_], KKF[0][:, W_], AF.Square, [KKF[1]], [SQ[1]])
            P.chk("e1")
            pn, pn_b = k.psn()
            op_mm(k, pn[:, W_], rw.boF[0][:], SQ[0][:, W_], True, True, [rw.boF[1], SQ[1]], [pn_b])
            op_act(k, SQ[0][:, W_], pn[:, W_], AF.Sqrt, [pn_b], [SQ[1]])
            op_ts(k, 'dve', SQ[0][:, W_], SQ[0][:, W_], 1e-12, None, ALU.max, None, [SQ[1]], [SQ[1]])
            P.op('dve', lambda e, W_=W_: e.reciprocal(out=SQ[0][:, W_], in_=SQ[0][:, W_]), [SQ[1]], [SQ[1]])
            op_tt(k, 'dve', KKF[0][:, W_], KKF[0][:, W_], SQ[0][:, W_], ALU.mult, [KKF[1], SQ[1]], [KKF[1]])
            op_tt(k, 'dve', Bt[0][:, W_], KKF[0][:, W_], A[0][:, W_], ALU.mult, [KKF[1], A[1]], [Bt[1]])
            P.chk("e2")
            op_act(k, R[0][:, W_], pr[:, W_], AF.Copy, [pr_b], [R[1]])
            op_act(k, V[0][:, W_], pv[:, W_], AF.Copy, [pv_b], [V[1]])
            op_stt(k, 'dve', RK[0][:, W_], R[0][:, W_], par("r_k"), KP[0][:, W_], ALU.mult, ALU.mult, [R[1], KP[1], parb("r_k")], [RK[1]])
            pb, pb_b = k.psn()
            op_mm(k, pb[:, W_], rw.boF[0][:], RK[0][:, W_], True, True, [rw.boF[1], RK[1]], [pb_b])
            op_tt(k, 'dve', BON[0][:, W_], pb[:, W_], V[0][:, W_], ALU.mult, [pb_b, V[1]], [BON[1]])
            ZS = rw.h16["ZS"]
            op_act(k, ZS[0][:, W_], pz[:, W_], AF.Silu, [pz_b], [ZS[1]])
            P.chk("elem")
            if not sample:
                ysrc, ysrc_b = rwkv_chunks(k, hp, ti, c0, A, SG, TMP, KP, KKF, Bt, R, V, PP, pv, pv_b)
            else:
                ysrc, ysrc_b = rwkv_sample(k, hp, SG, KP, KKF, Bt, R, V)
            P.chk("chunks")
            YB, YQ = rw.h16["YB"], rw.h16["YQ"]
            op_act(k, YB[0][:, W_], ysrc[:, W_], AF.Copy, [ysrc_b], [YB[1]])
            op_act(k, YQ[0][:, W_], ysrc[:, W_], AF.Square, [ysrc_b], [YQ[1]])
            pm, pm_b = k.psn()
            pq, pq_b = k.psn()
            op_mm(k, pm[:, W_], rw.boB[0][:], YB[0][:, W_], True, True, [rw.boB[1], YB[1]], [pm_b])
            op_mm(k, pq[:, W_], rw.boB[0][:], YQ[0][:, W_], True, True, [rw.boB[1], YQ[1]], [pq_b])
            MEAN, MSQ, RS = A, SG, TMP
            op_act(k, MEAN[0][:, W_], pm[:, W_], AF.Copy, [pm_b], [MEAN[1]], scale=1.0 / 64)
            op_act(k, MSQ[0][:, W_], pm[:, W_], AF.Square, [pm_b], [MSQ[1]], scale=1.0 / 64)
            op_stt(k, 'dve', RS[0][:, W_], pq[:, W_], 1.0 / 64, MSQ[0][:, W_], ALU.mult, ALU.subtract, [pq_b, MSQ[1]], [RS[1]])
            op_act(k, RS[0][:, W_], RS[0][:, W_], AF.Sqrt, [RS[1], k.epsr_b], [RS[1]], bias=k.epsr[:, 2:3])
            P.op('dve', lambda e, W_=W_, RS=RS: e.reciprocal(out=RS[0][:, W_], in_=RS[0][:, W_]), [RS[1]], [RS[1]])
            op_tt(k, 'dve', YN[0][:, W_], ysrc[:, W_], MEAN[0][:, W_], ALU.subtract, [ysrc_b, MEAN[1]], [YN[1]])
            op_tt(k, 'dve', YN[0][:, W_], YN[0][:, W_], RS[0][:, W_], ALU.mult, [YN[1], RS[1]], [YN[1]])
            op_act(k, YN[0][:, W_], YN[0][:, W_], AF.Identity, [YN[1], parb("ln_g"), parb("ln_b")], [YN[1]], scale=par("ln_g"), bias=par("ln_b"))
            op_tt(k, 'dve', YN[0][:, W_], YN[0][:, W_], BON[0][:, W_], ALU.add, [YN[1], BON[1]], [YN[1]])
            op_tt(k, 'dve', k.GT[:, hp, c0:c0 + w], YN[0][:, W_], ZS[0][:, W_], ALU.mult, [YN[1], ZS[1]], [k.GT_b.sub(hp).sub(ti)])
            P.chk("gn")
            if ti == 3:
                pt, pt_b = k.psn()
                for h in range(2):
                    hP = slice(h * 64, h * 64 + 64)
                    op_mm(k, pt[hP, 0:64], H[hP, :], k.identF[hP, hP], True, True, [H_b, k.identF_b], [pt_b])
                sm, sm_b, _ = rw.small
                P.op('dve', lambda e, pt=pt: e.tensor_copy(out=sm[:, :], in_=pt[:, 0:64]), [pt_b], [sm_b])
                op_dma(k, 'sp', dr["wkv_p"][ps_, 2 * hp:2 * hp + 2, :, :].rearrange("h i j -> (h i) j"), sm[:, :], [sm_b], [k.dbuf["wkv_p"]])


def rwkv_chunks(k, hp, ti, c0, A, SG, TMP, KP, KK, Bt, R, V, PP, pv, pv_b):
    P, rw = k.P, k.rw
    H, H_b, _ = rw.H
    Hbf, Hbf_b, _ = rw.Hbf
    KT, RT, BT, KKT, VT = [rw.h16[n] for n in ["KT", "RT", "BT", "KKT", "VT"]]
    ms = rw.mscan
    CS = TMP
    P.op('dve', lambda e: e.tensor_tensor_scan(out=CS[0][:], data0=ms[0][:], data1=SG[0][:], initial=0.0, op0=ALU.mult, op1=ALU.add),
         [ms[1], SG[1]], [CS[1]])
    op_tt(k, 'dve', SG[0][:], CS[0][:], SG[0][:], ALU.subtract, [CS[1], SG[1]], [SG[1]])
    op_act(k, PP[0][:], CS[0][:], AF.Exp, [CS[1]], [PP[1]], scale=-DECAY_C)
    op_act(k, CS[0][:], CS[0][:], AF.Exp, [CS[1]], [CS[1]], scale=DECAY_C)
    op_act(k, SG[0][:], SG[0][:], AF.Exp, [SG[1]], [SG[1]], scale=-DECAY_C)
    op_tt(k, 'dve', KT[0][:], KK[0][:], SG[0][:], ALU.mult, [KK[1], SG[1]], [KT[1]])
    op_tt(k, 'dve', RT[0][:], R[0][:], PP[0][:], ALU.mult, [R[1], PP[1]], [RT[1]])
    op_tt(k, 'dve', BT[0][:], Bt[0][:], CS[0][:], ALU.mult, [Bt[1], CS[1]], [BT[1]])
    op_tt(k, 'dve', KKT[0][:], KP[0][:], CS[0][:], ALU.mult, [KP[1], CS[1]], [KKT[1]])
    op_act(k, VT[0][:], pv[:, :], AF.Copy, [pv_b], [VT[1]])
    for n, X in (("B", BT), ("K", KKT), ("V", VT)):
        pt, pt_b = k.psn()
        for c in range(8):
            for h in range(2):
                hP = slice(h * 64, h * 64 + 64)
                op_mm(k, pt[hP, c * 64:(c + 1) * 64], X[0][hP, c * 64:(c + 1) * 64], k.identB[hP, hP], True, True,
                      [X[1], k.identB_b], [pt_b])
        op_act(k, rw.tok[n][0][:, :, :], pt[:, 0:512].rearrange("p (c t) -> p c t", t=64), AF.Copy, [pt_b], [rw.tok[n][1]])
    Btok, Ktok, Vtok = rw.tok["B"], rw.tok["K"], rw.tok["V"]
    P.chk("tok")
    grams = [("AbT", BT, KT), ("Ab", KT, BT), ("AkT", KKT, KT), ("BrT", BT, RT), ("KrT", KKT, RT)]
    gps = {}
    for n, L, Rr in grams:
        pg, pg_b = k.psn()
        for c in range(8):
            cs_ = slice(c * 64, (c + 1) * 64)
            for h in range(2):
                hP = slice(h * 64, h * 64 + 64)
                op_mm(k, pg[hP, cs_], L[0][hP, cs_], Rr[0][hP, cs_], True, True, [L[1], Rr[1]], [pg_b])
        gps[n] = (pg, pg_b)
    Pc, Qc = rw.Pp[0], rw.Qp[0]
    S, Sbf = rw.S, rw.Sbf
    op_tt(k, 'dve', Qc[0][:], gps["AbT"][0][:, :], rw.mk["nUs"][0][:], ALU.mult, [gps["AbT"][1], rw.mk["nUs"][1]], [Qc[1]])
    op_tt(k, 'dve', Pc[0][:], gps["Ab"][0][:, :], rw.mk["nLs"][0][:], ALU.mult, [gps["Ab"][1], rw.mk["nLs"][1]], [Pc[1]])
    op_tt(k, 'dve', rw.g["AkT"][0][:], gps["AkT"][0][:, :], rw.mk["Us"][0][:], ALU.mult, [gps["AkT"][1], rw.mk["Us"][1]], [rw.g["AkT"][1]])
    op_tt(k, 'dve', rw.g["BrT"][0][:], gps["BrT"][0][:, :], rw.mk["Ui"][0][:], ALU.mult, [gps["BrT"][1], rw.mk["Ui"][1]], [rw.g["BrT"][1]])
    op_tt(k, 'dve', rw.g["KrT"][0][:], gps["KrT"][0][:, :], rw.mk["Ui"][0][:], ALU.mult, [gps["KrT"][1], rw.mk["Ui"][1]], [rw.g["KrT"][1]])
    op_tt(k, 'dve', S[0][:].rearrange("p (c t) -> p c t", t=64), Qc[0][:].rearrange("p (c t) -> p c t", t=64),
          rw.irep[0][:, :].unsqueeze(1).to_broadcast([128, 8, 64]), ALU.add, [Qc[1], rw.irep[1]], [S[1]])
    op_act(k, Sbf[0][:], S[0][:], AF.Copy, [S[1]], [Sbf[1]])
    for it in range(1, 6):
        Pn, Qn = rw.Pp[it % 2], rw.Qp[it % 2]
        pP, pP_b = k.psn()
        for c in range(8):
            cs_ = slice(c * 64, (c + 1) * 64)
            for h in range(2):
                hP = slice(h * 64, h * 64 + 64)
                op_mm(k, pP[hP, cs_], Qc[0][hP, cs_], Pc[0][hP, cs_], True, True, [Qc[1], Pc[1]], [pP_b])
        if it < 5:
            pQ, pQ_b = k.psn()
            for c in range(8):
                cs_ = slice(c * 64, (c + 1) * 64)
                for h in range(2):
                    hP = slice(h * 64, h * 64 + 64)
                    op_mm(k, pQ[hP, cs_], Pc[0][hP, cs_], Qc[0][hP, cs_], True, True, [Qc[1], Pc[1]], [pQ_b])
        op_act(k, Pn[0][:], pP[:, :], AF.Copy, [pP_b], [Pn[1]])
        if it < 5:
            P.op('dve', lambda e, Qn=Qn, pQ=pQ: e.tensor_copy(out=Qn[0][:], in_=pQ[:, :]), [pQ_b], [Qn[1]])
        pS, pS_b = k.psn()
        for c in range(8):
            cs_ = slice(c * 64, (c + 1) * 64)
            for h in range(2):
                hP = slice(h * 64, h * 64 + 64)
                op_mm(k, pS[hP, cs_], Pn[0][hP, cs_], Sbf[0][hP, cs_], True, True, [Pn[1], Sbf[1]], [pS_b])
        op_tt(k, 'dve', S[0][:], S[0][:], pS[:, :], ALU.add, [S[1], pS_b], [S[1]])
        dst = Sbf if it < 5 else rw.g["Inv"]
        op_act(k, dst[0][:], S[0][:], AF.Copy, [S[1]], [dst[1]])
        Pc, Qc = Pn, Qn
    Inv, AkT, BrT, KrT = [rw.g[n] for n in ["Inv", "AkT", "BrT", "KrT"]]
    P.chk("inv")
    py, py_b = k.ps[6]

    def ps6():
        i = k.ps_i[0] % 6
        k.ps_i[0] += 1
        return k.ps[i]
    Xsb, Usb, TH = rw.Xsb, rw.Usb, rw.TH
    for c in range(8):
        cs_ = slice(c * 64, (c + 1) * 64)
        px, px_b = ps6()
        for h in range(2):
            hP = slice(h * 64, h * 64 + 64)
            op_mm(k, px[hP, 0:64], KT[0][hP, cs_], Hbf[hP, :], True, False, [KT[1], Hbf_b], [px_b])
            op_mm(k, px[hP, 0:64], AkT[0][hP, cs_], Vtok[0][hP, c, :], False, True, [AkT[1], Vtok[1]], [px_b])
        op_act(k, Xsb[0][:, :], px[:, 0:64], AF.Copy, [px_b], [Xsb[1]], scale=-1.0)
        pu, pu_b = ps6()
        for h in range(2):
            hP = slice(h * 64, h * 64 + 64)
            op_mm(k, pu[hP, 0:64], Inv[0][hP, cs_], Xsb[0][hP, :], True, True, [Inv[1], Xsb[1]], [pu_b])
        P.op('dve', lambda e, pu=pu: e.tensor_copy(out=Usb[0][:, :], in_=pu[:, 0:64]), [pu_b], [Usb[1]])
        for h in range(2):
            hP = slice(h * 64, h * 64 + 64)
            op_mm(k, py[hP, cs_], Hbf[hP, :], RT[0][hP, cs_], True, False, [Hbf_b, RT[1]], [py_b])
            op_mm(k, py[hP, cs_], Usb[0][hP, :], BrT[0][hP, cs_], False, False, [Usb[1], BrT[1]], [py_b])
            op_mm(k, py[hP, cs_], Vtok[0][hP, c, :], KrT[0][hP, cs_], False, True, [Vtok[1], KrT[1]], [py_b])
        ph, ph_b = ps6()
        for h in range(2):
            hP = slice(h * 64, h * 64 + 64)
            op_mm(k, ph[hP, 0:64], Btok[0][hP, c, :], Usb[0][hP, :], True, False, [Btok[1], Usb[1]], [ph_b])
            op_mm(k, ph[hP, 0:64], Ktok[0][hP, c, :], Vtok[0][hP, c, :], False, True, [Ktok[1], Vtok[1]], [ph_b])
        pc_ap = PP[0][:, c * 64 + 63:c * 64 + 64]
        op_tt(k, 'dve', TH[0][:, :], ph[:, 0:64], H[:, :], ALU.add, [ph_b, H_b], [TH[1]])
        op_ts(k, 'dve', H[:, :], TH[0][:, :], pc_ap, None, ALU.mult, None, [TH[1], PP[1]], [H_b])
        op_act(k, Hbf[:, :], TH[0][:, :], AF.Identity, [TH[1], PP[1], k.epsr_b], [Hbf_b], scale=pc_ap, bias=k.epsr[:, 3:4])
    Ysb = rw.f[5]
    op_act(k, Ysb[0][:, :], py[:, :], AF.Copy, [py_b], [Ysb[1]])
    return Ysb[0], Ysb[1]


def rwkv_sample(k, hp, SG, KP, KK, Bt, R, V):
    P, rw, dr = k.P, k.rw, k.dr
    F = rw.f
    Wd = F[2]
    op_act(k, Wd[0][:, 0:NS], SG[0][:, 0:NS], AF.Exp, [SG[1]], [Wd[1]], scale=-DECAY_C)
    ysm, ysm_b, _ = rw.small
    Sst_t, Sn_t, T1_t, RX_t = F[5], F[9], F[11], rw.S
    sa_t = F[12]
    for half in range(2):
        s0 = half * 8
        ss_ = slice(s0, s0 + 8)
        v3 = lambda t: t[0][:, :].rearrange("p (s j) -> p s j", j=64)
        op_dma(k, 'sp', v3(Sst_t), dr["state_wkv"][s0:s0 + 8, 2 * hp:2 * hp + 2, :, :].rearrange("s h i j -> (h i) s j"),
               [k.dbuf["state_wkv"]], [Sst_t[1]])

        def bcast(X):
            op_tt(k, 'dve', v3(RX_t), rw.irep[0][:, :].unsqueeze(1).to_broadcast([128, 8, 64]),
                  X[0][:, ss_].unsqueeze(2).to_broadcast([128, 8, 64]), ALU.mult, [rw.irep[1], X[1]], [RX_t[1]])
            pb, pb_b = k.psn()
            op_mm(k, pb[:, :], rw.boF[0][:], RX_t[0][:, :], True, True, [rw.boF[1], RX_t[1]], [pb_b])
            return pb[:, :].rearrange("p (s j) -> p s j", j=64), pb_b

        kkb, kkb_b = bcast(KK)
        op_tt(k, 'dve', v3(T1_t), v3(Sst_t), kkb, ALU.mult, [Sst_t[1], kkb_b], [T1_t[1]])
        P.op('dve', lambda e, ss_=ss_: e.tensor_reduce(out=sa_t[0][:, ss_], in_=v3(T1_t), axis=AX.X, op=ALU.add, negate=True),
             [T1_t[1]], [sa_t[1]])
        wb_, wb_b = bcast(Wd)
        op_tt(k, 'dve', v3(Sn_t), v3(Sst_t), wb_, ALU.mult, [Sst_t[1], wb_b], [Sn_t[1]])
        bb_, bb_b = bcast(Bt)
        op_tt(k, 'dve', v3(T1_t), bb_, sa_t[0][:, ss_].unsqueeze(2).to_broadcast([128, 8, 64]), ALU.mult, [bb_b, sa_t[1]], [T1_t[1]])
        op_tt(k, 'dve', v3(Sn_t), v3(Sn_t), v3(T1_t), ALU.add, [Sn_t[1], T1_t[1]], [Sn_t[1]])
        kb_, kb_b = bcast(KP)
        op_tt(k, 'dve', v3(T1_t), kb_, V[0][:, ss_].unsqueeze(2).to_broadcast([128, 8, 64]), ALU.mult, [kb_b, V[1]], [T1_t[1]])
        op_tt(k, 'dve', v3(Sn_t), v3(Sn_t), v3(T1_t), ALU.add, [Sn_t[1], T1_t[1]], [Sn_t[1]])
        rb_, rb_b = bcast(R)
        op_tt(k, 'dve', v3(T1_t), v3(Sn_t), rb_, ALU.mult, [Sn_t[1], rb_b], [T1_t[1]])
        P.op('dve', lambda e, ss_=ss_: e.tensor_reduce(out=ysm[:, ss_], in_=v3(T1_t), axis=AX.X, op=ALU.add), [T1_t[1]], [ysm_b])
        op_dma(k, 'sp', dr["wkv_s"][s0:s0 + 8, 2 * hp:2 * hp + 2, :, :].rearrange("s h i j -> (h i) s j"), v3(Sn_t),
               [Sn_t[1]], [k.dbuf["wkv_s"]])
    return ysm, ysm_b

MLA_SCALE_ = 192.0 ** -0.5
PI_ = 3.141592653589793
NPOOL = 10240


def mla_setup(k):
    P, dr, nc, sb = k.P, k.dr, k.nc, k.sb
    ml = K()
    k.ml = ml
    ml.cst = sb("ml_cst", [128, 4], F32)
    ml.Qs = sb("ml_Qs", [128, 2, NS, 16], BF16)
    ml.QRs = sb("ml_QRs", [64, NS, 16], BF16)
    ml.OLs = sb("ml_OLs", [128, 2, 16, NS], BF16)
    ml.ZSs = sb("ml_ZSs", [128, 16, NS], BF16)
    ml.KsT = sb("ml_KsT", [128, 3, NS], BF16)
    ml.ckvN = sb("ml_ckvN", [NS, 258], BF16)
    ml.wuv = sb("ml_wuv", [128, 2, E], BF16)
    ml.mskc = sb("ml_mskc", [NS, NS], F32)
    ml.ones16 = sb("ml_ones16", [NS, 128], F32)
    ml.maskB = sb("ml_maskB", [128, 128], BF16)
    ml.qg = sb("ml_qg", [128, 3], F32)
    ml.big_base = k.off[0]
    ml.cqn = sb("ml_cqn", [128, 3, TWA], BF16)
    ml.KTc = sb("ml_KTc", [128, 2, TWA], BF16)
    ml.KTr = sb("ml_KTr", [64, TWA], BF16)
    ml.ctok = sb("ml_ctok", [128, 17, 256], BF16)
    ml.cos2 = sb("ml_cos2", [64, TWA], BF16)
    ml.sin2 = sb("ml_sin2", [64, TWA], BF16)
    ml.t = [sb(f"ml_t{i}", [128, 576], F32) for i in range(3)]
    ml.sqb = sb("ml_sqb", [128, 512], BF16)
    ml.rA = sb("ml_rA", [128, 64], F32)
    ml.rB = sb("ml_rB", [128, 64], F32)
    ml.st = [sb(f"ml_st{i}", [128, 16], F32) for i in range(4)]
    ml.st_i = [0]
    early = k.off[0]
    ml.wkv = sb("ml_wkv", [128, 8, 320], BF16)
    ml.wq = sb("ml_wq", [128, 8, 384], BF16)
    ml.kvg = sb("ml_kvg", [128, 256], F32)
    ml.costk = sb("ml_costk", [128, 17, 32], F32)
    ml.sintk = sb("ml_sintk", [128, 17, 32], F32)
    ml.CK = sb("ml_CK", [128, 256], F32)
    ml.KRf = sb("ml_KRf", [128, 64], F32)
    ml.KRb = sb("ml_KRb", [128, 64], BF16)
    end_early = k.off[0]
    k.off[0] = early
    ml.QL = sb("ml_QL", [128, 2, TWA], BF16)
    ml.QR = sb("ml_QR", [64, TWA], BF16)
    ml.Pbf = sb("ml_Pbf", [128, T], BF16)
    ml.PT = sb("ml_PT", [128, 16, 128], BF16)
    ml.OLT = sb("ml_OLT", [128, 2, 512], BF16)
    ml.ZS = sb("ml_ZS", [128, 4, 512], BF16)
    ml.QN = sb("ml_QN", [128, 512], BF16)
    ml.wuq = sb("ml_wuq", [128, 3, 192], BF16)
    ml.wsw = sb("ml_wsw", [128, 3, 64], BF16)
    ml.wukr = sb("ml_wukr", [128, 2, 128], F32)
    ml.wukT = sb("ml_wukT", [128, 256], BF16)
    ml.wz = sb("ml_wz", [128, 8, 128], BF16)
    ml.Dg = sb("ml_Dg", [128, 128], BF16)
    k.off[0] = max(k.off[0], end_early)
    ml.end_a = k.off[0]
    k.off[0] = ml.big_base
    ml.KP = sb("ml_KP", [128, 64, 322], BF16)
    ml.ST = sb("ml_ST", [128, 65, 16], F32)
    ml.PTs = sb("ml_PTs", [128, 65, 16], BF16)
    ml.KTp = sb("ml_KTp", [128, 2, 384], BF16)
    ml.ptf = sb("ml_ptf", [128, NS * 64], F32)
    ml.idx = sb("ml_idx", [128, NS * 64], I32)
    ml.pti = sb("ml_pti", [128, NS * 64], I32)
    ml.sm = [sb(f"ml_sm{i}", [128, 32], F32) for i in range(4)]
    ml.olat = sb("ml_olat", [NS, 256], BF16)
    k.off[0] = max(k.off[0], ml.end_a)


def mla_trig(k, out_ap, out_b, ang_ap, shift, np_, w):
    ml = k.ml
    P = k.P
    t0, t0_b, _ = ml.t[0]
    t1, t1_b, _ = ml.t[1]
    a0 = t0[0:np_, 0:w]
    a1 = t1[0:np_, 0:w]
    a1i = t1[:].bitcast(I32)[0:np_, 0:w]
    TWO_PI = 2 * PI_
    rd = [ml.t[2][1]]
    op_ts(k, 'dve', a0, ang_ap, 1.0 / TWO_PI, shift / TWO_PI, ALU.mult, ALU.add, rd, [t0_b])
    P.op('dve', lambda e: e.tensor_copy(out=a1i, in_=a0), [t0_b], [t1_b])
    P.op('dve', lambda e: e.tensor_copy(out=a0, in_=a1i), [t1_b], [t0_b])
    op_stt(k, 'dve', a0, a0, -TWO_PI, ang_ap, ALU.mult, ALU.add, [t0_b] + rd, [t0_b])
    op_ts(k, 'dve', a1, a0, shift, 0.0, ALU.add, ALU.is_lt, [t0_b], [t1_b])
    op_stt(k, 'dve', a0, a1, TWO_PI, a0, ALU.mult, ALU.add, [t0_b, t1_b], [t0_b])
    col = 1 if abs(shift - PI_) < 1e-9 else 2
    return op_act(k, out_ap, a0, AF.Sin, [t0_b, ml.cst[1]], [out_b], bias=ml.cst[0][0:np_, col:col + 1])


def layer_mla(k, ps_):
    P, dr, nc = k.P, k.dr, k.nc
    if not hasattr(k, "ml"):
        mla_setup(k)
    ml = k.ml
    TW = TWA if ps_ == 0 else T
    tiles = ntiles(ps_)
    c_w_in, c_w_in_b = dr["c_w_in"], k.dbuf["c_w_in"]

    def nst():
        s_ = ml.st[ml.st_i[0] % 4]
        ml.st_i[0] += 1
        return s_

    def ps5():
        i = k.ps_i[0] % 5
        k.ps_i[0] += 1
        return k.ps[i]

    cst, cst_b, _ = ml.cst
    P.op('dve', lambda e: e.memset(cst[:, 0:1], -PI_), writes=[cst_b])
    P.op('dve', lambda e: e.memset(cst[:, 1:2], 0.0), writes=[cst_b])
    P.op('dve', lambda e: e.memset(cst[:, 2:3], 0.5 * PI_), writes=[cst_b])
    load_w_cols(k, ml.wkv[0], ml.wkv[1], c_w_in, c_w_in_b, 384, ncols=320)
    load_w_cols(k, ml.wq[0], ml.wq[1], c_w_in, c_w_in_b, 0, ncols=384)
    op_dma(k, 'pool', ml.wuv[0][:, :, :], dr["c_w_uv"].rearrange("(c p) h v -> p c (h v)", p=128), [k.dbuf["c_w_uv"]], [ml.wuv[1]])
    op_dma(k, 'sp', ml.kvg[0][:], dr["c_kv_norm"].rearrange("(o c) -> o c", o=1).partition_broadcast(128), [k.dbuf["c_kv_norm"]], [ml.kvg[1]])
    op_dma(k, 'sp', ml.qg[0][:], dr["c_q_norm"].rearrange("(c p) -> p c", p=128), [k.dbuf["c_q_norm"]], [ml.qg[1]], slow=True)
    tf, tf_b, _ = ml.t[0]
    P.op('pool', lambda e: e.memset(tf[:, 0:128], 0.0), writes=[tf_b])
    P.op('pool', lambda e: e.affine_select(out=tf[:, 0:128], in_=tf[:, 0:128], pattern=[[-1, 128]], compare_op=ALU.is_ge, fill=-1e9,
                                           base=0, channel_multiplier=1), reads=[tf_b], writes=[tf_b])
    P.op('dve', lambda e: e.tensor_copy(out=ml.maskB[0][:], in_=tf[:, 0:128]), reads=[tf_b], writes=[ml.maskB[1]])
    P.op('pool', lambda e: e.memset(ml.mskc[0][:], 0.0), writes=[ml.mskc[1]])
    P.op('pool', lambda e: e.affine_select(out=ml.mskc[0][:], in_=ml.mskc[0][:], pattern=[[-1, NS]], compare_op=ALU.is_equal, fill=-1e9,
                                           base=0, channel_multiplier=1), reads=[ml.mskc[1]], writes=[ml.mskc[1]])
    P.op('dve', lambda e: e.memset(ml.ones16[0][:], 1.0), writes=[ml.ones16[1]])
    ti_, ti_b, _ = ml.t[1]
    tii = ti_[:].bitcast(I32)
    P.op('pool', lambda e: e.iota(tii[:, 0:32], pattern=[[1, 32]], base=0, channel_multiplier=0), writes=[ti_b])
    invf, invf_b, _ = ml.rA
    P.op('dve', lambda e: e.tensor_copy(out=invf[:, 0:32], in_=tii[:, 0:32]), reads=[ti_b], writes=[invf_b])
    op_act(k, invf[:, 0:32], invf[:, 0:32], AF.Exp, [invf_b], [invf_b], scale=-math.log(10000.0) / 32.0)
    P.op('pool', lambda e: e.iota(tii[:, 64:80], pattern=[[128, 16]], base=0, channel_multiplier=1), writes=[ti_b])
    posf, posf_b, _ = ml.rB
    P.op('dve', lambda e: e.tensor_copy(out=posf[:, 0:16], in_=tii[:, 64:80]), reads=[ti_b], writes=[posf_b])
    P.op('dve', lambda e: e.memset(posf[:, 16:17], 8192.0), writes=[posf_b])
    ang, ang_b, _ = ml.t[2]
    angv = ang[:, 0:17 * 32].rearrange("p (a i) -> p a i", i=32)
    op_tt(k, 'dve', angv, posf[:, 0:17].unsqueeze(2).to_broadcast([128, 17, 32]), invf[:, 0:32].unsqueeze(1).to_broadcast([128, 17, 32]),
          ALU.mult, [posf_b, invf_b], [ang_b])
    tmp, tmp_b, _ = ml.t[0]
    mla_trig(k, ml.sintk[0][:, :, :].rearrange("p a i -> p (a i)"), ml.sintk[1], ang[:, 0:544], PI_, 128, 544)
    mla_trig(k, ml.costk[0][:, :, :].rearrange("p a i -> p (a i)"), ml.costk[1], ang[:, 0:544], 1.5 * PI_, 128, 544)
    P.op('pool', lambda e: e.iota(tii[0:32, 0:1], pattern=[[1, 1]], base=0, channel_multiplier=1), writes=[ti_b])
    P.op('pool', lambda e: e.iota(tii[32:64, 0:1], pattern=[[1, 1]], base=0, channel_multiplier=1), writes=[ti_b])
    P.op('dve', lambda e: e.tensor_copy(out=invf[0:64, 32:33], in_=tii[0:64, 0:1]), reads=[ti_b], writes=[invf_b])
    op_act(k, invf[0:64, 33:34], invf[0:64, 32:33], AF.Exp, [invf_b], [invf_b], scale=-math.log(10000.0) / 32.0)
    P.op('dve', lambda e: e.memset(invf[0:32, 34:35], -1.0), writes=[invf_b])
    P.op('dve', lambda e: e.memset(invf[32:64, 34:35], 1.0), writes=[invf_b])
    for ti, (c0, w) in enumerate(tiles):
        if c0 < T:
            P.op('pool', lambda e, c0=c0: e.iota(tii[0:64, 0:512], pattern=[[1, 512]], base=c0, channel_multiplier=0), writes=[ti_b])
            P.op('dve', lambda e: e.tensor_copy(out=ang[0:64, 0:512], in_=tii[0:64, 0:512]), reads=[ti_b], writes=[ang_b])
        else:
            P.op('dve', lambda e: e.memset(ang[0:64, 0:NS], 8192.0), writes=[ang_b])
        op_ts(k, 'dve', ang[0:64, 0:w], ang[0:64, 0:w], invf[0:64, 33:34], None, ALU.mult, None, [ang_b, invf_b], [ang_b])
        mla_trig(k, ml.cos2[0][:, c0:c0 + w], ml.cos2[1], ang[0:64, 0:w], 1.5 * PI_, 64, w)
        mla_trig(k, ti_[0:64, 0:w], ti_b, ang[0:64, 0:w], PI_, 64, w)
        op_ts(k, 'dve', ml.sin2[0][:, c0:c0 + w], ti_[0:64, 0:w], invf[0:64, 34:35], None, ALU.mult, None, [ti_b, invf_b], [ml.sin2[1]])
    P.chk("m_tab")
    ttiles = [(tt * 128, 128, tt) for tt in range(16)]
    if ps_ == 0:
        ttiles.append((T, NS, 16))
    ctok, ctok_b, _ = ml.ctok
    for (c0, np_, tt) in ttiles:
        pkv, pkv_b = k.psn()
        for kk in range(8):
            op_mm(k, pkv[0:np_, 0:320], k.hT[:, kk, c0 + 1:c0 + 1 + np_], ml.wkv[0][:, kk, :], kk == 0, kk == 7,
                  [ml.wkv[1], k.hT_b.sub(c0 // 128)], [pkv_b])
        st, st_b, _ = nst()
        P.op('act', lambda e, np_=np_, pkv=pkv, st=st: e.activation(out=ml.sqb[0][0:np_, 0:256], in_=pkv[0:np_, 0:256],
                                                                    func=AF.Square, accum_out=st[0:np_, 0:1]),
             [pkv_b], [ml.sqb[1], st_b])
        rstd_from_ss(k, st[0:np_, 0:1], st_b, st[0:np_, 1:2], st_b, np_, 1.0 / 256, 0)
        CK, CK_b, _ = ml.CK
        op_stt(k, 'dve', CK[0:np_, :], pkv[0:np_, 0:256], st[0:np_, 1:2], ml.kvg[0][0:np_, :], ALU.mult, ALU.mult,
               [pkv_b, st_b, ml.kvg[1]], [CK_b])
        if c0 < T:
            op_dma(k, 'sp', dr["ckv_p"][ps_, c0:c0 + 128, :], CK[:, :], [CK_b], [k.dbuf["ckv_p"]])
        else:
            op_dma(k, 'sp', dr["ckv_s"][:, :], CK[0:NS, :], [CK_b], [k.dbuf["ckv_s"]])
            op_act(k, ml.ckvN[0][:, 0:256], CK[0:NS, :], AF.Copy, [CK_b], [ml.ckvN[1]])
            P.op('dve', lambda e: e.memset(ml.ckvN[0][:, 256:258], 1.0), writes=[ml.ckvN[1]])
        op_act(k, ctok[0:np_, tt, :], CK[0:np_, :], AF.Copy, [CK_b], [ctok_b.sub(tt)])
        rA, rA_b, _ = ml.rA
        rB, rB_b, _ = ml.rB
        KRf, KRf_b, _ = ml.KRf
        kr3 = pkv[0:np_, 256:320].rearrange("p (a i) -> p a i", i=32)
        op_tt(k, 'dve', rA[0:np_, 0:64].rearrange("p (a i) -> p a i", i=32), kr3,
              ml.costk[0][0:np_, tt:tt + 1, :].to_broadcast([np_, 2, 32]), ALU.mult, [pkv_b, ml.costk[1]], [rA_b])
        op_tt(k, 'dve', rB[0:np_, 0:64].rearrange("p (a i) -> p a i", i=32), kr3,
              ml.sintk[0][0:np_, tt:tt + 1, :].to_broadcast([np_, 2, 32]), ALU.mult, [pkv_b, ml.sintk[1]], [rB_b])
        op_tt(k, 'dve', KRf[0:np_, 0:32], rA[0:np_, 0:32], rB[0:np_, 32:64], ALU.subtract, [rA_b, rB_b], [KRf_b])
        op_tt(k, 'dve', KRf[0:np_, 32:64], rB[0:np_, 0:32], rA[0:np_, 32:64], ALU.add, [rA_b, rB_b], [KRf_b])
        if c0 < T:
            op_dma(k, 'sp', dr["kr_p"][ps_, c0:c0 + 128, :], KRf[:, :], [KRf_b], [k.dbuf["kr_p"]])
        else:
            op_dma(k, 'sp', dr["kr_s"][:, :], KRf[0:NS, :], [KRf_b], [k.dbuf["kr_s"]])
        KRb, KRb_b, _ = ml.KRb
        op_act(k, KRb[0:np_, :], KRf[0:np_, :], AF.Copy, [KRf_b], [KRb_b])
        psT, psT_b = k.psT
        for j in range(2):
            op_tr(k, psT[:, j * 128:j * 128 + np_], ctok[0:np_, tt, j * 128:(j + 1) * 128], k.identB[0:np_, 0:np_],
                  [ctok_b.sub(tt), k.identB_b], [psT_b])
        op_tr(k, psT[0:64, 256:256 + np_], KRb[0:np_, :], k.identB[0:np_, 0:np_], [KRb_b, k.identB_b], [psT_b])
        op_act(k, ml.KTc[0][:, :, c0:c0 + np_], psT[:, 0:256].rearrange("p (j t) -> p j t", t=128)[:, :, 0:np_], AF.Copy,
               [psT_b], [ml.KTc[1].sub(tt)])
        P.op('dve', lambda e, c0=c0, np_=np_: e.tensor_copy(out=ml.KTr[0][:, c0:c0 + np_], in_=psT[0:64, 256:256 + np_]),
             [psT_b], [ml.KTr[1].sub(tt)])
    if ps_ == 0:
        op_act(k, ml.KsT[0][:, 0:2, :], ml.KTc[0][:, :, T:T + NS], AF.Copy, [ml.KTc[1].sub(16)], [ml.KsT[1]])
        op_act(k, ml.KsT[0][0:64, 2, :], ml.KTr[0][:, T:T + NS], AF.Copy, [ml.KTr[1].sub(16)], [ml.KsT[1]])
    P.chk("m_1")
    for ti, (c0, w) in enumerate(tiles):
        pqs = []
        for qc in range(3):
            pq, pq_b = k.psn()
            for kk in range(8):
                op_mm(k, pq[:, 0:w], ml.wq[0][:, kk, qc * 128:(qc + 1) * 128], k.hT[:, kk, c0 + 1:c0 + 1 + w], kk == 0, kk == 7,
                      [ml.wq[1]] + hT_reads(k, c0, w), [pq_b])
            pqs.append((pq, pq_b))
        pss, pss_b = k.psn()
        for qc in range(3):
            sqb, sqb_b, _ = ml.sqb
            op_act(k, sqb[:, 0:w], pqs[qc][0][:, 0:w], AF.Square, [pqs[qc][1]], [sqb_b])
            op_mm(k, pss[:, 0:w], k.onesB[:], sqb[:, 0:w], qc == 0, qc == 2, [k.onesB_b, sqb_b], [pss_b])
        rst, rst_b, _ = ml.t[0]
        op_act(k, rst[:, 0:w], pss[:, 0:w], AF.Sqrt, [pss_b, k.epsr_b], [rst_b], scale=1.0 / 384, bias=k.epsr[:, 0:1])
        P.op('dve', lambda e, w=w: e.reciprocal(out=rst[:, 0:w], in_=rst[:, 0:w]), [rst_b], [rst_b])
        for qc in range(3):
            op_stt(k, 'dve', ml.cqn[0][:, qc, c0:c0 + w], pqs[qc][0][:, 0:w], ml.qg[0][:, qc:qc + 1], rst[:, 0:w], ALU.mult, ALU.mult,
                   [pqs[qc][1], ml.qg[1], rst_b], [ml.cqn[1].sub(ti)])
    P.chk("m_2")
    P.barrier()
    QL, QL_b, _ = ml.QL
    QR, QR_b, _ = ml.QR
    for h in range(16):
        wuq, wuq_b, _ = ml.wuq
        op_dma(k, 'pool', wuq[:, :, :], dr["c_w_uq"][:, h, :].rearrange("(c p) e -> p c e", p=128), [k.dbuf["c_w_uq"]], [wuq_b])
        wsw, wsw_b, _ = ml.wsw
        P.op('dve', lambda e: e.tensor_copy(out=wsw[:, :, 0:32], in_=wuq[:, :, 160:192]), [wuq_b], [wsw_b])
        P.op('dve', lambda e: e.tensor_copy(out=wsw[:, :, 32:64], in_=wuq[:, :, 128:160]), [wuq_b], [wsw_b])
        wukr, wukr_b, _ = ml.wukr
        op_dma(k, 'sp', wukr[:, :, :], dr["c_w_uk"][:, h, :].rearrange("(c p) n -> p c n", p=128), [k.dbuf["c_w_uk"]], [wukr_b])
        pw_, pw_b = k.psn()
        for cc in range(2):
            op_tr(k, pw_[:, cc * 128:(cc + 1) * 128], wukr[:, cc, :], k.identF[:], [wukr_b, k.identF_b], [pw_b])
        op_act(k, ml.wukT[0][:, :], pw_[:, 0:256], AF.Copy, [pw_b], [ml.wukT[1]])
        load_w_cols(k, ml.wz[0], ml.wz[1], c_w_in, c_w_in_b, 704 + h * 128)
        for ti, (c0, w) in enumerate(tiles):
            rq = [ml.cqn[1].sub(ti)]
            pqn, pqn_b = k.psn()
            for qc in range(3):
                op_mm(k, pqn[:, 0:w], wuq[:, qc, 0:128], ml.cqn[0][:, qc, c0:c0 + w], qc == 0, qc == 2, [wuq_b] + rq, [pqn_b])
            pra, pra_b = k.psn()
            for qc in range(3):
                op_mm(k, pra[0:64, 0:w], wuq[:, qc, 128:192], ml.cqn[0][:, qc, c0:c0 + w], qc == 0, qc == 2, [wuq_b] + rq, [pra_b])
            prb, prb_b = k.psn()
            for qc in range(3):
                op_mm(k, prb[0:64, 0:w], wsw[:, qc, :], ml.cqn[0][:, qc, c0:c0 + w], qc == 0, qc == 2, [wsw_b] + rq, [prb_b])
            QN, QN_b, _ = ml.QN
            op_act(k, QN[:, 0:w], pqn[:, 0:w], AF.Copy, [pqn_b], [QN_b])
            t1, t1_b, _ = ml.t[1]
            t2, t2_b, _ = ml.t[2]
            op_tt(k, 'dve', t1[0:64, 0:w], pra[0:64, 0:w], ml.cos2[0][:, c0:c0 + w], ALU.mult, [pra_b, ml.cos2[1]], [t1_b])
            op_tt(k, 'dve', t2[0:64, 0:w], prb[0:64, 0:w], ml.sin2[0][:, c0:c0 + w], ALU.mult, [prb_b, ml.sin2[1]], [t2_b])
            op_tt(k, 'dve', QR[:, c0:c0 + w], t1[0:64, 0:w], t2[0:64, 0:w], ALU.add, [t1_b, t2_b], [QR_b.sub(ti)])
            for cc in range(2):
                pql, pql_b = k.psn()
                op_mm(k, pql[:, 0:w], ml.wukT[0][:, cc * 128:(cc + 1) * 128], QN[:, 0:w], True, True, [ml.wukT[1], QN_b], [pql_b])
                op_act(k, QL[:, cc, c0:c0 + w], pql[:, 0:w], AF.Copy, [pql_b], [QL_b.sub(ti)])
            pz, pz_b = k.psn()
            for kk in range(8):
                op_mm(k, pz[:, 0:w], ml.wz[0][:, kk, :], k.hT[:, kk, c0 + 1:c0 + 1 + w], kk == 0, kk == 7,
                      [ml.wz[1]] + hT_reads(k, c0, w), [pz_b])
            if c0 < T:
                op_act(k, ml.ZS[0][:, ti, :], pz[:, :], AF.Silu, [pz_b], [ml.ZS[1].sub(ti)])
            else:
                op_act(k, ml.ZSs[0][:, h, :], pz[:, 0:NS], AF.Silu, [pz_b], [ml.ZSs[1]])
                P.op('dve', lambda e, h=h: e.tensor_copy(out=ml.Qs[0][:, :, :, h], in_=QL[:, :, T:T + NS]), [QL_b.sub(ti)], [ml.Qs[1]])
                P.op('dve', lambda e, h=h: e.tensor_copy(out=ml.QRs[0][:, :, h], in_=QR[:, T:T + NS]), [QR_b.sub(ti)], [ml.QRs[1]])
        P.chk("m_q")
        Pbf, Pbf_b, _ = ml.Pbf
        PT, PT_b, _ = ml.PT
        pov = [k.ps[5], k.ps[6]]
        for qb in range(16):
            t0 = qb * 128
            L = t0 + 128
            nb = (L + 511) // 512
            tq = qb // 4
            banks = [ps5() for _ in range(nb)]
            kt_r = [ml.KTc[1].sub(x) for x in range(qb + 1)] + [ml.KTr[1].sub(x) for x in range(qb + 1)]
            for b in range(nb):
                l0 = b * 512
                lw = min(512, L - l0)
                bk, bk_b = banks[b]
                last = (b == nb - 1)
                op_mm(k, bk[:, 0:lw], QL[:, 0, t0:t0 + 128], ml.KTc[0][:, 0, l0:l0 + lw], True, False, [QL_b.sub(tq)] + kt_r, [bk_b])
                op_mm(k, bk[:, 0:lw], QL[:, 1, t0:t0 + 128], ml.KTc[0][:, 1, l0:l0 + lw], False, False, [QL_b.sub(tq)] + kt_r, [bk_b])
                op_mm(k, bk[:, 0:lw], QR[:, t0:t0 + 128], ml.KTr[0][:, l0:l0 + lw], False, not last, [QR_b.sub(tq)] + kt_r, [bk_b])
                if last:
                    dcol = t0 - l0
                    op_mm(k, bk[:, dcol:dcol + 128], k.identB[:], ml.maskB[0][:], False, True, [k.identB_b, ml.maskB[1]], [bk_b])
            st, st_b, _ = nst()
            for b in range(nb):
                lw = min(512, L - b * 512)
                bk, bk_b = banks[b]
                P.op('dve', lambda e, b=b, lw=lw, bk=bk, st=st: e.tensor_reduce(out=st[:, b:b + 1], in_=bk[:, 0:lw], axis=AX.X, op=ALU.max),
                     [bk_b], [st_b])
            if nb > 1:
                P.op('dve', lambda e, nb=nb, st=st: e.tensor_reduce(out=st[:, 4:5], in_=st[:, 0:nb], axis=AX.X, op=ALU.max), [st_b], [st_b])
                mcol = st[:, 4:5]
            else:
                mcol = st[:, 0:1]
            op_ts(k, 'dve', st[:, 5:6], mcol, -MLA_SCALE_, None, ALU.mult, None, [st_b], [st_b])
            for b in range(nb):
                l0 = b * 512
                lw = min(512, L - l0)
                bk, bk_b = banks[b]
                P.op('act', lambda e, b=b, l0=l0, lw=lw, bk=bk, st=st: e.activation(out=Pbf[:, l0:l0 + lw], in_=bk[:, 0:lw], func=AF.Exp,
                                                                                   scale=MLA_SCALE_, bias=st[:, 5:6],
                                                                                   accum_out=st[:, 8 + b:9 + b]),
                     [bk_b, st_b], [Pbf_b.sub(b), st_b])
            if nb > 1:
                P.op('dve', lambda e, nb=nb, st=st: e.tensor_reduce(out=st[:, 6:7], in_=st[:, 8:8 + nb], axis=AX.X, op=ALU.add), [st_b], [st_b])
                scol = st[:, 6:7]
            else:
                scol = st[:, 8:9]
            P.op('dve', lambda e, st=st, scol=scol: e.reciprocal(out=st[:, 7:8], in_=scol), [st_b], [st_b])
            Dg, Dg_b, _ = ml.Dg
            op_ts(k, 'dve', Dg[:, :], k.identF[:, :], st[:, 7:8], None, ALU.mult, None, [k.identF_b, st_b], [Dg_b])
            for g0 in range(0, qb + 1, 4):
                g1 = min(qb + 1, g0 + 4)
                pt, pt_b = ps5()
                for kb in range(g0, g1):
                    op_mm(k, pt[:, (kb - g0) * 128:(kb - g0 + 1) * 128], Pbf[:, kb * 128:(kb + 1) * 128], Dg[:, :], True, True,
                          [Pbf_b.sub(kb // 4), Dg_b], [pt_b])
                n_ = g1 - g0
                if (g0 // 4) % 2 == 0:
                    op_act(k, PT[:, g0:g1, :], pt[:, 0:n_ * 128].rearrange("p (g t) -> p g t", t=128), AF.Copy, [pt_b], [PT_b.sub(g0 // 4)])
                else:
                    P.op('dve', lambda e, g0=g0, g1=g1, n_=n_, pt=pt: e.tensor_copy(
                        out=PT[:, g0:g1, :], in_=pt[:, 0:n_ * 128].rearrange("p (g t) -> p g t", t=128)), [pt_b], [PT_b.sub(g0 // 4)])
            qc_ = slice((qb % 4) * 128, (qb % 4 + 1) * 128)
            for cc in range(2):
                pv_, pv_b = pov[cc]
                for kb in range(qb + 1):
                    op_mm(k, pv_[:, qc_], ctok[:, kb, cc * 128:(cc + 1) * 128], PT[:, kb, :], kb == 0, kb == qb,
                          [ctok_b.sub(kb), PT_b.sub(kb // 4)], [pv_b])
            if qb % 4 == 3:
                OLT, OLT_b, _ = ml.OLT
                op_act(k, OLT[:, 0, :], pov[0][0][:, :], AF.Copy, [pov[0][1]], [OLT_b])
                P.op('dve', lambda e: e.tensor_copy(out=OLT[:, 1, :], in_=pov[1][0][:, :]), [pov[1][1]], [OLT_b])
                po, po_b = ps5()
                for cc in range(2):
                    op_mm(k, po[:, :], ml.wuv[0][:, cc, h * 128:(h + 1) * 128], OLT[:, cc, :], cc == 0, cc == 1, [ml.wuv[1], OLT_b], [po_b])
                op_tt(k, 'dve', k.GT[:, h, tq * 512:(tq + 1) * 512], po[:, :], ml.ZS[0][:, tq, :], ALU.mult, [po_b, ml.ZS[1].sub(tq)],
                      [k.GT_b.sub(h).sub(tq)])
            P.chk("m_att")
    if ps_ == 0:
        P.barrier()
        mla_sample(k)


def mla_sample(k):
    P, dr, nc, ml = k.P, k.dr, k.nc, k.ml
    KP, KP_b, _ = ml.KP
    ST, ST_b, _ = ml.ST
    PTs, PTs_b, _ = ml.PTs
    ptf, ptf_b, _ = ml.ptf
    pti, pti_b, _ = ml.pti
    idx, idx_b, _ = ml.idx
    op_dma(k, 'sp', pti[:, :], dr["page_table"].rearrange("s j -> (s j)").rearrange("(o n) -> o n", o=1).partition_broadcast(128),
           [k.dbuf["page_table"]], [pti_b])
    P.op('dve', lambda e: e.tensor_copy(out=ptf[:, :], in_=pti[:, :]), [pti_b], [ptf_b])
    sm0, sm0_b, _ = ml.sm[0]
    smi = sm0[:].bitcast(I32)
    P.op('pool', lambda e: e.iota(smi[:, 0:1], pattern=[[1, 1]], base=0, channel_multiplier=1), writes=[sm0_b])
    P.op('dve', lambda e: e.tensor_copy(out=sm0[:, 1:2], in_=smi[:, 0:1]), [sm0_b], [sm0_b])
    op_ts(k, 'dve', ptf[:, :], ptf[:, :], 128.0, sm0[:, 1:2], ALU.mult, ALU.add, [ptf_b, sm0_b], [ptf_b])
    P.op('dve', lambda e: e.tensor_copy(out=idx[:, :], in_=ptf[:, :]), [ptf_b], [idx_b])
    P.op('dve', lambda e: e.memset(KP[:, :, 320:322], 1.0), writes=[KP_b])
    ckv_rows = dr["cache_ckv"].rearrange("n t c -> (n t) c")
    kr_rows = dr["cache_krope"].rearrange("n t c -> (n t) c")
    P.chk("s_idx")
    for s in range(NS):
        for j in range(64):
            n = s * 64 + j
            P.op('pool', lambda e, j=j, n=n: e.indirect_dma_start(out=KP[:, j, 64:320], out_offset=None, in_=ckv_rows,
                                                                  in_offset=bass.IndirectOffsetOnAxis(ap=idx[:, n:n + 1], axis=0)),
                 [idx_b, k.dbuf["cache_ckv"]], [KP_b.sub(j)], dma=True)
            P.op('pool', lambda e, j=j, n=n: e.indirect_dma_start(out=KP[:, j, 0:64], out_offset=None, in_=kr_rows,
                                                                  in_offset=bass.IndirectOffsetOnAxis(ap=idx[:, n:n + 1], axis=0)),
                 [idx_b, k.dbuf["cache_krope"]], [KP_b.sub(j)], dma=True)
        P.chk("s_gather")
        psT, psT_b = k.psT
        pS = [k.psn(), k.psn()]
        for j in range(64):
            o_ = (j % 2) * 384
            if True:
                op_tr(k, psT[0:64, o_:o_ + 128], KP[:, j, 0:64], k.identB[:], [KP_b.sub(j), k.identB_b], [psT_b])
                op_tr(k, psT[:, o_ + 128:o_ + 256], KP[:, j, 64:192], k.identB[:], [KP_b.sub(j), k.identB_b], [psT_b])
                op_tr(k, psT[:, o_ + 256:o_ + 384], KP[:, j, 192:320], k.identB[:], [KP_b.sub(j), k.identB_b], [psT_b])
            if j % 2 == 1:
                KTp, KTp_b, _ = ml.KTp
                op_act(k, KTp[:, :, :], psT[:, 0:768].rearrange("p (a c) -> p a c", c=384), AF.Copy, [psT_b], [KTp_b])
                for jj in (j - 1, j):
                    a = jj % 2
                    bk, bk_b = pS[jj // 32]
                    cs_ = slice((jj % 32) * 16, (jj % 32 + 1) * 16)
                    op_mm(k, bk[:, cs_], KTp[0:64, a, 0:128], ml.QRs[0][:, s, :], True, False, [KTp_b, ml.QRs[1]], [bk_b])
                    op_mm(k, bk[:, cs_], KTp[:, a, 128:256], ml.Qs[0][:, 0, s, :], False, False, [KTp_b, ml.Qs[1]], [bk_b])
                    op_mm(k, bk[:, cs_], KTp[:, a, 256:384], ml.Qs[0][:, 1, s, :], False, True, [KTp_b, ml.Qs[1]], [bk_b])
        for g in range(2):
            bk, bk_b = pS[g]
            P.op('dve', lambda e, g=g, bk=bk: e.tensor_copy(out=ST[:, g * 32:(g + 1) * 32, :], in_=bk[:, :].rearrange("p (j h) -> p j h", h=16)),
                 [bk_b], [ST_b])
        pN, pN_b = k.psn()
        op_mm(k, pN[0:NS, 0:16], ml.KsT[0][0:64, 2, :], ml.QRs[0][:, s, :], True, False, [ml.KsT[1], ml.QRs[1]], [pN_b])
        op_mm(k, pN[0:NS, 0:16], ml.KsT[0][:, 0, :], ml.Qs[0][:, 0, s, :], False, False, [ml.KsT[1], ml.Qs[1]], [pN_b])
        op_mm(k, pN[0:NS, 0:16], ml.KsT[0][:, 1, :], ml.Qs[0][:, 1, s, :], False, True, [ml.KsT[1], ml.Qs[1]], [pN_b])
        P.op('dve', lambda e: e.memset(ST[:, 64, :], -1e9), writes=[ST_b])
        op_ts(k, 'dve', ST[0:NS, 64, :], pN[0:NS, 0:16], ml.mskc[0][:, s:s + 1], None, ALU.add, None, [pN_b, ml.mskc[1]], [ST_b])
        sm1, sm1_b, _ = ml.sm[1]
        P.op('dve', lambda e: e.tensor_reduce(out=sm1[:, 0:16], in_=ST[:, :, :].rearrange("p j h -> p h j"), axis=AX.X, op=ALU.max),
             [ST_b], [sm1_b])
        pM, pM_b = k.psn()
        op_tr(k, pM[0:16, 0:128], sm1[:, 0:16], k.identF[:], [sm1_b, k.identF_b], [pM_b])
        sm2, sm2_b, _ = ml.sm[2]
        P.op('dve', lambda e, pM=pM: e.tensor_reduce(out=sm2[0:16, 0:1], in_=pM[0:16, 0:128], axis=AX.X, op=ALU.max), [pM_b], [sm2_b])
        op_ts(k, 'dve', sm2[0:16, 16:32], k.identF[0:16, 0:16], sm2[0:16, 0:1], None, ALU.mult, None, [k.identF_b, sm2_b], [sm2_b])
        pB, pB_b = k.psn()
        op_mm(k, pB[:, 0:16], ml.ones16[0][:, :], sm2[0:16, 16:32], True, True, [ml.ones16[1], sm2_b], [pB_b])
        sm3, sm3_b, _ = ml.sm[3]
        op_act(k, sm3[:, 0:16], pB[:, 0:16], AF.Copy, [pB_b], [sm3_b], scale=-MLA_SCALE_)
        op_stt(k, 'dve', ST[:, :, :], ST[:, :, :], MLA_SCALE_, sm3[:, 0:16].unsqueeze(1).to_broadcast([128, 65, 16]), ALU.mult, ALU.add,
               [ST_b, sm3_b], [ST_b])
        op_act(k, PTs[:, :, :], ST[:, :, :], AF.Exp, [ST_b], [PTs_b])
        pO, pO_b = k.psn()
        for j in range(64):
            op_mm(k, pO[0:16, 0:257], PTs[:, j, :], KP[:, j, 64:321], j == 0, False, [PTs_b, KP_b.sub(j)], [pO_b])
        op_mm(k, pO[0:16, 0:257], PTs[0:NS, 64, :], ml.ckvN[0][:, 0:257], False, True, [PTs_b, ml.ckvN[1]], [pO_b])
        P.op('dve', lambda e, pO=pO: e.reciprocal(out=sm2[0:16, 1:2], in_=pO[0:16, 256:257]), [pO_b, sm2_b], [sm2_b])
        olat, olat_b, _ = ml.olat
        op_ts(k, 'dve', olat[:, :], pO[0:16, 0:256], sm2[0:16, 1:2], None, ALU.mult, None, [pO_b, sm2_b], [olat_b])
        for cc in range(2):
            op_tr(k, psT[:, cc * 16:(cc + 1) * 16], olat[:, cc * 128:(cc + 1) * 128], k.identB[0:16, 0:16], [olat_b, k.identB_b], [psT_b])
        op_act(k, ml.OLs[0][:, :, :, s], psT[:, 0:32].rearrange("p (c h) -> p c h", h=16), AF.Copy, [psT_b], [ml.OLs[1]])
        P.chk("s_one")
    for h in range(16):
        po, po_b = k.psn()
        for cc in range(2):
            op_mm(k, po[:, 0:NS], ml.wuv[0][:, cc, h * 128:(h + 1) * 128], ml.OLs[0][:, cc, h, :], cc == 0, cc == 1,
                  [ml.wuv[1], ml.OLs[1]], [po_b])
        op_tt(k, 'dve', k.GT[:, h, T:T + NS], po[:, 0:NS], ml.ZSs[0][:, h, :], ALU.mult, [po_b, ml.ZSs[1]], [k.GT_b.sub(h).sub(4)])

S5C = 64
S5NCH = T // S5C


def s5_setup(k):
    P, dr, nc, sb = k.P, k.dr, k.nc, k.sb
    s5 = K()
    k.s5 = s5
    s5.cst = sb("s5_cst", [128, 4], F32)
    s5.P64 = {n: sb("s5_p_" + n, [128, 64], F32) for n in
              ["lr", "li", "dt", "m", "th", "are", "aim", "fre", "fim", "t0", "t1", "lnm", "m64", "ph"]}
    s5.LB = [sb(f"s5_LB{i}", [128, 16, 128], BF16) for i in range(2)]
    s5.LC = [sb(f"s5_LC{i}", [128, 16, 128], BF16) for i in range(2)]
    s5.LC3 = [sb(f"s5_LC3{i}", [128, 16, 64], BF16) for i in range(2)]
    s5.Uz = sb("s5_Uz", [128, TWA], BF16)
    s5.mask4 = sb("s5_mask4", [128, 128], F32)
    s5.par = {n: sb("s5_par_" + n, [128, 16], F32) for n in ["d", "bg"]}
    s5.m01 = sb("s5_m01", [128, T], BF16)
    s5.U = sb("s5_U", [128, TWA], BF16)
    s5.wt = sb("s5_wt", [128, 8, 128], BF16)
    s5.tab = {n: sb("s5_tab_" + n, [128, 64], F32) for n in ["Fc", "Fs", "Bc", "Bs", "ang", "mg", "a", "b", "c", "Gc", "Gs"]}
    s5.big = [sb(f"s5_big{i}", [128, T], F32) for i in range(5)]
    s5.sbf = [sb(f"s5_sbf{i}", [128, TWA], BF16) for i in range(2)]
    s5.ec_ = {n: sb("s5_e_" + n, [128, 64], F32) for n in ["er", "ei", "hr", "hi", "Er", "Ei", "cr", "ci"]}
    s5.fin = [sb(f"s5_fin{i}", [128, 64], F32) for i in range(2)]
    s5.s0T = [sb(f"s5_s0T{i}", [128, 64, NS], F32) for i in range(2)]
    s5.tmpn = [sb(f"s5_tmpn{i}", [128, NS], F32) for i in range(2)]
    s5.g = [sb(f"s5_g{i}", [128, 512], F32) for i in range(3)]
    s5.gb = sb("s5_gb", [128, 512], BF16)
    al = lambda name, shape, dt, o: (nc.alloc_sbuf_tensor_at(name, shape, dt, offset=o), Buf(name), o)
    s5.srow = al("s5_srow", [NS, 2048], F32, s5.big[0][2])
    s5.wg = al("s5_wg", [128, 16, 128], BF16, s5.big[1][2])
    s5.XX = [al("s5_XX0", [128, 64, 32], F32, s5.big[3][2]), al("s5_XX1", [128, 64, 32], F32, s5.big[4][2])]
    s5.bre = al("s5_bre", [128, 64, 16], F32, s5.sbf[0][2])
    s5.bim = al("s5_bim", [128, 64, 16], F32, s5.sbf[1][2])
    s5.CN = [al("s5_CN0", [128, 16, 64], F32, s5.big[2][2] + 4096), al("s5_CN1", [128, 16, 64], F32, s5.big[1][2] + 4096)]


def s5_trig(k, out_ap, out_b, ang_ap, ang_b, shift, w):
    P, s5 = k.P, k.s5
    a0, a0_b = s5.tab["a"][0][:, 0:w], s5.tab["a"][1]
    a1, a1_b = s5.tab["b"][0][:, 0:w], s5.tab["b"][1]
    a1i = s5.tab["b"][0][:].bitcast(I32)[:, 0:w]
    TWO_PI = 2 * PI_
    op_ts(k, 'dve', a0, ang_ap, 1.0 / TWO_PI, shift / TWO_PI, ALU.mult, ALU.add, [ang_b], [a0_b])
    P.op('dve', lambda e: e.tensor_copy(out=a1i, in_=a0), [a0_b], [a1_b])
    P.op('dve', lambda e: e.tensor_copy(out=a0, in_=a1i), [a1_b], [a0_b])
    op_stt(k, 'dve', a0, a0, -TWO_PI, ang_ap, ALU.mult, ALU.add, [a0_b, ang_b], [a0_b])
    op_ts(k, 'dve', a1, a0, shift, 0.0, ALU.add, ALU.is_lt, [a0_b], [a1_b])
    op_stt(k, 'dve', a0, a1, TWO_PI, a0, ALU.mult, ALU.add, [a0_b, a1_b], [a0_b])
    col = 1 if abs(shift - PI_) < 1e-9 else 2
    op_act(k, out_ap, a0, AF.Sin, [a0_b, s5.cst[1]], [out_b], bias=s5.cst[0][:, col:col + 1])


def s5_load(k):
    P, dr, nc, s5 = k.P, k.dr, k.nc, k.s5
    cst, cst_b, _ = s5.cst
    P.op('dve', lambda e: e.memset(cst[:, 0:1], -PI_), writes=[cst_b])
    P.op('dve', lambda e: e.memset(cst[:, 1:2], 0.0), writes=[cst_b])
    P.op('dve', lambda e: e.memset(cst[:, 2:3], 0.5 * PI_), writes=[cst_b])
    p = s5.P64
    for n, src in (("lr", "d_lambda_re"), ("li", "d_lambda_im")):
        for g2 in range(2):
            op_dma(k, 'sp', p[n][0][g2 * 64:(g2 + 1) * 64, :], dr[src].rearrange("(c g2) p -> g2 p c", g2=2)[g2],
                   [k.dbuf[src]], [p[n][1]], slow=True)
    for g2 in range(2):
        op_dma(k, 'sp', p["dt"][0][g2 * 64:(g2 + 1) * 64, :],
               dr["d_log_dt"].rearrange("(c g2) -> g2 c", g2=2)[g2:g2 + 1, :].partition_broadcast(64), [k.dbuf["d_log_dt"]], [p["dt"][1]],
               slow=True)
    A = lambda n: p[n][0][:, :]
    Bf = lambda n: p[n][1]
    op_act(k, A("dt"), A("dt"), AF.Exp, [Bf("dt")], [Bf("dt")])
    op_tt(k, 'dve', A("lnm"), A("lr"), A("dt"), ALU.mult, [Bf("lr"), Bf("dt")], [Bf("lnm")])
    op_act(k, A("m"), A("lnm"), AF.Exp, [Bf("lnm")], [Bf("m")])
    op_act(k, A("m64"), A("lnm"), AF.Exp, [Bf("lnm")], [Bf("m64")], scale=float(S5C))
    op_tt(k, 'dve', A("th"), A("li"), A("dt"), ALU.mult, [Bf("li"), Bf("dt")], [Bf("th")])
    op_ts(k, 'dve', A("ph"), A("th"), float(S5C), None, ALU.mult, None, [Bf("th")], [Bf("ph")])
    s5_trig(k, A("aim"), Bf("aim"), A("th"), Bf("th"), PI_, 64)
    s5_trig(k, A("are"), Bf("are"), A("th"), Bf("th"), 1.5 * PI_, 64)
    op_tt(k, 'dve', A("are"), A("are"), A("m"), ALU.mult, [Bf("are"), Bf("m")], [Bf("are")])
    op_tt(k, 'dve', A("aim"), A("aim"), A("m"), ALU.mult, [Bf("aim"), Bf("m")], [Bf("aim")])
    op_tt(k, 'dve', A("t0"), A("lr"), A("lr"), ALU.mult, [Bf("lr")], [Bf("t0")])
    op_tt(k, 'dve', A("t1"), A("li"), A("li"), ALU.mult, [Bf("li")], [Bf("t1")])
    op_tt(k, 'dve', A("t0"), A("t0"), A("t1"), ALU.add, [Bf("t0"), Bf("t1")], [Bf("t0")])
    P.op('dve', lambda e: e.reciprocal(out=A("t0"), in_=A("t0")), [Bf("t0")], [Bf("t0")])
    op_ts(k, 'dve', A("t1"), A("are"), -1.0, None, ALU.add, None, [Bf("are")], [Bf("t1")])
    op_tt(k, 'dve', A("fre"), A("t1"), A("lr"), ALU.mult, [Bf("t1"), Bf("lr")], [Bf("fre")])
    op_tt(k, 'dve', A("fim"), A("aim"), A("li"), ALU.mult, [Bf("aim"), Bf("li")], [Bf("fim")])
    op_tt(k, 'dve', A("fre"), A("fre"), A("fim"), ALU.add, [Bf("fre"), Bf("fim")], [Bf("fre")])
    op_tt(k, 'dve', A("fim"), A("aim"), A("lr"), ALU.mult, [Bf("aim"), Bf("lr")], [Bf("fim")])
    op_tt(k, 'dve', A("t1"), A("t1"), A("li"), ALU.mult, [Bf("t1"), Bf("li")], [Bf("t1")])
    op_tt(k, 'dve', A("fim"), A("fim"), A("t1"), ALU.subtract, [Bf("fim"), Bf("t1")], [Bf("fim")])
    op_tt(k, 'dve', A("fre"), A("fre"), A("t0"), ALU.mult, [Bf("fre"), Bf("t0")], [Bf("fre")])
    op_tt(k, 'dve', A("fim"), A("fim"), A("t0"), ALU.mult, [Bf("fim"), Bf("t0")], [Bf("fim")])
    for t_, src in ((s5.bre, "d_b_re"), (s5.bim, "d_b_im")):
        for g2 in range(2):
            op_dma(k, 'sp', t_[0][g2 * 64:(g2 + 1) * 64, :, :], dr[src].rearrange("(c g2) p q -> g2 p c q", g2=2)[g2],
                   [k.dbuf[src]], [t_[1]])
    for i in range(2):
        P.op('pool', lambda e, i=i: e.memset(s5.XX[i][0][:], 0.0), writes=[s5.XX[i][1]])
    big0, big0_b, _ = s5.big[0]
    big1, big1_b, _ = s5.big[1]
    v1 = lambda t: t[:, 0:1024].rearrange("p (c q) -> p c q", q=16)
    frb = A("fre").unsqueeze(2).to_broadcast([128, 64, 16])
    fib = A("fim").unsqueeze(2).to_broadcast([128, 64, 16])
    op_tt(k, 'dve', v1(big0), s5.bre[0][:], frb, ALU.mult, [s5.bre[1], Bf("fre")], [big0_b])
    op_tt(k, 'dve', v1(big1), s5.bim[0][:], fib, ALU.mult, [s5.bim[1], Bf("fim")], [big1_b])
    op_tt(k, 'dve', v1(big0), v1(big0), v1(big1), ALU.subtract, [big0_b, big1_b], [big0_b])
    for g2 in range(2):
        hP = slice(g2 * 64, g2 * 64 + 64)
        P.op('dve', lambda e, g2=g2, hP=hP: e.tensor_copy(out=s5.XX[0][0][hP, :, g2 * 16:(g2 + 1) * 16], in_=v1(big0)[hP]),
             [big0_b], [s5.XX[0][1]])
    op_tt(k, 'dve', v1(big0), s5.bim[0][:], frb, ALU.mult, [s5.bim[1], Bf("fre")], [big0_b])
    op_tt(k, 'dve', v1(big1), s5.bre[0][:], fib, ALU.mult, [s5.bre[1], Bf("fim")], [big1_b])
    op_tt(k, 'dve', v1(big0), v1(big0), v1(big1), ALU.add, [big0_b, big1_b], [big0_b])
    for g2 in range(2):
        hP = slice(g2 * 64, g2 * 64 + 64)
        P.op('dve', lambda e, g2=g2, hP=hP: e.tensor_copy(out=s5.XX[1][0][hP, :, g2 * 16:(g2 + 1) * 16], in_=v1(big0)[hP]),
             [big0_b], [s5.XX[1][1]])
    for i in range(2):
        for ec in range(16):
            pt, pt_b = k.psn()
            op_tr(k, pt[:, 0:128], s5.XX[i][0][:, 4 * ec:4 * ec + 4, :].rearrange("p a b -> p (a b)"), k.identF[:],
                  [s5.XX[i][1], k.identF_b], [pt_b])
            op_act(k, s5.LB[i][0][:, ec, :], pt[:, 0:128], AF.Copy, [pt_b], [s5.LB[i][1]])
    m4, m4_b, _ = s5.mask4
    P.op('dve', lambda e: e.memset(m4[:], 0.0), writes=[m4_b])
    big2, big2_b, _ = s5.big[2]
    P.op('dve', lambda e: e.tensor_reduce(out=big2[:, 4:6], in_=k.identF[:, :].rearrange("p (q g c) -> p g q c", q=4, g=2),
                                          axis=AX.XY, op=ALU.add), [k.identF_b], [big2_b])
    P.op('dve', lambda e: e.memset(m4[:], 1.0), writes=[m4_b])
    op_ts(k, 'dve', m4[:, 0:64], m4[:, 0:64], big2[:, 4:5], None, ALU.mult, None, [m4_b, big2_b], [m4_b])
    op_ts(k, 'dve', m4[:, 64:128], m4[:, 64:128], big2[:, 5:6], None, ALU.mult, None, [m4_b, big2_b], [m4_b])
    for i, src in ((0, "d_c_re"), (1, "d_c_im")):
        op_dma(k, 'sp', s5.CN[i][0][:, :, :], dr[src].rearrange("(e a) k p -> (a k) e p", a=8), [k.dbuf[src]], [s5.CN[i][1]])
        for ec in range(16):
            y4, y4_b, _ = s5.g[0]
            op_tt(k, 'dve', y4[:, 0:128].rearrange("p (a b) -> p a b", b=64), s5.CN[i][0][:, ec:ec + 1, :].to_broadcast([128, 2, 64]),
                  m4[:, :].rearrange("p (a b) -> p a b", b=64), ALU.mult, [s5.CN[i][1], m4_b], [y4_b])
            pt, pt_b = k.psn()
            op_tr(k, pt[:, 0:128], y4[:, 0:128], k.identF[:], [y4_b, k.identF_b], [pt_b])
            if i == 0:
                op_act(k, s5.LC[i][0][:, ec, :], pt[:, 0:128], AF.Copy, [pt_b], [s5.LC[i][1]])
            else:
                op_act(k, s5.LC[i][0][:, ec, :], pt[:, 0:128], AF.Copy, [pt_b], [s5.LC[i][1]], scale=-1.0)
    for i in range(2):
        P.op('dve', lambda e, i=i: e.tensor_copy(out=s5.LC3[i][0][:, :, :], in_=s5.LC[i][0][:, :, 64:128]), [s5.LC[i][1]], [s5.LC3[i][1]])
        P.op('dve', lambda e, i=i: e.memset(s5.LC3[i][0][:, :, 0:32], 0.0), writes=[s5.LC3[i][1]])
    for n, src in (("d", "d_d"), ("bg", "d_b_glu")):
        op_dma(k, 'sp', s5.par[n][0][:], dr[src].rearrange("(c p) -> p c", p=128), [k.dbuf[src]], [s5.par[n][1]], slow=True)
    m01, m01_b, _ = s5.m01
    P.op('pool', lambda e: e.memset(m01[:], 1.0), writes=[m01_b])
    P.op('pool', lambda e: e.memset(m01[:].rearrange("p (c t) -> p c t", t=S5C)[:, :, 0:1], 0.0), writes=[m01_b])


def layer_s5(k, ps_):
    P, dr, nc = k.P, k.dr, k.nc
    if not hasattr(k, "s5"):
        s5_setup(k)
    s5 = k.s5
    s5_load(k)
    P.barrier()
    P.chk("s5_load")
    tiles = ntiles(ps_)
    p = s5.P64
    A = lambda n: p[n][0]
    Bf = lambda n: p[n][1]
    tab = s5.tab
    TA = lambda n: tab[n][0][:, :]
    TB = lambda n: tab[n][1]
    d_w_in, d_w_in_b = dr["d_w_in"], k.dbuf["d_w_in"]
    U, U_b, _ = s5.U
    if ps_ == 0:
        for i, src in ((0, "state_ssm_re"), (1, "state_ssm_im")):
            for q4 in range(4):
                sr, sr_b, _ = s5.srow
                op_dma(k, 'sp', sr[:, :], dr[src].rearrange("s g p -> s (g p)")[:, q4 * 2048:(q4 + 1) * 2048], [k.dbuf[src]], [sr_b])
                for j in range(16):
                    sc = q4 * 16 + j
                    if j % 8 == 0:
                        pt, pt_b = k.psn()
                    op_tr(k, pt[:, (j % 8) * NS:(j % 8 + 1) * NS], sr[0:NS, j * 128:(j + 1) * 128], k.identF[0:NS, 0:NS],
                          [sr_b, k.identF_b], [pt_b])
                    if j % 8 == 7:
                        P.op('dve', lambda e, i=i, sc=sc, pt=pt: e.tensor_copy(
                            out=s5.s0T[i][0][:, sc - 7:sc + 1, :], in_=pt[:, 0:8 * NS].rearrange("p (a s) -> p a s", s=NS)),
                            [pt_b], [s5.s0T[i][1]])
    big = s5.big
    P.barrier()

    def ps56():
        i = 5 + k.ps_i[0] % 2
        k.ps_i[0] += 1
        return k.ps[i]
    for ec in range(16):
        load_w_cols(k, s5.wt[0], s5.wt[1], d_w_in, d_w_in_b, ec * 128)
        for ti, (c0, w) in enumerate(tiles):
            pu, pu_b = k.psn()
            for kk in range(8):
                op_mm(k, pu[:, 0:w], s5.wt[0][:, kk, :], k.hT[:, kk, c0 + 1:c0 + 1 + w], kk == 0, kk == 7,
                      [s5.wt[1]] + hT_reads(k, c0, w), [pu_b])
            op_act(k, U[:, c0:c0 + w], pu[:, 0:w], AF.Copy, [pu_b], [U_b.sub(ti)])
        Uz, Uz_b, _ = s5.Uz
        P.op('dve', lambda e: e.tensor_copy(out=Uz[64:128, :], in_=U[64:128, :]), [U_b], [Uz_b])
        P.op('dve', lambda e: e.memset(Uz[64:96, :], 0.0), writes=[Uz_b])
        P.chk("s5_u")
        py = {}
        for q in (0, 1, 3, 2):
            sc = ec * 4 + q
            col = slice(sc, sc + 1)
            qP = slice(q * 32, q * 32 + 32)
            P.op('pool', lambda e: e.iota(tab["c"][0][:].bitcast(I32)[:, 0:64], pattern=[[1, 64]], base=0, channel_multiplier=0),
                 writes=[TB("c")])
            P.op('dve', lambda e: e.tensor_copy(out=TA("c"), in_=tab["c"][0][:].bitcast(I32)[:, 0:64]), [TB("c")], [TB("c")])
            op_ts(k, 'dve', TA("ang"), TA("c"), A("th")[:, col], None, ALU.mult, None, [TB("c"), Bf("th")], [TB("ang")])
            op_ts(k, 'dve', TA("mg"), TA("c"), A("lnm")[:, col], None, ALU.mult, None, [TB("c"), Bf("lnm")], [TB("mg")])
            s5_trig(k, TA("Fs"), TB("Fs"), TA("ang"), TB("ang"), PI_, 64)
            s5_trig(k, TA("Fc"), TB("Fc"), TA("ang"), TB("ang"), 1.5 * PI_, 64)
            op_act(k, TA("c"), TA("mg"), AF.Exp, [TB("mg")], [TB("c")])
            op_tt(k, 'dve', TA("Bc"), TA("Fc"), TA("c"), ALU.mult, [TB("Fc"), TB("c")], [TB("Bc")])
            op_tt(k, 'dve', TA("Bs"), TA("Fs"), TA("c"), ALU.mult, [TB("Fs"), TB("c")], [TB("Bs")])
            op_act(k, TA("c"), TA("mg"), AF.Exp, [TB("mg")], [TB("c")], scale=-1.0)
            op_tt(k, 'dve', TA("Fc"), TA("Fc"), TA("c"), ALU.mult, [TB("Fc"), TB("c")], [TB("Fc")])
            op_tt(k, 'dve', TA("Fs"), TA("Fs"), TA("c"), ALU.mult, [TB("Fs"), TB("c")], [TB("Fs")])
            P.op('pool', lambda e: e.iota(tab["c"][0][:].bitcast(I32)[:, 0:32], pattern=[[1, 32]], base=0, channel_multiplier=0),
                 writes=[TB("c")])
            P.op('dve', lambda e: e.tensor_copy(out=tab["c"][0][:, 0:32], in_=tab["c"][0][:].bitcast(I32)[:, 0:32]), [TB("c")], [TB("c")])
            op_ts(k, 'dve', tab["ang"][0][:, 0:32], tab["c"][0][:, 0:32], A("ph")[:, col], None, ALU.mult, None, [TB("c"), Bf("ph")], [TB("ang")])
            s5_trig(k, tab["Gs"][0][:, 0:32], TB("Gs"), tab["ang"][0][:, 0:32], TB("ang"), PI_, 32)
            s5_trig(k, tab["Gc"][0][:, 0:32], TB("Gc"), tab["ang"][0][:, 0:32], TB("ang"), 1.5 * PI_, 32)
            XR, XI, T1, T2 = [b_[0] for b_ in big[0:4]]
            XR_b, XI_b, T1_b, T2_b = [b_[1] for b_ in big[0:4]]
            QR_, QI_, QR_b, QI_b = XR, XI, XR_b, XI_b
            v3 = lambda t: t[:, :].rearrange("p (c t) -> p c t", t=S5C)
            bc = lambda n: tab[n][0][:, :].unsqueeze(1).to_broadcast([128, S5NCH, S5C])
            for ti, (c0, w) in enumerate(tiles):
                pbr, pbr_b = ps56()
                pbi, pbi_b = ps56()
                if q < 3:
                    op_mm(k, pbr[:, 0:w], s5.LB[0][0][qP, ec, :], U[qP, c0:c0 + w], True, True, [s5.LB[0][1], U_b.sub(ti)], [pbr_b])
                    op_mm(k, pbi[:, 0:w], s5.LB[1][0][qP, ec, :], U[qP, c0:c0 + w], True, True, [s5.LB[1][1], U_b.sub(ti)], [pbi_b])
                else:
                    op_mm(k, pbr[:, 0:w], s5.LB[0][0][64:128, ec, :], Uz[64:128, c0:c0 + w], True, True, [s5.LB[0][1], Uz_b], [pbr_b])
                    op_mm(k, pbi[:, 0:w], s5.LB[1][0][64:128, ec, :], Uz[64:128, c0:c0 + w], True, True, [s5.LB[1][1], Uz_b], [pbi_b])
                if c0 < T:
                    P.op('act', lambda e, c0=c0, pbr=pbr: e.activation(out=XR[:, c0:c0 + 512], in_=pbr[:, :], func=AF.Copy), [pbr_b], [XR_b])
                    P.op('act', lambda e, c0=c0, pbi=pbi: e.activation(out=XI[:, c0:c0 + 512], in_=pbi[:, :], func=AF.Copy), [pbi_b], [XI_b])
                else:
                    s0r, s0i = s5.s0T[0], s5.s0T[1]
                    tr_, ti__ = s5.tmpn[0], s5.tmpn[1]
                    op_stt(k, 'dve', tr_[0][:, :], s0r[0][:, sc, :], A("are")[:, col], pbr[:, 0:NS], ALU.mult, ALU.add,
                           [s0r[1], Bf("are"), pbr_b], [tr_[1]])
                    op_ts(k, 'dve', s5.g[1][0][:, 0:NS], s0i[0][:, sc, :], A("aim")[:, col], None, ALU.mult, None, [s0i[1], Bf("aim")], [s5.g[1][1]])
                    op_tt(k, 'dve', tr_[0][:, :], tr_[0][:, :], s5.g[1][0][:, 0:NS], ALU.subtract, [tr_[1], s5.g[1][1]], [tr_[1]])
                    op_stt(k, 'dve', ti__[0][:, :], s0i[0][:, sc, :], A("are")[:, col], pbi[:, 0:NS], ALU.mult, ALU.add,
                           [s0i[1], Bf("are"), pbi_b], [ti__[1]])
                    op_stt(k, 'dve', ti__[0][:, :], s0r[0][:, sc, :], A("aim")[:, col], ti__[0][:, :], ALU.mult, ALU.add,
                           [s0r[1], Bf("aim"), ti__[1]], [ti__[1]])
                    P.op('dve', lambda e, sc=sc: e.tensor_copy(out=s0r[0][:, sc, :], in_=tr_[0][:, :]), [tr_[1]], [s0r[1]])
                    P.op('dve', lambda e, sc=sc: e.tensor_copy(out=s0i[0][:, sc, :], in_=ti__[0][:, :]), [ti__[1]], [s0i[1]])
                    op_act(k, s5.sbf[0][0][:, T:T + NS], tr_[0][:, :], AF.Copy, [tr_[1]], [s5.sbf[0][1].sub(4)])
                    op_act(k, s5.sbf[1][0][:, T:T + NS], ti__[0][:, :], AF.Copy, [ti__[1]], [s5.sbf[1][1].sub(4)])
            op_tt(k, 'dve', v3(T1), v3(XR), bc("Fc"), ALU.mult, [XR_b, TB("Fc")], [T1_b])
            op_tt(k, 'pool', v3(T2), v3(XI), bc("Fs"), ALU.mult, [XI_b, TB("Fs")], [T2_b])
            op_tt(k, 'dve', v3(T1), v3(T1), v3(T2), ALU.add, [T1_b, T2_b], [T1_b])
            op_tt(k, 'pool', v3(T2), v3(XI), bc("Fc"), ALU.mult, [XI_b, TB("Fc")], [T2_b])
            op_tt(k, 'dve', v3(XI), v3(XR), bc("Fs"), ALU.mult, [XR_b, TB("Fs")], [XI_b])
            op_tt(k, 'pool', v3(T2), v3(T2), v3(XI), ALU.subtract, [T2_b, XI_b], [T2_b])
            m01 = s5.m01
            P.op('dve', lambda e: e.tensor_tensor_scan(out=QR_[:], data0=m01[0][:], data1=T1[:], initial=0.0, op0=ALU.mult, op1=ALU.add),
                 [m01[1], T1_b], [QR_b])
            P.op('dve', lambda e: e.tensor_tensor_scan(out=QI_[:], data0=m01[0][:], data1=T2[:], initial=0.0, op0=ALU.mult, op1=ALU.add),
                 [m01[1], T2_b], [QI_b])
            EE = s5.ec_
            e = lambda n: EE[n][0][:, 0:S5NCH]
            eb = lambda n: EE[n][1]
            qr_end = v3(QR_)[:, :, S5C - 1]
            qi_end = v3(QI_)[:, :, S5C - 1]
            bc63 = tab["Bc"][0][:, S5C - 1:S5C]
            bs63 = tab["Bs"][0][:, S5C - 1:S5C]
            op_ts(k, 'dve', e("er"), qr_end, bc63, None, ALU.mult, None, [QR_b, TB("Bc")], [eb("er")])
            op_ts(k, 'dve', e("hr"), qi_end, bs63, None, ALU.mult, None, [QI_b, TB("Bs")], [eb("hr")])
            op_tt(k, 'dve', e("er"), e("er"), e("hr"), ALU.subtract, [eb("er"), eb("hr")], [eb("er")])
            op_ts(k, 'dve', e("ei"), qr_end, bs63, None, ALU.mult, None, [QR_b, TB("Bs")], [eb("ei")])
            op_ts(k, 'dve', e("hr"), qi_end, bc63, None, ALU.mult, None, [QI_b, TB("Bc")], [eb("hr")])
            op_tt(k, 'dve', e("ei"), e("ei"), e("hr"), ALU.add, [eb("ei"), eb("hr")], [eb("ei")])
            Gc = tab["Gc"][0][:, 0:S5NCH]
            Gs = tab["Gs"][0][:, 0:S5NCH]
            op_tt(k, 'dve', e("hr"), e("er"), Gc, ALU.mult, [eb("er"), TB("Gc")], [eb("hr")])
            op_tt(k, 'dve', e("hi"), e("ei"), Gs, ALU.mult, [eb("ei"), TB("Gs")], [eb("hi")])
            op_tt(k, 'dve', e("hr"), e("hr"), e("hi"), ALU.add, [eb("hr"), eb("hi")], [eb("hr")])
            op_tt(k, 'dve', e("hi"), e("ei"), Gc, ALU.mult, [eb("ei"), TB("Gc")], [eb("hi")])
            op_tt(k, 'dve', e("cr"), e("er"), Gs, ALU.mult, [eb("er"), TB("Gs")], [eb("cr")])
            op_tt(k, 'dve', e("hi"), e("hi"), e("cr"), ALU.subtract, [eb("hi"), eb("cr")], [eb("hi")])
            op_ts(k, 'dve', e("ci"), Gc, 0.0, A("m64")[:, col], ALU.mult, ALU.add, [TB("Gc"), Bf("m64")], [eb("ci")])
            P.op('dve', lambda e_: e_.tensor_tensor_scan(out=e("Er"), data0=e("ci"), data1=e("hr"), initial=0.0, op0=ALU.mult, op1=ALU.add),
                 [eb("ci"), eb("hr")], [eb("Er")])
            P.op('dve', lambda e_: e_.tensor_tensor_scan(out=e("Ei"), data0=e("ci"), data1=e("hi"), initial=0.0, op0=ALU.mult, op1=ALU.add),
                 [eb("ci"), eb("hi")], [eb("Ei")])
            op_tt(k, 'dve', e("hr"), e("Er"), Gc, ALU.mult, [eb("Er"), TB("Gc")], [eb("hr")])
            op_tt(k, 'dve', e("cr"), e("Ei"), Gs, ALU.mult, [eb("Ei"), TB("Gs")], [eb("cr")])
            op_tt(k, 'dve', e("hr"), e("hr"), e("cr"), ALU.subtract, [eb("hr"), eb("cr")], [eb("hr")])
            op_tt(k, 'dve', e("hi"), e("Er"), Gs, ALU.mult, [eb("Er"), TB("Gs")], [eb("hi")])
            op_tt(k, 'dve', e("cr"), e("Ei"), Gc, ALU.mult, [eb("Ei"), TB("Gc")], [eb("cr")])
            op_tt(k, 'dve', e("hi"), e("hi"), e("cr"), ALU.add, [eb("hi"), eb("cr")], [eb("hi")])
            P.op('dve', lambda e_, sc=sc: e_.tensor_copy(out=s5.fin[0][0][:, sc:sc + 1], in_=EE["hr"][0][:, S5NCH - 1:S5NCH]), [eb("hr")], [s5.fin[0][1]])
            P.op('dve', lambda e_, sc=sc: e_.tensor_copy(out=s5.fin[1][0][:, sc:sc + 1], in_=EE["hi"][0][:, S5NCH - 1:S5NCH]), [eb("hi")], [s5.fin[1][1]])
            n1 = S5NCH - 1
            op_ts(k, 'dve', EE["cr"][0][:, 0:n1], EE["hr"][0][:, 0:n1], A("are")[:, col], None, ALU.mult, None, [eb("hr"), Bf("are")], [eb("cr")])
            op_ts(k, 'dve', EE["ci"][0][:, 0:n1], EE["hi"][0][:, 0:n1], A("aim")[:, col], None, ALU.mult, None, [eb("hi"), Bf("aim")], [eb("ci")])
            op_tt(k, 'dve', EE["cr"][0][:, 0:n1], EE["cr"][0][:, 0:n1], EE["ci"][0][:, 0:n1], ALU.subtract, [eb("cr"), eb("ci")], [eb("cr")])
            op_ts(k, 'dve', EE["ci"][0][:, 0:n1], EE["hr"][0][:, 0:n1], A("aim")[:, col], None, ALU.mult, None, [eb("hr"), Bf("aim")], [eb("ci")])
            op_ts(k, 'dve', EE["er"][0][:, 0:n1], EE["hi"][0][:, 0:n1], A("are")[:, col], None, ALU.mult, None, [eb("hi"), Bf("are")], [eb("er")])
            op_tt(k, 'dve', EE["ci"][0][:, 0:n1], EE["ci"][0][:, 0:n1], EE["er"][0][:, 0:n1], ALU.add, [eb("ci"), eb("er")], [eb("ci")])
            op_tt(k, 'dve', v3(T1)[:, 1:S5NCH, 0], v3(T1)[:, 1:S5NCH, 0], EE["cr"][0][:, 0:n1], ALU.add, [T1_b, eb("cr")], [T1_b])
            op_tt(k, 'dve', v3(T2)[:, 1:S5NCH, 0], v3(T2)[:, 1:S5NCH, 0], EE["ci"][0][:, 0:n1], ALU.add, [T2_b, eb("ci")], [T2_b])
            P.op('dve', lambda e_: e_.tensor_tensor_scan(out=QR_[:], data0=m01[0][:], data1=T1[:], initial=0.0, op0=ALU.mult, op1=ALU.add),
                 [m01[1], T1_b], [QR_b])
            P.op('dve', lambda e_: e_.tensor_tensor_scan(out=QI_[:], data0=m01[0][:], data1=T2[:], initial=0.0, op0=ALU.mult, op1=ALU.add),
                 [m01[1], T2_b], [QI_b])
            op_tt(k, 'dve', v3(T1), v3(QR_), bc("Bc"), ALU.mult, [QR_b, TB("Bc")], [T1_b])
            op_tt(k, 'pool', v3(T2), v3(QI_), bc("Bs"), ALU.mult, [QI_b, TB("Bs")], [T2_b])
            op_tt(k, 'dve', s5.sbf[0][0][:, 0:T].rearrange("p (c t) -> p c t", t=S5C), v3(T1), v3(T2), ALU.subtract, [T1_b, T2_b],
                  [s5.sbf[0][1].sub(0)])
            op_tt(k, 'pool', v3(T1), v3(QR_), bc("Bs"), ALU.mult, [QR_b, TB("Bs")], [T1_b])
            op_tt(k, 'dve', v3(T2), v3(QI_), bc("Bc"), ALU.mult, [QI_b, TB("Bc")], [T2_b])
            op_tt(k, 'pool', s5.sbf[1][0][:, 0:T].rearrange("p (c t) -> p c t", t=S5C), v3(T1), v3(T2), ALU.add, [T1_b, T2_b],
                  [s5.sbf[1][1].sub(0)])
            for ti, (c0, w) in enumerate(tiles):
                if q == 0:
                    py[ti] = k.ps[ti]
                pyt, pyt_b = py[ti]
                rs_ = [s5.sbf[0][1].sub(0 if c0 < T else 4), s5.sbf[1][1].sub(0 if c0 < T else 4)]
                if q < 2:
                    op_mm(k, pyt[qP, 0:w], s5.LC[0][0][:, ec, qP], s5.sbf[0][0][:, c0:c0 + w], True, False, [s5.LC[0][1]] + rs_, [pyt_b])
                    op_mm(k, pyt[qP, 0:w], s5.LC[1][0][:, ec, qP], s5.sbf[1][0][:, c0:c0 + w], False, True, [s5.LC[1][1]] + rs_, [pyt_b])
                elif q == 3:
                    op_mm(k, pyt[64:128, 0:w], s5.LC3[0][0][:, ec, :], s5.sbf[0][0][:, c0:c0 + w], True, False, [s5.LC3[0][1]] + rs_, [pyt_b])
                    op_mm(k, pyt[64:128, 0:w], s5.LC3[1][0][:, ec, :], s5.sbf[1][0][:, c0:c0 + w], False, False, [s5.LC3[1][1]] + rs_, [pyt_b])
                else:
                    op_mm(k, pyt[64:96, 0:w], s5.LC[0][0][:, ec, 64:96], s5.sbf[0][0][:, c0:c0 + w], False, False, [s5.LC[0][1]] + rs_, [pyt_b])
                    op_mm(k, pyt[64:96, 0:w], s5.LC[1][0][:, ec, 64:96], s5.sbf[1][0][:, c0:c0 + w], False, True, [s5.LC[1][1]] + rs_, [pyt_b])
            P.chk("s5_sc")
        for ti, (c0, w) in enumerate(tiles):
            pyt, pyt_b = py[ti]
            y_, y_b, _ = s5.g[0]
            t_, t_b, _ = s5.g[1]
            s_, s_b, _ = s5.g[2]
            op_stt(k, 'dve', y_[:, 0:w], U[:, c0:c0 + w], s5.par["d"][0][:, ec:ec + 1], pyt[:, 0:w], ALU.mult, ALU.add,
                   [U_b.sub(ti), s5.par["d"][1], pyt_b], [y_b])
            op_act(k, t_[:, 0:w], y_[:, 0:w], AF.Square, [y_b], [t_b])
            op_ts(k, 'dve', t_[:, 0:w], t_[:, 0:w], 0.044715, 1.0, ALU.mult, ALU.add, [t_b], [t_b])
            op_tt(k, 'dve', t_[:, 0:w], t_[:, 0:w], y_[:, 0:w], ALU.mult, [t_b, y_b], [t_b])
            op_act(k, s_[:, 0:w], t_[:, 0:w], AF.Sigmoid, [t_b], [s_b], scale=1.5957691216057308)
            op_tt(k, 'dve', k.GT[:, ec, c0:c0 + w], y_[:, 0:w], s_[:, 0:w], ALU.mult, [y_b, s_b], [k.GT_b.sub(ec).sub(ti)])
        P.chk("s5_ec")
    P.barrier()
    for i, nm in ((0, "sre_p"), (1, "sim_p")):
        for g2 in range(2):
            op_dma(k, 'sp', dr[nm][ps_].rearrange("(c g2) p -> g2 p c", g2=2)[g2], s5.fin[i][0][g2 * 64:(g2 + 1) * 64, :],
                   [s5.fin[i][1]], [k.dbuf[nm]], slow=True)
    if ps_ == 0:
        for i, nm in ((0, "sre_s"), (1, "sim_s")):
            for q4 in range(4):
                sr, sr_b, _ = s5.srow
                for j in range(16):
                    sc = q4 * 16 + j
                    if j % 4 == 0:
                        pt, pt_b = k.psn()
                    op_tr(k, pt[0:NS, (j % 4) * 128:(j % 4 + 1) * 128], s5.s0T[i][0][:, sc, :], k.identF[:], [s5.s0T[i][1], k.identF_b], [pt_b])
                    if j % 4 == 3:
                        P.op('dve', lambda e, j=j, pt=pt: e.tensor_copy(out=sr[0:NS, (j - 3) * 128:(j + 1) * 128], in_=pt[0:NS, 0:512]),
                             [pt_b], [sr_b])
                op_dma(k, 'sp', dr[nm].rearrange("s g p -> s (g p)")[:, q4 * 2048:(q4 + 1) * 2048], sr[0:NS, :], [sr_b], [k.dbuf[nm]])
    P.barrier()
    gscr, gscr_b = dr["gscr"], k.dbuf["gscr"]
    for e2 in range(16):
        wg, wg_b, _ = s5.wg
        op_dma(k, 'pool', wg[:, :, :], dr["d_w_glu"][:, e2 * 128:(e2 + 1) * 128].rearrange("(c p) e -> p c e", p=128), [k.dbuf["d_w_glu"]], [wg_b])
        load_w_cols(k, s5.wt[0], s5.wt[1], d_w_in, d_w_in_b, E + e2 * 128)
        for ti, (c0, w) in enumerate(tiles):
            pg, pg_b = k.psn()
            for ec in range(16):
                op_mm(k, pg[:, 0:w], wg[:, ec, :], k.GT[:, ec, c0:c0 + w], ec == 0, ec == 15, [wg_b, k.GT_b.sub(ec).sub(ti)], [pg_b])
            pz, pz_b = k.psn()
            for kk in range(8):
                op_mm(k, pz[:, 0:w], s5.wt[0][:, kk, :], k.hT[:, kk, c0 + 1:c0 + 1 + w], kk == 0, kk == 7,
                      [s5.wt[1]] + hT_reads(k, c0, w), [pz_b])
            s_, s_b, _ = s5.g[0]
            z_, z_b, _ = s5.g[1]
            op_act(k, s_[:, 0:w], pg[:, 0:w], AF.Sigmoid, [pg_b, s5.par["bg"][1]], [s_b], bias=s5.par["bg"][0][:, e2:e2 + 1])
            op_act(k, z_[:, 0:w], pz[:, 0:w], AF.Silu, [pz_b], [z_b])
            op_tt(k, 'dve', s_[:, 0:w], s_[:, 0:w], k.GT[:, e2, c0:c0 + w], ALU.mult, [s_b, k.GT_b.sub(e2).sub(ti)], [s_b])
            gb, gb_b, _ = s5.gb
            op_tt(k, 'dve', gb[:, 0:w], s_[:, 0:w], z_[:, 0:w], ALU.mult, [s_b, z_b], [gb_b])
            op_dma(k, 'sp', gscr[e2, :, c0:c0 + w], gb[:, 0:w], [gb_b], [gscr_b])
    P.barrier()
    TW = TWA if ps_ == 0 else T
    for e2 in range(16):
        op_dma(k, 'sp', k.GT[:, e2, 0:TW], gscr[e2, :, 0:TW], [gscr_b], [k.GT_b])
N_LAYERS = 4

_CACHE = {}


def kernel(**inp):
    n_layers = N_LAYERS
    N_IN = N_IN_BY_LAYER[n_layers]
    if "nc" not in _CACHE:
        _CACHE["nc"] = build(n_layers)
    nc, k = _CACHE["nc"]
    f = lambda a: np.ascontiguousarray(np.asarray(a))
    shared = {}
    for name, shp, dt in IN_SHAPES[:N_IN]:
        if name in ("xp", "xs", "state_conv", "state_shift", "state_wkv", "state_ssm_re", "state_ssm_im", "page_table"):
            continue
        shared[name] = f(inp[name])
    in_maps = []
    for c in range(NCORES):
        m = dict(shared)
        m["xp"] = f(inp["x_prompt"][2 * c:2 * c + 2])
        m["xs"] = f(inp["x_sample"][NS * c:NS * (c + 1), 0, :])
        for nm in ("state_conv", "state_shift", "state_wkv", "state_ssm_re", "state_ssm_im", "page_table"):
            if any(nm == x[0] for x in IN_SHAPES[:N_IN]):
                m[nm] = f(inp[nm][NS * c:NS * (c + 1)])
        in_maps.append(m)
    res = run_bass_kernel_spmd(nc, in_maps, core_ids=list(range(NCORES)))
    R = res.results
    cat = lambda nm: np.concatenate([np.asarray(R[c][nm]) for c in range(NCORES)], axis=0)
    y_p = cat("y_p")
    y_s = cat("y_s").reshape(128, 1, D)
    outs = (y_p, y_s, cat("conv_p"), cat("conv_s"), cat("shift_p"), cat("shift_s"), cat("wkv_p"), cat("wkv_s"),
            cat("ckv_p"), cat("ckv_s").reshape(128, 1, 256), cat("kr_p"), cat("kr_s").reshape(128, 1, 64),
            cat("sre_p"), cat("sre_s"), cat("sim_p"), cat("sim_s"))
    return tuple(np.ascontiguousarray(o, dtype=np.float32) for o in outs)
```

```python
from concourse.bass_utils import run_bass_kernel_spmd
import numpy as np
import concourse.bass as bass
import concourse.mybir as mybir

F32 = mybir.dt.float32
BF16 = mybir.dt.bfloat16
I32 = mybir.dt.int32
AF = mybir.ActivationFunctionType
ALU = mybir.AluOpType
AX = mybir.AxisListType

NDS = 6
ENG = ['pe', 'act', 'dve', 'pool', 'sp']
BLK = {'pe': 'tensor', 'act': 'scalar', 'dve': 'vector', 'pool': 'gpsimd', 'sp': 'sync'}


class Buf:
    __slots__ = ('name', 'w', 'r', 'parent', 'kids')

    def __init__(s, name, parent=None):
        s.name = name
        s.w = None
        s.r = []
        s.parent = parent
        s.kids = {}

    def sub(s, key):
        k = s.kids.get(key)
        if k is None:
            k = Buf(f'{s.name}.{key}', s)
            s.kids[key] = k
        return k

    def family(s):
        out = [s]
        p = s.parent
        while p is not None:
            out.append(p)
            p = p.parent
        if s.kids:
            st = list(s.kids.values())
            while st:
                k = st.pop()
                out.append(k)
                if k.kids:
                    st.extend(k.kids.values())
        return out


class Op:
    __slots__ = ('eng', 'fn', 'waits', 'idx', 'inc', 'dma', 'semk', 'val', 'seq')


class Prog:
    def __init__(s):
        s.q = {e: [] for e in ENG}
        s.seen = {e: {} for e in ENG}
        s.rr = {e: 0 for e in ENG}
        s.dlast = {}
        s.dcount = {}
        s.alldma = []

    def op(s, eng, fn, reads=(), writes=(), dma=False):
        if getattr(s, "dead", False):
            return None
        o = Op()
        o.eng = eng
        o.fn = fn
        o.dma = dma
        o.inc = bool(dma)
        o.waits = []
        o.idx = len(s.q[eng])
        o.semk = None
        o.val = None
        o.seq = None
        deps = {}
        for b in reads:
            for f in b.family():
                if f.w is not None:
                    deps[id(f.w)] = f.w
        for b in writes:
            for f in b.family():
                if f.w is not None:
                    deps[id(f.w)] = f.w
                for r in f.r:
                    deps[id(r)] = r
        if dma:
            k = s.rr[eng] % NDS
            s.rr[eng] += 1
            o.semk = k
            prev = s.dlast.get((eng, k))
            if prev is not None:
                deps[id(prev)] = prev
            s.dlast[(eng, k)] = o
            o.seq = s.dcount.get((eng, k), 0) + 1
            s.dcount[(eng, k)] = o.seq
            s.alldma.append(o)
        seen = s.seen[eng]
        for d in deps.values():
            if d.dma:
                key = (d.eng, d.semk)
                v = d.seq
            else:
                if d.eng == eng and eng == 'pe':
                    continue
                key = d.eng
                v = d.idx
            if seen.get(key, -1) >= v:
                continue
            seen[key] = v
            o.waits.append(d)
            d.inc = True
        for b in reads:
            b.r.append(o)
        for b in writes:
            b.w = o
            b.r = []
        s.q[eng].append(o)
        return o

    def chk(s, name):
        import os
        if os.environ.get("KSTOP", "") == name:
            s.dead = True

    def barrier(s):
        if getattr(s, "dead", False):
            return
        lasts = []
        for e in ENG:
            for o in reversed(s.q[e]):
                if o.fn is not None and not o.dma:
                    lasts.append(o)
                    break
        dl = list(s.dlast.values())
        for e in ENG:
            o = Op()
            o.eng = e
            o.fn = None
            o.dma = False
            o.inc = False
            o.waits = []
            o.idx = len(s.q[e])
            o.semk = None
            o.val = None
            o.seq = None
            seen = s.seen[e]
            for d in lasts + dl:
                if d.dma:
                    key = (d.eng, d.semk)
                    v = d.seq
                else:
                    if d.eng == e:
                        continue
                    key = d.eng
                    v = d.idx
                if seen.get(key, -1) >= v:
                    continue
                seen[key] = v
                o.waits.append(d)
                d.inc = True
            s.q[e].append(o)

    def finish(s, eng='sp'):
        o = Op()
        o.eng = eng
        o.fn = None
        o.dma = False
        o.inc = False
        o.waits = list(s.dlast.values())
        o.idx = len(s.q[eng])
        s.q[eng].append(o)

    def emit(s, nc):
        sems = {}
        for e in ENG:
            sems[e] = nc.alloc_semaphore(f'S_{e}')
            for k in range(NDS):
                sems[(e, k)] = nc.alloc_semaphore(f'D_{e}_{k}')
        for e in ENG:
            c = 0
            for o in s.q[e]:
                if o.dma:
                    o.val = 16 * o.seq
                elif o.inc:
                    c += 1
                    o.val = c
        n_ins = {e: 0 for e in ENG}
        with nc.Block() as block:
            for e in ENG:
                def body(eng, e=e):
                    for o in s.q[e]:
                        for d in o.waits:
                            sm = sems[(d.eng, d.semk)] if d.dma else sems[d.eng]
                            eng.wait_ge(sm, d.val)
                            n_ins[e] += 1
                        if o.fn is None:
                            continue
                        ins = o.fn(eng)
                        n_ins[e] += 1
                        if o.dma:
                            ins.then_inc(sems[(e, o.semk)], 16)
                        elif o.inc:
                            ins.then_inc(sems[e], 1)
                getattr(block, BLK[e])(body)
        return n_ins

import math
NCORES = 8
T = 2048
D = 1024
E = 2048
NS = 16
TWA = T + NS
SB0 = 16640
SBMAX = 229376

OUT_SHAPES = [
    ("y_p", [2, T, D]), ("y_s", [NS, D]),
    ("conv_p", [2, 30, E]), ("conv_s", [NS, 30, E]),
    ("shift_p", [2, D]), ("shift_s", [NS, D]),
    ("wkv_p", [2, 32, 64, 64]), ("wkv_s", [NS, 32, 64, 64]),
    ("ckv_p", [2, T, 256]), ("ckv_s", [NS, 256]),
    ("kr_p", [2, T, 64]), ("kr_s", [NS, 64]),
    ("sre_p", [2, 128, 64]), ("sre_s", [NS, 128, 64]),
    ("sim_p", [2, 128, 64]), ("sim_s", [NS, 128, 64]),
]

IN_SHAPES = [
    ("xp", [2, T, D], F32), ("xs", [NS, D], F32),
    ("state_conv", [NS, 30, E], F32), ("state_shift", [NS, D], F32),
    ("norm_pre", [4, D], F32), ("norm_post", [4, D], F32),
    ("a_w_in", [D, 3 * E], F32), ("a_b_in", [3 * E], F32), ("a_conv_w", [31, E], F32),
    ("a_conv_b", [E], F32), ("a_ln_g", [E], F32), ("a_ln_b", [E], F32), ("a_w_out", [E, D], F32),
    ("state_wkv", [NS, 32, 64, 64], F32),
    ("b_mu", [6, D], F32), ("b_w_rkvz", [4, D, E], F32), ("b_w0", [E], F32), ("b_w1", [D, 64], F32), ("b_w2", [64, E], F32),
    ("b_a0", [E], F32), ("b_a1", [D, 64], F32), ("b_a2", [64, E], F32), ("b_k_k", [E], F32), ("b_k_a", [E], F32),
    ("b_r_k", [32, 64], F32), ("b_ln_g", [E], F32), ("b_ln_b", [E], F32), ("b_w_out", [E, D], F32),
    ("cache_cat", [10240, 128, 320], F32), ("page_table", [NS, 64], I32),
    ("c_w_in", [D, 2752], F32), ("c_q_norm", [384], F32), ("c_kv_norm", [256], F32), ("c_w_uq", [384, 16, 192], F32),
    ("c_w_uk", [256, 16, 128], F32), ("c_w_uv", [256, 16, 128], F32), ("c_w_out", [E, D], F32),
    ("state_ssm_re", [NS, 128, 64], F32), ("state_ssm_im", [NS, 128, 64], F32),
    ("d_w_in", [D, 2 * E], F32), ("d_lambda_re", [128, 64], F32), ("d_lambda_im", [128, 64], F32), ("d_log_dt", [128], F32),
    ("d_b_re", [128, 64, 16], F32), ("d_b_im", [128, 64, 16], F32), ("d_c_re", [128, 16, 64], F32), ("d_c_im", [128, 16, 64], F32),
    ("d_d", [E], F32), ("d_w_glu", [E, E], F32), ("d_b_glu", [E], F32), ("d_w_out", [E, D], F32),
]


N_IN_BY_LAYER = {1: 13, 2: 28, 3: 37, 4: 51}


class K:
    pass


def build(n_layers=1):
    nc = bass.Bass("TRN2", target_bir_lowering=False)
    P = Prog()
    k = K()
    k.nc = nc
    k.P = P
    dr = {}
    for name, shp, dt in IN_SHAPES[:N_IN_BY_LAYER[n_layers]]:
        dr[name] = nc.dram_tensor(name, shp, dt, kind="ExternalInput").ap()
    for name, shp in OUT_SHAPES:
        dr[name] = nc.dram_tensor(name, shp, F32, kind="ExternalOutput").ap()
    dr["xscr"] = nc.dram_tensor("xscr", [2, T, D], F32, kind="Internal").ap()
    dr["xscr_s"] = nc.dram_tensor("xscr_s", [NS, D], F32, kind="Internal").ap()
    dr["xscr2"] = nc.dram_tensor("xscr2", [2, T, D], F32, kind="Internal").ap()
    dr["xscr2_s"] = nc.dram_tensor("xscr2_s", [NS, D], F32, kind="Internal").ap()
    dr["gscr"] = nc.dram_tensor("gscr", [16, 128, TWA], BF16, kind="Internal").ap()
    k.dr = dr
    k.dbuf = {n: Buf("dram_" + n) for n in dr}

    off = [SB0]

    def sb(name, shape, dt, at=None):
        esz = 2 if dt == BF16 else 4
        nbytes = int(np.prod(shape[1:])) * esz
        if at is None:
            o = off[0]
            off[0] += (nbytes + 63) // 64 * 64
            assert off[0] <= SBMAX, (name, off[0])
        else:
            o = at
        t = nc.alloc_sbuf_tensor_at(name, shape, dt, offset=o)
        return t, Buf(name), o

    k.sb = sb
    k.ps = []
    for i in range(7):
        k.ps.append((nc.alloc_psum_tensor(f"ps{i}", [128, 512], F32), Buf(f"ps{i}")))
    k.psT = (nc.alloc_psum_tensor("psT", [128, 1024], BF16), Buf("psT"))
    k.ps_i = [0]

    def psn():
        i = k.ps_i[0] % 7
        k.ps_i[0] += 1
        return k.ps[i]
    k.psn = psn

    k.identF, k.identF_b, _ = sb("identF", [128, 128], F32)
    k.identB, k.identB_b, _ = sb("identB", [128, 128], BF16)
    k.onesB, k.onesB_b, _ = sb("onesB", [128, 128], BF16)
    k.epsr, k.epsr_b, _ = sb("epsr", [128, 4], F32)
    P.op('pool', lambda e: e.memset(k.identF[:], 0.0), writes=[k.identF_b])
    P.op('pool', lambda e: e.affine_select(out=k.identF[:], in_=k.identF[:], pattern=[[-1, 128]],
                                           compare_op=ALU.not_equal, fill=1.0, base=0, channel_multiplier=1),
         reads=[k.identF_b], writes=[k.identF_b])
    P.op('dve', lambda e: e.tensor_copy(out=k.identB[:], in_=k.identF[:]), reads=[k.identF_b], writes=[k.identB_b])
    P.op('dve', lambda e: e.memset(k.onesB[:], 1.0), writes=[k.onesB_b])
    P.op('dve', lambda e: e.memset(k.epsr[:, 0:1], 1e-6), writes=[k.epsr_b])
    P.op('dve', lambda e: e.memset(k.epsr[:, 1:2], 1e-5), writes=[k.epsr_b])
    P.op('dve', lambda e: e.memset(k.epsr[:, 2:3], 64e-5), writes=[k.epsr_b])
    P.op('dve', lambda e: e.memset(k.epsr[:, 3:4], 0.0), writes=[k.epsr_b])

    k.hT, k.hT_b, k.hT_off = sb("hT", [128, 8, TWA + 1], BF16)
    k.GT, k.GT_b, _ = sb("GT", [128, 16, TWA], BF16)
    k.wo = (nc.alloc_sbuf_tensor_at("wo", [128, 16, D], BF16, offset=k.hT_off), k.hT_b)
    k.layer_base = off[0]
    k.gpre, k.gpre_b, _ = sb("gpre", [128, D], F32)
    k.gpost, k.gpost_b = k.gpre, k.gpre_b
    k.xt = [sb(f"xt{i}", [128, D], F32) for i in range(2)]
    hn_par = Buf("hnpar")
    k.hn = []
    for i in range(2):
        t_, _, o_ = sb(f"hn{i}", [128, D], BF16)
        k.hn.append((t_, hn_par.sub(i), o_))
    k.of = nc.alloc_sbuf_tensor_at("of", [128, D], F32, offset=k.hn[0][2])
    k.of_b = hn_par
    k.sqf = lambda np_, h: k.of[0:np_, h * 512:(h + 1) * 512]
    k.sq = sb("sqjunk", [128, D], BF16)
    k.st = [sb(f"stat{i}", [128, 8], F32) for i in range(4)]
    k.st_i = [0]
    k.hf = sb("hf32", [128, D], F32)
    k.off = off

    x_src = (dr["xp"], dr["xs"], k.dbuf["xp"], k.dbuf["xs"])
    wouts = ["a_w_out", "b_w_out", "c_w_out", "d_w_out"]
    for l in range(n_layers):
        x_dst = (dr["y_p"], dr["y_s"], k.dbuf["y_p"], k.dbuf["y_s"]) if (l == n_layers - 1) else \
            ((dr["xscr"], dr["xscr_s"], k.dbuf["xscr"], k.dbuf["xscr_s"]) if l % 2 == 0 else
             (dr["xscr2"], dr["xscr2_s"], k.dbuf["xscr2"], k.dbuf["xscr2_s"]))
        for ps_ in range(2):
            stage1(k, l, ps_, x_src)
            P.barrier()
            off[0] = k.layer_base
            if l == 0:
                layer_conv(k, ps_)
            elif l == 1:
                layer_rwkv(k, ps_)
            elif l == 2:
                layer_mla(k, ps_)
            elif l == 3:
                layer_s5(k, ps_)
            P.barrier()
            stage3(k, l, ps_, x_src, x_dst, dr[wouts[l]], k.dbuf[wouts[l]])
            P.barrier()
        x_src = x_dst
    P.finish('sp')
    k.n_ins = P.emit(nc)
    return nc, k


def ntiles(ps_):
    tl = [(i * 512, 512) for i in range(4)]
    if ps_ == 0:
        tl.append((T, NS))
    return tl


def rstd_from_ss(k, ss_ap, ss_b, out_ap, out_b, npart, inv_n, eps_col):
    P = k.P
    P.op('act', lambda e: e.activation(out=out_ap, in_=ss_ap, func=AF.Sqrt, scale=inv_n,
                                       bias=k.epsr[0:npart, eps_col:eps_col + 1]),
         reads=[ss_b, k.epsr_b], writes=[out_b])
    P.op('dve', lambda e: e.reciprocal(out=out_ap, in_=out_ap), reads=[out_b], writes=[out_b])


def stage1(k, l, ps_, x_src):
    P, dr = k.P, k.dr
    xp, xs, xp_b, xs_b = x_src
    P.op('dve', lambda e: e.memset(k.hT[:, :, 0:1], 0.0), writes=[k.hT_b])
    P.op('sp', lambda e: e.dma_start(out=k.gpre[:], in_=dr["norm_pre"][l:l + 1, :].partition_broadcast(128)),
         writes=[k.gpre_b], dma=True)
    tiles = [(xp[ps_, tt * 128:(tt + 1) * 128, :], 128, tt * 128, xp_b) for tt in range(16)]
    if ps_ == 0:
        tiles.append((xs[:, :], NS, T, xs_b))
    for i, (src, np_, c0, src_b) in enumerate(tiles):
        xt, xt_b, _ = k.xt[i % 2]
        hn, hn_b, _ = k.hn[i % 2]
        sq, sq_b, _ = k.sq
        st, st_b, _ = k.st[k.st_i[0] % 4]
        k.st_i[0] += 1
        P.op('sp', lambda e, xt=xt, src=src, np_=np_: e.dma_start(out=xt[0:np_, :], in_=src),
             reads=[src_b], writes=[xt_b], dma=True)
        P.op('act', lambda e, xt=xt, sq=sq, st=st, np_=np_: e.activation(out=sq[0:np_, :], in_=xt[0:np_, :], func=AF.Square,
                                                                         accum_out=st[0:np_, 0:1]),
             reads=[xt_b], writes=[sq_b, st_b])
        rstd_from_ss(k, st[0:np_, 0:1], st_b, st[0:np_, 1:2], st_b, np_, 1.0 / D, 0)
        P.op('dve', lambda e, hn=hn, xt=xt, st=st, np_=np_: e.scalar_tensor_tensor(
            out=hn[0:np_, :], in0=xt[0:np_, :], scalar=st[0:np_, 1:2], in1=k.gpre[0:np_, :], op0=ALU.mult, op1=ALU.mult),
            reads=[xt_b, st_b, k.gpre_b], writes=[hn_b])
        psT, psT_b = k.psT
        for kk in range(8):
            P.op('pe', lambda e, hn=hn, kk=kk, np_=np_: e.transpose(out=psT[:, kk * 128:kk * 128 + np_],
                                                                    in_=hn[0:np_, kk * 128:(kk + 1) * 128],
                                                                    identity=k.identB[0:np_, 0:np_]),
                 reads=[hn_b, k.identB_b], writes=[psT_b])
        P.op('act', lambda e, c0=c0, np_=np_: e.activation(
            out=k.hT[:, :, c0 + 1:c0 + 1 + np_], in_=psT[:].rearrange("p (k t) -> p k t", k=8)[:, :, 0:np_], func=AF.Copy),
            reads=[psT_b], writes=[k.hT_b.sub(c0 // 128)])
        if l == 1 and (i == 15 or np_ == NS):
            hf, hf_b, _ = k.hf
            P.op('dve', lambda e, hf=hf, xt=xt, st=st, np_=np_: e.scalar_tensor_tensor(
                out=hf[0:np_, :], in0=xt[0:np_, :], scalar=st[0:np_, 1:2], in1=k.gpre[0:np_, :], op0=ALU.mult, op1=ALU.mult),
                reads=[xt_b, st_b, k.gpre_b], writes=[hf_b])
            if np_ == NS:
                P.op('sp', lambda e, hf=hf: e.dma_start(out=dr["shift_s"][:, :], in_=hf[0:NS, :]), reads=[hf_b],
                     writes=[k.dbuf["shift_s"]], dma=True)
            else:
                P.op('sp', lambda e, hf=hf: e.dma_start(out=dr["shift_p"][ps_:ps_ + 1, :], in_=hf[127:128, :]), reads=[hf_b],
                     writes=[k.dbuf["shift_p"]], dma=True)


def hT_reads(k, c0, w):
    return [k.hT_b.sub(c) for c in range(c0 // 128, (c0 + w + 127) // 128)]


def load_w_cols(k, wt, wt_b, w_dram, w_dram_b, col0, ncols=128, nk=8):
    src = w_dram[:, col0:col0 + ncols].rearrange("(k p) e -> p k e", p=128)
    k.P.op('pool', lambda e: e.dma_start(out=wt[:, 0:nk, 0:ncols], in_=src), reads=[w_dram_b], writes=[wt_b], dma=True)


def layer_conv(k, ps_):
    P, dr, nc, sb = k.P, k.dr, k.nc, k.sb
    TW = TWA if ps_ == 0 else T
    if not hasattr(k, "cv"):
        cv = K()
        k.cv = cv
        cv.wt = [[sb(f"cvw{j}_{i}", [128, 8, 128], BF16) for i in range(2)] for j in range(2)]
        cv.wt.append(cv.wt[0])
        cv.cw = sb("cv_cw", [128, 16, 31], F32)
        cv.bin = sb("cv_bin", [128, 48], F32)
        cv.cb = sb("cv_cb", [128, 16], F32)
        cv.lg = sb("cv_lg", [128, 16], F32)
        cv.lb = sb("cv_lb", [128, 16], F32)
        cv.dg = sb("cv_dg", [128, 31, 128], BF16)
        cv.u = sb("cv_u", [128, 30 + T], BF16)
        cv.us = sb("cv_us", [128, NS], F32)
        cv.uf = [sb(f"cv_uf{i}", [128, 512], F32) for i in range(2)]
        cv.sg = [sb(f"cv_sg{i}", [128, 512], F32) for i in range(2)]
        cv.mean = sb("cv_mean", [128, TWA], F32)
        cv.rstd = sb("cv_rstd", [128, TWA], F32)
        cv.tmp = [cv.uf[0], cv.sg[0], cv.uf[1]]
        cv.csq = [(nc.alloc_sbuf_tensor_at(f"cv_csq{i}", [128, 512], BF16, offset=cv.sg[1][2] + i * 1024), cv.sg[1][1].sub(i), 0)
                  for i in range(2)]
        cv.cpt = (nc.alloc_sbuf_tensor_at("cv_cpt", [32, E], F32, offset=cv.mean[2]), cv.mean[1], 0)
        cv.ust = (nc.alloc_sbuf_tensor_at("cv_ust", [16, E], F32, offset=cv.rstd[2]), cv.rstd[1], 0)
        cv.strow = [sb(f"cv_strow{i}", [128, 128], F32) for i in range(4)]
        cv.stT = sb("cv_stT", [128, 480], F32)
        cv.prod = sb("cv_prod", [128, 480], F32)
        cv.cs = sb("cv_cs", [128, NS], F32)
    cv = k.cv
    if True:
        for c_ in range(16):
            P.op('sp', lambda e, c_=c_: e.dma_start(out=cv.cw[0][:, c_, :],
                                                    in_=dr["a_conv_w"][:, c_ * 128:(c_ + 1) * 128].rearrange("k p -> p k"),
                                                    allow_slow_non_contiguous=True), writes=[cv.cw[1]], dma=True)
        P.op('sp', lambda e: e.dma_start(out=cv.bin[0][:], in_=dr["a_b_in"].rearrange("(c p) -> p c", p=128),
                                         allow_slow_non_contiguous=True), writes=[cv.bin[1]], dma=True)
        for t_, nm in ((cv.cb, "a_conv_b"), (cv.lg, "a_ln_g"), (cv.lb, "a_ln_b")):
            P.op('sp', lambda e, t_=t_, nm=nm: e.dma_start(out=t_[0][:], in_=dr[nm].rearrange("(c p) -> p c", p=128),
                                                           allow_slow_non_contiguous=True), writes=[t_[1]], dma=True)
        P.op('dve', lambda e: e.memset(cv.u[0][:, 0:30], 0.0), writes=[cv.u[1].sub('pad')])
    cv = k.cv
    tiles = ntiles(ps_)
    u, u_b, _ = cv.u
    a_w_in, a_w_in_b = dr["a_w_in"], k.dbuf["a_w_in"]
    for ec in range(16):
        wa, wa_b, _ = cv.wt[0][ec % 2]
        wb, wb_b, _ = cv.wt[1][ec % 2]
        load_w_cols(k, wa, wa_b, a_w_in, a_w_in_b, ec * 128)
        load_w_cols(k, wb, wb_b, a_w_in, a_w_in_b, E + ec * 128)
        dg, dg_b, _ = cv.dg
        for kk in range(31):
            P.op('dve', lambda e, kk=kk, ec=ec: e.tensor_scalar(out=dg[:, kk, :], in0=k.identF[:], scalar1=cv.cw[0][:, ec, kk:kk + 1],
                                                                scalar2=None, op0=ALU.mult),
                 reads=[k.identF_b, cv.cw[1]], writes=[dg_b.sub(kk)])
        for ti, (c0, w) in enumerate(tiles):
            pa, pa_b = k.psn()
            pb, pb_b = k.psn()
            for kk in range(8):
                P.op('pe', lambda e, kk=kk, c0=c0, w=w, pa=pa, wa=wa: e.matmul(pa[:, 0:w], lhsT=wa[:, kk, :], rhs=k.hT[:, kk, c0 + 1:c0 + 1 + w],
                                                                              start=(kk == 0), stop=(kk == 7)),
                     reads=[wa_b] + hT_reads(k, c0, w), writes=[pa_b])
            for kk in range(8):
                P.op('pe', lambda e, kk=kk, c0=c0, w=w, pb=pb, wb=wb: e.matmul(pb[:, 0:w], lhsT=wb[:, kk, :], rhs=k.hT[:, kk, c0 + 1:c0 + 1 + w],
                                                                              start=(kk == 0), stop=(kk == 7)),
                     reads=[wb_b] + hT_reads(k, c0, w), writes=[pb_b])
            sg, sg_b, _ = cv.sg[ti % 2]
            uf, uf_b, _ = cv.uf[ti % 2]
            P.op('act', lambda e, sg=sg, pb=pb, w=w, ec=ec: e.activation(out=sg[:, 0:w], in_=pb[:, 0:w], func=AF.Sigmoid,
                                                                        bias=cv.bin[0][:, 16 + ec:17 + ec]),
                 reads=[pb_b, cv.bin[1]], writes=[sg_b])
            if w == 512:
                P.op('dve', lambda e, uf=uf, pa=pa, sg=sg, ec=ec: e.scalar_tensor_tensor(
                    out=uf[:], in0=pa[:], scalar=cv.bin[0][:, ec:ec + 1], in1=sg[:], op0=ALU.add, op1=ALU.mult),
                    reads=[pa_b, sg_b, cv.bin[1]], writes=[uf_b])
                P.op('act', lambda e, uf=uf, c0=c0: e.activation(out=u[:, 30 + c0:30 + c0 + 512], in_=uf[:], func=AF.Copy),
                     reads=[uf_b], writes=[u_b.sub(ti)])
                if ti == 3:
                    pt, pt_b = k.psn()
                    P.op('pe', lambda e, uf=uf, pt=pt: e.transpose(out=pt[0:30, 0:128], in_=uf[:, 482:512], identity=k.identF[:]),
                         reads=[uf_b, k.identF_b], writes=[pt_b])
                    P.op('dve', lambda e, pt=pt, ec=ec: e.tensor_copy(out=cv.cpt[0][0:30, ec * 128:(ec + 1) * 128], in_=pt[0:30, 0:128]),
                         reads=[pt_b], writes=[cv.cpt[1].sub(ec)])
            else:
                us, us_b, _ = cv.us
                P.op('dve', lambda e, pa=pa, sg=sg, ec=ec: e.scalar_tensor_tensor(
                    out=us[:, :], in0=pa[:, 0:NS], scalar=cv.bin[0][:, ec:ec + 1], in1=sg[:, 0:NS], op0=ALU.add, op1=ALU.mult),
                    reads=[pa_b, sg_b, cv.bin[1]], writes=[us_b])
        for ti in range(4):
            c0 = ti * 512
            pc, pc_b = k.psn()
            rd = [u_b.sub(ti), u_b.sub('pad')] + ([u_b.sub(ti - 1)] if ti > 0 else [])
            for kk in range(31):
                P.op('pe', lambda e, kk=kk, c0=c0, pc=pc: e.matmul(pc[:, :], lhsT=dg[:, kk, :], rhs=u[:, c0 + kk:c0 + kk + 512],
                                                                   start=(kk == 0), stop=(kk == 30)),
                     reads=[dg_b.sub(kk)] + rd, writes=[pc_b])
            P.op('act', lambda e, pc=pc, c0=c0, ec=ec: e.activation(out=k.GT[:, ec, c0:c0 + 512], in_=pc[:, :], func=AF.Identity,
                                                                   bias=cv.cb[0][:, ec:ec + 1]),
                 reads=[pc_b, cv.cb[1]], writes=[k.GT_b.sub(ec).sub(ti)])
        if ps_ == 0:
            conv_sample(k, ec)
    P.op('sp', lambda e: e.dma_start(out=dr["conv_p"][ps_, :, :], in_=cv.cpt[0][0:30, :]), reads=[cv.cpt[1]],
         writes=[k.dbuf["conv_p"]], dma=True)
    if ps_ == 0:
        P.op('sp', lambda e: e.dma_start(out=dr["conv_s"][:, 29, :], in_=cv.ust[0][:, :]), reads=[cv.ust[1]],
             writes=[k.dbuf["conv_s"]], dma=True)
        P.op('sp', lambda e: e.dma_start(out=dr["conv_s"][:, 0:29, :], in_=dr["state_conv"][:, 1:30, :]),
             reads=[k.dbuf["state_conv"]], writes=[k.dbuf["conv_s"]], dma=True)
    mean, mean_b, _ = cv.mean
    rstd, rstd_b, _ = cv.rstd
    for ti, (c0, w) in enumerate(tiles):
        p1, p1_b = k.psn()
        p2, p2_b = k.psn()
        for ec in range(16):
            P.op('pe', lambda e, ec=ec, c0=c0, w=w, p1=p1: e.matmul(p1[:, 0:w], lhsT=k.onesB[:], rhs=k.GT[:, ec, c0:c0 + w],
                                                                   start=(ec == 0), stop=(ec == 15)),
                 reads=[k.onesB_b, k.GT_b.sub(ec).sub(ti)], writes=[p1_b])
        for ec in range(16):
            cq, cq_b, _ = cv.csq[ec % 2]
            P.op('act', lambda e, ec=ec, c0=c0, w=w, cq=cq: e.activation(out=cq[:, 0:w], in_=k.GT[:, ec, c0:c0 + w], func=AF.Square),
                 reads=[k.GT_b.sub(ec).sub(ti)], writes=[cq_b])
            P.op('pe', lambda e, ec=ec, w=w, p2=p2, cq=cq: e.matmul(p2[:, 0:w], lhsT=k.onesB[:], rhs=cq[:, 0:w],
                                                                   start=(ec == 0), stop=(ec == 15)),
                 reads=[k.onesB_b, cq_b], writes=[p2_b])
        tm, tm_b, _ = cv.tmp[0]
        P.op('act', lambda e, c0=c0, w=w, p1=p1: e.activation(out=mean[:, c0:c0 + w], in_=p1[:, 0:w], func=AF.Copy, scale=1.0 / E),
             reads=[p1_b], writes=[mean_b.sub(ti)])
        P.op('act', lambda e, w=w, p1=p1: e.activation(out=tm[:, 0:w], in_=p1[:, 0:w], func=AF.Square, scale=1.0 / E),
             reads=[p1_b], writes=[tm_b])
        P.op('dve', lambda e, c0=c0, w=w, p2=p2: e.scalar_tensor_tensor(out=rstd[:, c0:c0 + w], in0=p2[:, 0:w], scalar=1.0 / E,
                                                                        in1=tm[:, 0:w], op0=ALU.mult, op1=ALU.subtract),
             reads=[p2_b, tm_b], writes=[rstd_b.sub(ti)])
        P.op('act', lambda e, c0=c0, w=w: e.activation(out=rstd[:, c0:c0 + w], in_=rstd[:, c0:c0 + w], func=AF.Sqrt,
                                                       bias=k.epsr[:, 1:2]),
             reads=[rstd_b.sub(ti), k.epsr_b], writes=[rstd_b.sub(ti)])
        P.op('dve', lambda e, c0=c0, w=w: e.reciprocal(out=rstd[:, c0:c0 + w], in_=rstd[:, c0:c0 + w]),
             reads=[rstd_b.sub(ti)], writes=[rstd_b.sub(ti)])
    for ec in range(16):
        wz, wz_b, _ = cv.wt[2][ec % 2]
        load_w_cols(k, wz, wz_b, a_w_in, a_w_in_b, 2 * E + ec * 128)
        for ti, (c0, w) in enumerate(tiles):
            pz, pz_b = k.psn()
            for kk in range(8):
                P.op('pe', lambda e, kk=kk, c0=c0, w=w, pz=pz, wz=wz: e.matmul(pz[:, 0:w], lhsT=wz[:, kk, :], rhs=k.hT[:, kk, c0 + 1:c0 + 1 + w],
                                                                              start=(kk == 0), stop=(kk == 7)),
                     reads=[wz_b] + hT_reads(k, c0, w), writes=[pz_b])
            t0, t0_b, _ = cv.tmp[1]
            t1, t1_b, _ = cv.tmp[2]
            gb = k.GT_b.sub(ec).sub(ti)
            P.op('dve', lambda e, ec=ec, c0=c0, w=w: e.tensor_tensor(out=t0[:, 0:w], in0=k.GT[:, ec, c0:c0 + w], in1=mean[:, c0:c0 + w],
                                                                    op=ALU.subtract),
                 reads=[gb, mean_b.sub(ti)], writes=[t0_b])
            P.op('dve', lambda e, c0=c0, w=w: e.tensor_tensor(out=t0[:, 0:w], in0=t0[:, 0:w], in1=rstd[:, c0:c0 + w], op=ALU.mult),
                 reads=[t0_b, rstd_b.sub(ti)], writes=[t0_b])
            P.op('act', lambda e, ec=ec, w=w: e.activation(out=t0[:, 0:w], in_=t0[:, 0:w], func=AF.Silu,
                                                           scale=cv.lg[0][:, ec:ec + 1], bias=cv.lb[0][:, ec:ec + 1]),
                 reads=[t0_b, cv.lg[1], cv.lb[1]], writes=[t0_b])
            P.op('act', lambda e, ec=ec, w=w, pz=pz: e.activation(out=t1[:, 0:w], in_=pz[:, 0:w], func=AF.Silu,
                                                                  bias=cv.bin[0][:, 32 + ec:33 + ec]),
                 reads=[pz_b, cv.bin[1]], writes=[t1_b])
            P.op('dve', lambda e, ec=ec, c0=c0, w=w: e.tensor_tensor(out=k.GT[:, ec, c0:c0 + w], in0=t0[:, 0:w], in1=t1[:, 0:w], op=ALU.mult),
                 reads=[t0_b, t1_b], writes=[gb])


def conv_sample(k, ec):
    P, dr, cv = k.P, k.dr, k.cv
    us, us_b, _ = cv.us
    stT, stT_b, _ = cv.stT
    pt, pt_b = k.psn()
    for r in range(4):
        nr = 128 if r < 3 else 96
        sr, sr_b, _ = cv.strow[r]
        src = dr["state_conv"].rearrange("s k e -> (s k) e")[r * 128:r * 128 + nr, ec * 128:(ec + 1) * 128]
        P.op('sp', lambda e, sr=sr, src=src, nr=nr: e.dma_start(out=sr[0:nr, :], in_=src), reads=[k.dbuf["state_conv"]],
             writes=[sr_b], dma=True)
        P.op('pe', lambda e, sr=sr, nr=nr, r=r, pt=pt: e.transpose(out=pt[:, r * 128:r * 128 + nr], in_=sr[0:nr, :],
                                                                  identity=k.identF[0:nr, 0:nr]),
             reads=[sr_b, k.identF_b], writes=[pt_b])
    P.op('dve', lambda e, pt=pt: e.tensor_copy(out=stT[:, :], in_=pt[:, 0:480]), reads=[pt_b], writes=[stT_b])
    prod, prod_b, _ = cv.prod
    cs, cs_b, _ = cv.cs
    P.op('dve', lambda e, ec=ec: e.tensor_tensor(out=prod[:, :].rearrange("p (s k) -> p s k", k=30),
                                                 in0=stT[:, :].rearrange("p (s k) -> p s k", k=30),
                                                 in1=cv.cw[0][:, ec:ec + 1, 0:30].to_broadcast([128, NS, 30]), op=ALU.mult),
         reads=[stT_b, cv.cw[1]], writes=[prod_b])
    P.op('dve', lambda e: e.tensor_reduce(out=cs[:, :], in_=prod[:, :].rearrange("p (s k) -> p s k", k=30), axis=AX.X, op=ALU.add),
         reads=[prod_b], writes=[cs_b])
    P.op('dve', lambda e, ec=ec: e.scalar_tensor_tensor(out=cs[:, :], in0=us[:, :], scalar=cv.cw[0][:, ec, 30:31], in1=cs[:, :],
                                                        op0=ALU.mult, op1=ALU.add),
         reads=[us_b, cs_b, cv.cw[1]], writes=[cs_b])
    P.op('act', lambda e, ec=ec: e.activation(out=k.GT[:, ec, T:T + NS], in_=cs[:, :], func=AF.Identity, bias=cv.cb[0][:, ec:ec + 1]),
         reads=[cs_b, cv.cb[1]], writes=[k.GT_b.sub(ec).sub(4)])
    p2, p2_b = k.psn()
    P.op('pe', lambda e, p2=p2: e.transpose(out=p2[0:NS, 0:128], in_=us[:, :], identity=k.identF[:]),
         reads=[us_b, k.identF_b], writes=[p2_b])
    P.op('dve', lambda e, p2=p2, ec=ec: e.tensor_copy(out=cv.ust[0][:, ec * 128:(ec + 1) * 128], in_=p2[0:NS, 0:128]),
         reads=[p2_b], writes=[cv.ust[1].sub(ec)])


def stage3(k, l, ps_, x_src, x_dst, w_out, w_out_b):
    P, dr = k.P, k.dr
    xp, xs, xp_b, xs_b = x_src
    yp, ys, yp_b, ys_b = x_dst
    wo, wo_b = k.wo
    P.op('sp', lambda e: e.dma_start(out=k.gpost[:], in_=dr["norm_post"][l:l + 1, :].partition_broadcast(128)),
         writes=[k.gpost_b], dma=True)
    for ec in range(16):
        P.op('pool', lambda e, ec=ec: e.dma_start(out=wo[:, ec, :], in_=w_out[ec * 128:(ec + 1) * 128, :]),
             reads=[w_out_b], writes=[wo_b], dma=True)
    tiles = [(xp[ps_, tt * 128:(tt + 1) * 128, :], yp[ps_, tt * 128:(tt + 1) * 128, :], 128, tt * 128, xp_b, yp_b) for tt in range(16)]
    if ps_ == 0:
        tiles.append((xs[:, :], ys[:, :], NS, T, xs_b, ys_b))
    for i, (src, dst, np_, c0, src_b, dst_b) in enumerate(tiles):
        xt, xt_b, _ = k.xt[i % 2]
        hn, hn_b, _ = k.hn[i % 2]
        sq, sq_b, _ = k.sq
        st, st_b, _ = k.st[k.st_i[0] % 4]
        k.st_i[0] += 1
        P.op('sp', lambda e, xt=xt, src=src, np_=np_: e.dma_start(out=xt[0:np_, :], in_=src),
             reads=[src_b], writes=[xt_b], dma=True)
        pp = [k.psn(), k.psn()]
        gtr = [k.GT_b.sub(ec).sub(min(c0 // 512, 4)) for ec in range(16)]
        for h in range(2):
            ph, ph_b = pp[h]
            for ec in range(16):
                P.op('pe', lambda e, ec=ec, h=h, ph=ph, c0=c0, np_=np_: e.matmul(ph[0:np_, :], lhsT=k.GT[:, ec, c0:c0 + np_],
                                                                                rhs=wo[:, ec, h * 512:(h + 1) * 512],
                                                                                start=(ec == 0), stop=(ec == 15)),
                     reads=[wo_b, gtr[ec]], writes=[ph_b])
            P.op('act', lambda e, h=h, ph=ph, st=st, np_=np_: e.activation(out=sq[0:np_, 0:512], in_=ph[0:np_, :], func=AF.Square,
                                                                          accum_out=st[0:np_, 2 + h:3 + h]),
                 reads=[ph_b], writes=[sq_b, st_b])
        P.op('dve', lambda e, st=st, np_=np_: e.tensor_tensor(out=st[0:np_, 4:5], in0=st[0:np_, 2:3], in1=st[0:np_, 3:4], op=ALU.add),
             reads=[st_b], writes=[st_b])
        rstd_from_ss(k, st[0:np_, 4:5], st_b, st[0:np_, 5:6], st_b, np_, 1.0 / D, 0)
        for h in range(2):
            ph, ph_b = pp[h]
            P.op('dve', lambda e, h=h, ph=ph, st=st, np_=np_, hn=hn: e.scalar_tensor_tensor(
                out=k.sqf(np_, h), in0=ph[0:np_, :], scalar=st[0:np_, 5:6], in1=k.gpost[0:np_, h * 512:(h + 1) * 512],
                op0=ALU.mult, op1=ALU.mult),
                reads=[ph_b, st_b, k.gpost_b], writes=[k.of_b])
        P.op('dve', lambda e, xt=xt, np_=np_: e.tensor_tensor(out=xt[0:np_, :], in0=xt[0:np_, :], in1=k.of[0:np_, :], op=ALU.add),
             reads=[xt_b, k.of_b], writes=[xt_b])
        P.op('sp', lambda e, xt=xt, dst=dst, np_=np_: e.dma_start(out=dst, in_=xt[0:np_, :]),
             reads=[xt_b], writes=[dst_b], dma=True)

def op_tt(k, eng, out, in0, in1, op, reads, writes):
    return k.P.op(eng, lambda e: e.tensor_tensor(out=out, in0=in0, in1=in1, op=op), reads, writes)


def op_ts(k, eng, out, in0, s1, s2, op0, op1, reads, writes):
    if s2 is None:
        return k.P.op(eng, lambda e: e.tensor_scalar(out=out, in0=in0, scalar1=s1, scalar2=None, op0=op0), reads, writes)
    return k.P.op(eng, lambda e: e.tensor_scalar(out=out, in0=in0, scalar1=s1, scalar2=s2, op0=op0, op1=op1), reads, writes)


def op_stt(k, eng, out, in0, scalar, in1, op0, op1, reads, writes):
    return k.P.op(eng, lambda e: e.scalar_tensor_tensor(out=out, in0=in0, scalar=scalar, in1=in1, op0=op0, op1=op1), reads, writes)


def op_act(k, out, in_, func, reads, writes, scale=None, bias=None):
    kw = {}
    if scale is not None:
        kw['scale'] = scale
    if bias is not None:
        kw['bias'] = bias
    return k.P.op('act', lambda e: e.activation(out=out, in_=in_, func=func, **kw), reads, writes)


def op_mm(k, out, lhsT, rhs, start, stop, reads, writes):
    return k.P.op('pe', lambda e: e.matmul(out, lhsT=lhsT, rhs=rhs, start=start, stop=stop), reads, writes)


def op_tr(k, out, in_, ident, reads, writes):
    return k.P.op('pe', lambda e: e.transpose(out=out, in_=in_, identity=ident), reads, writes)


def op_dma(k, eng, out, in_, reads, writes, slow=False):
    if slow:
        return k.P.op(eng, lambda e: e.dma_start(out=out, in_=in_, allow_slow_non_contiguous=True), reads, writes, dma=True)
    return k.P.op(eng, lambda e: e.dma_start(out=out, in_=in_), reads, writes, dma=True)


def hT_reads_prev(k, c0, w):
    out = []
    if c0 == 0:
        out.append(k.hT_b.sub('pad'))
    lo = max(c0 - 1, 0) // 128
    hi = (c0 + w - 2) // 128
    for c in range(lo, hi + 1):
        out.append(k.hT_b.sub(c))
    return out


DECAY_C = 0.6065306597126334


def rwkv_setup(k):
    P, dr, nc, sb = k.P, k.dr, k.nc, k.sb
    rw = K()
    k.rw = rw
    rw.W = [[sb(f"rwW{j}{ab}", [128, 8, 128], BF16) for ab in range(2)] for j in range(4)]
    rw.wraw = sb("rw_wraw", [128, 8, 128], BF16)
    rw.mu = sb("rw_mu", [128, 6, 8], F32)
    rw.omu = sb("rw_omu", [128, 6, 8], F32)
    rw.par = {n: sb("rw_p_" + n, [128, 16], F32) for n in ["w0", "a0", "k_k", "k_a", "omk_a", "r_k", "ln_g", "ln_b"]}
    rw.lw = [sb(f"rw_lw{i}", [128, 8, 128], BF16) for i in range(2)]
    rw.w2a2 = sb("rw_w2a2", [128, E], BF16)
    rw.lora = sb("rw_lora", [128, TWA], BF16)
    rw.mscan = sb("rw_mscan", [128, 512], F32)
    rw.mk = {n: sb("rw_mk_" + n, [128, 512], BF16) for n in ["nUs", "nLs", "Us", "Ui"]}
    rw.irep = sb("rw_irep", [128, 64], F32)
    rw.boB = sb("rw_boB", [128, 128], BF16)
    rw.boF = sb("rw_boF", [128, 128], F32)
    rw.f = [sb(f"rw_f{i}", [128, 512], F32) for i in range(13)]
    rw.h16 = {n: sb("rw_h_" + n, [128, 512], BF16) for n in ["KT", "RT", "BT", "KKT", "VT", "ZS", "YB", "YQ"]}
    rw.tok = {n: sb("rw_tok_" + n, [128, 8, 64], BF16) for n in ["B", "K", "V"]}
    rw.g = {n: sb("rw_g_" + n, [128, 512], BF16) for n in ["Inv", "AkT", "BrT", "KrT"]}
    rw.Pp = [sb(f"rw_P{i}", [128, 512], BF16) for i in range(2)]
    rw.Qp = [sb(f"rw_Q{i}", [128, 512], BF16) for i in range(2)]
    rw.S = sb("rw_S", [128, 512], F32)
    rw.Sbf = sb("rw_Sbf", [128, 512], BF16)
    rw.tokKap = sb("rw_tokKap", [128, 8, 64], BF16)
    rw.M1Tn = sb("rw_M1Tn", [128, 512], BF16)
    rw.AVsb = sb("rw_AVsb", [128, 8, 64], BF16)
    rw.Wn = sb("rw_Wn", [128, 8, 64], BF16)
    rw.Xsb = sb("rw_Xsb", [128, 64], BF16)
    rw.Usb = sb("rw_Usb", [128, 64], BF16)
    rw.H = sb("rw_H", [128, 64], F32)
    rw.Hbf = sb("rw_Hbf", [128, 64], BF16)
    rw.TH = sb("rw_TH", [128, 64], F32)
    rw.shT = sb("rw_shT", [128, 8, NS], BF16)
    rw.shrow = sb("rw_shrow", [NS, D], F32)
    rw.small = sb("rw_small", [128, 64], F32)


def rwkv_load(k):
    P, dr, nc, rw = k.P, k.dr, k.nc, k.rw
    for j in range(6):
        op_dma(k, 'sp', rw.mu[0][:, j, :], dr["b_mu"][j].rearrange("(k p) -> p k", p=128), [k.dbuf["b_mu"]], [rw.mu[1]], slow=True)
    op_ts(k, 'dve', rw.omu[0][:], rw.mu[0][:], -1.0, 1.0, ALU.mult, ALU.add, [rw.mu[1]], [rw.omu[1]])
    for n, src in [("w0", "b_w0"), ("a0", "b_a0"), ("k_k", "b_k_k"), ("k_a", "b_k_a"), ("ln_g", "b_ln_g"), ("ln_b", "b_ln_b")]:
        op_dma(k, 'sp', rw.par[n][0][:], dr[src].rearrange("(c p) -> p c", p=128), [k.dbuf[src]], [rw.par[n][1]], slow=True)
    op_dma(k, 'sp', rw.par["r_k"][0][:], dr["b_r_k"].rearrange("(c h2) j -> (h2 j) c", h2=2), [k.dbuf["b_r_k"]],
           [rw.par["r_k"][1]], slow=True)
    op_ts(k, 'dve', rw.par["omk_a"][0][:], rw.par["k_a"][0][:], -1.0, 1.0, ALU.mult, ALU.add, [rw.par["k_a"][1]], [rw.par["omk_a"][1]])
    wr, wr_b, _ = rw.wraw
    op_dma(k, 'pool', wr[:, :, 0:64], dr["b_w1"].rearrange("(k p) e -> p k e", p=128), [k.dbuf["b_w1"]], [wr_b])
    op_dma(k, 'pool', wr[:, :, 64:128], dr["b_a1"].rearrange("(k p) e -> p k e", p=128), [k.dbuf["b_a1"]], [wr_b])
    for half, j in ((0, 4), (1, 5)):
        cs_ = slice(half * 64, half * 64 + 64)
        op_tt(k, 'dve', rw.lw[0][0][:, :, cs_], wr[:, :, cs_], rw.omu[0][:, j, :].unsqueeze(2).to_broadcast([128, 8, 64]), ALU.mult,
              [wr_b, rw.omu[1]], [rw.lw[0][1]])
        op_tt(k, 'dve', rw.lw[1][0][:, :, cs_], wr[:, :, cs_], rw.mu[0][:, j, :].unsqueeze(2).to_broadcast([128, 8, 64]), ALU.mult,
              [wr_b, rw.mu[1]], [rw.lw[1][1]])
    op_dma(k, 'pool', rw.w2a2[0][0:64, :], dr["b_w2"], [k.dbuf["b_w2"]], [rw.w2a2[1]])
    op_dma(k, 'pool', rw.w2a2[0][64:128, :], dr["b_a2"], [k.dbuf["b_a2"]], [rw.w2a2[1]])
    ms, ms_b, _ = rw.mscan
    P.op('dve', lambda e: e.memset(ms[:], 1.0), writes=[ms_b])
    P.op('dve', lambda e: e.memset(ms[:].rearrange("p (c t) -> p c t", t=64)[:, :, 0:1], 0.0), writes=[ms_b])
    for n, val, cm, pat, cmp_ in [("nUs", -1.0, -1, 1, ALU.is_gt), ("Us", 1.0, -1, 1, ALU.is_gt), ("Ui", 1.0, -1, 1, ALU.is_ge),
                                  ("nLs", -1.0, 1, -1, ALU.is_gt)]:
        t_, b_, _ = rw.mk[n]
        tf, tf_b, _ = rw.f[0]
        P.op('pool', lambda e, tf=tf, val=val: e.memset(tf[0:64, :], val), writes=[tf_b])
        P.op('pool', lambda e, tf=tf, cm=cm, pat=pat, cmp_=cmp_: e.affine_select(
            out=tf[0:64, :].rearrange("p (c t) -> p c t", t=64), in_=tf[0:64, :].rearrange("p (c t) -> p c t", t=64),
            pattern=[[0, 8], [pat, 64]], compare_op=cmp_, fill=0.0, base=0, channel_multiplier=cm),
            reads=[tf_b], writes=[tf_b])
        P.op('dve', lambda e, t_=t_, tf=tf: e.tensor_copy(out=t_[0:64, :], in_=tf[0:64, :]), reads=[tf_b], writes=[b_])
        op_dma(k, 'sp', t_[64:128, :], t_[0:64, :], [b_], [b_])
    op_tt(k, 'dve', rw.irep[0][:], k.identF[:, 0:64], k.identF[:, 64:128], ALU.add, [k.identF_b], [rw.irep[1]])
    for t_, b_, _ in (rw.boB, rw.boF):
        P.op('dve', lambda e, t_=t_: e.memset(t_[:], 0.0), writes=[b_])
        P.op('dve', lambda e, t_=t_: e.memset(t_[0:64, 0:64], 1.0), writes=[b_])
        P.op('dve', lambda e, t_=t_: e.memset(t_[64:128, 64:128], 1.0), writes=[b_])


def layer_rwkv(k, ps_):
    P, dr, nc = k.P, k.dr, k.nc
    if not hasattr(k, "rw"):
        rwkv_setup(k)
    rwkv_load(k)
    rw = k.rw
    P.chk("setup")
    tiles = ntiles(ps_)
    lora, lora_b, _ = rw.lora
    if ps_ == 0:
        sr, sr_b, _ = rw.shrow
        op_dma(k, 'sp', sr[:, :], dr["state_shift"], [k.dbuf["state_shift"]], [sr_b])
        pt, pt_b = k.psn()
        for kk in range(8):
            op_tr(k, pt[:, kk * NS:(kk + 1) * NS], sr[0:NS, kk * 128:(kk + 1) * 128], k.identF[0:NS, 0:NS], [sr_b, k.identF_b], [pt_b])
        op_act(k, rw.shT[0][:, :, :], pt[:, 0:8 * NS].rearrange("p (k s) -> p k s", s=NS), AF.Copy, [pt_b], [rw.shT[1]])

    def prev_rhs(kk, c0, w):
        if c0 >= T:
            return rw.shT[0][:, kk, :], [rw.shT[1]]
        return k.hT[:, kk, c0:c0 + w], hT_reads_prev(k, c0, w)

    for ti, (c0, w) in enumerate(tiles):
        pl, pl_b = k.psn()
        for kk in range(8):
            op_mm(k, pl[:, 0:w], rw.lw[0][0][:, kk, :], k.hT[:, kk, c0 + 1:c0 + 1 + w], kk == 0, False,
                  [rw.lw[0][1]] + hT_reads(k, c0, w), [pl_b])
        for kk in range(8):
            r_, rb_ = prev_rhs(kk, c0, w)
            op_mm(k, pl[:, 0:w], rw.lw[1][0][:, kk, :], r_, False, kk == 7, [rw.lw[1][1]] + rb_, [pl_b])
        op_act(k, lora[0:64, c0:c0 + w], pl[0:64, 0:w], AF.Tanh, [pl_b], [lora_b.sub(ti)])
        op_act(k, lora[64:128, c0:c0 + w], pl[64:128, 0:w], AF.Copy, [pl_b], [lora_b.sub(ti)])

    P.chk("lora")
    F = rw.f
    for hp in range(16):
        hc = slice(hp * 128, (hp + 1) * 128)
        wr, wr_b, _ = rw.wraw
        for j in range(4):
            op_dma(k, 'pool', wr[:, :, :], dr["b_w_rkvz"][j][:, hc].rearrange("(k p) e -> p k e", p=128), [k.dbuf["b_w_rkvz"]], [wr_b])
            op_tt(k, 'dve', rw.W[j][0][0][:], wr[:], rw.omu[0][:, j, :].unsqueeze(2).to_broadcast([128, 8, 128]), ALU.mult,
                  [wr_b, rw.omu[1]], [rw.W[j][0][1]])
            op_tt(k, 'pool', rw.W[j][1][0][:], wr[:], rw.mu[0][:, j, :].unsqueeze(2).to_broadcast([128, 8, 128]), ALU.mult,
                  [wr_b, rw.mu[1]], [rw.W[j][1][1]])
        P.chk("w")
        H, H_b, _ = rw.H
        Hbf, Hbf_b, _ = rw.Hbf
        P.op('dve', lambda e: e.memset(H[:], 0.0), writes=[H_b])
        P.op('dve', lambda e: e.memset(Hbf[:], 0.0), writes=[Hbf_b])
        par = lambda n: rw.par[n][0][:, hp:hp + 1]
        parb = lambda n: rw.par[n][1]
        for ti, (c0, w) in enumerate(tiles):
            sample = c0 >= T
            pj = []
            for j in range(4):
                pp, pp_b = k.psn()
                for kk in range(8):
                    op_mm(k, pp[:, 0:w], rw.W[j][0][0][:, kk, :], k.hT[:, kk, c0 + 1:c0 + 1 + w], kk == 0, False,
                          [rw.W[j][0][1]] + hT_reads(k, c0, w), [pp_b])
                for kk in range(8):
                    r_, rb_ = prev_rhs(kk, c0, w)
                    op_mm(k, pp[:, 0:w], rw.W[j][1][0][:, kk, :], r_, False, kk == 7, [rw.W[j][1][1]] + rb_, [pp_b])
                pj.append((pp, pp_b))
            (pr, pr_b), (pk, pk_b), (pv, pv_b), (pz, pz_b) = pj
            P.chk("proj")
            pw, pw_b = k.psn()
            op_mm(k, pw[:, 0:w], rw.w2a2[0][0:64, hc], lora[0:64, c0:c0 + w], True, True, [rw.w2a2[1], lora_b.sub(ti)], [pw_b])
            pa, pa_b = k.psn()
            op_mm(k, pa[:, 0:w], rw.w2a2[0][64:128, hc], lora[64:128, c0:c0 + w], True, True, [rw.w2a2[1], lora_b.sub(ti)], [pa_b])
            P.chk("pwpa")
            W_ = slice(0, w)
            A, SG, TMP, KP, KKF, SQ, Bt, R, V, RK, BON, PP, YN = [F[i] for i in range(13)]
            op_act(k, A[0][:, W_], pa[:, W_], AF.Sigmoid, [pa_b, parb("a0")], [A[1]], bias=par("a0"))
            op_act(k, SG[0][:, W_], pw[:, W_], AF.Sigmoid, [pw_b, parb("w0")], [SG[1]], bias=par("w0"))
            P.chk("e0a")
            op_act(k, TMP[0][:, W_], A[0][:, W_], AF.Identity, [A[1], parb("k_a"), parb("omk_a")], [TMP[1]], scale=par("k_a"), bias=par("omk_a"))
            P.chk("e0b")
            op_tt(k, 'dve', KP[0][:, W_], pk[:, W_], TMP[0][:, W_], ALU.mult, [pk_b, TMP[1]], [KP[1]])
            P.chk("e0c")
            op_ts(k, 'dve', KKF[0][:, W_], pk[:, W_], par("k_k"), None, ALU.mult, None, [pk_b, parb("k_k")], [KKF[1]])
            P.chk("e0d")
            op_act(k, SQ[0][:, W_], KKF[0][:, W_], AF.Square, [KKF[1]], [SQ[1]])
            P.chk("e1")
            pn, pn_b = k.psn()
            op_mm(k, pn[:, W_], rw.boF[0][:], SQ[0][:, W_], True, True, [rw.boF[1], SQ[1]], [pn_b])
            op_act(k, SQ[0][:, W_], pn[:, W_], AF.Sqrt, [pn_b], [SQ[1]])
            op_ts(k, 'dve', SQ[0][:, W_], SQ[0][:, W_], 1e-12, None, ALU.max, None, [SQ[1]], [SQ[1]])
            P.op('dve', lambda e, W_=W_: e.reciprocal(out=SQ[0][:, W_], in_=SQ[0][:, W_]), [SQ[1]], [SQ[1]])
            op_tt(k, 'dve', KKF[0][:, W_], KKF[0][:, W_], SQ[0][:, W_], ALU.mult, [KKF[1], SQ[1]], [KKF[1]])
            op_tt(k, 'dve', Bt[0][:, W_], KKF[0][:, W_], A[0][:, W_], ALU.mult, [KKF[1], A[1]], [Bt[1]])
            P.chk("e2")
            op_act(k, R[0][:, W_], pr[:, W_], AF.Copy, [pr_b], [R[1]])
            op_act(k, V[0][:, W_], pv[:, W_], AF.Copy, [pv_b], [V[1]])
            op_stt(k, 'dve', RK[0][:, W_], R[0][:, W_], par("r_k"), KP[0][:, W_], ALU.mult, ALU.mult, [R[1], KP[1], parb("r_k")], [RK[1]])
            pb, pb_b = k.psn()
            op_mm(k, pb[:, W_], rw.boF[0][:], RK[0][:, W_], True, True, [rw.boF[1], RK[1]], [pb_b])
            op_tt(k, 'dve', BON[0][:, W_], pb[:, W_], V[0][:, W_], ALU.mult, [pb_b, V[1]], [BON[1]])
            ZS = rw.h16["ZS"]
            op_act(k, ZS[0][:, W_], pz[:, W_], AF.Silu, [pz_b], [ZS[1]])
            P.chk("elem")
            if not sample:
                ysrc, ysrc_b = rwkv_chunks(k, hp, ti, c0, A, SG, TMP, KP, KKF, Bt, R, V, PP, pv, pv_b)
            else:
                ysrc, ysrc_b = rwkv_sample(k, hp, SG, KP, KKF, Bt, R, V)
            P.chk("chunks")
            YB, YQ = rw.h16["YB"], rw.h16["YQ"]
            op_act(k, YB[0][:, W_], ysrc[:, W_], AF.Copy, [ysrc_b], [YB[1]])
            op_act(k, YQ[0][:, W_], ysrc[:, W_], AF.Square, [ysrc_b], [YQ[1]])
            pm, pm_b = k.psn()
            pq, pq_b = k.psn()
            op_mm(k, pm[:, W_], rw.boB[0][:], YB[0][:, W_], True, True, [rw.boB[1], YB[1]], [pm_b])
            op_mm(k, pq[:, W_], rw.boB[0][:], YQ[0][:, W_], True, True, [rw.boB[1], YQ[1]], [pq_b])
            MEAN, MSQ, RS = A, SG, TMP
            op_act(k, MEAN[0][:, W_], pm[:, W_], AF.Copy, [pm_b], [MEAN[1]], scale=1.0 / 64)
            op_act(k, MSQ[0][:, W_], pm[:, W_], AF.Square, [pm_b], [MSQ[1]], scale=1.0 / 64)
            op_stt(k, 'dve', RS[0][:, W_], pq[:, W_], 1.0 / 64, MSQ[0][:, W_], ALU.mult, ALU.subtract, [pq_b, MSQ[1]], [RS[1]])
            op_act(k, RS[0][:, W_], RS[0][:, W_], AF.Sqrt, [RS[1], k.epsr_b], [RS[1]], bias=k.epsr[:, 2:3])
            P.op('dve', lambda e, W_=W_, RS=RS: e.reciprocal(out=RS[0][:, W_], in_=RS[0][:, W_]), [RS[1]], [RS[1]])
            op_tt(k, 'dve', YN[0][:, W_], ysrc[:, W_], MEAN[0][:, W_], ALU.subtract, [ysrc_b, MEAN[1]], [YN[1]])
            op_tt(k, 'dve', YN[0][:, W_], YN[0][:, W_], RS[0][:, W_], ALU.mult, [YN[1], RS[1]], [YN[1]])
            op_act(k, YN[0][:, W_], YN[0][:, W_], AF.Identity, [YN[1], parb("ln_g"), parb("ln_b")], [YN[1]], scale=par("ln_g"), bias=par("ln_b"))
            op_tt(k, 'dve', YN[0][:, W_], YN[0][:, W_], BON[0][:, W_], ALU.add, [YN[1], BON[1]], [YN[1]])
            op_tt(k, 'dve', k.GT[:, hp, c0:c0 + w], YN[0][:, W_], ZS[0][:, W_], ALU.mult, [YN[1], ZS[1]], [k.GT_b.sub(hp).sub(ti)])
            P.chk("gn")
            if ti == 3:
                pt, pt_b = k.psn()
                for h in range(2):
                    hP = slice(h * 64, h * 64 + 64)
                    op_mm(k, pt[hP, 0:64], H[hP, :], k.identF[hP, hP], True, True, [H_b, k.identF_b], [pt_b])
                sm, sm_b, _ = rw.small
                P.op('dve', lambda e, pt=pt: e.tensor_copy(out=sm[:, :], in_=pt[:, 0:64]), [pt_b], [sm_b])
                op_dma(k, 'sp', dr["wkv_p"][ps_, 2 * hp:2 * hp + 2, :, :].rearrange("h i j -> (h i) j"), sm[:, :], [sm_b], [k.dbuf["wkv_p"]])


def rwkv_chunks(k, hp, ti, c0, A, SG, TMP, KP, KK, Bt, R, V, PP, pv, pv_b):
    P, rw = k.P, k.rw
    H, H_b, _ = rw.H
    Hbf, Hbf_b, _ = rw.Hbf
    KT, RT, BT, KKT, VT = [rw.h16[n] for n in ["KT", "RT", "BT", "KKT", "VT"]]
    ms = rw.mscan
    CS = TMP
    P.op('dve', lambda e: e.tensor_tensor_scan(out=CS[0][:], data0=ms[0][:], data1=SG[0][:], initial=0.0, op0=ALU.mult, op1=ALU.add),
         [ms[1], SG[1]], [CS[1]])
    op_tt(k, 'dve', SG[0][:], CS[0][:], SG[0][:], ALU.subtract, [CS[1], SG[1]], [SG[1]])
    op_act(k, PP[0][:], CS[0][:], AF.Exp, [CS[1]], [PP[1]], scale=-DECAY_C)
    op_act(k, CS[0][:], CS[0][:], AF.Exp, [CS[1]], [CS[1]], scale=DECAY_C)
    op_act(k, SG[0][:], SG[0][:], AF.Exp, [SG[1]], [SG[1]], scale=-DECAY_C)
    op_tt(k, 'dve', KT[0][:], KK[0][:], SG[0][:], ALU.mult, [KK[1], SG[1]], [KT[1]])
    op_tt(k, 'dve', RT[0][:], R[0][:], PP[0][:], ALU.mult, [R[1], PP[1]], [RT[1]])
    op_tt(k, 'dve', BT[0][:], Bt[0][:], CS[0][:], ALU.mult, [Bt[1], CS[1]], [BT[1]])
    op_tt(k, 'dve', KKT[0][:], KP[0][:], CS[0][:], ALU.mult, [KP[1], CS[1]], [KKT[1]])
    op_act(k, VT[0][:], pv[:, :], AF.Copy, [pv_b], [VT[1]])
    rw.tok["Kap"] = rw.tokKap
    for n, X in (("B", BT), ("K", KKT), ("V", VT), ("Kap", KT)):
        pt, pt_b = k.psn()
        for c in range(8):
            for h in range(2):
                hP = slice(h * 64, h * 64 + 64)
                op_mm(k, pt[hP, c * 64:(c + 1) * 64], X[0][hP, c * 64:(c + 1) * 64], k.identB[hP, hP], True, True,
                      [X[1], k.identB_b], [pt_b])
        op_act(k, rw.tok[n][0][:, :, :], pt[:, 0:512].rearrange("p (c t) -> p c t", t=64), AF.Copy, [pt_b], [rw.tok[n][1]])
    Btok, Ktok, Vtok = rw.tok["B"], rw.tok["K"], rw.tok["V"]
    P.chk("tok")
    grams = [("AbT", BT, KT), ("Ab", KT, BT), ("AkT", KKT, KT), ("BrT", BT, RT), ("KrT", KKT, RT)]
    gps = {}
    for n, L, Rr in grams:
        pg, pg_b = k.psn()
        for c in range(8):
            cs_ = slice(c * 64, (c + 1) * 64)
            for h in range(2):
                hP = slice(h * 64, h * 64 + 64)
                op_mm(k, pg[hP, cs_], L[0][hP, cs_], Rr[0][hP, cs_], True, True, [L[1], Rr[1]], [pg_b])
        gps[n] = (pg, pg_b)
    Pc, Qc = rw.Pp[0], rw.Qp[0]
    S, Sbf = rw.S, rw.Sbf
    op_tt(k, 'dve', Qc[0][:], gps["AbT"][0][:, :], rw.mk["nUs"][0][:], ALU.mult, [gps["AbT"][1], rw.mk["nUs"][1]], [Qc[1]])
    op_tt(k, 'dve', Pc[0][:], gps["Ab"][0][:, :], rw.mk["nLs"][0][:], ALU.mult, [gps["Ab"][1], rw.mk["nLs"][1]], [Pc[1]])
    op_tt(k, 'dve', rw.g["AkT"][0][:], gps["AkT"][0][:, :], rw.mk["Us"][0][:], ALU.mult, [gps["AkT"][1], rw.mk["Us"][1]], [rw.g["AkT"][1]])
    op_tt(k, 'dve', rw.g["BrT"][0][:], gps["BrT"][0][:, :], rw.mk["Ui"][0][:], ALU.mult, [gps["BrT"][1], rw.mk["Ui"][1]], [rw.g["BrT"][1]])
    op_tt(k, 'dve', rw.g["KrT"][0][:], gps["KrT"][0][:, :], rw.mk["Ui"][0][:], ALU.mult, [gps["KrT"][1], rw.mk["Ui"][1]], [rw.g["KrT"][1]])
    op_tt(k, 'dve', S[0][:].rearrange("p (c t) -> p c t", t=64), Qc[0][:].rearrange("p (c t) -> p c t", t=64),
          rw.irep[0][:, :].unsqueeze(1).to_broadcast([128, 8, 64]), ALU.add, [Qc[1], rw.irep[1]], [S[1]])
    op_act(k, Sbf[0][:], S[0][:], AF.Copy, [S[1]], [Sbf[1]])
    for it in range(1, 6):
        Pn, Qn = rw.Pp[it % 2], rw.Qp[it % 2]
        pP, pP_b = k.psn()
        for c in range(8):
            cs_ = slice(c * 64, (c + 1) * 64)
            for h in range(2):
                hP = slice(h * 64, h * 64 + 64)
                op_mm(k, pP[hP, cs_], Qc[0][hP, cs_], Pc[0][hP, cs_], True, True, [Qc[1], Pc[1]], [pP_b])
        if it < 5:
            pQ, pQ_b = k.psn()
            for c in range(8):
                cs_ = slice(c * 64, (c + 1) * 64)
                for h in range(2):
                    hP = slice(h * 64, h * 64 + 64)
                    op_mm(k, pQ[hP, cs_], Pc[0][hP, cs_], Qc[0][hP, cs_], True, True, [Qc[1], Pc[1]], [pQ_b])
        op_act(k, Pn[0][:], pP[:, :], AF.Copy, [pP_b], [Pn[1]])
        if it < 5:
            P.op('dve', lambda e, Qn=Qn, pQ=pQ: e.tensor_copy(out=Qn[0][:], in_=pQ[:, :]), [pQ_b], [Qn[1]])
        pS, pS_b = k.psn()
        for c in range(8):
            cs_ = slice(c * 64, (c + 1) * 64)
            for h in range(2):
                hP = slice(h * 64, h * 64 + 64)
                op_mm(k, pS[hP, cs_], Pn[0][hP, cs_], Sbf[0][hP, cs_], True, True, [Pn[1], Sbf[1]], [pS_b])
        op_tt(k, 'dve', S[0][:], S[0][:], pS[:, :], ALU.add, [S[1], pS_b], [S[1]])
        dst = Sbf if it < 5 else rw.g["Inv"]
        op_act(k, dst[0][:], S[0][:], AF.Copy, [S[1]], [dst[1]])
        Pc, Qc = Pn, Qn
    Inv, AkT, BrT, KrT = [rw.g[n] for n in ["Inv", "AkT", "BrT", "KrT"]]
    tokKap, M1Tn, AVsb, Wn = rw.tokKap, rw.M1Tn, rw.AVsb, rw.Wn
    pm1, pm1_b = k.psn()
    pav, pav_b = k.psn()
    for c in range(8):
        cs_ = slice(c * 64, (c + 1) * 64)
        for h in range(2):
            hP = slice(h * 64, h * 64 + 64)
            op_mm(k, pm1[hP, cs_], tokKap[0][hP, c, :], Inv[0][hP, cs_], True, True, [tokKap[1], Inv[1]], [pm1_b])
            op_mm(k, pav[hP, cs_], AkT[0][hP, cs_], Vtok[0][hP, c, :], True, True, [AkT[1], Vtok[1]], [pav_b])
    op_act(k, M1Tn[0][:, :], pm1[:, :], AF.Copy, [pm1_b], [M1Tn[1]], scale=-1.0)
    P.op('dve', lambda e: e.tensor_copy(out=AVsb[0][:, :, :], in_=pav[:, :].rearrange("p (c i) -> p c i", i=64)), [pav_b], [AVsb[1]])
    pw2, pw2_b = k.psn()
    for c in range(8):
        cs_ = slice(c * 64, (c + 1) * 64)
        for h in range(2):
            hP = slice(h * 64, h * 64 + 64)
            op_mm(k, pw2[hP, cs_], Inv[0][hP, cs_], AVsb[0][hP, c, :], True, True, [Inv[1], AVsb[1]], [pw2_b])
    op_act(k, Wn[0][:, :, :], pw2[:, :].rearrange("p (c i) -> p c i", i=64), AF.Copy, [pw2_b], [Wn[1]], scale=-1.0)
    P.chk("inv")
    py, py_b = k.ps[6]

    def ps6():
        i = k.ps_i[0] % 6
        k.ps_i[0] += 1
        return k.ps[i]
    Xsb, Usb, TH = rw.Xsb, rw.Usb, rw.TH
    op_ts(k, 'dve', TH[0][:, :], H[:, :], PP[0][:, 63:64], None, ALU.mult, None, [H_b, PP[1]], [TH[1]])
    for c in range(8):
        cs_ = slice(c * 64, (c + 1) * 64)
        pu, pu_b = ps6()
        for h in range(2):
            hP = slice(h * 64, h * 64 + 64)
            op_mm(k, pu[hP, 0:64], M1Tn[0][hP, cs_], Hbf[hP, :], True, False, [M1Tn[1], Hbf_b], [pu_b])
            op_mm(k, pu[hP, 0:64], k.identB[hP, hP], Wn[0][hP, c, :], False, True, [k.identB_b, Wn[1]], [pu_b])
        P.op('dve', lambda e, pu=pu: e.tensor_copy(out=Usb[0][:, :], in_=pu[:, 0:64]), [pu_b], [Usb[1]])
        for h in range(2):
            hP = slice(h * 64, h * 64 + 64)
            op_mm(k, py[hP, cs_], Hbf[hP, :], RT[0][hP, cs_], True, False, [Hbf_b, RT[1]], [py_b])
            op_mm(k, py[hP, cs_], Usb[0][hP, :], BrT[0][hP, cs_], False, False, [Usb[1], BrT[1]], [py_b])
            op_mm(k, py[hP, cs_], Vtok[0][hP, c, :], KrT[0][hP, cs_], False, True, [Vtok[1], KrT[1]], [py_b])
        ph, ph_b = ps6()
        for h in range(2):
            hP = slice(h * 64, h * 64 + 64)
            op_mm(k, ph[hP, 0:64], Btok[0][hP, c, :], Usb[0][hP, :], True, False, [Btok[1], Usb[1]], [ph_b])
            op_mm(k, ph[hP, 0:64], Ktok[0][hP, c, :], Vtok[0][hP, c, :], False, True, [Ktok[1], Vtok[1]], [ph_b])
        pc_ap = PP[0][:, c * 64 + 63:c * 64 + 64]
        op_stt(k, 'dve', Hbf[:, :], ph[:, 0:64], pc_ap, TH[0][:, :], ALU.mult, ALU.add, [ph_b, PP[1], TH[1]], [Hbf_b])
        op_stt(k, 'dve', H[:, :], ph[:, 0:64], pc_ap, TH[0][:, :], ALU.mult, ALU.add, [ph_b, PP[1], TH[1]], [H_b])
        if c < 7:
            op_ts(k, 'dve', TH[0][:, :], H[:, :], PP[0][:, (c + 1) * 64 + 63:(c + 1) * 64 + 64], None, ALU.mult, None, [H_b, PP[1]], [TH[1]])
    Ysb = rw.f[5]
    op_act(k, Ysb[0][:, :], py[:, :], AF.Copy, [py_b], [Ysb[1]])
    return Ysb[0], Ysb[1]


def rwkv_sample(k, hp, SG, KP, KK, Bt, R, V):
    P, rw, dr = k.P, k.rw, k.dr
    F = rw.f
    Wd = F[2]
    op_act(k, Wd[0][:, 0:NS], SG[0][:, 0:NS], AF.Exp, [SG[1]], [Wd[1]], scale=-DECAY_C)
    ysm, ysm_b, _ = rw.small
    Sst_t, Sn_t, T1_t, RX_t = F[5], F[9], F[11], rw.S
    sa_t = F[12]
    for half in range(2):
        s0 = half * 8
        ss_ = slice(s0, s0 + 8)
        v3 = lambda t: t[0][:, :].rearrange("p (s j) -> p s j", j=64)
        op_dma(k, 'sp', v3(Sst_t), dr["state_wkv"][s0:s0 + 8, 2 * hp:2 * hp + 2, :, :].rearrange("s h i j -> (h i) s j"),
               [k.dbuf["state_wkv"]], [Sst_t[1]])

        def bcast(X):
            op_tt(k, 'dve', v3(RX_t), rw.irep[0][:, :].unsqueeze(1).to_broadcast([128, 8, 64]),
                  X[0][:, ss_].unsqueeze(2).to_broadcast([128, 8, 64]), ALU.mult, [rw.irep[1], X[1]], [RX_t[1]])
            pb, pb_b = k.psn()
            op_mm(k, pb[:, :], rw.boF[0][:], RX_t[0][:, :], True, True, [rw.boF[1], RX_t[1]], [pb_b])
            return pb[:, :].rearrange("p (s j) -> p s j", j=64), pb_b

        kkb, kkb_b = bcast(KK)
        op_tt(k, 'dve', v3(T1_t), v3(Sst_t), kkb, ALU.mult, [Sst_t[1], kkb_b], [T1_t[1]])
        P.op('dve', lambda e, ss_=ss_: e.tensor_reduce(out=sa_t[0][:, ss_], in_=v3(T1_t), axis=AX.X, op=ALU.add, negate=True),
             [T1_t[1]], [sa_t[1]])
        wb_, wb_b = bcast(Wd)
        op_tt(k, 'dve', v3(Sn_t), v3(Sst_t), wb_, ALU.mult, [Sst_t[1], wb_b], [Sn_t[1]])
        bb_, bb_b = bcast(Bt)
        op_tt(k, 'dve', v3(T1_t), bb_, sa_t[0][:, ss_].unsqueeze(2).to_broadcast([128, 8, 64]), ALU.mult, [bb_b, sa_t[1]], [T1_t[1]])
        op_tt(k, 'dve', v3(Sn_t), v3(Sn_t), v3(T1_t), ALU.add, [Sn_t[1], T1_t[1]], [Sn_t[1]])
        kb_, kb_b = bcast(KP)
        op_tt(k, 'dve', v3(T1_t), kb_, V[0][:, ss_].unsqueeze(2).to_broadcast([128, 8, 64]), ALU.mult, [kb_b, V[1]], [T1_t[1]])
        op_tt(k, 'dve', v3(Sn_t), v3(Sn_t), v3(T1_t), ALU.add, [Sn_t[1], T1_t[1]], [Sn_t[1]])
        rb_, rb_b = bcast(R)
        op_tt(k, 'dve', v3(T1_t), v3(Sn_t), rb_, ALU.mult, [Sn_t[1], rb_b], [T1_t[1]])
        P.op('dve', lambda e, ss_=ss_: e.tensor_reduce(out=ysm[:, ss_], in_=v3(T1_t), axis=AX.X, op=ALU.add), [T1_t[1]], [ysm_b])
        op_dma(k, 'sp', dr["wkv_s"][s0:s0 + 8, 2 * hp:2 * hp + 2, :, :].rearrange("s h i j -> (h i) s j"), v3(Sn_t),
               [Sn_t[1]], [k.dbuf["wkv_s"]])
    return ysm, ysm_b

MLA_SCALE_ = 192.0 ** -0.5
PI_ = 3.141592653589793
NPOOL = 10240


def mla_setup(k):
    P, dr, nc, sb = k.P, k.dr, k.nc, k.sb
    ml = K()
    k.ml = ml
    ml.cst = sb("ml_cst", [128, 4], F32)
    ml.Qs = sb("ml_Qs", [128, 2, NS, 16], BF16)
    ml.QRs = sb("ml_QRs", [64, NS, 16], BF16)
    ml.OLs = sb("ml_OLs", [128, 2, 16, NS], BF16)
    ml.ZSs = sb("ml_ZSs", [128, 16, NS], BF16)
    ml.KsT = sb("ml_KsT", [128, 3, NS], BF16)
    ml.ckvN = sb("ml_ckvN", [NS, 258], BF16)
    ml.wuv = sb("ml_wuv", [128, 2, E], BF16)
    ml.mskc = sb("ml_mskc", [NS, NS], F32)
    ml.ones16 = sb("ml_ones16", [NS, 128], F32)
    ml.maskB = sb("ml_maskB", [128, 128], BF16)
    ml.qg = sb("ml_qg", [128, 3], F32)
    ml.big_base = k.off[0]
    ml.cqn = sb("ml_cqn", [128, 3, TWA], BF16)
    ml.KTc = sb("ml_KTc", [128, 2, TWA], BF16)
    ml.KTr = sb("ml_KTr", [64, TWA], BF16)
    ml.ctok = sb("ml_ctok", [128, 17, 256], BF16)
    ml.cos2 = sb("ml_cos2", [64, TWA], BF16)
    ml.sin2 = sb("ml_sin2", [64, TWA], BF16)
    ml.t = [sb(f"ml_t{i}", [128, 576], F32) for i in range(3)]
    ml.sqb = sb("ml_sqb", [128, 512], BF16)
    ml.rA = sb("ml_rA", [128, 64], F32)
    ml.rB = sb("ml_rB", [128, 64], F32)
    ml.st = [sb(f"ml_st{i}", [128, 16], F32) for i in range(4)]
    ml.st_i = [0]
    early = k.off[0]
    ml.wkv = sb("ml_wkv", [128, 8, 320], BF16)
    ml.wq = sb("ml_wq", [128, 8, 384], BF16)
    ml.kvg = sb("ml_kvg", [128, 256], F32)
    ml.costk = sb("ml_costk", [128, 17, 32], F32)
    ml.sintk = sb("ml_sintk", [128, 17, 32], F32)
    ml.CK = sb("ml_CK", [128, 256], F32)
    ml.KRf = sb("ml_KRf", [128, 64], F32)
    ml.KRb = sb("ml_KRb", [128, 64], BF16)
    end_early = k.off[0]
    k.off[0] = early
    ml.QL = sb("ml_QL", [128, 2, TWA], BF16)
    ml.QR = sb("ml_QR", [64, TWA], BF16)
    ml.Pbf = sb("ml_Pbf", [128, T], BF16)
    ml.PT = sb("ml_PT", [128, 16, 128], BF16)
    ml.OLT = sb("ml_OLT", [128, 2, 512], BF16)
    ml.ZS = sb("ml_ZS", [128, 4, 512], BF16)
    ml.QN = sb("ml_QN", [128, 512], BF16)
    ml.wuq = sb("ml_wuq", [128, 3, 192], BF16)
    ml.wsw = sb("ml_wsw", [128, 3, 64], BF16)
    ml.wukr = sb("ml_wukr", [128, 2, 128], F32)
    ml.wukT = sb("ml_wukT", [128, 256], BF16)
    ml.wz = sb("ml_wz", [128, 8, 128], BF16)
    ml.Dg = sb("ml_Dg", [128, 128], BF16)
    k.off[0] = max(k.off[0], end_early)
    ml.end_a = k.off[0]
    k.off[0] = ml.big_base
    ml.KPs = [sb(f"ml_KP{i}", [128, 64, 322], BF16) for i in range(2)]
    ml.ST = sb("ml_ST", [128, 65, 16], F32)
    ml.PTs = sb("ml_PTs", [128, 65, 16], BF16)
    ml.KTp = sb("ml_KTp", [128, 2, 384], BF16)
    ml.pti = sb("ml_pti", [128, NS * 64], I32)
    ml.sm = [sb(f"ml_sm{i}", [128, 32], F32) for i in range(4)]
    ml.olat = sb("ml_olat", [NS, 256], BF16)
    k.off[0] = max(k.off[0], ml.end_a)


def mla_trig(k, out_ap, out_b, ang_ap, shift, np_, w):
    ml = k.ml
    P = k.P
    t0, t0_b, _ = ml.t[0]
    t1, t1_b, _ = ml.t[1]
    a0 = t0[0:np_, 0:w]
    a1 = t1[0:np_, 0:w]
    a1i = t1[:].bitcast(I32)[0:np_, 0:w]
    TWO_PI = 2 * PI_
    rd = [ml.t[2][1]]
    op_ts(k, 'dve', a0, ang_ap, 1.0 / TWO_PI, shift / TWO_PI, ALU.mult, ALU.add, rd, [t0_b])
    P.op('dve', lambda e: e.tensor_copy(out=a1i, in_=a0), [t0_b], [t1_b])
    P.op('dve', lambda e: e.tensor_copy(out=a0, in_=a1i), [t1_b], [t0_b])
    op_stt(k, 'dve', a0, a0, -TWO_PI, ang_ap, ALU.mult, ALU.add, [t0_b] + rd, [t0_b])
    op_ts(k, 'dve', a1, a0, shift, 0.0, ALU.add, ALU.is_lt, [t0_b], [t1_b])
    op_stt(k, 'dve', a0, a1, TWO_PI, a0, ALU.mult, ALU.add, [t0_b, t1_b], [t0_b])
    col = 1 if abs(shift - PI_) < 1e-9 else 2
    return op_act(k, out_ap, a0, AF.Sin, [t0_b, ml.cst[1]], [out_b], bias=ml.cst[0][0:np_, col:col + 1])


def layer_mla(k, ps_):
    P, dr, nc = k.P, k.dr, k.nc
    if not hasattr(k, "ml"):
        mla_setup(k)
    ml = k.ml
    TW = TWA if ps_ == 0 else T
    tiles = ntiles(ps_)
    c_w_in, c_w_in_b = dr["c_w_in"], k.dbuf["c_w_in"]

    def nst():
        s_ = ml.st[ml.st_i[0] % 4]
        ml.st_i[0] += 1
        return s_

    def ps5():
        i = k.ps_i[0] % 5
        k.ps_i[0] += 1
        return k.ps[i]

    cst, cst_b, _ = ml.cst
    P.op('dve', lambda e: e.memset(cst[:, 0:1], -PI_), writes=[cst_b])
    P.op('dve', lambda e: e.memset(cst[:, 1:2], 0.0), writes=[cst_b])
    P.op('dve', lambda e: e.memset(cst[:, 2:3], 0.5 * PI_), writes=[cst_b])
    load_w_cols(k, ml.wkv[0], ml.wkv[1], c_w_in, c_w_in_b, 384, ncols=320)
    load_w_cols(k, ml.wq[0], ml.wq[1], c_w_in, c_w_in_b, 0, ncols=384)
    op_dma(k, 'pool', ml.wuv[0][:, :, :], dr["c_w_uv"].rearrange("(c p) h v -> p c (h v)", p=128), [k.dbuf["c_w_uv"]], [ml.wuv[1]])
    op_dma(k, 'sp', ml.kvg[0][:], dr["c_kv_norm"].rearrange("(o c) -> o c", o=1).partition_broadcast(128), [k.dbuf["c_kv_norm"]], [ml.kvg[1]])
    op_dma(k, 'sp', ml.qg[0][:], dr["c_q_norm"].rearrange("(c p) -> p c", p=128), [k.dbuf["c_q_norm"]], [ml.qg[1]], slow=True)
    tf, tf_b, _ = ml.t[0]
    P.op('pool', lambda e: e.memset(tf[:, 0:128], 0.0), writes=[tf_b])
    P.op('pool', lambda e: e.affine_select(out=tf[:, 0:128], in_=tf[:, 0:128], pattern=[[-1, 128]], compare_op=ALU.is_ge, fill=-1e9,
                                           base=0, channel_multiplier=1), reads=[tf_b], writes=[tf_b])
    P.op('dve', lambda e: e.tensor_copy(out=ml.maskB[0][:], in_=tf[:, 0:128]), reads=[tf_b], writes=[ml.maskB[1]])
    P.op('pool', lambda e: e.memset(ml.mskc[0][:], 0.0), writes=[ml.mskc[1]])
    P.op('pool', lambda e: e.affine_select(out=ml.mskc[0][:], in_=ml.mskc[0][:], pattern=[[-1, NS]], compare_op=ALU.is_equal, fill=-1e9,
                                           base=0, channel_multiplier=1), reads=[ml.mskc[1]], writes=[ml.mskc[1]])
    P.op('dve', lambda e: e.memset(ml.ones16[0][:], 1.0), writes=[ml.ones16[1]])
    ti_, ti_b, _ = ml.t[1]
    tii = ti_[:].bitcast(I32)
    P.op('pool', lambda e: e.iota(tii[:, 0:32], pattern=[[1, 32]], base=0, channel_multiplier=0), writes=[ti_b])
    invf, invf_b, _ = ml.rA
    P.op('dve', lambda e: e.tensor_copy(out=invf[:, 0:32], in_=tii[:, 0:32]), reads=[ti_b], writes=[invf_b])
    op_act(k, invf[:, 0:32], invf[:, 0:32], AF.Exp, [invf_b], [invf_b], scale=-math.log(10000.0) / 32.0)
    P.op('pool', lambda e: e.iota(tii[:, 64:80], pattern=[[128, 16]], base=0, channel_multiplier=1), writes=[ti_b])
    posf, posf_b, _ = ml.rB
    P.op('dve', lambda e: e.tensor_copy(out=posf[:, 0:16], in_=tii[:, 64:80]), reads=[ti_b], writes=[posf_b])
    P.op('dve', lambda e: e.memset(posf[:, 16:17], 8192.0), writes=[posf_b])
    ang, ang_b, _ = ml.t[2]
    angv = ang[:, 0:17 * 32].rearrange("p (a i) -> p a i", i=32)
    op_tt(k, 'dve', angv, posf[:, 0:17].unsqueeze(2).to_broadcast([128, 17, 32]), invf[:, 0:32].unsqueeze(1).to_broadcast([128, 17, 32]),
          ALU.mult, [posf_b, invf_b], [ang_b])
    tmp, tmp_b, _ = ml.t[0]
    mla_trig(k, ml.sintk[0][:, :, :].rearrange("p a i -> p (a i)"), ml.sintk[1], ang[:, 0:544], PI_, 128, 544)
    mla_trig(k, ml.costk[0][:, :, :].rearrange("p a i -> p (a i)"), ml.costk[1], ang[:, 0:544], 1.5 * PI_, 128, 544)
    P.op('pool', lambda e: e.iota(tii[0:32, 0:1], pattern=[[1, 1]], base=0, channel_multiplier=1), writes=[ti_b])
    P.op('pool', lambda e: e.iota(tii[32:64, 0:1], pattern=[[1, 1]], base=0, channel_multiplier=1), writes=[ti_b])
    P.op('dve', lambda e: e.tensor_copy(out=invf[0:64, 32:33], in_=tii[0:64, 0:1]), reads=[ti_b], writes=[invf_b])
    op_act(k, invf[0:64, 33:34], invf[0:64, 32:33], AF.Exp, [invf_b], [invf_b], scale=-math.log(10000.0) / 32.0)
    P.op('dve', lambda e: e.memset(invf[0:32, 34:35], -1.0), writes=[invf_b])
    P.op('dve', lambda e: e.memset(invf[32:64, 34:35], 1.0), writes=[invf_b])
    for ti, (c0, w) in enumerate(tiles):
        if c0 < T:
            P.op('pool', lambda e, c0=c0: e.iota(tii[0:64, 0:512], pattern=[[1, 512]], base=c0, channel_multiplier=0), writes=[ti_b])
            P.op('dve', lambda e: e.tensor_copy(out=ang[0:64, 0:512], in_=tii[0:64, 0:512]), reads=[ti_b], writes=[ang_b])
        else:
            P.op('dve', lambda e: e.memset(ang[0:64, 0:NS], 8192.0), writes=[ang_b])
        op_ts(k, 'dve', ang[0:64, 0:w], ang[0:64, 0:w], invf[0:64, 33:34], None, ALU.mult, None, [ang_b, invf_b], [ang_b])
        mla_trig(k, ml.cos2[0][:, c0:c0 + w], ml.cos2[1], ang[0:64, 0:w], 1.5 * PI_, 64, w)
        mla_trig(k, ti_[0:64, 0:w], ti_b, ang[0:64, 0:w], PI_, 64, w)
        op_ts(k, 'dve', ml.sin2[0][:, c0:c0 + w], ti_[0:64, 0:w], invf[0:64, 34:35], None, ALU.mult, None, [ti_b, invf_b], [ml.sin2[1]])
    P.chk("m_tab")
    ttiles = [(tt * 128, 128, tt) for tt in range(16)]
    if ps_ == 0:
        ttiles.append((T, NS, 16))
    ctok, ctok_b, _ = ml.ctok
    for (c0, np_, tt) in ttiles:
        pkv, pkv_b = k.psn()
        for kk in range(8):
            op_mm(k, pkv[0:np_, 0:320], k.hT[:, kk, c0 + 1:c0 + 1 + np_], ml.wkv[0][:, kk, :], kk == 0, kk == 7,
                  [ml.wkv[1], k.hT_b.sub(c0 // 128)], [pkv_b])
        st, st_b, _ = nst()
        P.op('act', lambda e, np_=np_, pkv=pkv, st=st: e.activation(out=ml.sqb[0][0:np_, 0:256], in_=pkv[0:np_, 0:256],
                                                                    func=AF.Square, accum_out=st[0:np_, 0:1]),
             [pkv_b], [ml.sqb[1], st_b])
        rstd_from_ss(k, st[0:np_, 0:1], st_b, st[0:np_, 1:2], st_b, np_, 1.0 / 256, 0)
        CK, CK_b, _ = ml.CK
        op_stt(k, 'dve', CK[0:np_, :], pkv[0:np_, 0:256], st[0:np_, 1:2], ml.kvg[0][0:np_, :], ALU.mult, ALU.mult,
               [pkv_b, st_b, ml.kvg[1]], [CK_b])
        if c0 < T:
            op_dma(k, 'sp', dr["ckv_p"][ps_, c0:c0 + 128, :], CK[:, :], [CK_b], [k.dbuf["ckv_p"]])
        else:
            op_dma(k, 'sp', dr["ckv_s"][:, :], CK[0:NS, :], [CK_b], [k.dbuf["ckv_s"]])
            op_act(k, ml.ckvN[0][:, 0:256], CK[0:NS, :], AF.Copy, [CK_b], [ml.ckvN[1]])
            P.op('dve', lambda e: e.memset(ml.ckvN[0][:, 256:258], 1.0), writes=[ml.ckvN[1]])
        op_act(k, ctok[0:np_, tt, :], CK[0:np_, :], AF.Copy, [CK_b], [ctok_b.sub(tt)])
        rA, rA_b, _ = ml.rA
        rB, rB_b, _ = ml.rB
        KRf, KRf_b, _ = ml.KRf
        kr3 = pkv[0:np_, 256:320].rearrange("p (a i) -> p a i", i=32)
        op_tt(k, 'dve', rA[0:np_, 0:64].rearrange("p (a i) -> p a i", i=32), kr3,
              ml.costk[0][0:np_, tt:tt + 1, :].to_broadcast([np_, 2, 32]), ALU.mult, [pkv_b, ml.costk[1]], [rA_b])
        op_tt(k, 'dve', rB[0:np_, 0:64].rearrange("p (a i) -> p a i", i=32), kr3,
              ml.sintk[0][0:np_, tt:tt + 1, :].to_broadcast([np_, 2, 32]), ALU.mult, [pkv_b, ml.sintk[1]], [rB_b])
        op_tt(k, 'dve', KRf[0:np_, 0:32], rA[0:np_, 0:32], rB[0:np_, 32:64], ALU.subtract, [rA_b, rB_b], [KRf_b])
        op_tt(k, 'dve', KRf[0:np_, 32:64], rB[0:np_, 0:32], rA[0:np_, 32:64], ALU.add, [rA_b, rB_b], [KRf_b])
        if c0 < T:
            op_dma(k, 'sp', dr["kr_p"][ps_, c0:c0 + 128, :], KRf[:, :], [KRf_b], [k.dbuf["kr_p"]])
        else:
            op_dma(k, 'sp', dr["kr_s"][:, :], KRf[0:NS, :], [KRf_b], [k.dbuf["kr_s"]])
        KRb, KRb_b, _ = ml.KRb
        op_act(k, KRb[0:np_, :], KRf[0:np_, :], AF.Copy, [KRf_b], [KRb_b])
        psT, psT_b = k.psT
        for j in range(2):
            op_tr(k, psT[:, j * 128:j * 128 + np_], ctok[0:np_, tt, j * 128:(j + 1) * 128], k.identB[0:np_, 0:np_],
                  [ctok_b.sub(tt), k.identB_b], [psT_b])
        op_tr(k, psT[0:64, 256:256 + np_], KRb[0:np_, :], k.identB[0:np_, 0:np_], [KRb_b, k.identB_b], [psT_b])
        op_act(k, ml.KTc[0][:, :, c0:c0 + np_], psT[:, 0:256].rearrange("p (j t) -> p j t", t=128)[:, :, 0:np_], AF.Copy,
               [psT_b], [ml.KTc[1].sub(tt)])
        P.op('dve', lambda e, c0=c0, np_=np_: e.tensor_copy(out=ml.KTr[0][:, c0:c0 + np_], in_=psT[0:64, 256:256 + np_]),
             [psT_b], [ml.KTr[1].sub(tt)])
    if ps_ == 0:
        op_act(k, ml.KsT[0][:, 0:2, :], ml.KTc[0][:, :, T:T + NS], AF.Copy, [ml.KTc[1].sub(16)], [ml.KsT[1]])
        op_act(k, ml.KsT[0][0:64, 2, :], ml.KTr[0][:, T:T + NS], AF.Copy, [ml.KTr[1].sub(16)], [ml.KsT[1]])
    P.chk("m_1")
    for ti, (c0, w) in enumerate(tiles):
        pqs = []
        for qc in range(3):
            pq, pq_b = k.psn()
            for kk in range(8):
                op_mm(k, pq[:, 0:w], ml.wq[0][:, kk, qc * 128:(qc + 1) * 128], k.hT[:, kk, c0 + 1:c0 + 1 + w], kk == 0, kk == 7,
                      [ml.wq[1]] + hT_reads(k, c0, w), [pq_b])
            pqs.append((pq, pq_b))
        pss, pss_b = k.psn()
        for qc in range(3):
            sqb, sqb_b, _ = ml.sqb
            op_act(k, sqb[:, 0:w], pqs[qc][0][:, 0:w], AF.Square, [pqs[qc][1]], [sqb_b])
            op_mm(k, pss[:, 0:w], k.onesB[:], sqb[:, 0:w], qc == 0, qc == 2, [k.onesB_b, sqb_b], [pss_b])
        rst, rst_b, _ = ml.t[0]
        op_act(k, rst[:, 0:w], pss[:, 0:w], AF.Sqrt, [pss_b, k.epsr_b], [rst_b], scale=1.0 / 384, bias=k.epsr[:, 0:1])
        P.op('dve', lambda e, w=w: e.reciprocal(out=rst[:, 0:w], in_=rst[:, 0:w]), [rst_b], [rst_b])
        for qc in range(3):
            op_stt(k, 'dve', ml.cqn[0][:, qc, c0:c0 + w], pqs[qc][0][:, 0:w], ml.qg[0][:, qc:qc + 1], rst[:, 0:w], ALU.mult, ALU.mult,
                   [pqs[qc][1], ml.qg[1], rst_b], [ml.cqn[1].sub(ti)])
    P.chk("m_2")
    P.barrier()
    QL, QL_b, _ = ml.QL
    QR, QR_b, _ = ml.QR
    for h in range(16):
        wuq, wuq_b, _ = ml.wuq
        op_dma(k, 'pool', wuq[:, :, :], dr["c_w_uq"][:, h, :].rearrange("(c p) e -> p c e", p=128), [k.dbuf["c_w_uq"]], [wuq_b])
        wsw, wsw_b, _ = ml.wsw
        P.op('dve', lambda e: e.tensor_copy(out=wsw[:, :, 0:32], in_=wuq[:, :, 160:192]), [wuq_b], [wsw_b])
        P.op('dve', lambda e: e.tensor_copy(out=wsw[:, :, 32:64], in_=wuq[:, :, 128:160]), [wuq_b], [wsw_b])
        wukr, wukr_b, _ = ml.wukr
        op_dma(k, 'sp', wukr[:, :, :], dr["c_w_uk"][:, h, :].rearrange("(c p) n -> p c n", p=128), [k.dbuf["c_w_uk"]], [wukr_b])
        pw_, pw_b = k.psn()
        for cc in range(2):
            op_tr(k, pw_[:, cc * 128:(cc + 1) * 128], wukr[:, cc, :], k.identF[:], [wukr_b, k.identF_b], [pw_b])
        op_act(k, ml.wukT[0][:, :], pw_[:, 0:256], AF.Copy, [pw_b], [ml.wukT[1]])
        load_w_cols(k, ml.wz[0], ml.wz[1], c_w_in, c_w_in_b, 704 + h * 128)
        for ti, (c0, w) in enumerate(tiles):
            rq = [ml.cqn[1].sub(ti)]
            pqn, pqn_b = k.psn()
            for qc in range(3):
                op_mm(k, pqn[:, 0:w], wuq[:, qc, 0:128], ml.cqn[0][:, qc, c0:c0 + w], qc == 0, qc == 2, [wuq_b] + rq, [pqn_b])
            pra, pra_b = k.psn()
            for qc in range(3):
                op_mm(k, pra[0:64, 0:w], wuq[:, qc, 128:192], ml.cqn[0][:, qc, c0:c0 + w], qc == 0, qc == 2, [wuq_b] + rq, [pra_b])
            prb, prb_b = k.psn()
            for qc in range(3):
                op_mm(k, prb[0:64, 0:w], wsw[:, qc, :], ml.cqn[0][:, qc, c0:c0 + w], qc == 0, qc == 2, [wsw_b] + rq, [prb_b])
            QN, QN_b, _ = ml.QN
            op_act(k, QN[:, 0:w], pqn[:, 0:w], AF.Copy, [pqn_b], [QN_b])
            t1, t1_b, _ = ml.t[1]
            t2, t2_b, _ = ml.t[2]
            op_tt(k, 'dve', t1[0:64, 0:w], pra[0:64, 0:w], ml.cos2[0][:, c0:c0 + w], ALU.mult, [pra_b, ml.cos2[1]], [t1_b])
            op_tt(k, 'dve', t2[0:64, 0:w], prb[0:64, 0:w], ml.sin2[0][:, c0:c0 + w], ALU.mult, [prb_b, ml.sin2[1]], [t2_b])
            op_tt(k, 'dve', QR[:, c0:c0 + w], t1[0:64, 0:w], t2[0:64, 0:w], ALU.add, [t1_b, t2_b], [QR_b.sub(ti)])
            for cc in range(2):
                pql, pql_b = k.psn()
                op_mm(k, pql[:, 0:w], ml.wukT[0][:, cc * 128:(cc + 1) * 128], QN[:, 0:w], True, True, [ml.wukT[1], QN_b], [pql_b])
                op_act(k, QL[:, cc, c0:c0 + w], pql[:, 0:w], AF.Copy, [pql_b], [QL_b.sub(ti)])
            pz, pz_b = k.psn()
            for kk in range(8):
                op_mm(k, pz[:, 0:w], ml.wz[0][:, kk, :], k.hT[:, kk, c0 + 1:c0 + 1 + w], kk == 0, kk == 7,
                      [ml.wz[1]] + hT_reads(k, c0, w), [pz_b])
            if c0 < T:
                op_act(k, ml.ZS[0][:, ti, :], pz[:, :], AF.Silu, [pz_b], [ml.ZS[1].sub(ti)])
            else:
                op_act(k, ml.ZSs[0][:, h, :], pz[:, 0:NS], AF.Silu, [pz_b], [ml.ZSs[1]])
                P.op('dve', lambda e, h=h: e.tensor_copy(out=ml.Qs[0][:, :, :, h], in_=QL[:, :, T:T + NS]), [QL_b.sub(ti)], [ml.Qs[1]])
                P.op('dve', lambda e, h=h: e.tensor_copy(out=ml.QRs[0][:, :, h], in_=QR[:, T:T + NS]), [QR_b.sub(ti)], [ml.QRs[1]])
        P.chk("m_q")
        Pbf, Pbf_b, _ = ml.Pbf
        PT, PT_b, _ = ml.PT
        pov = [k.ps[5], k.ps[6]]
        for qb in range(16):
            t0 = qb * 128
            L = t0 + 128
            nb = (L + 511) // 512
            tq = qb // 4
            banks = [ps5() for _ in range(nb)]
            kt_r = [ml.KTc[1].sub(x) for x in range(qb + 1)] + [ml.KTr[1].sub(x) for x in range(qb + 1)]
            for b in range(nb):
                l0 = b * 512
                lw = min(512, L - l0)
                bk, bk_b = banks[b]
                last = (b == nb - 1)
                op_mm(k, bk[:, 0:lw], QL[:, 0, t0:t0 + 128], ml.KTc[0][:, 0, l0:l0 + lw], True, False, [QL_b.sub(tq)] + kt_r, [bk_b])
                op_mm(k, bk[:, 0:lw], QL[:, 1, t0:t0 + 128], ml.KTc[0][:, 1, l0:l0 + lw], False, False, [QL_b.sub(tq)] + kt_r, [bk_b])
                op_mm(k, bk[:, 0:lw], QR[:, t0:t0 + 128], ml.KTr[0][:, l0:l0 + lw], False, not last, [QR_b.sub(tq)] + kt_r, [bk_b])
                if last:
                    dcol = t0 - l0
                    op_mm(k, bk[:, dcol:dcol + 128], k.identB[:], ml.maskB[0][:], False, True, [k.identB_b, ml.maskB[1]], [bk_b])
            st, st_b, _ = nst()
            for b in range(nb):
                lw = min(512, L - b * 512)
                bk, bk_b = banks[b]
                P.op('dve', lambda e, b=b, lw=lw, bk=bk, st=st: e.tensor_reduce(out=st[:, b:b + 1], in_=bk[:, 0:lw], axis=AX.X, op=ALU.max),
                     [bk_b], [st_b])
            if nb > 1:
                P.op('dve', lambda e, nb=nb, st=st: e.tensor_reduce(out=st[:, 4:5], in_=st[:, 0:nb], axis=AX.X, op=ALU.max), [st_b], [st_b])
                mcol = st[:, 4:5]
            else:
                mcol = st[:, 0:1]
            op_ts(k, 'dve', st[:, 5:6], mcol, -MLA_SCALE_, None, ALU.mult, None, [st_b], [st_b])
            for b in range(nb):
                l0 = b * 512
                lw = min(512, L - l0)
                bk, bk_b = banks[b]
                P.op('act', lambda e, b=b, l0=l0, lw=lw, bk=bk, st=st: e.activation(out=Pbf[:, l0:l0 + lw], in_=bk[:, 0:lw], func=AF.Exp,
                                                                                   scale=MLA_SCALE_, bias=st[:, 5:6],
                                                                                   accum_out=st[:, 8 + b:9 + b]),
                     [bk_b, st_b], [Pbf_b.sub(b), st_b])
            if nb > 1:
                P.op('dve', lambda e, nb=nb, st=st: e.tensor_reduce(out=st[:, 6:7], in_=st[:, 8:8 + nb], axis=AX.X, op=ALU.add), [st_b], [st_b])
                scol = st[:, 6:7]
            else:
                scol = st[:, 8:9]
            P.op('dve', lambda e, st=st, scol=scol: e.reciprocal(out=st[:, 7:8], in_=scol), [st_b], [st_b])
            Dg, Dg_b, _ = ml.Dg
            op_ts(k, 'dve', Dg[:, :], k.identF[:, :], st[:, 7:8], None, ALU.mult, None, [k.identF_b, st_b], [Dg_b])
            for g0 in range(0, qb + 1, 4):
                g1 = min(qb + 1, g0 + 4)
                pt, pt_b = ps5()
                for kb in range(g0, g1):
                    op_mm(k, pt[:, (kb - g0) * 128:(kb - g0 + 1) * 128], Pbf[:, kb * 128:(kb + 1) * 128], Dg[:, :], True, True,
                          [Pbf_b.sub(kb // 4), Dg_b], [pt_b])
                n_ = g1 - g0
                if (g0 // 4) % 2 == 0:
                    op_act(k, PT[:, g0:g1, :], pt[:, 0:n_ * 128].rearrange("p (g t) -> p g t", t=128), AF.Copy, [pt_b], [PT_b.sub(g0 // 4)])
                else:
                    P.op('dve', lambda e, g0=g0, g1=g1, n_=n_, pt=pt: e.tensor_copy(
                        out=PT[:, g0:g1, :], in_=pt[:, 0:n_ * 128].rearrange("p (g t) -> p g t", t=128)), [pt_b], [PT_b.sub(g0 // 4)])
            qc_ = slice((qb % 4) * 128, (qb % 4 + 1) * 128)
            for cc in range(2):
                pv_, pv_b = pov[cc]
                for kb in range(qb + 1):
                    op_mm(k, pv_[:, qc_], ctok[:, kb, cc * 128:(cc + 1) * 128], PT[:, kb, :], kb == 0, kb == qb,
                          [ctok_b.sub(kb), PT_b.sub(kb // 4)], [pv_b])
            if qb % 4 == 3:
                OLT, OLT_b, _ = ml.OLT
                op_act(k, OLT[:, 0, :], pov[0][0][:, :], AF.Copy, [pov[0][1]], [OLT_b])
                P.op('dve', lambda e: e.tensor_copy(out=OLT[:, 1, :], in_=pov[1][0][:, :]), [pov[1][1]], [OLT_b])
                po, po_b = ps5()
                for cc in range(2):
                    op_mm(k, po[:, :], ml.wuv[0][:, cc, h * 128:(h + 1) * 128], OLT[:, cc, :], cc == 0, cc == 1, [ml.wuv[1], OLT_b], [po_b])
                op_tt(k, 'dve', k.GT[:, h, tq * 512:(tq + 1) * 512], po[:, :], ml.ZS[0][:, tq, :], ALU.mult, [po_b, ml.ZS[1].sub(tq)],
                      [k.GT_b.sub(h).sub(tq)])
            P.chk("m_att")
    if ps_ == 0:
        P.barrier()
        mla_sample(k)


def mla_sample(k):
    P, dr, nc, ml = k.P, k.dr, k.nc, k.ml
    ST, ST_b, _ = ml.ST
    PTs, PTs_b, _ = ml.PTs
    pti, pti_b, _ = ml.pti
    ptf, ptf_b = pti[:].bitcast(F32), pti_b
    idx, idx_b = pti, pti_b
    op_dma(k, 'sp', pti[:, :], dr["page_table"].rearrange("s j -> (s j)").rearrange("(o n) -> o n", o=1).partition_broadcast(128),
           [k.dbuf["page_table"]], [pti_b])
    P.op('dve', lambda e: e.tensor_copy(out=ptf[:, :], in_=pti[:, :]), [pti_b], [ptf_b])
    sm0, sm0_b, _ = ml.sm[0]
    smi = sm0[:].bitcast(I32)
    P.op('pool', lambda e: e.iota(smi[:, 0:1], pattern=[[1, 1]], base=0, channel_multiplier=1), writes=[sm0_b])
    P.op('dve', lambda e: e.tensor_copy(out=sm0[:, 1:2], in_=smi[:, 0:1]), [sm0_b], [sm0_b])
    op_ts(k, 'dve', ptf[:, :], ptf[:, :], 128.0, sm0[:, 1:2], ALU.mult, ALU.add, [ptf_b, sm0_b], [ptf_b])
    P.op('dve', lambda e: e.tensor_copy(out=idx[:, :], in_=ptf[:, :]), [ptf_b], [idx_b])
    for i_ in range(2):
        P.op('dve', lambda e, i_=i_: e.memset(ml.KPs[i_][0][:, :, 320:322], 1.0), writes=[ml.KPs[i_][1]])
    cat_rows = dr["cache_cat"].rearrange("n t c -> (n t) c")
    P.chk("s_idx")
    for s in range(NS):
        KP, KP_b, _ = ml.KPs[s % 2]
        for j in range(64):
            n = s * 64 + j
            P.op('pool', lambda e, j=j, n=n, KP=KP: e.indirect_dma_start(out=KP[:, j, 0:320], out_offset=None, in_=cat_rows,
                                                                        in_offset=bass.IndirectOffsetOnAxis(ap=idx[:, n:n + 1], axis=0)),
                 [idx_b, k.dbuf["cache_cat"]], [KP_b.sub(j)], dma=True)
        P.chk("s_gather")
        psT, psT_b = k.psT
        pS = [k.psn(), k.psn()]
        for j in range(64):
            o_ = (j % 2) * 384
            if True:
                op_tr(k, psT[0:64, o_:o_ + 128], KP[:, j, 0:64], k.identB[:], [KP_b.sub(j), k.identB_b], [psT_b])
                op_tr(k, psT[:, o_ + 128:o_ + 256], KP[:, j, 64:192], k.identB[:], [KP_b.sub(j), k.identB_b], [psT_b])
                op_tr(k, psT[:, o_ + 256:o_ + 384], KP[:, j, 192:320], k.identB[:], [KP_b.sub(j), k.identB_b], [psT_b])
            if j % 2 == 1:
                KTp, KTp_b, _ = ml.KTp
                op_act(k, KTp[:, :, :], psT[:, 0:768].rearrange("p (a c) -> p a c", c=384), AF.Copy, [psT_b], [KTp_b])
                for jj in (j - 1, j):
                    a = jj % 2
                    bk, bk_b = pS[jj // 32]
                    cs_ = slice((jj % 32) * 16, (jj % 32 + 1) * 16)
                    op_mm(k, bk[:, cs_], KTp[0:64, a, 0:128], ml.QRs[0][:, s, :], True, False, [KTp_b, ml.QRs[1]], [bk_b])
                    op_mm(k, bk[:, cs_], KTp[:, a, 128:256], ml.Qs[0][:, 0, s, :], False, False, [KTp_b, ml.Qs[1]], [bk_b])
                    op_mm(k, bk[:, cs_], KTp[:, a, 256:384], ml.Qs[0][:, 1, s, :], False, True, [KTp_b, ml.Qs[1]], [bk_b])
        for g in range(2):
            bk, bk_b = pS[g]
            P.op('dve', lambda e, g=g, bk=bk: e.tensor_copy(out=ST[:, g * 32:(g + 1) * 32, :], in_=bk[:, :].rearrange("p (j h) -> p j h", h=16)),
                 [bk_b], [ST_b])
        pN, pN_b = k.psn()
        op_mm(k, pN[0:NS, 0:16], ml.KsT[0][0:64, 2, :], ml.QRs[0][:, s, :], True, False, [ml.KsT[1], ml.QRs[1]], [pN_b])
        op_mm(k, pN[0:NS, 0:16], ml.KsT[0][:, 0, :], ml.Qs[0][:, 0, s, :], False, False, [ml.KsT[1], ml.Qs[1]], [pN_b])
        op_mm(k, pN[0:NS, 0:16], ml.KsT[0][:, 1, :], ml.Qs[0][:, 1, s, :], False, True, [ml.KsT[1], ml.Qs[1]], [pN_b])
        P.op('dve', lambda e: e.memset(ST[:, 64, :], -1e9), writes=[ST_b])
        op_ts(k, 'dve', ST[0:NS, 64, :], pN[0:NS, 0:16], ml.mskc[0][:, s:s + 1], None, ALU.add, None, [pN_b, ml.mskc[1]], [ST_b])
        sm1, sm1_b, _ = ml.sm[1]
        P.op('dve', lambda e: e.tensor_reduce(out=sm1[:, 0:16], in_=ST[:, :, :].rearrange("p j h -> p h j"), axis=AX.X, op=ALU.max),
             [ST_b], [sm1_b])
        pM, pM_b = k.psn()
        op_tr(k, pM[0:16, 0:128], sm1[:, 0:16], k.identF[:], [sm1_b, k.identF_b], [pM_b])
        sm2, sm2_b, _ = ml.sm[2]
        P.op('dve', lambda e, pM=pM: e.tensor_reduce(out=sm2[0:16, 0:1], in_=pM[0:16, 0:128], axis=AX.X, op=ALU.max), [pM_b], [sm2_b])
        op_ts(k, 'dve', sm2[0:16, 16:32], k.identF[0:16, 0:16], sm2[0:16, 0:1], None, ALU.mult, None, [k.identF_b, sm2_b], [sm2_b])
        pB, pB_b = k.psn()
        op_mm(k, pB[:, 0:16], ml.ones16[0][:, :], sm2[0:16, 16:32], True, True, [ml.ones16[1], sm2_b], [pB_b])
        sm3, sm3_b, _ = ml.sm[3]
        op_act(k, sm3[:, 0:16], pB[:, 0:16], AF.Copy, [pB_b], [sm3_b], scale=-MLA_SCALE_)
        op_stt(k, 'dve', ST[:, :, :], ST[:, :, :], MLA_SCALE_, sm3[:, 0:16].unsqueeze(1).to_broadcast([128, 65, 16]), ALU.mult, ALU.add,
               [ST_b, sm3_b], [ST_b])
        op_act(k, PTs[:, :, :], ST[:, :, :], AF.Exp, [ST_b], [PTs_b])
        pO, pO_b = k.psn()
        for j in range(64):
            op_mm(k, pO[0:16, 0:257], PTs[:, j, :], KP[:, j, 64:321], j == 0, False, [PTs_b, KP_b.sub(j)], [pO_b])
        op_mm(k, pO[0:16, 0:257], PTs[0:NS, 64, :], ml.ckvN[0][:, 0:257], False, True, [PTs_b, ml.ckvN[1]], [pO_b])
        P.op('dve', lambda e, pO=pO: e.reciprocal(out=sm2[0:16, 1:2], in_=pO[0:16, 256:257]), [pO_b, sm2_b], [sm2_b])
        olat, olat_b, _ = ml.olat
        op_ts(k, 'dve', olat[:, :], pO[0:16, 0:256], sm2[0:16, 1:2], None, ALU.mult, None, [pO_b, sm2_b], [olat_b])
        for cc in range(2):
            op_tr(k, psT[:, cc * 16:(cc + 1) * 16], olat[:, cc * 128:(cc + 1) * 128], k.identB[0:16, 0:16], [olat_b, k.identB_b], [psT_b])
        op_act(k, ml.OLs[0][:, :, :, s], psT[:, 0:32].rearrange("p (c h) -> p c h", h=16), AF.Copy, [psT_b], [ml.OLs[1]])
        P.chk("s_one")
    for h in range(16):
        po, po_b = k.psn()
        for cc in range(2):
            op_mm(k, po[:, 0:NS], ml.wuv[0][:, cc, h * 128:(h + 1) * 128], ml.OLs[0][:, cc, h, :], cc == 0, cc == 1,
                  [ml.wuv[1], ml.OLs[1]], [po_b])
        op_tt(k, 'dve', k.GT[:, h, T:T + NS], po[:, 0:NS], ml.ZSs[0][:, h, :], ALU.mult, [po_b, ml.ZSs[1]], [k.GT_b.sub(h).sub(4)])

S5C = 64
S5NCH = T // S5C


def s5_setup(k):
    P, dr, nc, sb = k.P, k.dr, k.nc, k.sb
    s5 = K()
    k.s5 = s5
    s5.cst = sb("s5_cst", [128, 4], F32)
    s5.P64 = {n: sb("s5_p_" + n, [128, 64], F32) for n in
              ["lr", "li", "dt", "m", "th", "are", "aim", "fre", "fim", "t0", "t1", "lnm", "m64", "ph"]}
    s5.LB = [sb(f"s5_LB{i}", [128, 16, 128], BF16) for i in range(2)]
    s5.LC = [sb(f"s5_LC{i}", [128, 16, 128], BF16) for i in range(2)]
    s5.LC3 = [sb(f"s5_LC3{i}", [128, 16, 64], BF16) for i in range(2)]
    s5.Uz = sb("s5_Uz", [128, TWA], BF16)
    s5.mask4 = sb("s5_mask4", [128, 128], F32)
    s5.par = {n: sb("s5_par_" + n, [128, 16], F32) for n in ["d", "bg"]}
    s5.m01 = sb("s5_m01", [128, T], BF16)
    s5.U = sb("s5_U", [128, TWA], BF16)
    s5.wt = sb("s5_wt", [128, 8, 128], BF16)
    s5.tab = {n: sb("s5_tab_" + n, [128, 64], F32) for n in ["Fc", "Fs", "Bc", "Bs", "ang", "mg", "a", "b", "c", "Gc", "Gs"]}
    s5.big = [sb(f"s5_big{i}", [128, T], F32) for i in range(5)]
    s5.sbf = [sb(f"s5_sbf{i}", [128, TWA], BF16) for i in range(2)]
    s5.ec_ = {n: sb("s5_e_" + n, [128, 64], F32) for n in ["er", "ei", "hr", "hi", "Er", "Ei", "cr", "ci"]}
    s5.fin = [sb(f"s5_fin{i}", [128, 64], F32) for i in range(2)]
    s5.s0T = [sb(f"s5_s0T{i}", [128, 64, NS], F32) for i in range(2)]
    s5.tmpn = [sb(f"s5_tmpn{i}", [128, NS], F32) for i in range(2)]
    s5.g = [sb(f"s5_g{i}", [128, 512], F32) for i in range(3)]
    s5.gb = sb("s5_gb", [128, 512], BF16)
    al = lambda name, shape, dt, o: (nc.alloc_sbuf_tensor_at(name, shape, dt, offset=o), Buf(name), o)
    s5.srow = al("s5_srow", [NS, 2048], F32, s5.big[0][2])
    s5.wg = al("s5_wg", [128, 16, 128], BF16, s5.big[1][2])
    s5.XX = [al("s5_XX0", [128, 64, 32], F32, s5.big[3][2]), al("s5_XX1", [128, 64, 32], F32, s5.big[4][2])]
    s5.bre = al("s5_bre", [128, 64, 16], F32, s5.sbf[0][2])
    s5.bim = al("s5_bim", [128, 64, 16], F32, s5.sbf[1][2])
    s5.CN = [al("s5_CN0", [128, 16, 64], F32, s5.big[2][2] + 4096), al("s5_CN1", [128, 16, 64], F32, s5.big[1][2] + 4096)]


def s5_trig(k, out_ap, out_b, ang_ap, ang_b, shift, w):
    P, s5 = k.P, k.s5
    a0, a0_b = s5.tab["a"][0][:, 0:w], s5.tab["a"][1]
    a1, a1_b = s5.tab["b"][0][:, 0:w], s5.tab["b"][1]
    a1i = s5.tab["b"][0][:].bitcast(I32)[:, 0:w]
    TWO_PI = 2 * PI_
    op_ts(k, 'dve', a0, ang_ap, 1.0 / TWO_PI, shift / TWO_PI, ALU.mult, ALU.add, [ang_b], [a0_b])
    P.op('dve', lambda e: e.tensor_copy(out=a1i, in_=a0), [a0_b], [a1_b])
    P.op('dve', lambda e: e.tensor_copy(out=a0, in_=a1i), [a1_b], [a0_b])
    op_stt(k, 'dve', a0, a0, -TWO_PI, ang_ap, ALU.mult, ALU.add, [a0_b, ang_b], [a0_b])
    op_ts(k, 'dve', a1, a0, shift, 0.0, ALU.add, ALU.is_lt, [a0_b], [a1_b])
    op_stt(k, 'dve', a0, a1, TWO_PI, a0, ALU.mult, ALU.add, [a0_b, a1_b], [a0_b])
    col = 1 if abs(shift - PI_) < 1e-9 else 2
    op_act(k, out_ap, a0, AF.Sin, [a0_b, s5.cst[1]], [out_b], bias=s5.cst[0][:, col:col + 1])


def s5_load(k):
    P, dr, nc, s5 = k.P, k.dr, k.nc, k.s5
    cst, cst_b, _ = s5.cst
    P.op('dve', lambda e: e.memset(cst[:, 0:1], -PI_), writes=[cst_b])
    P.op('dve', lambda e: e.memset(cst[:, 1:2], 0.0), writes=[cst_b])
    P.op('dve', lambda e: e.memset(cst[:, 2:3], 0.5 * PI_), writes=[cst_b])
    p = s5.P64
    for n, src in (("lr", "d_lambda_re"), ("li", "d_lambda_im")):
        for g2 in range(2):
            op_dma(k, 'sp', p[n][0][g2 * 64:(g2 + 1) * 64, :], dr[src].rearrange("(c g2) p -> g2 p c", g2=2)[g2],
                   [k.dbuf[src]], [p[n][1]], slow=True)
    for g2 in range(2):
        op_dma(k, 'sp', p["dt"][0][g2 * 64:(g2 + 1) * 64, :],
               dr["d_log_dt"].rearrange("(c g2) -> g2 c", g2=2)[g2:g2 + 1, :].partition_broadcast(64), [k.dbuf["d_log_dt"]], [p["dt"][1]],
               slow=True)
    A = lambda n: p[n][0][:, :]
    Bf = lambda n: p[n][1]
    op_act(k, A("dt"), A("dt"), AF.Exp, [Bf("dt")], [Bf("dt")])
    op_tt(k, 'dve', A("lnm"), A("lr"), A("dt"), ALU.mult, [Bf("lr"), Bf("dt")], [Bf("lnm")])
    op_act(k, A("m"), A("lnm"), AF.Exp, [Bf("lnm")], [Bf("m")])
    op_act(k, A("m64"), A("lnm"), AF.Exp, [Bf("lnm")], [Bf("m64")], scale=float(S5C))
    op_tt(k, 'dve', A("th"), A("li"), A("dt"), ALU.mult, [Bf("li"), Bf("dt")], [Bf("th")])
    op_ts(k, 'dve', A("ph"), A("th"), float(S5C), None, ALU.mult, None, [Bf("th")], [Bf("ph")])
    s5_trig(k, A("aim"), Bf("aim"), A("th"), Bf("th"), PI_, 64)
    s5_trig(k, A("are"), Bf("are"), A("th"), Bf("th"), 1.5 * PI_, 64)
    op_tt(k, 'dve', A("are"), A("are"), A("m"), ALU.mult, [Bf("are"), Bf("m")], [Bf("are")])
    op_tt(k, 'dve', A("aim"), A("aim"), A("m"), ALU.mult, [Bf("aim"), Bf("m")], [Bf("aim")])
    op_tt(k, 'dve', A("t0"), A("lr"), A("lr"), ALU.mult, [Bf("lr")], [Bf("t0")])
    op_tt(k, 'dve', A("t1"), A("li"), A("li"), ALU.mult, [Bf("li")], [Bf("t1")])
    op_tt(k, 'dve', A("t0"), A("t0"), A("t1"), ALU.add, [Bf("t0"), Bf("t1")], [Bf("t0")])
    P.op('dve', lambda e: e.reciprocal(out=A("t0"), in_=A("t0")), [Bf("t0")], [Bf("t0")])
    op_ts(k, 'dve', A("t1"), A("are"), -1.0, None, ALU.add, None, [Bf("are")], [Bf("t1")])
    op_tt(k, 'dve', A("fre"), A("t1"), A("lr"), ALU.mult, [Bf("t1"), Bf("lr")], [Bf("fre")])
    op_tt(k, 'dve', A("fim"), A("aim"), A("li"), ALU.mult, [Bf("aim"), Bf("li")], [Bf("fim")])
    op_tt(k, 'dve', A("fre"), A("fre"), A("fim"), ALU.add, [Bf("fre"), Bf("fim")], [Bf("fre")])
    op_tt(k, 'dve', A("fim"), A("aim"), A("lr"), ALU.mult, [Bf("aim"), Bf("lr")], [Bf("fim")])
    op_tt(k, 'dve', A("t1"), A("t1"), A("li"), ALU.mult, [Bf("t1"), Bf("li")], [Bf("t1")])
    op_tt(k, 'dve', A("fim"), A("fim"), A("t1"), ALU.subtract, [Bf("fim"), Bf("t1")], [Bf("fim")])
    op_tt(k, 'dve', A("fre"), A("fre"), A("t0"), ALU.mult, [Bf("fre"), Bf("t0")], [Bf("fre")])
    op_tt(k, 'dve', A("fim"), A("fim"), A("t0"), ALU.mult, [Bf("fim"), Bf("t0")], [Bf("fim")])
    for t_, src in ((s5.bre, "d_b_re"), (s5.bim, "d_b_im")):
        for g2 in range(2):
            op_dma(k, 'sp', t_[0][g2 * 64:(g2 + 1) * 64, :, :], dr[src].rearrange("(c g2) p q -> g2 p c q", g2=2)[g2],
                   [k.dbuf[src]], [t_[1]])
    for i in range(2):
        P.op('pool', lambda e, i=i: e.memset(s5.XX[i][0][:], 0.0), writes=[s5.XX[i][1]])
    big0, big0_b, _ = s5.big[0]
    big1, big1_b, _ = s5.big[1]
    v1 = lambda t: t[:, 0:1024].rearrange("p (c q) -> p c q", q=16)
    frb = A("fre").unsqueeze(2).to_broadcast([128, 64, 16])
    fib = A("fim").unsqueeze(2).to_broadcast([128, 64, 16])
    op_tt(k, 'dve', v1(big0), s5.bre[0][:], frb, ALU.mult, [s5.bre[1], Bf("fre")], [big0_b])
    op_tt(k, 'dve', v1(big1), s5.bim[0][:], fib, ALU.mult, [s5.bim[1], Bf("fim")], [big1_b])
    op_tt(k, 'dve', v1(big0), v1(big0), v1(big1), ALU.subtract, [big0_b, big1_b], [big0_b])
    for g2 in range(2):
        hP = slice(g2 * 64, g2 * 64 + 64)
        P.op('dve', lambda e, g2=g2, hP=hP: e.tensor_copy(out=s5.XX[0][0][hP, :, g2 * 16:(g2 + 1) * 16], in_=v1(big0)[hP]),
             [big0_b], [s5.XX[0][1]])
    op_tt(k, 'dve', v1(big0), s5.bim[0][:], frb, ALU.mult, [s5.bim[1], Bf("fre")], [big0_b])
    op_tt(k, 'dve', v1(big1), s5.bre[0][:], fib, ALU.mult, [s5.bre[1], Bf("fim")], [big1_b])
    op_tt(k, 'dve', v1(big0), v1(big0), v1(big1), ALU.add, [big0_b, big1_b], [big0_b])
    for g2 in range(2):
        hP = slice(g2 * 64, g2 * 64 + 64)
        P.op('dve', lambda e, g2=g2, hP=hP: e.tensor_copy(out=s5.XX[1][0][hP, :, g2 * 16:(g2 + 1) * 16], in_=v1(big0)[hP]),
             [big0_b], [s5.XX[1][1]])
    for i in range(2):
        for ec in range(16):
            pt, pt_b = k.psn()
            op_tr(k, pt[:, 0:128], s5.XX[i][0][:, 4 * ec:4 * ec + 4, :].rearrange("p a b -> p (a b)"), k.identF[:],
                  [s5.XX[i][1], k.identF_b], [pt_b])
            op_act(k, s5.LB[i][0][:, ec, :], pt[:, 0:128], AF.Copy, [pt_b], [s5.LB[i][1]])
    m4, m4_b, _ = s5.mask4
    P.op('dve', lambda e: e.memset(m4[:], 0.0), writes=[m4_b])
    big2, big2_b, _ = s5.big[2]
    P.op('dve', lambda e: e.tensor_reduce(out=big2[:, 4:6], in_=k.identF[:, :].rearrange("p (q g c) -> p g q c", q=4, g=2),
                                          axis=AX.XY, op=ALU.add), [k.identF_b], [big2_b])
    P.op('dve', lambda e: e.memset(m4[:], 1.0), writes=[m4_b])
    op_ts(k, 'dve', m4[:, 0:64], m4[:, 0:64], big2[:, 4:5], None, ALU.mult, None, [m4_b, big2_b], [m4_b])
    op_ts(k, 'dve', m4[:, 64:128], m4[:, 64:128], big2[:, 5:6], None, ALU.mult, None, [m4_b, big2_b], [m4_b])
    for i, src in ((0, "d_c_re"), (1, "d_c_im")):
        op_dma(k, 'sp', s5.CN[i][0][:, :, :], dr[src].rearrange("(e a) k p -> (a k) e p", a=8), [k.dbuf[src]], [s5.CN[i][1]])
        for ec in range(16):
            y4, y4_b, _ = s5.g[0]
            op_tt(k, 'dve', y4[:, 0:128].rearrange("p (a b) -> p a b", b=64), s5.CN[i][0][:, ec:ec + 1, :].to_broadcast([128, 2, 64]),
                  m4[:, :].rearrange("p (a b) -> p a b", b=64), ALU.mult, [s5.CN[i][1], m4_b], [y4_b])
            pt, pt_b = k.psn()
            op_tr(k, pt[:, 0:128], y4[:, 0:128], k.identF[:], [y4_b, k.identF_b], [pt_b])
            if i == 0:
                op_act(k, s5.LC[i][0][:, ec, :], pt[:, 0:128], AF.Copy, [pt_b], [s5.LC[i][1]])
            else:
                op_act(k, s5.LC[i][0][:, ec, :], pt[:, 0:128], AF.Copy, [pt_b], [s5.LC[i][1]], scale=-1.0)
    for i in range(2):
        P.op('dve', lambda e, i=i: e.tensor_copy(out=s5.LC3[i][0][:, :, :], in_=s5.LC[i][0][:, :, 64:128]), [s5.LC[i][1]], [s5.LC3[i][1]])
        P.op('dve', lambda e, i=i: e.memset(s5.LC3[i][0][:, :, 0:32], 0.0), writes=[s5.LC3[i][1]])
    for n, src in (("d", "d_d"), ("bg", "d_b_glu")):
        op_dma(k, 'sp', s5.par[n][0][:], dr[src].rearrange("(c p) -> p c", p=128), [k.dbuf[src]], [s5.par[n][1]], slow=True)
    m01, m01_b, _ = s5.m01
    P.op('pool', lambda e: e.memset(m01[:], 1.0), writes=[m01_b])
    P.op('pool', lambda e: e.memset(m01[:].rearrange("p (c t) -> p c t", t=S5C)[:, :, 0:1], 0.0), writes=[m01_b])


def layer_s5(k, ps_):
    P, dr, nc = k.P, k.dr, k.nc
    if not hasattr(k, "s5"):
        s5_setup(k)
    s5 = k.s5
    s5_load(k)
    P.barrier()
    P.chk("s5_load")
    tiles = ntiles(ps_)
    p = s5.P64
    A = lambda n: p[n][0]
    Bf = lambda n: p[n][1]
    tab = s5.tab
    TA = lambda n: tab[n][0][:, :]
    TB = lambda n: tab[n][1]
    d_w_in, d_w_in_b = dr["d_w_in"], k.dbuf["d_w_in"]
    U, U_b, _ = s5.U
    if ps_ == 0:
        for i, src in ((0, "state_ssm_re"), (1, "state_ssm_im")):
            for q4 in range(4):
                sr, sr_b, _ = s5.srow
                op_dma(k, 'sp', sr[:, :], dr[src].rearrange("s g p -> s (g p)")[:, q4 * 2048:(q4 + 1) * 2048], [k.dbuf[src]], [sr_b])
                for j in range(16):
                    sc = q4 * 16 + j
                    if j % 8 == 0:
                        pt, pt_b = k.psn()
                    op_tr(k, pt[:, (j % 8) * NS:(j % 8 + 1) * NS], sr[0:NS, j * 128:(j + 1) * 128], k.identF[0:NS, 0:NS],
                          [sr_b, k.identF_b], [pt_b])
                    if j % 8 == 7:
                        P.op('dve', lambda e, i=i, sc=sc, pt=pt: e.tensor_copy(
                            out=s5.s0T[i][0][:, sc - 7:sc + 1, :], in_=pt[:, 0:8 * NS].rearrange("p (a s) -> p a s", s=NS)),
                            [pt_b], [s5.s0T[i][1]])
    big = s5.big
    P.barrier()

    def ps56():
        i = 5 + k.ps_i[0] % 2
        k.ps_i[0] += 1
        return k.ps[i]
    for ec in range(16):
        load_w_cols(k, s5.wt[0], s5.wt[1], d_w_in, d_w_in_b, ec * 128)
        for ti, (c0, w) in enumerate(tiles):
            pu, pu_b = k.psn()
            for kk in range(8):
                op_mm(k, pu[:, 0:w], s5.wt[0][:, kk, :], k.hT[:, kk, c0 + 1:c0 + 1 + w], kk == 0, kk == 7,
                      [s5.wt[1]] + hT_reads(k, c0, w), [pu_b])
            op_act(k, U[:, c0:c0 + w], pu[:, 0:w], AF.Copy, [pu_b], [U_b.sub(ti)])
        Uz, Uz_b, _ = s5.Uz
        P.op('dve', lambda e: e.tensor_copy(out=Uz[64:128, :], in_=U[64:128, :]), [U_b], [Uz_b])
        P.op('dve', lambda e: e.memset(Uz[64:96, :], 0.0), writes=[Uz_b])
        P.chk("s5_u")
        py = {}
        for q in (0, 1, 3, 2):
            sc = ec * 4 + q
            col = slice(sc, sc + 1)
            qP = slice(q * 32, q * 32 + 32)
            P.op('pool', lambda e: e.iota(tab["c"][0][:].bitcast(I32)[:, 0:64], pattern=[[1, 64]], base=0, channel_multiplier=0),
                 writes=[TB("c")])
            P.op('dve', lambda e: e.tensor_copy(out=TA("c"), in_=tab["c"][0][:].bitcast(I32)[:, 0:64]), [TB("c")], [TB("c")])
            op_ts(k, 'dve', TA("ang"), TA("c"), A("th")[:, col], None, ALU.mult, None, [TB("c"), Bf("th")], [TB("ang")])
            op_ts(k, 'dve', TA("mg"), TA("c"), A("lnm")[:, col], None, ALU.mult, None, [TB("c"), Bf("lnm")], [TB("mg")])
            s5_trig(k, TA("Fs"), TB("Fs"), TA("ang"), TB("ang"), PI_, 64)
            s5_trig(k, TA("Fc"), TB("Fc"), TA("ang"), TB("ang"), 1.5 * PI_, 64)
            op_act(k, TA("c"), TA("mg"), AF.Exp, [TB("mg")], [TB("c")])
            op_tt(k, 'dve', TA("Bc"), TA("Fc"), TA("c"), ALU.mult, [TB("Fc"), TB("c")], [TB("Bc")])
            op_tt(k, 'dve', TA("Bs"), TA("Fs"), TA("c"), ALU.mult, [TB("Fs"), TB("c")], [TB("Bs")])
            op_act(k, TA("c"), TA("mg"), AF.Exp, [TB("mg")], [TB("c")], scale=-1.0)
            op_tt(k, 'dve', TA("Fc"), TA("Fc"), TA("c"), ALU.mult, [TB("Fc"), TB("c")], [TB("Fc")])
            op_tt(k, 'dve', TA("Fs"), TA("Fs"), TA("c"), ALU.mult, [TB("Fs"), TB("c")], [TB("Fs")])
            P.op('pool', lambda e: e.iota(tab["c"][0][:].bitcast(I32)[:, 0:32], pattern=[[1, 32]], base=0, channel_multiplier=0),
                 writes=[TB("c")])
            P.op('dve', lambda e: e.tensor_copy(out=tab["c"][0][:, 0:32], in_=tab["c"][0][:].bitcast(I32)[:, 0:32]), [TB("c")], [TB("c")])
            op_ts(k, 'dve', tab["ang"][0][:, 0:32], tab["c"][0][:, 0:32], A("ph")[:, col], None, ALU.mult, None, [TB("c"), Bf("ph")], [TB("ang")])
            s5_trig(k, tab["Gs"][0][:, 0:32], TB("Gs"), tab["ang"][0][:, 0:32], TB("ang"), PI_, 32)
            s5_trig(k, tab["Gc"][0][:, 0:32], TB("Gc"), tab["ang"][0][:, 0:32], TB("ang"), 1.5 * PI_, 32)
            XR, XI, T1, T2 = [b_[0] for b_ in big[0:4]]
            XR_b, XI_b, T1_b, T2_b = [b_[1] for b_ in big[0:4]]
            QR_, QI_, QR_b, QI_b = XR, XI, XR_b, XI_b
            v3 = lambda t: t[:, :].rearrange("p (c t) -> p c t", t=S5C)
            bc = lambda n: tab[n][0][:, :].unsqueeze(1).to_broadcast([128, S5NCH, S5C])
            for ti, (c0, w) in enumerate(tiles):
                pbr, pbr_b = ps56()
                pbi, pbi_b = ps56()
                if q < 3:
                    op_mm(k, pbr[:, 0:w], s5.LB[0][0][qP, ec, :], U[qP, c0:c0 + w], True, True, [s5.LB[0][1], U_b.sub(ti)], [pbr_b])
                    op_mm(k, pbi[:, 0:w], s5.LB[1][0][qP, ec, :], U[qP, c0:c0 + w], True, True, [s5.LB[1][1], U_b.sub(ti)], [pbi_b])
                else:
                    op_mm(k, pbr[:, 0:w], s5.LB[0][0][64:128, ec, :], Uz[64:128, c0:c0 + w], True, True, [s5.LB[0][1], Uz_b], [pbr_b])
                    op_mm(k, pbi[:, 0:w], s5.LB[1][0][64:128, ec, :], Uz[64:128, c0:c0 + w], True, True, [s5.LB[1][1], Uz_b], [pbi_b])
                if c0 < T:
                    P.op('act', lambda e, c0=c0, pbr=pbr: e.activation(out=XR[:, c0:c0 + 512], in_=pbr[:, :], func=AF.Copy), [pbr_b], [XR_b])
                    P.op('act', lambda e, c0=c0, pbi=pbi: e.activation(out=XI[:, c0:c0 + 512], in_=pbi[:, :], func=AF.Copy), [pbi_b], [XI_b])
                else:
                    s0r, s0i = s5.s0T[0], s5.s0T[1]
                    tr_, ti__ = s5.tmpn[0], s5.tmpn[1]
                    op_stt(k, 'dve', tr_[0][:, :], s0r[0][:, sc, :], A("are")[:, col], pbr[:, 0:NS], ALU.mult, ALU.add,
                           [s0r[1], Bf("are"), pbr_b], [tr_[1]])
                    op_ts(k, 'dve', s5.g[1][0][:, 0:NS], s0i[0][:, sc, :], A("aim")[:, col], None, ALU.mult, None, [s0i[1], Bf("aim")], [s5.g[1][1]])
                    op_tt(k, 'dve', tr_[0][:, :], tr_[0][:, :], s5.g[1][0][:, 0:NS], ALU.subtract, [tr_[1], s5.g[1][1]], [tr_[1]])
                    op_stt(k, 'dve', ti__[0][:, :], s0i[0][:, sc, :], A("are")[:, col], pbi[:, 0:NS], ALU.mult, ALU.add,
                           [s0i[1], Bf("are"), pbi_b], [ti__[1]])
                    op_stt(k, 'dve', ti__[0][:, :], s0r[0][:, sc, :], A("aim")[:, col], ti__[0][:, :], ALU.mult, ALU.add,
                           [s0r[1], Bf("aim"), ti__[1]], [ti__[1]])
                    P.op('dve', lambda e, sc=sc: e.tensor_copy(out=s0r[0][:, sc, :], in_=tr_[0][:, :]), [tr_[1]], [s0r[1]])
                    P.op('dve', lambda e, sc=sc: e.tensor_copy(out=s0i[0][:, sc, :], in_=ti__[0][:, :]), [ti__[1]], [s0i[1]])
                    op_act(k, s5.sbf[0][0][:, T:T + NS], tr_[0][:, :], AF.Copy, [tr_[1]], [s5.sbf[0][1].sub(4)])
                    op_act(k, s5.sbf[1][0][:, T:T + NS], ti__[0][:, :], AF.Copy, [ti__[1]], [s5.sbf[1][1].sub(4)])
            op_tt(k, 'dve', v3(T1), v3(XR), bc("Fc"), ALU.mult, [XR_b, TB("Fc")], [T1_b])
            op_tt(k, 'pool', v3(T2), v3(XI), bc("Fs"), ALU.mult, [XI_b, TB("Fs")], [T2_b])
            op_tt(k, 'dve', v3(T1), v3(T1), v3(T2), ALU.add, [T1_b, T2_b], [T1_b])
            op_tt(k, 'pool', v3(T2), v3(XI), bc("Fc"), ALU.mult, [XI_b, TB("Fc")], [T2_b])
            op_tt(k, 'dve', v3(XI), v3(XR), bc("Fs"), ALU.mult, [XR_b, TB("Fs")], [XI_b])
            op_tt(k, 'pool', v3(T2), v3(T2), v3(XI), ALU.subtract, [T2_b, XI_b], [T2_b])
            m01 = s5.m01
            P.op('dve', lambda e: e.tensor_tensor_scan(out=QR_[:], data0=m01[0][:], data1=T1[:], initial=0.0, op0=ALU.mult, op1=ALU.add),
                 [m01[1], T1_b], [QR_b])
            P.op('dve', lambda e: e.tensor_tensor_scan(out=QI_[:], data0=m01[0][:], data1=T2[:], initial=0.0, op0=ALU.mult, op1=ALU.add),
                 [m01[1], T2_b], [QI_b])
            EE = s5.ec_
            e = lambda n: EE[n][0][:, 0:S5NCH]
            eb = lambda n: EE[n][1]
            qr_end = v3(QR_)[:, :, S5C - 1]
            qi_end = v3(QI_)[:, :, S5C - 1]
            bc63 = tab["Bc"][0][:, S5C - 1:S5C]
            bs63 = tab["Bs"][0][:, S5C - 1:S5C]
            op_ts(k, 'dve', e("er"), qr_end, bc63, None, ALU.mult, None, [QR_b, TB("Bc")], [eb("er")])
            op_ts(k, 'dve', e("hr"), qi_end, bs63, None, ALU.mult, None, [QI_b, TB("Bs")], [eb("hr")])
            op_tt(k, 'dve', e("er"), e("er"), e("hr"), ALU.subtract, [eb("er"), eb("hr")], [eb("er")])
            op_ts(k, 'dve', e("ei"), qr_end, bs63, None, ALU.mult, None, [QR_b, TB("Bs")], [eb("ei")])
            op_ts(k, 'dve', e("hr"), qi_end, bc63, None, ALU.mult, None, [QI_b, TB("Bc")], [eb("hr")])
            op_tt(k, 'dve', e("ei"), e("ei"), e("hr"), ALU.add, [eb("ei"), eb("hr")], [eb("ei")])
            Gc = tab["Gc"][0][:, 0:S5NCH]
            Gs = tab["Gs"][0][:, 0:S5NCH]
            op_tt(k, 'dve', e("hr"), e("er"), Gc, ALU.mult, [eb("er"), TB("Gc")], [eb("hr")])
            op_tt(k, 'dve', e("hi"), e("ei"), Gs, ALU.mult, [eb("ei"), TB("Gs")], [eb("hi")])
            op_tt(k, 'dve', e("hr"), e("hr"), e("hi"), ALU.add, [eb("hr"), eb("hi")], [eb("hr")])
            op_tt(k, 'dve', e("hi"), e("ei"), Gc, ALU.mult, [eb("ei"), TB("Gc")], [eb("hi")])
            op_tt(k, 'dve', e("cr"), e("er"), Gs, ALU.mult, [eb("er"), TB("Gs")], [eb("cr")])
            op_tt(k, 'dve', e("hi"), e("hi"), e("cr"), ALU.subtract, [eb("hi"), eb("cr")], [eb("hi")])
            op_ts(k, 'dve', e("ci"), Gc, 0.0, A("m64")[:, col], ALU.mult, ALU.add, [TB("Gc"), Bf("m64")], [eb("ci")])
            P.op('dve', lambda e_: e_.tensor_tensor_scan(out=e("Er"), data0=e("ci"), data1=e("hr"), initial=0.0, op0=ALU.mult, op1=ALU.add),
                 [eb("ci"), eb("hr")], [eb("Er")])
            P.op('dve', lambda e_: e_.tensor_tensor_scan(out=e("Ei"), data0=e("ci"), data1=e("hi"), initial=0.0, op0=ALU.mult, op1=ALU.add),
                 [eb("ci"), eb("hi")], [eb("Ei")])
            op_tt(k, 'dve', e("hr"), e("Er"), Gc, ALU.mult, [eb("Er"), TB("Gc")], [eb("hr")])
            op_tt(k, 'dve', e("cr"), e("Ei"), Gs, ALU.mult, [eb("Ei"), TB("Gs")], [eb("cr")])
            op_tt(k, 'dve', e("hr"), e("hr"), e("cr"), ALU.subtract, [eb("hr"), eb("cr")], [eb("hr")])
            op_tt(k, 'dve', e("hi"), e("Er"), Gs, ALU.mult, [eb("Er"), TB("Gs")], [eb("hi")])
            op_tt(k, 'dve', e("cr"), e("Ei"), Gc, ALU.mult, [eb("Ei"), TB("Gc")], [eb("cr")])
            op_tt(k, 'dve', e("hi"), e("hi"), e("cr"), ALU.add, [eb("hi"), eb("cr")], [eb("hi")])
            P.op('dve', lambda e_, sc=sc: e_.tensor_copy(out=s5.fin[0][0][:, sc:sc + 1], in_=EE["hr"][0][:, S5NCH - 1:S5NCH]), [eb("hr")], [s5.fin[0][1]])
            P.op('dve', lambda e_, sc=sc: e_.tensor_copy(out=s5.fin[1][0][:, sc:sc + 1], in_=EE["hi"][0][:, S5NCH - 1:S5NCH]), [eb("hi")], [s5.fin[1][1]])
            n1 = S5NCH - 1
            op_ts(k, 'dve', EE["cr"][0][:, 0:n1], EE["hr"][0][:, 0:n1], A("are")[:, col], None, ALU.mult, None, [eb("hr"), Bf("are")], [eb("cr")])
            op_ts(k, 'dve', EE["ci"][0][:, 0:n1], EE["hi"][0][:, 0:n1], A("aim")[:, col], None, ALU.mult, None, [eb("hi"), Bf("aim")], [eb("ci")])
            op_tt(k, 'dve', EE["cr"][0][:, 0:n1], EE["cr"][0][:, 0:n1], EE["ci"][0][:, 0:n1], ALU.subtract, [eb("cr"), eb("ci")], [eb("cr")])
            op_ts(k, 'dve', EE["ci"][0][:, 0:n1], EE["hr"][0][:, 0:n1], A("aim")[:, col], None, ALU.mult, None, [eb("hr"), Bf("aim")], [eb("ci")])
            op_ts(k, 'dve', EE["er"][0][:, 0:n1], EE["hi"][0][:, 0:n1], A("are")[:, col], None, ALU.mult, None, [eb("hi"), Bf("are")], [eb("er")])
            op_tt(k, 'dve', EE["ci"][0][:, 0:n1], EE["ci"][0][:, 0:n1], EE["er"][0][:, 0:n1], ALU.add, [eb("ci"), eb("er")], [eb("ci")])
            op_tt(k, 'dve', v3(T1)[:, 1:S5NCH, 0], v3(T1)[:, 1:S5NCH, 0], EE["cr"][0][:, 0:n1], ALU.add, [T1_b, eb("cr")], [T1_b])
            op_tt(k, 'dve', v3(T2)[:, 1:S5NCH, 0], v3(T2)[:, 1:S5NCH, 0], EE["ci"][0][:, 0:n1], ALU.add, [T2_b, eb("ci")], [T2_b])
            P.op('dve', lambda e_: e_.tensor_tensor_scan(out=QR_[:], data0=m01[0][:], data1=T1[:], initial=0.0, op0=ALU.mult, op1=ALU.add),
                 [m01[1], T1_b], [QR_b])
            P.op('dve', lambda e_: e_.tensor_tensor_scan(out=QI_[:], data0=m01[0][:], data1=T2[:], initial=0.0, op0=ALU.mult, op1=ALU.add),
                 [m01[1], T2_b], [QI_b])
            op_tt(k, 'dve', v3(T1), v3(QR_), bc("Bc"), ALU.mult, [QR_b, TB("Bc")], [T1_b])
            op_tt(k, 'pool', v3(T2), v3(QI_), bc("Bs"), ALU.mult, [QI_b, TB("Bs")], [T2_b])
            op_tt(k, 'dve', s5.sbf[0][0][:, 0:T].rearrange("p (c t) -> p c t", t=S5C), v3(T1), v3(T2), ALU.subtract, [T1_b, T2_b],
                  [s5.sbf[0][1].sub(0)])
            op_tt(k, 'pool', v3(T1), v3(QR_), bc("Bs"), ALU.mult, [QR_b, TB("Bs")], [T1_b])
            op_tt(k, 'dve', v3(T2), v3(QI_), bc("Bc"), ALU.mult, [QI_b, TB("Bc")], [T2_b])
            op_tt(k, 'pool', s5.sbf[1][0][:, 0:T].rearrange("p (c t) -> p c t", t=S5C), v3(T1), v3(T2), ALU.add, [T1_b, T2_b],
                  [s5.sbf[1][1].sub(0)])
            for ti, (c0, w) in enumerate(tiles):
                if q == 0:
                    py[ti] = k.ps[ti]
                pyt, pyt_b = py[ti]
                rs_ = [s5.sbf[0][1].sub(0 if c0 < T else 4), s5.sbf[1][1].sub(0 if c0 < T else 4)]
                if q < 2:
                    op_mm(k, pyt[qP, 0:w], s5.LC[0][0][:, ec, qP], s5.sbf[0][0][:, c0:c0 + w], True, False, [s5.LC[0][1]] + rs_, [pyt_b])
                    op_mm(k, pyt[qP, 0:w], s5.LC[1][0][:, ec, qP], s5.sbf[1][0][:, c0:c0 + w], False, True, [s5.LC[1][1]] + rs_, [pyt_b])
                elif q == 3:
                    op_mm(k, pyt[64:128, 0:w], s5.LC3[0][0][:, ec, :], s5.sbf[0][0][:, c0:c0 + w], True, False, [s5.LC3[0][1]] + rs_, [pyt_b])
                    op_mm(k, pyt[64:128, 0:w], s5.LC3[1][0][:, ec, :], s5.sbf[1][0][:, c0:c0 + w], False, False, [s5.LC3[1][1]] + rs_, [pyt_b])
                else:
                    op_mm(k, pyt[64:96, 0:w], s5.LC[0][0][:, ec, 64:96], s5.sbf[0][0][:, c0:c0 + w], False, False, [s5.LC[0][1]] + rs_, [pyt_b])
                    op_mm(k, pyt[64:96, 0:w], s5.LC[1][0][:, ec, 64:96], s5.sbf[1][0][:, c0:c0 + w], False, True, [s5.LC[1][1]] + rs_, [pyt_b])
            P.chk("s5_sc")
        for ti, (c0, w) in enumerate(tiles):
            pyt, pyt_b = py[ti]
            y_, y_b, _ = s5.g[0]
            t_, t_b, _ = s5.g[1]
            s_, s_b, _ = s5.g[2]
            op_stt(k, 'dve', y_[:, 0:w], U[:, c0:c0 + w], s5.par["d"][0][:, ec:ec + 1], pyt[:, 0:w], ALU.mult, ALU.add,
                   [U_b.sub(ti), s5.par["d"][1], pyt_b], [y_b])
            op_act(k, t_[:, 0:w], y_[:, 0:w], AF.Square, [y_b], [t_b])
            op_ts(k, 'dve', t_[:, 0:w], t_[:, 0:w], 0.044715, 1.0, ALU.mult, ALU.add, [t_b], [t_b])
            op_tt(k, 'dve', t_[:, 0:w], t_[:, 0:w], y_[:, 0:w], ALU.mult, [t_b, y_b], [t_b])
            op_act(k, s_[:, 0:w], t_[:, 0:w], AF.Sigmoid, [t_b], [s_b], scale=1.5957691216057308)
            op_tt(k, 'dve', k.GT[:, ec, c0:c0 + w], y_[:, 0:w], s_[:, 0:w], ALU.mult, [y_b, s_b], [k.GT_b.sub(ec).sub(ti)])
        P.chk("s5_ec")
    P.barrier()
    for i, nm in ((0, "sre_p"), (1, "sim_p")):
        for g2 in range(2):
            op_dma(k, 'sp', dr[nm][ps_].rearrange("(c g2) p -> g2 p c", g2=2)[g2], s5.fin[i][0][g2 * 64:(g2 + 1) * 64, :],
                   [s5.fin[i][1]], [k.dbuf[nm]], slow=True)
    if ps_ == 0:
        for i, nm in ((0, "sre_s"), (1, "sim_s")):
            for q4 in range(4):
                sr, sr_b, _ = s5.srow
                for j in range(16):
                    sc = q4 * 16 + j
                    if j % 4 == 0:
                        pt, pt_b = k.psn()
                    op_tr(k, pt[0:NS, (j % 4) * 128:(j % 4 + 1) * 128], s5.s0T[i][0][:, sc, :], k.identF[:], [s5.s0T[i][1], k.identF_b], [pt_b])
                    if j % 4 == 3:
                        P.op('dve', lambda e, j=j, pt=pt: e.tensor_copy(out=sr[0:NS, (j - 3) * 128:(j + 1) * 128], in_=pt[0:NS, 0:512]),
                             [pt_b], [sr_b])
                op_dma(k, 'sp', dr[nm].rearrange("s g p -> s (g p)")[:, q4 * 2048:(q4 + 1) * 2048], sr[0:NS, :], [sr_b], [k.dbuf[nm]])
    P.barrier()
    gscr, gscr_b = dr["gscr"], k.dbuf["gscr"]
    for e2 in range(16):
        wg, wg_b, _ = s5.wg
        op_dma(k, 'pool', wg[:, :, :], dr["d_w_glu"][:, e2 * 128:(e2 + 1) * 128].rearrange("(c p) e -> p c e", p=128), [k.dbuf["d_w_glu"]], [wg_b])
        load_w_cols(k, s5.wt[0], s5.wt[1], d_w_in, d_w_in_b, E + e2 * 128)
        for ti, (c0, w) in enumerate(tiles):
            pg, pg_b = k.psn()
            for ec in range(16):
                op_mm(k, pg[:, 0:w], wg[:, ec, :], k.GT[:, ec, c0:c0 + w], ec == 0, ec == 15, [wg_b, k.GT_b.sub(ec).sub(ti)], [pg_b])
            pz, pz_b = k.psn()
            for kk in range(8):
                op_mm(k, pz[:, 0:w], s5.wt[0][:, kk, :], k.hT[:, kk, c0 + 1:c0 + 1 + w], kk == 0, kk == 7,
                      [s5.wt[1]] + hT_reads(k, c0, w), [pz_b])
            s_, s_b, _ = s5.g[0]
            z_, z_b, _ = s5.g[1]
            op_act(k, s_[:, 0:w], pg[:, 0:w], AF.Sigmoid, [pg_b, s5.par["bg"][1]], [s_b], bias=s5.par["bg"][0][:, e2:e2 + 1])
            op_act(k, z_[:, 0:w], pz[:, 0:w], AF.Silu, [pz_b], [z_b])
            op_tt(k, 'dve', s_[:, 0:w], s_[:, 0:w], k.GT[:, e2, c0:c0 + w], ALU.mult, [s_b, k.GT_b.sub(e2).sub(ti)], [s_b])
            gb, gb_b, _ = s5.gb
            op_tt(k, 'dve', gb[:, 0:w], s_[:, 0:w], z_[:, 0:w], ALU.mult, [s_b, z_b], [gb_b])
            op_dma(k, 'sp', gscr[e2, :, c0:c0 + w], gb[:, 0:w], [gb_b], [gscr_b])
    P.barrier()
    TW = TWA if ps_ == 0 else T
    for e2 in range(16):
        op_dma(k, 'sp', k.GT[:, e2, 0:TW], gscr[e2, :, 0:TW], [gscr_b], [k.GT_b])
N_LAYERS = 4

_CACHE = {}


def kernel(**inp):
    n_layers = N_LAYERS
    N_IN = N_IN_BY_LAYER[n_layers]
    if "nc" not in _CACHE:
        _CACHE["nc"] = build(n_layers)
    nc, k = _CACHE["nc"]
    f = lambda a: np.ascontiguousarray(np.asarray(a))
    shared = {}
    for name, shp, dt in IN_SHAPES[:N_IN]:
        if name in ("xp", "xs", "state_conv", "state_shift", "state_wkv", "state_ssm_re", "state_ssm_im", "page_table"):
            continue
        if name == "cache_cat":
            shared[name] = np.concatenate([np.asarray(inp["cache_krope"]), np.asarray(inp["cache_ckv"])], axis=-1)
            continue
        shared[name] = f(inp[name])
    in_maps = []
    for c in range(NCORES):
        m = dict(shared)
        m["xp"] = f(inp["x_prompt"][2 * c:2 * c + 2])
        m["xs"] = f(inp["x_sample"][NS * c:NS * (c + 1), 0, :])
        for nm in ("state_conv", "state_shift", "state_wkv", "state_ssm_re", "state_ssm_im", "page_table"):
            if any(nm == x[0] for x in IN_SHAPES[:N_IN]):
                m[nm] = f(inp[nm][NS * c:NS * (c + 1)])
        in_maps.append(m)
    res = run_bass_kernel_spmd(nc, in_maps, core_ids=list(range(NCORES)))
    R = res.results
    cat = lambda nm: np.concatenate([np.asarray(R[c][nm]) for c in range(NCORES)], axis=0)
    y_p = cat("y_p")
    y_s = cat("y_s").reshape(128, 1, D)
    outs = (y_p, y_s, cat("conv_p"), cat("conv_s"), cat("shift_p"), cat("shift_s"), cat("wkv_p"), cat("wkv_s"),
            cat("ckv_p"), cat("ckv_s").reshape(128, 1, 256), cat("kr_p"), cat("kr_s").reshape(128, 1, 64),
            cat("sre_p"), cat("sre_s"), cat("sim_p"), cat("sim_s"))
    return tuple(np.ascontiguousarray(o, dtype=np.float32) for o in outs)
```

```python
from concourse.bass_utils import run_bass_kernel_spmd
import numpy as np
import concourse.bass as bass
import concourse.mybir as mybir

F32 = mybir.dt.float32
BF16 = mybir.dt.bfloat16
I32 = mybir.dt.int32
AF = mybir.ActivationFunctionType
ALU = mybir.AluOpType
AX = mybir.AxisListType

NDS = 6
ENG = ['pe', 'act', 'dve', 'pool', 'sp']
BLK = {'pe': 'tensor', 'act': 'scalar', 'dve': 'vector', 'pool': 'gpsimd', 'sp': 'sync'}


class Buf:
    __slots__ = ('name', 'w', 'r', 'parent', 'kids')

    def __init__(s, name, parent=None):
        s.name = name
        s.w = None
        s.r = []
        s.parent = parent
        s.kids = {}

    def sub(s, key):
        k = s.kids.get(key)
        if k is None:
            k = Buf(f'{s.name}.{key}', s)
            s.kids[key] = k
        return k

    def family(s):
        out = [s]
        p = s.parent
        while p is not None:
            out.append(p)
            p = p.parent
        if s.kids:
            st = list(s.kids.values())
            while st:
                k = st.pop()
                out.append(k)
                if k.kids:
                    st.extend(k.kids.values())
        return out


class Op:
    __slots__ = ('eng', 'fn', 'waits', 'idx', 'inc', 'dma', 'semk', 'val', 'seq')


class Prog:
    def __init__(s):
        s.q = {e: [] for e in ENG}
        s.seen = {e: {} for e in ENG}
        s.rr = {e: 0 for e in ENG}
        s.dlast = {}
        s.dcount = {}
        s.alldma = []

    def op(s, eng, fn, reads=(), writes=(), dma=False):
        if getattr(s, "dead", False):
            return None
        o = Op()
        o.eng = eng
        o.fn = fn
        o.dma = dma
        o.inc = bool(dma)
        o.waits = []
        o.idx = len(s.q[eng])
        o.semk = None
        o.val = None
        o.seq = None
        deps = {}
        for b in reads:
            for f in b.family():
                if f.w is not None:
                    deps[id(f.w)] = f.w
        for b in writes:
            for f in b.family():
                if f.w is not None:
                    deps[id(f.w)] = f.w
                for r in f.r:
                    deps[id(r)] = r
        if dma:
            k = s.rr[eng] % NDS
            s.rr[eng] += 1
            o.semk = k
            prev = s.dlast.get((eng, k))
            if prev is not None:
                deps[id(prev)] = prev
            s.dlast[(eng, k)] = o
            o.seq = s.dcount.get((eng, k), 0) + 1
            s.dcount[(eng, k)] = o.seq
            s.alldma.append(o)
        seen = s.seen[eng]
        for d in deps.values():
            if d.dma:
                key = (d.eng, d.semk)
                v = d.seq
            else:
                if d.eng == eng and eng == 'pe':
                    continue
                key = d.eng
                v = d.idx
            if seen.get(key, -1) >= v:
                continue
            seen[key] = v
            o.waits.append(d)
            d.inc = True
        for b in reads:
            b.r.append(o)
        for b in writes:
            b.w = o
            b.r = []
        s.q[eng].append(o)
        return o

    def chk(s, name):
        import os
        if os.environ.get("KSTOP", "") == name:
            s.dead = True

    def barrier(s):
        if getattr(s, "dead", False):
            return
        lasts = []
        for e in ENG:
            for o in reversed(s.q[e]):
                if o.fn is not None and not o.dma:
                    lasts.append(o)
                    break
        dl = list(s.dlast.values())
        for e in ENG:
            o = Op()
            o.eng = e
            o.fn = None
            o.dma = False
            o.inc = False
            o.waits = []
            o.idx = len(s.q[e])
            o.semk = None
            o.val = None
            o.seq = None
            seen = s.seen[e]
            for d in lasts + dl:
                if d.dma:
                    key = (d.eng, d.semk)
                    v = d.seq
                else:
                    if d.eng == e:
                        continue
                    key = d.eng
                    v = d.idx
                if seen.get(key, -1) >= v:
                    continue
                seen[key] = v
                o.waits.append(d)
                d.inc = True
            s.q[e].append(o)

    def finish(s, eng='sp'):
        o = Op()
        o.eng = eng
        o.fn = None
        o.dma = False
        o.inc = False
        o.waits = list(s.dlast.values())
        o.idx = len(s.q[eng])
        s.q[eng].append(o)

    def emit(s, nc):
        sems = {}
        for e in ENG:
            sems[e] = nc.alloc_semaphore(f'S_{e}')
            for k in range(NDS):
                sems[(e, k)] = nc.alloc_semaphore(f'D_{e}_{k}')
        for e in ENG:
            c = 0
            for o in s.q[e]:
                if o.dma:
                    o.val = 16 * o.seq
                elif o.inc:
                    c += 1
                    o.val = c
        n_ins = {e: 0 for e in ENG}
        with nc.Block() as block:
            for e in ENG:
                def body(eng, e=e):
                    for o in s.q[e]:
                        for d in o.waits:
                            sm = sems[(d.eng, d.semk)] if d.dma else sems[d.eng]
                            eng.wait_ge(sm, d.val)
                            n_ins[e] += 1
                        if o.fn is None:
                            continue
                        ins = o.fn(eng)
                        n_ins[e] += 1
                        if o.dma:
                            ins.then_inc(sems[(e, o.semk)], 16)
                        elif o.inc:
                            ins.then_inc(sems[e], 1)
                getattr(block, BLK[e])(body)
        return n_ins

import math
NCORES = 8
T = 2048
D = 1024
E = 2048
NS = 16
TWA = T + NS
SB0 = 16640
SBMAX = 229376

OUT_SHAPES = [
    ("y_p", [2, T, D]), ("y_s", [NS, D]),
    ("conv_p", [2, 30, E]), ("conv_s", [NS, 30, E]),
    ("shift_p", [2, D]), ("shift_s", [NS, D]),
    ("wkv_p", [2, 32, 64, 64]), ("wkv_s", [NS, 32, 64, 64]),
    ("ckv_p", [2, T, 256]), ("ckv_s", [NS, 256]),
    ("kr_p", [2, T, 64]), ("kr_s", [NS, 64]),
    ("sre_p", [2, 128, 64]), ("sre_s", [NS, 128, 64]),
    ("sim_p", [2, 128, 64]), ("sim_s", [NS, 128, 64]),
]

IN_SHAPES = [
    ("xp", [2, T, D], F32), ("xs", [NS, D], F32),
    ("state_conv", [NS, 30, E], F32), ("state_shift", [NS, D], F32),
    ("norm_pre", [4, D], F32), ("norm_post", [4, D], F32),
    ("a_w_in", [D, 3 * E], F32), ("a_b_in", [3 * E], F32), ("a_conv_w", [31, E], F32),
    ("a_conv_b", [E], F32), ("a_ln_g", [E], F32), ("a_ln_b", [E], F32), ("a_w_out", [E, D], F32),
    ("state_wkv", [NS, 32, 64, 64], F32),
    ("b_mu", [6, D], F32), ("b_w_rkvz", [4, D, E], F32), ("b_w0", [E], F32), ("b_w1", [D, 64], F32), ("b_w2", [64, E], F32),
    ("b_a0", [E], F32), ("b_a1", [D, 64], F32), ("b_a2", [64, E], F32), ("b_k_k", [E], F32), ("b_k_a", [E], F32),
    ("b_r_k", [32, 64], F32), ("b_ln_g", [E], F32), ("b_ln_b", [E], F32), ("b_w_out", [E, D], F32),
    ("cache_cat", [10240, 128, 320], F32), ("page_table", [NS, 64], I32),
    ("c_w_in", [D, 2752], F32), ("c_q_norm", [384], F32), ("c_kv_norm", [256], F32), ("c_w_uq", [384, 16, 192], F32),
    ("c_w_uk", [256, 16, 128], F32), ("c_w_uv", [256, 16, 128], F32), ("c_w_out", [E, D], F32),
    ("state_ssm_re", [NS, 128, 64], F32), ("state_ssm_im", [NS, 128, 64], F32),
    ("d_w_in", [D, 2 * E], F32), ("d_lambda_re", [128, 64], F32), ("d_lambda_im", [128, 64], F32), ("d_log_dt", [128], F32),
    ("d_b_re", [128, 64, 16], F32), ("d_b_im", [128, 64, 16], F32), ("d_c_re", [128, 16, 64], F32), ("d_c_im", [128, 16, 64], F32),
    ("d_d", [E], F32), ("d_w_glu", [E, E], F32), ("d_b_glu", [E], F32), ("d_w_out", [E, D], F32),
]


N_IN_BY_LAYER = {1: 13, 2: 28, 3: 37, 4: 51}


class K:
    pass


def build(n_layers=1):
    nc = bass.Bass("TRN2", target_bir_lowering=False)
    P = Prog()
    k = K()
    k.nc = nc
    k.P = P
    dr = {}
    for name, shp, dt in IN_SHAPES[:N_IN_BY_LAYER[n_layers]]:
        dr[name] = nc.dram_tensor(name, shp, dt, kind="ExternalInput").ap()
    for name, shp in OUT_SHAPES:
        dr[name] = nc.dram_tensor(name, shp, F32, kind="ExternalOutput").ap()
    dr["xscr"] = nc.dram_tensor("xscr", [2, T, D], F32, kind="Internal").ap()
    dr["xscr_s"] = nc.dram_tensor("xscr_s", [NS, D], F32, kind="Internal").ap()
    dr["xscr2"] = nc.dram_tensor("xscr2", [2, T, D], F32, kind="Internal").ap()
    dr["xscr2_s"] = nc.dram_tensor("xscr2_s", [NS, D], F32, kind="Internal").ap()
    dr["gscr"] = nc.dram_tensor("gscr", [16, 128, TWA], BF16, kind="Internal").ap()
    dr["tabscr"] = nc.dram_tensor("tabscr", [64, 128, 6, 64], F32, kind="Internal").ap()
    k.dr = dr
    k.dbuf = {n: Buf("dram_" + n) for n in dr}

    off = [SB0]

    def sb(name, shape, dt, at=None):
        esz = 2 if dt == BF16 else 4
        nbytes = int(np.prod(shape[1:])) * esz
        if at is None:
            o = off[0]
            off[0] += (nbytes + 63) // 64 * 64
            assert off[0] <= SBMAX, (name, off[0])
        else:
            o = at
        t = nc.alloc_sbuf_tensor_at(name, shape, dt, offset=o)
        return t, Buf(name), o

    k.sb = sb
    k.ps = []
    for i in range(7):
        k.ps.append((nc.alloc_psum_tensor(f"ps{i}", [128, 512], F32), Buf(f"ps{i}")))
    k.psT = (nc.alloc_psum_tensor("psT", [128, 1024], BF16), Buf("psT"))
    k.ps_i = [0]

    def psn():
        i = k.ps_i[0] % 7
        k.ps_i[0] += 1
        return k.ps[i]
    k.psn = psn

    k.identF, k.identF_b, _ = sb("identF", [128, 128], F32)
    k.identB, k.identB_b, _ = sb("identB", [128, 128], BF16)
    k.onesB, k.onesB_b, _ = sb("onesB", [128, 128], BF16)
    k.epsr, k.epsr_b, _ = sb("epsr", [128, 4], F32)
    P.op('pool', lambda e: e.memset(k.identF[:], 0.0), writes=[k.identF_b])
    P.op('pool', lambda e: e.affine_select(out=k.identF[:], in_=k.identF[:], pattern=[[-1, 128]],
                                           compare_op=ALU.not_equal, fill=1.0, base=0, channel_multiplier=1),
         reads=[k.identF_b], writes=[k.identF_b])
    P.op('dve', lambda e: e.tensor_copy(out=k.identB[:], in_=k.identF[:]), reads=[k.identF_b], writes=[k.identB_b])
    P.op('dve', lambda e: e.memset(k.onesB[:], 1.0), writes=[k.onesB_b])
    P.op('dve', lambda e: e.memset(k.epsr[:, 0:1], 1e-6), writes=[k.epsr_b])
    P.op('dve', lambda e: e.memset(k.epsr[:, 1:2], 1e-5), writes=[k.epsr_b])
    P.op('dve', lambda e: e.memset(k.epsr[:, 2:3], 64e-5), writes=[k.epsr_b])
    P.op('dve', lambda e: e.memset(k.epsr[:, 3:4], 0.0), writes=[k.epsr_b])

    k.hT, k.hT_b, k.hT_off = sb("hT", [128, 8, TWA + 1], BF16)
    k.GT, k.GT_b, _ = sb("GT", [128, 16, TWA], BF16)
    k.wo = (nc.alloc_sbuf_tensor_at("wo", [128, 16, D], BF16, offset=k.hT_off), k.hT_b)
    k.layer_base = off[0]
    k.gpre, k.gpre_b, _ = sb("gpre", [128, D], F32)
    k.gpost, k.gpost_b = k.gpre, k.gpre_b
    k.xt = [sb(f"xt{i}", [128, D], F32) for i in range(2)]
    hn_par = Buf("hnpar")
    k.hn = []
    for i in range(2):
        t_, _, o_ = sb(f"hn{i}", [128, D], BF16)
        k.hn.append((t_, hn_par.sub(i), o_))
    k.of = nc.alloc_sbuf_tensor_at("of", [128, D], F32, offset=k.hn[0][2])
    k.of_b = hn_par
    k.sqf = lambda np_, h: k.of[0:np_, h * 512:(h + 1) * 512]
    k.sq = sb("sqjunk", [128, D], BF16)
    k.st = [sb(f"stat{i}", [128, 8], F32) for i in range(4)]
    k.st_i = [0]
    k.hf = sb("hf32", [128, D], F32)
    k.off = off

    x_src = (dr["xp"], dr["xs"], k.dbuf["xp"], k.dbuf["xs"])
    wouts = ["a_w_out", "b_w_out", "c_w_out", "d_w_out"]
    for l in range(n_layers):
        x_dst = (dr["y_p"], dr["y_s"], k.dbuf["y_p"], k.dbuf["y_s"]) if (l == n_layers - 1) else \
            ((dr["xscr"], dr["xscr_s"], k.dbuf["xscr"], k.dbuf["xscr_s"]) if l % 2 == 0 else
             (dr["xscr2"], dr["xscr2_s"], k.dbuf["xscr2"], k.dbuf["xscr2_s"]))
        for ps_ in range(2):
            stage1(k, l, ps_, x_src)
            P.barrier()
            off[0] = k.layer_base
            if l == 0:
                layer_conv(k, ps_)
            elif l == 1:
                layer_rwkv(k, ps_)
            elif l == 2:
                layer_mla(k, ps_)
            elif l == 3:
                layer_s5(k, ps_)
            P.barrier()
            stage3(k, l, ps_, x_src, x_dst, dr[wouts[l]], k.dbuf[wouts[l]])
            P.barrier()
        x_src = x_dst
    P.finish('sp')
    k.n_ins = P.emit(nc)
    return nc, k


def ntiles(ps_):
    tl = [(i * 512, 512) for i in range(4)]
    if ps_ == 0:
        tl.append((T, NS))
    return tl


def rstd_from_ss(k, ss_ap, ss_b, out_ap, out_b, npart, inv_n, eps_col):
    P = k.P
    P.op('act', lambda e: e.activation(out=out_ap, in_=ss_ap, func=AF.Sqrt, scale=inv_n,
                                       bias=k.epsr[0:npart, eps_col:eps_col + 1]),
         reads=[ss_b, k.epsr_b], writes=[out_b])
    P.op('dve', lambda e: e.reciprocal(out=out_ap, in_=out_ap), reads=[out_b], writes=[out_b])


def stage1(k, l, ps_, x_src):
    P, dr = k.P, k.dr
    xp, xs, xp_b, xs_b = x_src
    P.op('dve', lambda e: e.memset(k.hT[:, :, 0:1], 0.0), writes=[k.hT_b])
    P.op('sp', lambda e: e.dma_start(out=k.gpre[:], in_=dr["norm_pre"][l:l + 1, :].partition_broadcast(128)),
         writes=[k.gpre_b], dma=True)
    tiles = [(xp[ps_, tt * 128:(tt + 1) * 128, :], 128, tt * 128, xp_b) for tt in range(16)]
    if ps_ == 0:
        tiles.append((xs[:, :], NS, T, xs_b))
    for i, (src, np_, c0, src_b) in enumerate(tiles):
        xt, xt_b, _ = k.xt[i % 2]
        hn, hn_b, _ = k.hn[i % 2]
        sq, sq_b, _ = k.sq
        st, st_b, _ = k.st[k.st_i[0] % 4]
        k.st_i[0] += 1
        P.op('sp', lambda e, xt=xt, src=src, np_=np_: e.dma_start(out=xt[0:np_, :], in_=src),
             reads=[src_b], writes=[xt_b], dma=True)
        P.op('act', lambda e, xt=xt, sq=sq, st=st, np_=np_: e.activation(out=sq[0:np_, :], in_=xt[0:np_, :], func=AF.Square,
                                                                         accum_out=st[0:np_, 0:1]),
             reads=[xt_b], writes=[sq_b, st_b])
        rstd_from_ss(k, st[0:np_, 0:1], st_b, st[0:np_, 1:2], st_b, np_, 1.0 / D, 0)
        P.op('dve', lambda e, hn=hn, xt=xt, st=st, np_=np_: e.scalar_tensor_tensor(
            out=hn[0:np_, :], in0=xt[0:np_, :], scalar=st[0:np_, 1:2], in1=k.gpre[0:np_, :], op0=ALU.mult, op1=ALU.mult),
            reads=[xt_b, st_b, k.gpre_b], writes=[hn_b])
        psT, psT_b = k.psT
        for kk in range(8):
            P.op('pe', lambda e, hn=hn, kk=kk, np_=np_: e.transpose(out=psT[:, kk * 128:kk * 128 + np_],
                                                                    in_=hn[0:np_, kk * 128:(kk + 1) * 128],
                                                                    identity=k.identB[0:np_, 0:np_]),
                 reads=[hn_b, k.identB_b], writes=[psT_b])
        P.op('act', lambda e, c0=c0, np_=np_: e.activation(
            out=k.hT[:, :, c0 + 1:c0 + 1 + np_], in_=psT[:].rearrange("p (k t) -> p k t", k=8)[:, :, 0:np_], func=AF.Copy),
            reads=[psT_b], writes=[k.hT_b.sub(c0 // 128)])
        if l == 1 and (i == 15 or np_ == NS):
            hf, hf_b, _ = k.hf
            P.op('dve', lambda e, hf=hf, xt=xt, st=st, np_=np_: e.scalar_tensor_tensor(
                out=hf[0:np_, :], in0=xt[0:np_, :], scalar=st[0:np_, 1:2], in1=k.gpre[0:np_, :], op0=ALU.mult, op1=ALU.mult),
                reads=[xt_b, st_b, k.gpre_b], writes=[hf_b])
            if np_ == NS:
                P.op('sp', lambda e, hf=hf: e.dma_start(out=dr["shift_s"][:, :], in_=hf[0:NS, :]), reads=[hf_b],
                     writes=[k.dbuf["shift_s"]], dma=True)
            else:
                P.op('sp', lambda e, hf=hf: e.dma_start(out=dr["shift_p"][ps_:ps_ + 1, :], in_=hf[127:128, :]), reads=[hf_b],
                     writes=[k.dbuf["shift_p"]], dma=True)


def hT_reads(k, c0, w):
    return [k.hT_b.sub(c) for c in range(c0 // 128, (c0 + w + 127) // 128)]


def load_w_cols(k, wt, wt_b, w_dram, w_dram_b, col0, ncols=128, nk=8):
    src = w_dram[:, col0:col0 + ncols].rearrange("(k p) e -> p k e", p=128)
    k.P.op('pool', lambda e: e.dma_start(out=wt[:, 0:nk, 0:ncols], in_=src), reads=[w_dram_b], writes=[wt_b], dma=True)


def layer_conv(k, ps_):
    P, dr, nc, sb = k.P, k.dr, k.nc, k.sb
    TW = TWA if ps_ == 0 else T
    if not hasattr(k, "cv"):
        cv = K()
        k.cv = cv
        cv.wt = [[sb(f"cvw{j}_{i}", [128, 8, 128], BF16) for i in range(2)] for j in range(2)]
        cv.wt.append(cv.wt[0])
        cv.cw = sb("cv_cw", [128, 16, 31], F32)
        cv.bin = sb("cv_bin", [128, 48], F32)
        cv.cb = sb("cv_cb", [128, 16], F32)
        cv.lg = sb("cv_lg", [128, 16], F32)
        cv.lb = sb("cv_lb", [128, 16], F32)
        cv.dg = sb("cv_dg", [128, 31, 128], BF16)
        cv.u = sb("cv_u", [128, 30 + T], BF16)
        cv.us = sb("cv_us", [128, NS], F32)
        cv.uf = [sb(f"cv_uf{i}", [128, 512], F32) for i in range(2)]
        cv.sg = [sb(f"cv_sg{i}", [128, 512], F32) for i in range(2)]
        cv.mean = sb("cv_mean", [128, TWA], F32)
        cv.rstd = sb("cv_rstd", [128, TWA], F32)
        cv.tmp = [cv.uf[0], cv.sg[0], cv.uf[1]]
        cv.csq = [(nc.alloc_sbuf_tensor_at(f"cv_csq{i}", [128, 512], BF16, offset=cv.sg[1][2] + i * 1024), cv.sg[1][1].sub(i), 0)
                  for i in range(2)]
        cv.cpt = (nc.alloc_sbuf_tensor_at("cv_cpt", [32, E], F32, offset=cv.mean[2]), cv.mean[1], 0)
        cv.ust = (nc.alloc_sbuf_tensor_at("cv_ust", [16, E], F32, offset=cv.rstd[2]), cv.rstd[1], 0)
        cv.strow = [sb(f"cv_strow{i}", [128, 128], F32) for i in range(4)]
        cv.stT = sb("cv_stT", [128, 480], F32)
        cv.prod = sb("cv_prod", [128, 480], F32)
        cv.cs = sb("cv_cs", [128, NS], F32)
    cv = k.cv
    if True:
        for c_ in range(16):
            P.op('sp', lambda e, c_=c_: e.dma_start(out=cv.cw[0][:, c_, :],
                                                    in_=dr["a_conv_w"][:, c_ * 128:(c_ + 1) * 128].rearrange("k p -> p k"),
                                                    allow_slow_non_contiguous=True), writes=[cv.cw[1]], dma=True)
        P.op('sp', lambda e: e.dma_start(out=cv.bin[0][:], in_=dr["a_b_in"].rearrange("(c p) -> p c", p=128),
                                         allow_slow_non_contiguous=True), writes=[cv.bin[1]], dma=True)
        for t_, nm in ((cv.cb, "a_conv_b"), (cv.lg, "a_ln_g"), (cv.lb, "a_ln_b")):
            P.op('sp', lambda e, t_=t_, nm=nm: e.dma_start(out=t_[0][:], in_=dr[nm].rearrange("(c p) -> p c", p=128),
                                                           allow_slow_non_contiguous=True), writes=[t_[1]], dma=True)
        P.op('dve', lambda e: e.memset(cv.u[0][:, 0:30], 0.0), writes=[cv.u[1].sub('pad')])
    cv = k.cv
    tiles = ntiles(ps_)
    u, u_b, _ = cv.u
    a_w_in, a_w_in_b = dr["a_w_in"], k.dbuf["a_w_in"]
    for ec in range(16):
        wa, wa_b, _ = cv.wt[0][ec % 2]
        wb, wb_b, _ = cv.wt[1][ec % 2]
        load_w_cols(k, wa, wa_b, a_w_in, a_w_in_b, ec * 128)
        load_w_cols(k, wb, wb_b, a_w_in, a_w_in_b, E + ec * 128)
        dg, dg_b, _ = cv.dg
        for kk in range(31):
            P.op('dve', lambda e, kk=kk, ec=ec: e.tensor_scalar(out=dg[:, kk, :], in0=k.identF[:], scalar1=cv.cw[0][:, ec, kk:kk + 1],
                                                                scalar2=None, op0=ALU.mult),
                 reads=[k.identF_b, cv.cw[1]], writes=[dg_b.sub(kk)])
        for ti, (c0, w) in enumerate(tiles):
            pa, pa_b = k.psn()
            pb, pb_b = k.psn()
            for kk in range(8):
                P.op('pe', lambda e, kk=kk, c0=c0, w=w, pa=pa, wa=wa: e.matmul(pa[:, 0:w], lhsT=wa[:, kk, :], rhs=k.hT[:, kk, c0 + 1:c0 + 1 + w],
                                                                              start=(kk == 0), stop=(kk == 7)),
                     reads=[wa_b] + hT_reads(k, c0, w), writes=[pa_b])
            for kk in range(8):
                P.op('pe', lambda e, kk=kk, c0=c0, w=w, pb=pb, wb=wb: e.matmul(pb[:, 0:w], lhsT=wb[:, kk, :], rhs=k.hT[:, kk, c0 + 1:c0 + 1 + w],
                                                                              start=(kk == 0), stop=(kk == 7)),
                     reads=[wb_b] + hT_reads(k, c0, w), writes=[pb_b])
            sg, sg_b, _ = cv.sg[ti % 2]
            uf, uf_b, _ = cv.uf[ti % 2]
            P.op('act', lambda e, sg=sg, pb=pb, w=w, ec=ec: e.activation(out=sg[:, 0:w], in_=pb[:, 0:w], func=AF.Sigmoid,
                                                                        bias=cv.bin[0][:, 16 + ec:17 + ec]),
                 reads=[pb_b, cv.bin[1]], writes=[sg_b])
            if w == 512:
                P.op('dve', lambda e, uf=uf, pa=pa, sg=sg, ec=ec: e.scalar_tensor_tensor(
                    out=uf[:], in0=pa[:], scalar=cv.bin[0][:, ec:ec + 1], in1=sg[:], op0=ALU.add, op1=ALU.mult),
                    reads=[pa_b, sg_b, cv.bin[1]], writes=[uf_b])
                P.op('act', lambda e, uf=uf, c0=c0: e.activation(out=u[:, 30 + c0:30 + c0 + 512], in_=uf[:], func=AF.Copy),
                     reads=[uf_b], writes=[u_b.sub(ti)])
                if ti == 3:
                    pt, pt_b = k.psn()
                    P.op('pe', lambda e, uf=uf, pt=pt: e.transpose(out=pt[0:30, 0:128], in_=uf[:, 482:512], identity=k.identF[:]),
                         reads=[uf_b, k.identF_b], writes=[pt_b])
                    P.op('dve', lambda e, pt=pt, ec=ec: e.tensor_copy(out=cv.cpt[0][0:30, ec * 128:(ec + 1) * 128], in_=pt[0:30, 0:128]),
                         reads=[pt_b], writes=[cv.cpt[1].sub(ec)])
            else:
                us, us_b, _ = cv.us
                P.op('dve', lambda e, pa=pa, sg=sg, ec=ec: e.scalar_tensor_tensor(
                    out=us[:, :], in0=pa[:, 0:NS], scalar=cv.bin[0][:, ec:ec + 1], in1=sg[:, 0:NS], op0=ALU.add, op1=ALU.mult),
                    reads=[pa_b, sg_b, cv.bin[1]], writes=[us_b])
        for ti in range(4):
            c0 = ti * 512
            pc, pc_b = k.psn()
            rd = [u_b.sub(ti), u_b.sub('pad')] + ([u_b.sub(ti - 1)] if ti > 0 else [])
            for kk in range(31):
                P.op('pe', lambda e, kk=kk, c0=c0, pc=pc: e.matmul(pc[:, :], lhsT=dg[:, kk, :], rhs=u[:, c0 + kk:c0 + kk + 512],
                                                                   start=(kk == 0), stop=(kk == 30)),
                     reads=[dg_b.sub(kk)] + rd, writes=[pc_b])
            P.op('act', lambda e, pc=pc, c0=c0, ec=ec: e.activation(out=k.GT[:, ec, c0:c0 + 512], in_=pc[:, :], func=AF.Identity,
                                                                   bias=cv.cb[0][:, ec:ec + 1]),
                 reads=[pc_b, cv.cb[1]], writes=[k.GT_b.sub(ec).sub(ti)])
        if ps_ == 0:
            conv_sample(k, ec)
    P.op('sp', lambda e: e.dma_start(out=dr["conv_p"][ps_, :, :], in_=cv.cpt[0][0:30, :]), reads=[cv.cpt[1]],
         writes=[k.dbuf["conv_p"]], dma=True)
    if ps_ == 0:
        P.op('sp', lambda e: e.dma_start(out=dr["conv_s"][:, 29, :], in_=cv.ust[0][:, :]), reads=[cv.ust[1]],
             writes=[k.dbuf["conv_s"]], dma=True)
        P.op('sp', lambda e: e.dma_start(out=dr["conv_s"][:, 0:29, :], in_=dr["state_conv"][:, 1:30, :]),
             reads=[k.dbuf["state_conv"]], writes=[k.dbuf["conv_s"]], dma=True)
    mean, mean_b, _ = cv.mean
    rstd, rstd_b, _ = cv.rstd
    for ti, (c0, w) in enumerate(tiles):
        p1, p1_b = k.psn()
        p2, p2_b = k.psn()
        for ec in range(16):
            P.op('pe', lambda e, ec=ec, c0=c0, w=w, p1=p1: e.matmul(p1[:, 0:w], lhsT=k.onesB[:], rhs=k.GT[:, ec, c0:c0 + w],
                                                                   start=(ec == 0), stop=(ec == 15)),
                 reads=[k.onesB_b, k.GT_b.sub(ec).sub(ti)], writes=[p1_b])
        for ec in range(16):
            cq, cq_b, _ = cv.csq[ec % 2]
            P.op('act', lambda e, ec=ec, c0=c0, w=w, cq=cq: e.activation(out=cq[:, 0:w], in_=k.GT[:, ec, c0:c0 + w], func=AF.Square),
                 reads=[k.GT_b.sub(ec).sub(ti)], writes=[cq_b])
            P.op('pe', lambda e, ec=ec, w=w, p2=p2, cq=cq: e.matmul(p2[:, 0:w], lhsT=k.onesB[:], rhs=cq[:, 0:w],
                                                                   start=(ec == 0), stop=(ec == 15)),
                 reads=[k.onesB_b, cq_b], writes=[p2_b])
        tm, tm_b, _ = cv.tmp[0]
        P.op('act', lambda e, c0=c0, w=w, p1=p1: e.activation(out=mean[:, c0:c0 + w], in_=p1[:, 0:w], func=AF.Copy, scale=1.0 / E),
             reads=[p1_b], writes=[mean_b.sub(ti)])
        P.op('act', lambda e, w=w, p1=p1: e.activation(out=tm[:, 0:w], in_=p1[:, 0:w], func=AF.Square, scale=1.0 / E),
             reads=[p1_b], writes=[tm_b])
        P.op('dve', lambda e, c0=c0, w=w, p2=p2: e.scalar_tensor_tensor(out=rstd[:, c0:c0 + w], in0=p2[:, 0:w], scalar=1.0 / E,
                                                                        in1=tm[:, 0:w], op0=ALU.mult, op1=ALU.subtract),
             reads=[p2_b, tm_b], writes=[rstd_b.sub(ti)])
        P.op('act', lambda e, c0=c0, w=w: e.activation(out=rstd[:, c0:c0 + w], in_=rstd[:, c0:c0 + w], func=AF.Sqrt,
                                                       bias=k.epsr[:, 1:2]),
             reads=[rstd_b.sub(ti), k.epsr_b], writes=[rstd_b.sub(ti)])
        P.op('dve', lambda e, c0=c0, w=w: e.reciprocal(out=rstd[:, c0:c0 + w], in_=rstd[:, c0:c0 + w]),
             reads=[rstd_b.sub(ti)], writes=[rstd_b.sub(ti)])
    for ec in range(16):
        wz, wz_b, _ = cv.wt[2][ec % 2]
        load_w_cols(k, wz, wz_b, a_w_in, a_w_in_b, 2 * E + ec * 128)
        for ti, (c0, w) in enumerate(tiles):
            pz, pz_b = k.psn()
            for kk in range(8):
                P.op('pe', lambda e, kk=kk, c0=c0, w=w, pz=pz, wz=wz: e.matmul(pz[:, 0:w], lhsT=wz[:, kk, :], rhs=k.hT[:, kk, c0 + 1:c0 + 1 + w],
                                                                              start=(kk == 0), stop=(kk == 7)),
                     reads=[wz_b] + hT_reads(k, c0, w), writes=[pz_b])
            t0, t0_b, _ = cv.tmp[1]
            t1, t1_b, _ = cv.tmp[2]
            gb = k.GT_b.sub(ec).sub(ti)
            P.op('dve', lambda e, ec=ec, c0=c0, w=w: e.tensor_tensor(out=t0[:, 0:w], in0=k.GT[:, ec, c0:c0 + w], in1=mean[:, c0:c0 + w],
                                                                    op=ALU.subtract),
                 reads=[gb, mean_b.sub(ti)], writes=[t0_b])
            P.op('dve', lambda e, c0=c0, w=w: e.tensor_tensor(out=t0[:, 0:w], in0=t0[:, 0:w], in1=rstd[:, c0:c0 + w], op=ALU.mult),
                 reads=[t0_b, rstd_b.sub(ti)], writes=[t0_b])
            P.op('act', lambda e, ec=ec, w=w: e.activation(out=t0[:, 0:w], in_=t0[:, 0:w], func=AF.Silu,
                                                           scale=cv.lg[0][:, ec:ec + 1], bias=cv.lb[0][:, ec:ec + 1]),
                 reads=[t0_b, cv.lg[1], cv.lb[1]], writes=[t0_b])
            P.op('act', lambda e, ec=ec, w=w, pz=pz: e.activation(out=t1[:, 0:w], in_=pz[:, 0:w], func=AF.Silu,
                                                                  bias=cv.bin[0][:, 32 + ec:33 + ec]),
                 reads=[pz_b, cv.bin[1]], writes=[t1_b])
            P.op('dve', lambda e, ec=ec, c0=c0, w=w: e.tensor_tensor(out=k.GT[:, ec, c0:c0 + w], in0=t0[:, 0:w], in1=t1[:, 0:w], op=ALU.mult),
                 reads=[t0_b, t1_b], writes=[gb])


def conv_sample(k, ec):
    P, dr, cv = k.P, k.dr, k.cv
    us, us_b, _ = cv.us
    stT, stT_b, _ = cv.stT
    pt, pt_b = k.psn()
    for r in range(4):
        nr = 128 if r < 3 else 96
        sr, sr_b, _ = cv.strow[r]
        src = dr["state_conv"].rearrange("s k e -> (s k) e")[r * 128:r * 128 + nr, ec * 128:(ec + 1) * 128]
        P.op('sp', lambda e, sr=sr, src=src, nr=nr: e.dma_start(out=sr[0:nr, :], in_=src), reads=[k.dbuf["state_conv"]],
             writes=[sr_b], dma=True)
        P.op('pe', lambda e, sr=sr, nr=nr, r=r, pt=pt: e.transpose(out=pt[:, r * 128:r * 128 + nr], in_=sr[0:nr, :],
                                                                  identity=k.identF[0:nr, 0:nr]),
             reads=[sr_b, k.identF_b], writes=[pt_b])
    P.op('dve', lambda e, pt=pt: e.tensor_copy(out=stT[:, :], in_=pt[:, 0:480]), reads=[pt_b], writes=[stT_b])
    prod, prod_b, _ = cv.prod
    cs, cs_b, _ = cv.cs
    P.op('dve', lambda e, ec=ec: e.tensor_tensor(out=prod[:, :].rearrange("p (s k) -> p s k", k=30),
                                                 in0=stT[:, :].rearrange("p (s k) -> p s k", k=30),
                                                 in1=cv.cw[0][:, ec:ec + 1, 0:30].to_broadcast([128, NS, 30]), op=ALU.mult),
         reads=[stT_b, cv.cw[1]], writes=[prod_b])
    P.op('dve', lambda e: e.tensor_reduce(out=cs[:, :], in_=prod[:, :].rearrange("p (s k) -> p s k", k=30), axis=AX.X, op=ALU.add),
         reads=[prod_b], writes=[cs_b])
    P.op('dve', lambda e, ec=ec: e.scalar_tensor_tensor(out=cs[:, :], in0=us[:, :], scalar=cv.cw[0][:, ec, 30:31], in1=cs[:, :],
                                                        op0=ALU.mult, op1=ALU.add),
         reads=[us_b, cs_b, cv.cw[1]], writes=[cs_b])
    P.op('act', lambda e, ec=ec: e.activation(out=k.GT[:, ec, T:T + NS], in_=cs[:, :], func=AF.Identity, bias=cv.cb[0][:, ec:ec + 1]),
         reads=[cs_b, cv.cb[1]], writes=[k.GT_b.sub(ec).sub(4)])
    p2, p2_b = k.psn()
    P.op('pe', lambda e, p2=p2: e.transpose(out=p2[0:NS, 0:128], in_=us[:, :], identity=k.identF[:]),
         reads=[us_b, k.identF_b], writes=[p2_b])
    P.op('dve', lambda e, p2=p2, ec=ec: e.tensor_copy(out=cv.ust[0][:, ec * 128:(ec + 1) * 128], in_=p2[0:NS, 0:128]),
         reads=[p2_b], writes=[cv.ust[1].sub(ec)])


def stage3(k, l, ps_, x_src, x_dst, w_out, w_out_b):
    P, dr = k.P, k.dr
    xp, xs, xp_b, xs_b = x_src
    yp, ys, yp_b, ys_b = x_dst
    wo, wo_b = k.wo
    P.op('sp', lambda e: e.dma_start(out=k.gpost[:], in_=dr["norm_post"][l:l + 1, :].partition_broadcast(128)),
         writes=[k.gpost_b], dma=True)
    for ec in range(16):
        P.op('pool', lambda e, ec=ec: e.dma_start(out=wo[:, ec, :], in_=w_out[ec * 128:(ec + 1) * 128, :]),
             reads=[w_out_b], writes=[wo_b], dma=True)
    tiles = [(xp[ps_, tt * 128:(tt + 1) * 128, :], yp[ps_, tt * 128:(tt + 1) * 128, :], 128, tt * 128, xp_b, yp_b) for tt in range(16)]
    if ps_ == 0:
        tiles.append((xs[:, :], ys[:, :], NS, T, xs_b, ys_b))
    for i, (src, dst, np_, c0, src_b, dst_b) in enumerate(tiles):
        xt, xt_b, _ = k.xt[i % 2]
        hn, hn_b, _ = k.hn[i % 2]
        sq, sq_b, _ = k.sq
        st, st_b, _ = k.st[k.st_i[0] % 4]
        k.st_i[0] += 1
        P.op('sp', lambda e, xt=xt, src=src, np_=np_: e.dma_start(out=xt[0:np_, :], in_=src),
             reads=[src_b], writes=[xt_b], dma=True)
        pp = [k.psn(), k.psn()]
        gtr = [k.GT_b.sub(ec).sub(min(c0 // 512, 4)) for ec in range(16)]
        for h in range(2):
            ph, ph_b = pp[h]
            for ec in range(16):
                P.op('pe', lambda e, ec=ec, h=h, ph=ph, c0=c0, np_=np_: e.matmul(ph[0:np_, :], lhsT=k.GT[:, ec, c0:c0 + np_],
                                                                                rhs=wo[:, ec, h * 512:(h + 1) * 512],
                                                                                start=(ec == 0), stop=(ec == 15)),
                     reads=[wo_b, gtr[ec]], writes=[ph_b])
            P.op('act', lambda e, h=h, ph=ph, st=st, np_=np_: e.activation(out=sq[0:np_, 0:512], in_=ph[0:np_, :], func=AF.Square,
                                                                          accum_out=st[0:np_, 2 + h:3 + h]),
                 reads=[ph_b], writes=[sq_b, st_b])
        P.op('dve', lambda e, st=st, np_=np_: e.tensor_tensor(out=st[0:np_, 4:5], in0=st[0:np_, 2:3], in1=st[0:np_, 3:4], op=ALU.add),
             reads=[st_b], writes=[st_b])
        rstd_from_ss(k, st[0:np_, 4:5], st_b, st[0:np_, 5:6], st_b, np_, 1.0 / D, 0)
        for h in range(2):
            ph, ph_b = pp[h]
            P.op('dve', lambda e, h=h, ph=ph, st=st, np_=np_, hn=hn: e.scalar_tensor_tensor(
                out=k.sqf(np_, h), in0=ph[0:np_, :], scalar=st[0:np_, 5:6], in1=k.gpost[0:np_, h * 512:(h + 1) * 512],
                op0=ALU.mult, op1=ALU.mult),
                reads=[ph_b, st_b, k.gpost_b], writes=[k.of_b])
        P.op('dve', lambda e, xt=xt, np_=np_: e.tensor_tensor(out=xt[0:np_, :], in0=xt[0:np_, :], in1=k.of[0:np_, :], op=ALU.add),
             reads=[xt_b, k.of_b], writes=[xt_b])
        P.op('sp', lambda e, xt=xt, dst=dst, np_=np_: e.dma_start(out=dst, in_=xt[0:np_, :]),
             reads=[xt_b], writes=[dst_b], dma=True)

def op_tt(k, eng, out, in0, in1, op, reads, writes):
    return k.P.op(eng, lambda e: e.tensor_tensor(out=out, in0=in0, in1=in1, op=op), reads, writes)


def op_ts(k, eng, out, in0, s1, s2, op0, op1, reads, writes):
    if s2 is None:
        return k.P.op(eng, lambda e: e.tensor_scalar(out=out, in0=in0, scalar1=s1, scalar2=None, op0=op0), reads, writes)
    return k.P.op(eng, lambda e: e.tensor_scalar(out=out, in0=in0, scalar1=s1, scalar2=s2, op0=op0, op1=op1), reads, writes)


def op_stt(k, eng, out, in0, scalar, in1, op0, op1, reads, writes):
    return k.P.op(eng, lambda e: e.scalar_tensor_tensor(out=out, in0=in0, scalar=scalar, in1=in1, op0=op0, op1=op1), reads, writes)


def op_act(k, out, in_, func, reads, writes, scale=None, bias=None):
    kw = {}
    if scale is not None:
        kw['scale'] = scale
    if bias is not None:
        kw['bias'] = bias
    return k.P.op('act', lambda e: e.activation(out=out, in_=in_, func=func, **kw), reads, writes)


def op_mm(k, out, lhsT, rhs, start, stop, reads, writes):
    return k.P.op('pe', lambda e: e.matmul(out, lhsT=lhsT, rhs=rhs, start=start, stop=stop), reads, writes)


def op_tr(k, out, in_, ident, reads, writes):
    return k.P.op('pe', lambda e: e.transpose(out=out, in_=in_, identity=ident), reads, writes)


def op_dma(k, eng, out, in_, reads, writes, slow=False):
    if slow:
        return k.P.op(eng, lambda e: e.dma_start(out=out, in_=in_, allow_slow_non_contiguous=True), reads, writes, dma=True)
    return k.P.op(eng, lambda e: e.dma_start(out=out, in_=in_), reads, writes, dma=True)


def hT_reads_prev(k, c0, w):
    out = []
    if c0 == 0:
        out.append(k.hT_b.sub('pad'))
    lo = max(c0 - 1, 0) // 128
    hi = (c0 + w - 2) // 128
    for c in range(lo, hi + 1):
        out.append(k.hT_b.sub(c))
    return out


DECAY_C = 0.6065306597126334


def rwkv_setup(k):
    P, dr, nc, sb = k.P, k.dr, k.nc, k.sb
    rw = K()
    k.rw = rw
    rw.W = [[sb(f"rwW{j}{ab}", [128, 8, 128], BF16) for ab in range(2)] for j in range(4)]
    rw.wraw = sb("rw_wraw", [128, 8, 128], BF16)
    rw.mu = sb("rw_mu", [128, 6, 8], F32)
    rw.omu = sb("rw_omu", [128, 6, 8], F32)
    rw.par = {n: sb("rw_p_" + n, [128, 16], F32) for n in ["w0", "a0", "k_k", "k_a", "omk_a", "r_k", "ln_g", "ln_b"]}
    rw.lw = [sb(f"rw_lw{i}", [128, 8, 128], BF16) for i in range(2)]
    rw.w2a2 = sb("rw_w2a2", [128, E], BF16)
    rw.lora = sb("rw_lora", [128, TWA], BF16)
    rw.mscan = sb("rw_mscan", [128, 512], F32)
    rw.mk = {n: sb("rw_mk_" + n, [128, 512], BF16) for n in ["nUs", "nLs", "Us", "Ui"]}
    rw.irep = sb("rw_irep", [128, 64], F32)
    rw.boB = sb("rw_boB", [128, 128], BF16)
    rw.boF = sb("rw_boF", [128, 128], F32)
    rw.f = [sb(f"rw_f{i}", [128, 512], F32) for i in range(13)]
    rw.h16 = {n: sb("rw_h_" + n, [128, 512], BF16) for n in ["KT", "RT", "BT", "KKT", "VT", "ZS", "YB", "YQ"]}
    rw.tok = {n: sb("rw_tok_" + n, [128, 8, 64], BF16) for n in ["B", "K", "V"]}
    rw.g = {n: sb("rw_g_" + n, [128, 512], BF16) for n in ["Inv", "AkT", "BrT", "KrT"]}
    rw.Pp = [sb(f"rw_P{i}", [128, 512], BF16) for i in range(2)]
    rw.Qp = [sb(f"rw_Q{i}", [128, 512], BF16) for i in range(2)]
    rw.S = sb("rw_S", [128, 512], F32)
    rw.Sbf = sb("rw_Sbf", [128, 512], BF16)
    rw.tokKap = sb("rw_tokKap", [128, 8, 64], BF16)
    rw.M1Tn = sb("rw_M1Tn", [128, 512], BF16)
    rw.AVsb = sb("rw_AVsb", [128, 8, 64], BF16)
    rw.Wn = sb("rw_Wn", [128, 8, 64], BF16)
    rw.Xsb = sb("rw_Xsb", [128, 64], BF16)
    rw.Usb = sb("rw_Usb", [128, 64], BF16)
    rw.H = sb("rw_H", [128, 64], F32)
    rw.Hbf = sb("rw_Hbf", [128, 64], BF16)
    rw.TH = sb("rw_TH", [128, 64], F32)
    rw.shT = sb("rw_shT", [128, 8, NS], BF16)
    rw.shrow = sb("rw_shrow", [NS, D], F32)
    rw.small = sb("rw_small", [128, 64], F32)


def rwkv_load(k):
    P, dr, nc, rw = k.P, k.dr, k.nc, k.rw
    for j in range(6):
        op_dma(k, 'sp', rw.mu[0][:, j, :], dr["b_mu"][j].rearrange("(k p) -> p k", p=128), [k.dbuf["b_mu"]], [rw.mu[1]], slow=True)
    op_ts(k, 'dve', rw.omu[0][:], rw.mu[0][:], -1.0, 1.0, ALU.mult, ALU.add, [rw.mu[1]], [rw.omu[1]])
    for n, src in [("w0", "b_w0"), ("a0", "b_a0"), ("k_k", "b_k_k"), ("k_a", "b_k_a"), ("ln_g", "b_ln_g"), ("ln_b", "b_ln_b")]:
        op_dma(k, 'sp', rw.par[n][0][:], dr[src].rearrange("(c p) -> p c", p=128), [k.dbuf[src]], [rw.par[n][1]], slow=True)
    op_dma(k, 'sp', rw.par["r_k"][0][:], dr["b_r_k"].rearrange("(c h2) j -> (h2 j) c", h2=2), [k.dbuf["b_r_k"]],
           [rw.par["r_k"][1]], slow=True)
    op_ts(k, 'dve', rw.par["omk_a"][0][:], rw.par["k_a"][0][:], -1.0, 1.0, ALU.mult, ALU.add, [rw.par["k_a"][1]], [rw.par["omk_a"][1]])
    wr, wr_b, _ = rw.wraw
    op_dma(k, 'pool', wr[:, :, 0:64], dr["b_w1"].rearrange("(k p) e -> p k e", p=128), [k.dbuf["b_w1"]], [wr_b])
    op_dma(k, 'pool', wr[:, :, 64:128], dr["b_a1"].rearrange("(k p) e -> p k e", p=128), [k.dbuf["b_a1"]], [wr_b])
    for half, j in ((0, 4), (1, 5)):
        cs_ = slice(half * 64, half * 64 + 64)
        op_tt(k, 'dve', rw.lw[0][0][:, :, cs_], wr[:, :, cs_], rw.omu[0][:, j, :].unsqueeze(2).to_broadcast([128, 8, 64]), ALU.mult,
              [wr_b, rw.omu[1]], [rw.lw[0][1]])
        op_tt(k, 'dve', rw.lw[1][0][:, :, cs_], wr[:, :, cs_], rw.mu[0][:, j, :].unsqueeze(2).to_broadcast([128, 8, 64]), ALU.mult,
              [wr_b, rw.mu[1]], [rw.lw[1][1]])
    op_dma(k, 'pool', rw.w2a2[0][0:64, :], dr["b_w2"], [k.dbuf["b_w2"]], [rw.w2a2[1]])
    op_dma(k, 'pool', rw.w2a2[0][64:128, :], dr["b_a2"], [k.dbuf["b_a2"]], [rw.w2a2[1]])
    ms, ms_b, _ = rw.mscan
    P.op('dve', lambda e: e.memset(ms[:], 1.0), writes=[ms_b])
    P.op('dve', lambda e: e.memset(ms[:].rearrange("p (c t) -> p c t", t=64)[:, :, 0:1], 0.0), writes=[ms_b])
    for n, val, cm, pat, cmp_ in [("nUs", -1.0, -1, 1, ALU.is_gt), ("Us", 1.0, -1, 1, ALU.is_gt), ("Ui", 1.0, -1, 1, ALU.is_ge),
                                  ("nLs", -1.0, 1, -1, ALU.is_gt)]:
        t_, b_, _ = rw.mk[n]
        tf, tf_b, _ = rw.f[0]
        P.op('pool', lambda e, tf=tf, val=val: e.memset(tf[0:64, :], val), writes=[tf_b])
        P.op('pool', lambda e, tf=tf, cm=cm, pat=pat, cmp_=cmp_: e.affine_select(
            out=tf[0:64, :].rearrange("p (c t) -> p c t", t=64), in_=tf[0:64, :].rearrange("p (c t) -> p c t", t=64),
            pattern=[[0, 8], [pat, 64]], compare_op=cmp_, fill=0.0, base=0, channel_multiplier=cm),
            reads=[tf_b], writes=[tf_b])
        P.op('dve', lambda e, t_=t_, tf=tf: e.tensor_copy(out=t_[0:64, :], in_=tf[0:64, :]), reads=[tf_b], writes=[b_])
        op_dma(k, 'sp', t_[64:128, :], t_[0:64, :], [b_], [b_])
    op_tt(k, 'dve', rw.irep[0][:], k.identF[:, 0:64], k.identF[:, 64:128], ALU.add, [k.identF_b], [rw.irep[1]])
    for t_, b_, _ in (rw.boB, rw.boF):
        P.op('dve', lambda e, t_=t_: e.memset(t_[:], 0.0), writes=[b_])
        P.op('dve', lambda e, t_=t_: e.memset(t_[0:64, 0:64], 1.0), writes=[b_])
        P.op('dve', lambda e, t_=t_: e.memset(t_[64:128, 64:128], 1.0), writes=[b_])


def layer_rwkv(k, ps_):
    P, dr, nc = k.P, k.dr, k.nc
    if not hasattr(k, "rw"):
        rwkv_setup(k)
    rwkv_load(k)
    rw = k.rw
    P.chk("setup")
    tiles = ntiles(ps_)
    lora, lora_b, _ = rw.lora
    if ps_ == 0:
        sr, sr_b, _ = rw.shrow
        op_dma(k, 'sp', sr[:, :], dr["state_shift"], [k.dbuf["state_shift"]], [sr_b])
        pt, pt_b = k.psn()
        for kk in range(8):
            op_tr(k, pt[:, kk * NS:(kk + 1) * NS], sr[0:NS, kk * 128:(kk + 1) * 128], k.identF[0:NS, 0:NS], [sr_b, k.identF_b], [pt_b])
        op_act(k, rw.shT[0][:, :, :], pt[:, 0:8 * NS].rearrange("p (k s) -> p k s", s=NS), AF.Copy, [pt_b], [rw.shT[1]])

    def prev_rhs(kk, c0, w):
        if c0 >= T:
            return rw.shT[0][:, kk, :], [rw.shT[1]]
        return k.hT[:, kk, c0:c0 + w], hT_reads_prev(k, c0, w)

    for ti, (c0, w) in enumerate(tiles):
        pl, pl_b = k.psn()
        for kk in range(8):
            op_mm(k, pl[:, 0:w], rw.lw[0][0][:, kk, :], k.hT[:, kk, c0 + 1:c0 + 1 + w], kk == 0, False,
                  [rw.lw[0][1]] + hT_reads(k, c0, w), [pl_b])
        for kk in range(8):
            r_, rb_ = prev_rhs(kk, c0, w)
            op_mm(k, pl[:, 0:w], rw.lw[1][0][:, kk, :], r_, False, kk == 7, [rw.lw[1][1]] + rb_, [pl_b])
        op_act(k, lora[0:64, c0:c0 + w], pl[0:64, 0:w], AF.Tanh, [pl_b], [lora_b.sub(ti)])
        op_act(k, lora[64:128, c0:c0 + w], pl[64:128, 0:w], AF.Copy, [pl_b], [lora_b.sub(ti)])

    P.chk("lora")
    F = rw.f
    for hp in range(16):
        hc = slice(hp * 128, (hp + 1) * 128)
        wr, wr_b, _ = rw.wraw
        for j in range(4):
            op_dma(k, 'pool', wr[:, :, :], dr["b_w_rkvz"][j][:, hc].rearrange("(k p) e -> p k e", p=128), [k.dbuf["b_w_rkvz"]], [wr_b])
            op_tt(k, 'dve', rw.W[j][0][0][:], wr[:], rw.omu[0][:, j, :].unsqueeze(2).to_broadcast([128, 8, 128]), ALU.mult,
                  [wr_b, rw.omu[1]], [rw.W[j][0][1]])
            op_tt(k, 'pool', rw.W[j][1][0][:], wr[:], rw.mu[0][:, j, :].unsqueeze(2).to_broadcast([128, 8, 128]), ALU.mult,
                  [wr_b, rw.mu[1]], [rw.W[j][1][1]])
        P.chk("w")
        H, H_b, _ = rw.H
        Hbf, Hbf_b, _ = rw.Hbf
        P.op('dve', lambda e: e.memset(H[:], 0.0), writes=[H_b])
        P.op('dve', lambda e: e.memset(Hbf[:], 0.0), writes=[Hbf_b])
        par = lambda n: rw.par[n][0][:, hp:hp + 1]
        parb = lambda n: rw.par[n][1]
        for ti, (c0, w) in enumerate(tiles):
            sample = c0 >= T
            pj = []
            for j in range(4):
                pp, pp_b = k.psn()
                for kk in range(8):
                    op_mm(k, pp[:, 0:w], rw.W[j][0][0][:, kk, :], k.hT[:, kk, c0 + 1:c0 + 1 + w], kk == 0, False,
                          [rw.W[j][0][1]] + hT_reads(k, c0, w), [pp_b])
                for kk in range(8):
                    r_, rb_ = prev_rhs(kk, c0, w)
                    op_mm(k, pp[:, 0:w], rw.W[j][1][0][:, kk, :], r_, False, kk == 7, [rw.W[j][1][1]] + rb_, [pp_b])
                pj.append((pp, pp_b))
            (pr, pr_b), (pk, pk_b), (pv, pv_b), (pz, pz_b) = pj
            P.chk("proj")
            pw, pw_b = k.psn()
            op_mm(k, pw[:, 0:w], rw.w2a2[0][0:64, hc], lora[0:64, c0:c0 + w], True, True, [rw.w2a2[1], lora_b.sub(ti)], [pw_b])
            pa, pa_b = k.psn()
            op_mm(k, pa[:, 0:w], rw.w2a2[0][64:128, hc], lora[64:128, c0:c0 + w], True, True, [rw.w2a2[1], lora_b.sub(ti)], [pa_b])
            P.chk("pwpa")
            W_ = slice(0, w)
            A, SG, TMP, KP, KKF, SQ, Bt, R, V, RK, BON, PP, YN = [F[i] for i in range(13)]
            op_act(k, A[0][:, W_], pa[:, W_], AF.Sigmoid, [pa_b, parb("a0")], [A[1]], bias=par("a0"))
            op_act(k, SG[0][:, W_], pw[:, W_], AF.Sigmoid, [pw_b, parb("w0")], [SG[1]], bias=par("w0"))
            P.chk("e0a")
            op_act(k, TMP[0][:, W_], A[0][:, W_], AF.Identity, [A[1], parb("k_a"), parb("omk_a")], [TMP[1]], scale=par("k_a"), bias=par("omk_a"))
            P.chk("e0b")
            op_tt(k, 'dve', KP[0][:, W_], pk[:, W_], TMP[0][:, W_], ALU.mult, [pk_b, TMP[1]], [KP[1]])
            P.chk("e0c")
            op_ts(k, 'dve', KKF[0][:, W_], pk[:, W_], par("k_k"), None, ALU.mult, None, [pk_b, parb("k_k")], [KKF[1]])
            P.chk("e0d")
            op_act(k, SQ[0][:, W_], KKF[0][:, W_], AF.Square, [KKF[1]], [SQ[1]])
            P.chk("e1")
            pn, pn_b = k.psn()
            op_mm(k, pn[:, W_], rw.boF[0][:], SQ[0][:, W_], True, True, [rw.boF[1], SQ[1]], [pn_b])
            op_act(k, SQ[0][:, W_], pn[:, W_], AF.Sqrt, [pn_b], [SQ[1]])
            op_ts(k, 'dve', SQ[0][:, W_], SQ[0][:, W_], 1e-12, None, ALU.max, None, [SQ[1]], [SQ[1]])
            P.op('dve', lambda e, W_=W_: e.reciprocal(out=SQ[0][:, W_], in_=SQ[0][:, W_]), [SQ[1]], [SQ[1]])
            op_tt(k, 'dve', KKF[0][:, W_], KKF[0][:, W_], SQ[0][:, W_], ALU.mult, [KKF[1], SQ[1]], [KKF[1]])
            op_tt(k, 'dve', Bt[0][:, W_], KKF[0][:, W_], A[0][:, W_], ALU.mult, [KKF[1], A[1]], [Bt[1]])
            P.chk("e2")
            op_act(k, R[0][:, W_], pr[:, W_], AF.Copy, [pr_b], [R[1]])
            op_act(k, V[0][:, W_], pv[:, W_], AF.Copy, [pv_b], [V[1]])
            op_stt(k, 'dve', RK[0][:, W_], R[0][:, W_], par("r_k"), KP[0][:, W_], ALU.mult, ALU.mult, [R[1], KP[1], parb("r_k")], [RK[1]])
            pb, pb_b = k.psn()
            op_mm(k, pb[:, W_], rw.boF[0][:], RK[0][:, W_], True, True, [rw.boF[1], RK[1]], [pb_b])
            op_tt(k, 'dve', BON[0][:, W_], pb[:, W_], V[0][:, W_], ALU.mult, [pb_b, V[1]], [BON[1]])
            ZS = rw.h16["ZS"]
            op_act(k, ZS[0][:, W_], pz[:, W_], AF.Silu, [pz_b], [ZS[1]])
            P.chk("elem")
            if not sample:
                ysrc, ysrc_b = rwkv_chunks(k, hp, ti, c0, A, SG, TMP, KP, KKF, Bt, R, V, PP, pv, pv_b)
            else:
                ysrc, ysrc_b = rwkv_sample(k, hp, SG, KP, KKF, Bt, R, V)
            P.chk("chunks")
            YB, YQ = rw.h16["YB"], rw.h16["YQ"]
            op_act(k, YB[0][:, W_], ysrc[:, W_], AF.Copy, [ysrc_b], [YB[1]])
            op_act(k, YQ[0][:, W_], ysrc[:, W_], AF.Square, [ysrc_b], [YQ[1]])
            pm, pm_b = k.psn()
            pq, pq_b = k.psn()
            op_mm(k, pm[:, W_], rw.boB[0][:], YB[0][:, W_], True, True, [rw.boB[1], YB[1]], [pm_b])
            op_mm(k, pq[:, W_], rw.boB[0][:], YQ[0][:, W_], True, True, [rw.boB[1], YQ[1]], [pq_b])
            MEAN, MSQ, RS = A, SG, TMP
            op_act(k, MEAN[0][:, W_], pm[:, W_], AF.Copy, [pm_b], [MEAN[1]], scale=1.0 / 64)
            op_act(k, MSQ[0][:, W_], pm[:, W_], AF.Square, [pm_b], [MSQ[1]], scale=1.0 / 64)
            op_stt(k, 'dve', RS[0][:, W_], pq[:, W_], 1.0 / 64, MSQ[0][:, W_], ALU.mult, ALU.subtract, [pq_b, MSQ[1]], [RS[1]])
            op_act(k, RS[0][:, W_], RS[0][:, W_], AF.Sqrt, [RS[1], k.epsr_b], [RS[1]], bias=k.epsr[:, 2:3])
            P.op('dve', lambda e, W_=W_, RS=RS: e.reciprocal(out=RS[0][:, W_], in_=RS[0][:, W_]), [RS[1]], [RS[1]])
            op_tt(k, 'dve', YN[0][:, W_], ysrc[:, W_], MEAN[0][:, W_], ALU.subtract, [ysrc_b, MEAN[1]], [YN[1]])
            op_tt(k, 'dve', YN[0][:, W_], YN[0][:, W_], RS[0][:, W_], ALU.mult, [YN[1], RS[1]], [YN[1]])
            op_act(k, YN[0][:, W_], YN[0][:, W_], AF.Identity, [YN[1], parb("ln_g"), parb("ln_b")], [YN[1]], scale=par("ln_g"), bias=par("ln_b"))
            op_tt(k, 'dve', YN[0][:, W_], YN[0][:, W_], BON[0][:, W_], ALU.add, [YN[1], BON[1]], [YN[1]])
            op_tt(k, 'dve', k.GT[:, hp, c0:c0 + w], YN[0][:, W_], ZS[0][:, W_], ALU.mult, [YN[1], ZS[1]], [k.GT_b.sub(hp).sub(ti)])
            P.chk("gn")
            if ti == 3:
                pt, pt_b = k.psn()
                for h in range(2):
                    hP = slice(h * 64, h * 64 + 64)
                    op_mm(k, pt[hP, 0:64], H[hP, :], k.identF[hP, hP], True, True, [H_b, k.identF_b], [pt_b])
                sm, sm_b, _ = rw.small
                P.op('dve', lambda e, pt=pt: e.tensor_copy(out=sm[:, :], in_=pt[:, 0:64]), [pt_b], [sm_b])
                op_dma(k, 'sp', dr["wkv_p"][ps_, 2 * hp:2 * hp + 2, :, :].rearrange("h i j -> (h i) j"), sm[:, :], [sm_b], [k.dbuf["wkv_p"]])


def rwkv_chunks(k, hp, ti, c0, A, SG, TMP, KP, KK, Bt, R, V, PP, pv, pv_b):
    P, rw = k.P, k.rw
    H, H_b, _ = rw.H
    Hbf, Hbf_b, _ = rw.Hbf
    KT, RT, BT, KKT, VT = [rw.h16[n] for n in ["KT", "RT", "BT", "KKT", "VT"]]
    ms = rw.mscan
    CS = TMP
    P.op('dve', lambda e: e.tensor_tensor_scan(out=CS[0][:], data0=ms[0][:], data1=SG[0][:], initial=0.0, op0=ALU.mult, op1=ALU.add),
         [ms[1], SG[1]], [CS[1]])
    op_tt(k, 'dve', SG[0][:], CS[0][:], SG[0][:], ALU.subtract, [CS[1], SG[1]], [SG[1]])
    op_act(k, PP[0][:], CS[0][:], AF.Exp, [CS[1]], [PP[1]], scale=-DECAY_C)
    op_act(k, CS[0][:], CS[0][:], AF.Exp, [CS[1]], [CS[1]], scale=DECAY_C)
    op_act(k, SG[0][:], SG[0][:], AF.Exp, [SG[1]], [SG[1]], scale=-DECAY_C)
    op_tt(k, 'dve', KT[0][:], KK[0][:], SG[0][:], ALU.mult, [KK[1], SG[1]], [KT[1]])
    op_tt(k, 'dve', RT[0][:], R[0][:], PP[0][:], ALU.mult, [R[1], PP[1]], [RT[1]])
    op_tt(k, 'dve', BT[0][:], Bt[0][:], CS[0][:], ALU.mult, [Bt[1], CS[1]], [BT[1]])
    op_tt(k, 'dve', KKT[0][:], KP[0][:], CS[0][:], ALU.mult, [KP[1], CS[1]], [KKT[1]])
    op_act(k, VT[0][:], pv[:, :], AF.Copy, [pv_b], [VT[1]])
    rw.tok["Kap"] = rw.tokKap
    for n, X in (("B", BT), ("K", KKT), ("V", VT), ("Kap", KT)):
        pt, pt_b = k.psn()
        for c in range(8):
            for h in range(2):
                hP = slice(h * 64, h * 64 + 64)
                op_mm(k, pt[hP, c * 64:(c + 1) * 64], X[0][hP, c * 64:(c + 1) * 64], k.identB[hP, hP], True, True,
                      [X[1], k.identB_b], [pt_b])
        op_act(k, rw.tok[n][0][:, :, :], pt[:, 0:512].rearrange("p (c t) -> p c t", t=64), AF.Copy, [pt_b], [rw.tok[n][1]])
    Btok, Ktok, Vtok = rw.tok["B"], rw.tok["K"], rw.tok["V"]
    P.chk("tok")
    grams = [("AbT", BT, KT), ("Ab", KT, BT), ("AkT", KKT, KT), ("BrT", BT, RT), ("KrT", KKT, RT)]
    gps = {}
    for n, L, Rr in grams:
        pg, pg_b = k.psn()
        for c in range(8):
            cs_ = slice(c * 64, (c + 1) * 64)
            for h in range(2):
                hP = slice(h * 64, h * 64 + 64)
                op_mm(k, pg[hP, cs_], L[0][hP, cs_], Rr[0][hP, cs_], True, True, [L[1], Rr[1]], [pg_b])
        gps[n] = (pg, pg_b)
    Pc, Qc = rw.Pp[0], rw.Qp[0]
    S, Sbf = rw.S, rw.Sbf
    op_tt(k, 'dve', Qc[0][:], gps["AbT"][0][:, :], rw.mk["nUs"][0][:], ALU.mult, [gps["AbT"][1], rw.mk["nUs"][1]], [Qc[1]])
    op_tt(k, 'dve', Pc[0][:], gps["Ab"][0][:, :], rw.mk["nLs"][0][:], ALU.mult, [gps["Ab"][1], rw.mk["nLs"][1]], [Pc[1]])
    op_tt(k, 'dve', rw.g["AkT"][0][:], gps["AkT"][0][:, :], rw.mk["Us"][0][:], ALU.mult, [gps["AkT"][1], rw.mk["Us"][1]], [rw.g["AkT"][1]])
    op_tt(k, 'dve', rw.g["BrT"][0][:], gps["BrT"][0][:, :], rw.mk["Ui"][0][:], ALU.mult, [gps["BrT"][1], rw.mk["Ui"][1]], [rw.g["BrT"][1]])
    op_tt(k, 'dve', rw.g["KrT"][0][:], gps["KrT"][0][:, :], rw.mk["Ui"][0][:], ALU.mult, [gps["KrT"][1], rw.mk["Ui"][1]], [rw.g["KrT"][1]])
    op_tt(k, 'dve', S[0][:].rearrange("p (c t) -> p c t", t=64), Qc[0][:].rearrange("p (c t) -> p c t", t=64),
          rw.irep[0][:, :].unsqueeze(1).to_broadcast([128, 8, 64]), ALU.add, [Qc[1], rw.irep[1]], [S[1]])
    op_act(k, Sbf[0][:], S[0][:], AF.Copy, [S[1]], [Sbf[1]])
    for it in range(1, 6):
        Pn, Qn = rw.Pp[it % 2], rw.Qp[it % 2]
        pP, pP_b = k.psn()
        for c in range(8):
            cs_ = slice(c * 64, (c + 1) * 64)
            for h in range(2):
                hP = slice(h * 64, h * 64 + 64)
                op_mm(k, pP[hP, cs_], Qc[0][hP, cs_], Pc[0][hP, cs_], True, True, [Qc[1], Pc[1]], [pP_b])
        if it < 5:
            pQ, pQ_b = k.psn()
            for c in range(8):
                cs_ = slice(c * 64, (c + 1) * 64)
                for h in range(2):
                    hP = slice(h * 64, h * 64 + 64)
                    op_mm(k, pQ[hP, cs_], Pc[0][hP, cs_], Qc[0][hP, cs_], True, True, [Qc[1], Pc[1]], [pQ_b])
        op_act(k, Pn[0][:], pP[:, :], AF.Copy, [pP_b], [Pn[1]])
        if it < 5:
            P.op('dve', lambda e, Qn=Qn, pQ=pQ: e.tensor_copy(out=Qn[0][:], in_=pQ[:, :]), [pQ_b], [Qn[1]])
        pS, pS_b = k.psn()
        for c in range(8):
            cs_ = slice(c * 64, (c + 1) * 64)
            for h in range(2):
                hP = slice(h * 64, h * 64 + 64)
                op_mm(k, pS[hP, cs_], Pn[0][hP, cs_], Sbf[0][hP, cs_], True, True, [Pn[1], Sbf[1]], [pS_b])
        op_tt(k, 'dve', S[0][:], S[0][:], pS[:, :], ALU.add, [S[1], pS_b], [S[1]])
        dst = Sbf if it < 5 else rw.g["Inv"]
        op_act(k, dst[0][:], S[0][:], AF.Copy, [S[1]], [dst[1]])
        Pc, Qc = Pn, Qn
    Inv, AkT, BrT, KrT = [rw.g[n] for n in ["Inv", "AkT", "BrT", "KrT"]]
    tokKap, M1Tn, AVsb, Wn = rw.tokKap, rw.M1Tn, rw.AVsb, rw.Wn
    pm1, pm1_b = k.psn()
    pav, pav_b = k.psn()
    for c in range(8):
        cs_ = slice(c * 64, (c + 1) * 64)
        for h in range(2):
            hP = slice(h * 64, h * 64 + 64)
            op_mm(k, pm1[hP, cs_], tokKap[0][hP, c, :], Inv[0][hP, cs_], True, True, [tokKap[1], Inv[1]], [pm1_b])
            op_mm(k, pav[hP, cs_], AkT[0][hP, cs_], Vtok[0][hP, c, :], True, True, [AkT[1], Vtok[1]], [pav_b])
    op_act(k, M1Tn[0][:, :], pm1[:, :], AF.Copy, [pm1_b], [M1Tn[1]], scale=-1.0)
    P.op('dve', lambda e: e.tensor_copy(out=AVsb[0][:, :, :], in_=pav[:, :].rearrange("p (c i) -> p c i", i=64)), [pav_b], [AVsb[1]])
    pw2, pw2_b = k.psn()
    for c in range(8):
        cs_ = slice(c * 64, (c + 1) * 64)
        for h in range(2):
            hP = slice(h * 64, h * 64 + 64)
            op_mm(k, pw2[hP, cs_], Inv[0][hP, cs_], AVsb[0][hP, c, :], True, True, [Inv[1], AVsb[1]], [pw2_b])
    op_act(k, Wn[0][:, :, :], pw2[:, :].rearrange("p (c i) -> p c i", i=64), AF.Copy, [pw2_b], [Wn[1]], scale=-1.0)
    P.chk("inv")
    py, py_b = k.ps[6]

    def ps6():
        i = k.ps_i[0] % 6
        k.ps_i[0] += 1
        return k.ps[i]
    Xsb, Usb, TH = rw.Xsb, rw.Usb, rw.TH
    op_ts(k, 'dve', TH[0][:, :], H[:, :], PP[0][:, 63:64], None, ALU.mult, None, [H_b, PP[1]], [TH[1]])
    for c in range(8):
        cs_ = slice(c * 64, (c + 1) * 64)
        pu, pu_b = ps6()
        for h in range(2):
            hP = slice(h * 64, h * 64 + 64)
            op_mm(k, pu[hP, 0:64], M1Tn[0][hP, cs_], Hbf[hP, :], True, False, [M1Tn[1], Hbf_b], [pu_b])
            op_mm(k, pu[hP, 0:64], k.identB[hP, hP], Wn[0][hP, c, :], False, True, [k.identB_b, Wn[1]], [pu_b])
        P.op('dve', lambda e, pu=pu: e.tensor_copy(out=Usb[0][:, :], in_=pu[:, 0:64]), [pu_b], [Usb[1]])
        for h in range(2):
            hP = slice(h * 64, h * 64 + 64)
            op_mm(k, py[hP, cs_], Hbf[hP, :], RT[0][hP, cs_], True, False, [Hbf_b, RT[1]], [py_b])
            op_mm(k, py[hP, cs_], Usb[0][hP, :], BrT[0][hP, cs_], False, False, [Usb[1], BrT[1]], [py_b])
            op_mm(k, py[hP, cs_], Vtok[0][hP, c, :], KrT[0][hP, cs_], False, True, [Vtok[1], KrT[1]], [py_b])
        ph, ph_b = ps6()
        for h in range(2):
            hP = slice(h * 64, h * 64 + 64)
            op_mm(k, ph[hP, 0:64], Btok[0][hP, c, :], Usb[0][hP, :], True, False, [Btok[1], Usb[1]], [ph_b])
            op_mm(k, ph[hP, 0:64], Ktok[0][hP, c, :], Vtok[0][hP, c, :], False, True, [Ktok[1], Vtok[1]], [ph_b])
        pc_ap = PP[0][:, c * 64 + 63:c * 64 + 64]
        op_stt(k, 'dve', Hbf[:, :], ph[:, 0:64], pc_ap, TH[0][:, :], ALU.mult, ALU.add, [ph_b, PP[1], TH[1]], [Hbf_b])
        op_stt(k, 'dve', H[:, :], ph[:, 0:64], pc_ap, TH[0][:, :], ALU.mult, ALU.add, [ph_b, PP[1], TH[1]], [H_b])
        if c < 7:
            op_ts(k, 'dve', TH[0][:, :], H[:, :], PP[0][:, (c + 1) * 64 + 63:(c + 1) * 64 + 64], None, ALU.mult, None, [H_b, PP[1]], [TH[1]])
    Ysb = rw.f[5]
    op_act(k, Ysb[0][:, :], py[:, :], AF.Copy, [py_b], [Ysb[1]])
    return Ysb[0], Ysb[1]


def rwkv_sample(k, hp, SG, KP, KK, Bt, R, V):
    P, rw, dr = k.P, k.rw, k.dr
    F = rw.f
    Wd = F[2]
    op_act(k, Wd[0][:, 0:NS], SG[0][:, 0:NS], AF.Exp, [SG[1]], [Wd[1]], scale=-DECAY_C)
    ysm, ysm_b, _ = rw.small
    Sst_t, Sn_t, T1_t, RX_t = F[5], F[9], F[11], rw.S
    sa_t = F[12]
    for half in range(2):
        s0 = half * 8
        ss_ = slice(s0, s0 + 8)
        v3 = lambda t: t[0][:, :].rearrange("p (s j) -> p s j", j=64)
        op_dma(k, 'sp', v3(Sst_t), dr["state_wkv"][s0:s0 + 8, 2 * hp:2 * hp + 2, :, :].rearrange("s h i j -> (h i) s j"),
               [k.dbuf["state_wkv"]], [Sst_t[1]])

        def bcast(X):
            op_tt(k, 'dve', v3(RX_t), rw.irep[0][:, :].unsqueeze(1).to_broadcast([128, 8, 64]),
                  X[0][:, ss_].unsqueeze(2).to_broadcast([128, 8, 64]), ALU.mult, [rw.irep[1], X[1]], [RX_t[1]])
            pb, pb_b = k.psn()
            op_mm(k, pb[:, :], rw.boF[0][:], RX_t[0][:, :], True, True, [rw.boF[1], RX_t[1]], [pb_b])
            return pb[:, :].rearrange("p (s j) -> p s j", j=64), pb_b

        kkb, kkb_b = bcast(KK)
        op_tt(k, 'dve', v3(T1_t), v3(Sst_t), kkb, ALU.mult, [Sst_t[1], kkb_b], [T1_t[1]])
        P.op('dve', lambda e, ss_=ss_: e.tensor_reduce(out=sa_t[0][:, ss_], in_=v3(T1_t), axis=AX.X, op=ALU.add, negate=True),
             [T1_t[1]], [sa_t[1]])
        wb_, wb_b = bcast(Wd)
        op_tt(k, 'dve', v3(Sn_t), v3(Sst_t), wb_, ALU.mult, [Sst_t[1], wb_b], [Sn_t[1]])
        bb_, bb_b = bcast(Bt)
        op_tt(k, 'dve', v3(T1_t), bb_, sa_t[0][:, ss_].unsqueeze(2).to_broadcast([128, 8, 64]), ALU.mult, [bb_b, sa_t[1]], [T1_t[1]])
        op_tt(k, 'dve', v3(Sn_t), v3(Sn_t), v3(T1_t), ALU.add, [Sn_t[1], T1_t[1]], [Sn_t[1]])
        kb_, kb_b = bcast(KP)
        op_tt(k, 'dve', v3(T1_t), kb_, V[0][:, ss_].unsqueeze(2).to_broadcast([128, 8, 64]), ALU.mult, [kb_b, V[1]], [T1_t[1]])
        op_tt(k, 'dve', v3(Sn_t), v3(Sn_t), v3(T1_t), ALU.add, [Sn_t[1], T1_t[1]], [Sn_t[1]])
        rb_, rb_b = bcast(R)
        op_tt(k, 'dve', v3(T1_t), v3(Sn_t), rb_, ALU.mult, [Sn_t[1], rb_b], [T1_t[1]])
        P.op('dve', lambda e, ss_=ss_: e.tensor_reduce(out=ysm[:, ss_], in_=v3(T1_t), axis=AX.X, op=ALU.add), [T1_t[1]], [ysm_b])
        op_dma(k, 'sp', dr["wkv_s"][s0:s0 + 8, 2 * hp:2 * hp + 2, :, :].rearrange("s h i j -> (h i) s j"), v3(Sn_t),
               [Sn_t[1]], [k.dbuf["wkv_s"]])
    return ysm, ysm_b

MLA_SCALE_ = 192.0 ** -0.5
PI_ = 3.141592653589793
NPOOL = 10240


def mla_setup(k):
    P, dr, nc, sb = k.P, k.dr, k.nc, k.sb
    ml = K()
    k.ml = ml
    ml.cst = sb("ml_cst", [128, 4], F32)
    ml.Qs = sb("ml_Qs", [128, 2, NS, 16], BF16)
    ml.QRs = sb("ml_QRs", [64, NS, 16], BF16)
    ml.OLs = sb("ml_OLs", [128, 2, 16, NS], BF16)
    ml.ZSs = sb("ml_ZSs", [128, 16, NS], BF16)
    ml.KsT = sb("ml_KsT", [128, 3, NS], BF16)
    ml.ckvN = sb("ml_ckvN", [NS, 258], BF16)
    ml.wuv = sb("ml_wuv", [128, 2, E], BF16)
    ml.mskc = sb("ml_mskc", [NS, NS], F32)
    ml.ones16 = sb("ml_ones16", [NS, 128], F32)
    ml.maskB = sb("ml_maskB", [128, 128], BF16)
    ml.qg = sb("ml_qg", [128, 3], F32)
    ml.big_base = k.off[0]
    ml.cqn = sb("ml_cqn", [128, 3, TWA], BF16)
    ml.KTc = sb("ml_KTc", [128, 2, TWA], BF16)
    ml.KTr = sb("ml_KTr", [64, TWA], BF16)
    ml.ctok = sb("ml_ctok", [128, 17, 256], BF16)
    ml.cos2 = sb("ml_cos2", [64, TWA], BF16)
    ml.sin2 = sb("ml_sin2", [64, TWA], BF16)
    ml.t = [sb(f"ml_t{i}", [128, 576], F32) for i in range(3)]
    ml.sqb = sb("ml_sqb", [128, 512], BF16)
    ml.rA = sb("ml_rA", [128, 64], F32)
    ml.rB = sb("ml_rB", [128, 64], F32)
    ml.st = [sb(f"ml_st{i}", [128, 16], F32) for i in range(4)]
    ml.st_i = [0]
    early = k.off[0]
    ml.wkv = sb("ml_wkv", [128, 8, 320], BF16)
    ml.wq = sb("ml_wq", [128, 8, 384], BF16)
    ml.kvg = sb("ml_kvg", [128, 256], F32)
    ml.costk = sb("ml_costk", [128, 17, 32], F32)
    ml.sintk = sb("ml_sintk", [128, 17, 32], F32)
    ml.CK = sb("ml_CK", [128, 256], F32)
    ml.KRf = sb("ml_KRf", [128, 64], F32)
    ml.KRb = sb("ml_KRb", [128, 64], BF16)
    end_early = k.off[0]
    k.off[0] = early
    ml.QL = sb("ml_QL", [128, 2, TWA], BF16)
    ml.QR = sb("ml_QR", [64, TWA], BF16)
    ml.Pbf = sb("ml_Pbf", [128, T], BF16)
    ml.PT = sb("ml_PT", [128, 16, 128], BF16)
    ml.OLT = sb("ml_OLT", [128, 2, 512], BF16)
    ml.ZS = sb("ml_ZS", [128, 4, 512], BF16)
    ml.QN = sb("ml_QN", [128, 512], BF16)
    ml.wuq = sb("ml_wuq", [128, 3, 192], BF16)
    ml.wsw = sb("ml_wsw", [128, 3, 64], BF16)
    ml.wukr = sb("ml_wukr", [128, 2, 128], F32)
    ml.wukT = sb("ml_wukT", [128, 256], BF16)
    ml.wz = sb("ml_wz", [128, 8, 128], BF16)
    ml.Dg = sb("ml_Dg", [128, 128], BF16)
    k.off[0] = max(k.off[0], end_early)
    ml.end_a = k.off[0]
    k.off[0] = ml.big_base
    ml.KPs = [sb(f"ml_KP{i}", [128, 64, 322], BF16) for i in range(2)]
    ml.ST = sb("ml_ST", [128, 65, 16], F32)
    ml.PTs = sb("ml_PTs", [128, 65, 16], BF16)
    ml.KTp = sb("ml_KTp", [128, 2, 384], BF16)
    ml.pti = sb("ml_pti", [128, NS * 64], I32)
    ml.sm = [sb(f"ml_sm{i}", [128, 32], F32) for i in range(4)]
    ml.olat = sb("ml_olat", [NS, 256], BF16)
    k.off[0] = max(k.off[0], ml.end_a)


def mla_trig(k, out_ap, out_b, ang_ap, shift, np_, w):
    ml = k.ml
    P = k.P
    t0, t0_b, _ = ml.t[0]
    t1, t1_b, _ = ml.t[1]
    a0 = t0[0:np_, 0:w]
    a1 = t1[0:np_, 0:w]
    a1i = t1[:].bitcast(I32)[0:np_, 0:w]
    TWO_PI = 2 * PI_
    rd = [ml.t[2][1]]
    op_ts(k, 'dve', a0, ang_ap, 1.0 / TWO_PI, shift / TWO_PI, ALU.mult, ALU.add, rd, [t0_b])
    P.op('dve', lambda e: e.tensor_copy(out=a1i, in_=a0), [t0_b], [t1_b])
    P.op('dve', lambda e: e.tensor_copy(out=a0, in_=a1i), [t1_b], [t0_b])
    op_stt(k, 'dve', a0, a0, -TWO_PI, ang_ap, ALU.mult, ALU.add, [t0_b] + rd, [t0_b])
    op_ts(k, 'dve', a1, a0, shift, 0.0, ALU.add, ALU.is_lt, [t0_b], [t1_b])
    op_stt(k, 'dve', a0, a1, TWO_PI, a0, ALU.mult, ALU.add, [t0_b, t1_b], [t0_b])
    col = 1 if abs(shift - PI_) < 1e-9 else 2
    return op_act(k, out_ap, a0, AF.Sin, [t0_b, ml.cst[1]], [out_b], bias=ml.cst[0][0:np_, col:col + 1])


def layer_mla(k, ps_):
    P, dr, nc = k.P, k.dr, k.nc
    if not hasattr(k, "ml"):
        mla_setup(k)
    ml = k.ml
    TW = TWA if ps_ == 0 else T
    tiles = ntiles(ps_)
    c_w_in, c_w_in_b = dr["c_w_in"], k.dbuf["c_w_in"]

    def nst():
        s_ = ml.st[ml.st_i[0] % 4]
        ml.st_i[0] += 1
        return s_

    def ps5():
        i = k.ps_i[0] % 5
        k.ps_i[0] += 1
        return k.ps[i]

    cst, cst_b, _ = ml.cst
    P.op('dve', lambda e: e.memset(cst[:, 0:1], -PI_), writes=[cst_b])
    P.op('dve', lambda e: e.memset(cst[:, 1:2], 0.0), writes=[cst_b])
    P.op('dve', lambda e: e.memset(cst[:, 2:3], 0.5 * PI_), writes=[cst_b])
    load_w_cols(k, ml.wkv[0], ml.wkv[1], c_w_in, c_w_in_b, 384, ncols=320)
    load_w_cols(k, ml.wq[0], ml.wq[1], c_w_in, c_w_in_b, 0, ncols=384)
    op_dma(k, 'pool', ml.wuv[0][:, :, :], dr["c_w_uv"].rearrange("(c p) h v -> p c (h v)", p=128), [k.dbuf["c_w_uv"]], [ml.wuv[1]])
    op_dma(k, 'sp', ml.kvg[0][:], dr["c_kv_norm"].rearrange("(o c) -> o c", o=1).partition_broadcast(128), [k.dbuf["c_kv_norm"]], [ml.kvg[1]])
    op_dma(k, 'sp', ml.qg[0][:], dr["c_q_norm"].rearrange("(c p) -> p c", p=128), [k.dbuf["c_q_norm"]], [ml.qg[1]], slow=True)
    tf, tf_b, _ = ml.t[0]
    P.op('pool', lambda e: e.memset(tf[:, 0:128], 0.0), writes=[tf_b])
    P.op('pool', lambda e: e.affine_select(out=tf[:, 0:128], in_=tf[:, 0:128], pattern=[[-1, 128]], compare_op=ALU.is_ge, fill=-1e9,
                                           base=0, channel_multiplier=1), reads=[tf_b], writes=[tf_b])
    P.op('dve', lambda e: e.tensor_copy(out=ml.maskB[0][:], in_=tf[:, 0:128]), reads=[tf_b], writes=[ml.maskB[1]])
    P.op('pool', lambda e: e.memset(ml.mskc[0][:], 0.0), writes=[ml.mskc[1]])
    P.op('pool', lambda e: e.affine_select(out=ml.mskc[0][:], in_=ml.mskc[0][:], pattern=[[-1, NS]], compare_op=ALU.is_equal, fill=-1e9,
                                           base=0, channel_multiplier=1), reads=[ml.mskc[1]], writes=[ml.mskc[1]])
    P.op('dve', lambda e: e.memset(ml.ones16[0][:], 1.0), writes=[ml.ones16[1]])
    ti_, ti_b, _ = ml.t[1]
    tii = ti_[:].bitcast(I32)
    P.op('pool', lambda e: e.iota(tii[:, 0:32], pattern=[[1, 32]], base=0, channel_multiplier=0), writes=[ti_b])
    invf, invf_b, _ = ml.rA
    P.op('dve', lambda e: e.tensor_copy(out=invf[:, 0:32], in_=tii[:, 0:32]), reads=[ti_b], writes=[invf_b])
    op_act(k, invf[:, 0:32], invf[:, 0:32], AF.Exp, [invf_b], [invf_b], scale=-math.log(10000.0) / 32.0)
    P.op('pool', lambda e: e.iota(tii[:, 64:80], pattern=[[128, 16]], base=0, channel_multiplier=1), writes=[ti_b])
    posf, posf_b, _ = ml.rB
    P.op('dve', lambda e: e.tensor_copy(out=posf[:, 0:16], in_=tii[:, 64:80]), reads=[ti_b], writes=[posf_b])
    P.op('dve', lambda e: e.memset(posf[:, 16:17], 8192.0), writes=[posf_b])
    ang, ang_b, _ = ml.t[2]
    angv = ang[:, 0:17 * 32].rearrange("p (a i) -> p a i", i=32)
    op_tt(k, 'dve', angv, posf[:, 0:17].unsqueeze(2).to_broadcast([128, 17, 32]), invf[:, 0:32].unsqueeze(1).to_broadcast([128, 17, 32]),
          ALU.mult, [posf_b, invf_b], [ang_b])
    tmp, tmp_b, _ = ml.t[0]
    mla_trig(k, ml.sintk[0][:, :, :].rearrange("p a i -> p (a i)"), ml.sintk[1], ang[:, 0:544], PI_, 128, 544)
    mla_trig(k, ml.costk[0][:, :, :].rearrange("p a i -> p (a i)"), ml.costk[1], ang[:, 0:544], 1.5 * PI_, 128, 544)
    P.op('pool', lambda e: e.iota(tii[0:32, 0:1], pattern=[[1, 1]], base=0, channel_multiplier=1), writes=[ti_b])
    P.op('pool', lambda e: e.iota(tii[32:64, 0:1], pattern=[[1, 1]], base=0, channel_multiplier=1), writes=[ti_b])
    P.op('dve', lambda e: e.tensor_copy(out=invf[0:64, 32:33], in_=tii[0:64, 0:1]), reads=[ti_b], writes=[invf_b])
    op_act(k, invf[0:64, 33:34], invf[0:64, 32:33], AF.Exp, [invf_b], [invf_b], scale=-math.log(10000.0) / 32.0)
    P.op('dve', lambda e: e.memset(invf[0:32, 34:35], -1.0), writes=[invf_b])
    P.op('dve', lambda e: e.memset(invf[32:64, 34:35], 1.0), writes=[invf_b])
    for ti, (c0, w) in enumerate(tiles):
        if c0 < T:
            P.op('pool', lambda e, c0=c0: e.iota(tii[0:64, 0:512], pattern=[[1, 512]], base=c0, channel_multiplier=0), writes=[ti_b])
            P.op('dve', lambda e: e.tensor_copy(out=ang[0:64, 0:512], in_=tii[0:64, 0:512]), reads=[ti_b], writes=[ang_b])
        else:
            P.op('dve', lambda e: e.memset(ang[0:64, 0:NS], 8192.0), writes=[ang_b])
        op_ts(k, 'dve', ang[0:64, 0:w], ang[0:64, 0:w], invf[0:64, 33:34], None, ALU.mult, None, [ang_b, invf_b], [ang_b])
        mla_trig(k, ml.cos2[0][:, c0:c0 + w], ml.cos2[1], ang[0:64, 0:w], 1.5 * PI_, 64, w)
        mla_trig(k, ti_[0:64, 0:w], ti_b, ang[0:64, 0:w], PI_, 64, w)
        op_ts(k, 'dve', ml.sin2[0][:, c0:c0 + w], ti_[0:64, 0:w], invf[0:64, 34:35], None, ALU.mult, None, [ti_b, invf_b], [ml.sin2[1]])
    P.chk("m_tab")
    ttiles = [(tt * 128, 128, tt) for tt in range(16)]
    if ps_ == 0:
        ttiles.append((T, NS, 16))
    ctok, ctok_b, _ = ml.ctok
    for (c0, np_, tt) in ttiles:
        pkv, pkv_b = k.psn()
        for kk in range(8):
            op_mm(k, pkv[0:np_, 0:320], k.hT[:, kk, c0 + 1:c0 + 1 + np_], ml.wkv[0][:, kk, :], kk == 0, kk == 7,
                  [ml.wkv[1], k.hT_b.sub(c0 // 128)], [pkv_b])
        st, st_b, _ = nst()
        P.op('act', lambda e, np_=np_, pkv=pkv, st=st: e.activation(out=ml.sqb[0][0:np_, 0:256], in_=pkv[0:np_, 0:256],
                                                                    func=AF.Square, accum_out=st[0:np_, 0:1]),
             [pkv_b], [ml.sqb[1], st_b])
        rstd_from_ss(k, st[0:np_, 0:1], st_b, st[0:np_, 1:2], st_b, np_, 1.0 / 256, 0)
        CK, CK_b, _ = ml.CK
        op_stt(k, 'dve', CK[0:np_, :], pkv[0:np_, 0:256], st[0:np_, 1:2], ml.kvg[0][0:np_, :], ALU.mult, ALU.mult,
               [pkv_b, st_b, ml.kvg[1]], [CK_b])
        if c0 < T:
            op_dma(k, 'sp', dr["ckv_p"][ps_, c0:c0 + 128, :], CK[:, :], [CK_b], [k.dbuf["ckv_p"]])
        else:
            op_dma(k, 'sp', dr["ckv_s"][:, :], CK[0:NS, :], [CK_b], [k.dbuf["ckv_s"]])
            op_act(k, ml.ckvN[0][:, 0:256], CK[0:NS, :], AF.Copy, [CK_b], [ml.ckvN[1]])
            P.op('dve', lambda e: e.memset(ml.ckvN[0][:, 256:258], 1.0), writes=[ml.ckvN[1]])
        op_act(k, ctok[0:np_, tt, :], CK[0:np_, :], AF.Copy, [CK_b], [ctok_b.sub(tt)])
        rA, rA_b, _ = ml.rA
        rB, rB_b, _ = ml.rB
        KRf, KRf_b, _ = ml.KRf
        kr3 = pkv[0:np_, 256:320].rearrange("p (a i) -> p a i", i=32)
        op_tt(k, 'dve', rA[0:np_, 0:64].rearrange("p (a i) -> p a i", i=32), kr3,
              ml.costk[0][0:np_, tt:tt + 1, :].to_broadcast([np_, 2, 32]), ALU.mult, [pkv_b, ml.costk[1]], [rA_b])
        op_tt(k, 'dve', rB[0:np_, 0:64].rearrange("p (a i) -> p a i", i=32), kr3,
              ml.sintk[0][0:np_, tt:tt + 1, :].to_broadcast([np_, 2, 32]), ALU.mult, [pkv_b, ml.sintk[1]], [rB_b])
        op_tt(k, 'dve', KRf[0:np_, 0:32], rA[0:np_, 0:32], rB[0:np_, 32:64], ALU.subtract, [rA_b, rB_b], [KRf_b])
        op_tt(k, 'dve', KRf[0:np_, 32:64], rB[0:np_, 0:32], rA[0:np_, 32:64], ALU.add, [rA_b, rB_b], [KRf_b])
        if c0 < T:
            op_dma(k, 'sp', dr["kr_p"][ps_, c0:c0 + 128, :], KRf[:, :], [KRf_b], [k.dbuf["kr_p"]])
        else:
            op_dma(k, 'sp', dr["kr_s"][:, :], KRf[0:NS, :], [KRf_b], [k.dbuf["kr_s"]])
        KRb, KRb_b, _ = ml.KRb
        op_act(k, KRb[0:np_, :], KRf[0:np_, :], AF.Copy, [KRf_b], [KRb_b])
        psT, psT_b = k.psT
        for j in range(2):
            op_tr(k, psT[:, j * 128:j * 128 + np_], ctok[0:np_, tt, j * 128:(j + 1) * 128], k.identB[0:np_, 0:np_],
                  [ctok_b.sub(tt), k.identB_b], [psT_b])
        op_tr(k, psT[0:64, 256:256 + np_], KRb[0:np_, :], k.identB[0:np_, 0:np_], [KRb_b, k.identB_b], [psT_b])
        op_act(k, ml.KTc[0][:, :, c0:c0 + np_], psT[:, 0:256].rearrange("p (j t) -> p j t", t=128)[:, :, 0:np_], AF.Copy,
               [psT_b], [ml.KTc[1].sub(tt)])
        P.op('dve', lambda e, c0=c0, np_=np_: e.tensor_copy(out=ml.KTr[0][:, c0:c0 + np_], in_=psT[0:64, 256:256 + np_]),
             [psT_b], [ml.KTr[1].sub(tt)])
    if ps_ == 0:
        op_act(k, ml.KsT[0][:, 0:2, :], ml.KTc[0][:, :, T:T + NS], AF.Copy, [ml.KTc[1].sub(16)], [ml.KsT[1]])
        op_act(k, ml.KsT[0][0:64, 2, :], ml.KTr[0][:, T:T + NS], AF.Copy, [ml.KTr[1].sub(16)], [ml.KsT[1]])
    P.chk("m_1")
    for ti, (c0, w) in enumerate(tiles):
        pqs = []
        for qc in range(3):
            pq, pq_b = k.psn()
            for kk in range(8):
                op_mm(k, pq[:, 0:w], ml.wq[0][:, kk, qc * 128:(qc + 1) * 128], k.hT[:, kk, c0 + 1:c0 + 1 + w], kk == 0, kk == 7,
                      [ml.wq[1]] + hT_reads(k, c0, w), [pq_b])
            pqs.append((pq, pq_b))
        pss, pss_b = k.psn()
        for qc in range(3):
            sqb, sqb_b, _ = ml.sqb
            op_act(k, sqb[:, 0:w], pqs[qc][0][:, 0:w], AF.Square, [pqs[qc][1]], [sqb_b])
            op_mm(k, pss[:, 0:w], k.onesB[:], sqb[:, 0:w], qc == 0, qc == 2, [k.onesB_b, sqb_b], [pss_b])
        rst, rst_b, _ = ml.t[0]
        op_act(k, rst[:, 0:w], pss[:, 0:w], AF.Sqrt, [pss_b, k.epsr_b], [rst_b], scale=1.0 / 384, bias=k.epsr[:, 0:1])
        P.op('dve', lambda e, w=w: e.reciprocal(out=rst[:, 0:w], in_=rst[:, 0:w]), [rst_b], [rst_b])
        for qc in range(3):
            op_stt(k, 'dve', ml.cqn[0][:, qc, c0:c0 + w], pqs[qc][0][:, 0:w], ml.qg[0][:, qc:qc + 1], rst[:, 0:w], ALU.mult, ALU.mult,
                   [pqs[qc][1], ml.qg[1], rst_b], [ml.cqn[1].sub(ti)])
    P.chk("m_2")
    P.barrier()
    QL, QL_b, _ = ml.QL
    QR, QR_b, _ = ml.QR
    for h in range(16):
        wuq, wuq_b, _ = ml.wuq
        op_dma(k, 'pool', wuq[:, :, :], dr["c_w_uq"][:, h, :].rearrange("(c p) e -> p c e", p=128), [k.dbuf["c_w_uq"]], [wuq_b])
        wsw, wsw_b, _ = ml.wsw
        P.op('dve', lambda e: e.tensor_copy(out=wsw[:, :, 0:32], in_=wuq[:, :, 160:192]), [wuq_b], [wsw_b])
        P.op('dve', lambda e: e.tensor_copy(out=wsw[:, :, 32:64], in_=wuq[:, :, 128:160]), [wuq_b], [wsw_b])
        wukr, wukr_b, _ = ml.wukr
        op_dma(k, 'sp', wukr[:, :, :], dr["c_w_uk"][:, h, :].rearrange("(c p) n -> p c n", p=128), [k.dbuf["c_w_uk"]], [wukr_b])
        pw_, pw_b = k.psn()
        for cc in range(2):
            op_tr(k, pw_[:, cc * 128:(cc + 1) * 128], wukr[:, cc, :], k.identF[:], [wukr_b, k.identF_b], [pw_b])
        op_act(k, ml.wukT[0][:, :], pw_[:, 0:256], AF.Copy, [pw_b], [ml.wukT[1]])
        load_w_cols(k, ml.wz[0], ml.wz[1], c_w_in, c_w_in_b, 704 + h * 128)
        for ti, (c0, w) in enumerate(tiles):
            rq = [ml.cqn[1].sub(ti)]
            pqn, pqn_b = k.psn()
            for qc in range(3):
                op_mm(k, pqn[:, 0:w], wuq[:, qc, 0:128], ml.cqn[0][:, qc, c0:c0 + w], qc == 0, qc == 2, [wuq_b] + rq, [pqn_b])
            pra, pra_b = k.psn()
            for qc in range(3):
                op_mm(k, pra[0:64, 0:w], wuq[:, qc, 128:192], ml.cqn[0][:, qc, c0:c0 + w], qc == 0, qc == 2, [wuq_b] + rq, [pra_b])
            prb, prb_b = k.psn()
            for qc in range(3):
                op_mm(k, prb[0:64, 0:w], wsw[:, qc, :], ml.cqn[0][:, qc, c0:c0 + w], qc == 0, qc == 2, [wsw_b] + rq, [prb_b])
            QN, QN_b, _ = ml.QN
            op_act(k, QN[:, 0:w], pqn[:, 0:w], AF.Copy, [pqn_b], [QN_b])
            t1, t1_b, _ = ml.t[1]
            t2, t2_b, _ = ml.t[2]
            op_tt(k, 'dve', t1[0:64, 0:w], pra[0:64, 0:w], ml.cos2[0][:, c0:c0 + w], ALU.mult, [pra_b, ml.cos2[1]], [t1_b])
            op_tt(k, 'dve', t2[0:64, 0:w], prb[0:64, 0:w], ml.sin2[0][:, c0:c0 + w], ALU.mult, [prb_b, ml.sin2[1]], [t2_b])
            op_tt(k, 'dve', QR[:, c0:c0 + w], t1[0:64, 0:w], t2[0:64, 0:w], ALU.add, [t1_b, t2_b], [QR_b.sub(ti)])
            for cc in range(2):
                pql, pql_b = k.psn()
                op_mm(k, pql[:, 0:w], ml.wukT[0][:, cc * 128:(cc + 1) * 128], QN[:, 0:w], True, True, [ml.wukT[1], QN_b], [pql_b])
                op_act(k, QL[:, cc, c0:c0 + w], pql[:, 0:w], AF.Copy, [pql_b], [QL_b.sub(ti)])
            pz, pz_b = k.psn()
            for kk in range(8):
                op_mm(k, pz[:, 0:w], ml.wz[0][:, kk, :], k.hT[:, kk, c0 + 1:c0 + 1 + w], kk == 0, kk == 7,
                      [ml.wz[1]] + hT_reads(k, c0, w), [pz_b])
            if c0 < T:
                op_act(k, ml.ZS[0][:, ti, :], pz[:, :], AF.Silu, [pz_b], [ml.ZS[1].sub(ti)])
            else:
                op_act(k, ml.ZSs[0][:, h, :], pz[:, 0:NS], AF.Silu, [pz_b], [ml.ZSs[1]])
                P.op('dve', lambda e, h=h: e.tensor_copy(out=ml.Qs[0][:, :, :, h], in_=QL[:, :, T:T + NS]), [QL_b.sub(ti)], [ml.Qs[1]])
                P.op('dve', lambda e, h=h: e.tensor_copy(out=ml.QRs[0][:, :, h], in_=QR[:, T:T + NS]), [QR_b.sub(ti)], [ml.QRs[1]])
        P.chk("m_q")
        Pbf, Pbf_b, _ = ml.Pbf
        PT, PT_b, _ = ml.PT
        pov = [k.ps[5], k.ps[6]]
        for qb in range(16):
            t0 = qb * 128
            L = t0 + 128
            nb = (L + 511) // 512
            tq = qb // 4
            banks = [ps5() for _ in range(nb)]
            kt_r = [ml.KTc[1].sub(x) for x in range(qb + 1)] + [ml.KTr[1].sub(x) for x in range(qb + 1)]
            for b in range(nb):
                l0 = b * 512
                lw = min(512, L - l0)
                bk, bk_b = banks[b]
                last = (b == nb - 1)
                op_mm(k, bk[:, 0:lw], QL[:, 0, t0:t0 + 128], ml.KTc[0][:, 0, l0:l0 + lw], True, False, [QL_b.sub(tq)] + kt_r, [bk_b])
                op_mm(k, bk[:, 0:lw], QL[:, 1, t0:t0 + 128], ml.KTc[0][:, 1, l0:l0 + lw], False, False, [QL_b.sub(tq)] + kt_r, [bk_b])
                op_mm(k, bk[:, 0:lw], QR[:, t0:t0 + 128], ml.KTr[0][:, l0:l0 + lw], False, not last, [QR_b.sub(tq)] + kt_r, [bk_b])
                if last:
                    dcol = t0 - l0
                    op_mm(k, bk[:, dcol:dcol + 128], k.identB[:], ml.maskB[0][:], False, True, [k.identB_b, ml.maskB[1]], [bk_b])
            st, st_b, _ = nst()
            for b in range(nb):
                lw = min(512, L - b * 512)
                bk, bk_b = banks[b]
                P.op('dve', lambda e, b=b, lw=lw, bk=bk, st=st: e.tensor_reduce(out=st[:, b:b + 1], in_=bk[:, 0:lw], axis=AX.X, op=ALU.max),
                     [bk_b], [st_b])
            if nb > 1:
                P.op('dve', lambda e, nb=nb, st=st: e.tensor_reduce(out=st[:, 4:5], in_=st[:, 0:nb], axis=AX.X, op=ALU.max), [st_b], [st_b])
                mcol = st[:, 4:5]
            else:
                mcol = st[:, 0:1]
            op_ts(k, 'dve', st[:, 5:6], mcol, -MLA_SCALE_, None, ALU.mult, None, [st_b], [st_b])
            for b in range(nb):
                l0 = b * 512
                lw = min(512, L - l0)
                bk, bk_b = banks[b]
                P.op('act', lambda e, b=b, l0=l0, lw=lw, bk=bk, st=st: e.activation(out=Pbf[:, l0:l0 + lw], in_=bk[:, 0:lw], func=AF.Exp,
                                                                                   scale=MLA_SCALE_, bias=st[:, 5:6],
                                                                                   accum_out=st[:, 8 + b:9 + b]),
                     [bk_b, st_b], [Pbf_b.sub(b), st_b])
            if nb > 1:
                P.op('dve', lambda e, nb=nb, st=st: e.tensor_reduce(out=st[:, 6:7], in_=st[:, 8:8 + nb], axis=AX.X, op=ALU.add), [st_b], [st_b])
                scol = st[:, 6:7]
            else:
                scol = st[:, 8:9]
            P.op('dve', lambda e, st=st, scol=scol: e.reciprocal(out=st[:, 7:8], in_=scol), [st_b], [st_b])
            Dg, Dg_b, _ = ml.Dg
            op_ts(k, 'dve', Dg[:, :], k.identF[:, :], st[:, 7:8], None, ALU.mult, None, [k.identF_b, st_b], [Dg_b])
            for g0 in range(0, qb + 1, 4):
                g1 = min(qb + 1, g0 + 4)
                pt, pt_b = ps5()
                for kb in range(g0, g1):
                    op_mm(k, pt[:, (kb - g0) * 128:(kb - g0 + 1) * 128], Pbf[:, kb * 128:(kb + 1) * 128], Dg[:, :], True, True,
                          [Pbf_b.sub(kb // 4), Dg_b], [pt_b])
                n_ = g1 - g0
                if (g0 // 4) % 2 == 0:
                    op_act(k, PT[:, g0:g1, :], pt[:, 0:n_ * 128].rearrange("p (g t) -> p g t", t=128), AF.Copy, [pt_b], [PT_b.sub(g0 // 4)])
                else:
                    P.op('dve', lambda e, g0=g0, g1=g1, n_=n_, pt=pt: e.tensor_copy(
                        out=PT[:, g0:g1, :], in_=pt[:, 0:n_ * 128].rearrange("p (g t) -> p g t", t=128)), [pt_b], [PT_b.sub(g0 // 4)])
            qc_ = slice((qb % 4) * 128, (qb % 4 + 1) * 128)
            for cc in range(2):
                pv_, pv_b = pov[cc]
                for kb in range(qb + 1):
                    op_mm(k, pv_[:, qc_], ctok[:, kb, cc * 128:(cc + 1) * 128], PT[:, kb, :], kb == 0, kb == qb,
                          [ctok_b.sub(kb), PT_b.sub(kb // 4)], [pv_b])
            if qb % 4 == 3:
                OLT, OLT_b, _ = ml.OLT
                op_act(k, OLT[:, 0, :], pov[0][0][:, :], AF.Copy, [pov[0][1]], [OLT_b])
                P.op('dve', lambda e: e.tensor_copy(out=OLT[:, 1, :], in_=pov[1][0][:, :]), [pov[1][1]], [OLT_b])
                po, po_b = ps5()
                for cc in range(2):
                    op_mm(k, po[:, :], ml.wuv[0][:, cc, h * 128:(h + 1) * 128], OLT[:, cc, :], cc == 0, cc == 1, [ml.wuv[1], OLT_b], [po_b])
                op_tt(k, 'dve', k.GT[:, h, tq * 512:(tq + 1) * 512], po[:, :], ml.ZS[0][:, tq, :], ALU.mult, [po_b, ml.ZS[1].sub(tq)],
                      [k.GT_b.sub(h).sub(tq)])
            P.chk("m_att")
    if ps_ == 0:
        P.barrier()
        mla_sample(k)


def mla_sample(k):
    P, dr, nc, ml = k.P, k.dr, k.nc, k.ml
    ST, ST_b, _ = ml.ST
    PTs, PTs_b, _ = ml.PTs
    pti, pti_b, _ = ml.pti
    ptf, ptf_b = pti[:].bitcast(F32), pti_b
    idx, idx_b = pti, pti_b
    op_dma(k, 'sp', pti[:, :], dr["page_table"].rearrange("s j -> (s j)").rearrange("(o n) -> o n", o=1).partition_broadcast(128),
           [k.dbuf["page_table"]], [pti_b])
    P.op('dve', lambda e: e.tensor_copy(out=ptf[:, :], in_=pti[:, :]), [pti_b], [ptf_b])
    sm0, sm0_b, _ = ml.sm[0]
    smi = sm0[:].bitcast(I32)
    P.op('pool', lambda e: e.iota(smi[:, 0:1], pattern=[[1, 1]], base=0, channel_multiplier=1), writes=[sm0_b])
    P.op('dve', lambda e: e.tensor_copy(out=sm0[:, 1:2], in_=smi[:, 0:1]), [sm0_b], [sm0_b])
    op_ts(k, 'dve', ptf[:, :], ptf[:, :], 128.0, sm0[:, 1:2], ALU.mult, ALU.add, [ptf_b, sm0_b], [ptf_b])
    P.op('dve', lambda e: e.tensor_copy(out=idx[:, :], in_=ptf[:, :]), [ptf_b], [idx_b])
    for i_ in range(2):
        P.op('dve', lambda e, i_=i_: e.memset(ml.KPs[i_][0][:, :, 320:322], 1.0), writes=[ml.KPs[i_][1]])
    cat_rows = dr["cache_cat"].rearrange("n t c -> (n t) c")
    P.chk("s_idx")
    for s in range(NS):
        KP, KP_b, _ = ml.KPs[s % 2]
        for j in range(64):
            n = s * 64 + j
            P.op('pool', lambda e, j=j, n=n, KP=KP: e.indirect_dma_start(out=KP[:, j, 0:320], out_offset=None, in_=cat_rows,
                                                                        in_offset=bass.IndirectOffsetOnAxis(ap=idx[:, n:n + 1], axis=0)),
                 [idx_b, k.dbuf["cache_cat"]], [KP_b.sub(j)], dma=True)
        P.chk("s_gather")
        psT, psT_b = k.psT
        pS = [k.psn(), k.psn()]
        for j in range(64):
            o_ = (j % 2) * 384
            if True:
                op_tr(k, psT[0:64, o_:o_ + 128], KP[:, j, 0:64], k.identB[:], [KP_b.sub(j), k.identB_b], [psT_b])
                op_tr(k, psT[:, o_ + 128:o_ + 256], KP[:, j, 64:192], k.identB[:], [KP_b.sub(j), k.identB_b], [psT_b])
                op_tr(k, psT[:, o_ + 256:o_ + 384], KP[:, j, 192:320], k.identB[:], [KP_b.sub(j), k.identB_b], [psT_b])
            if j % 2 == 1:
                KTp, KTp_b, _ = ml.KTp
                op_act(k, KTp[:, :, :], psT[:, 0:768].rearrange("p (a c) -> p a c", c=384), AF.Copy, [psT_b], [KTp_b])
                for jj in (j - 1, j):
                    a = jj % 2
                    bk, bk_b = pS[jj // 32]
                    cs_ = slice((jj % 32) * 16, (jj % 32 + 1) * 16)
                    op_mm(k, bk[:, cs_], KTp[0:64, a, 0:128], ml.QRs[0][:, s, :], True, False, [KTp_b, ml.QRs[1]], [bk_b])
                    op_mm(k, bk[:, cs_], KTp[:, a, 128:256], ml.Qs[0][:, 0, s, :], False, False, [KTp_b, ml.Qs[1]], [bk_b])
                    op_mm(k, bk[:, cs_], KTp[:, a, 256:384], ml.Qs[0][:, 1, s, :], False, True, [KTp_b, ml.Qs[1]], [bk_b])
        for g in range(2):
            bk, bk_b = pS[g]
            P.op('dve', lambda e, g=g, bk=bk: e.tensor_copy(out=ST[:, g * 32:(g + 1) * 32, :], in_=bk[:, :].rearrange("p (j h) -> p j h", h=16)),
                 [bk_b], [ST_b])
        pN, pN_b = k.psn()
        op_mm(k, pN[0:NS, 0:16], ml.KsT[0][0:64, 2, :], ml.QRs[0][:, s, :], True, False, [ml.KsT[1], ml.QRs[1]], [pN_b])
        op_mm(k, pN[0:NS, 0:16], ml.KsT[0][:, 0, :], ml.Qs[0][:, 0, s, :], False, False, [ml.KsT[1], ml.Qs[1]], [pN_b])
        op_mm(k, pN[0:NS, 0:16], ml.KsT[0][:, 1, :], ml.Qs[0][:, 1, s, :], False, True, [ml.KsT[1], ml.Qs[1]], [pN_b])
        P.op('dve', lambda e: e.memset(ST[:, 64, :], -1e9), writes=[ST_b])
        op_ts(k, 'dve', ST[0:NS, 64, :], pN[0:NS, 0:16], ml.mskc[0][:, s:s + 1], None, ALU.add, None, [pN_b, ml.mskc[1]], [ST_b])
        sm1, sm1_b, _ = ml.sm[1]
        P.op('dve', lambda e: e.tensor_reduce(out=sm1[:, 0:16], in_=ST[:, :, :].rearrange("p j h -> p h j"), axis=AX.X, op=ALU.max),
             [ST_b], [sm1_b])
        pM, pM_b = k.psn()
        op_tr(k, pM[0:16, 0:128], sm1[:, 0:16], k.identF[:], [sm1_b, k.identF_b], [pM_b])
        sm2, sm2_b, _ = ml.sm[2]
        P.op('dve', lambda e, pM=pM: e.tensor_reduce(out=sm2[0:16, 0:1], in_=pM[0:16, 0:128], axis=AX.X, op=ALU.max), [pM_b], [sm2_b])
        op_ts(k, 'dve', sm2[0:16, 16:32], k.identF[0:16, 0:16], sm2[0:16, 0:1], None, ALU.mult, None, [k.identF_b, sm2_b], [sm2_b])
        pB, pB_b = k.psn()
        op_mm(k, pB[:, 0:16], ml.ones16[0][:, :], sm2[0:16, 16:32], True, True, [ml.ones16[1], sm2_b], [pB_b])
        sm3, sm3_b, _ = ml.sm[3]
        op_act(k, sm3[:, 0:16], pB[:, 0:16], AF.Copy, [pB_b], [sm3_b], scale=-MLA_SCALE_)
        op_stt(k, 'dve', ST[:, :, :], ST[:, :, :], MLA_SCALE_, sm3[:, 0:16].unsqueeze(1).to_broadcast([128, 65, 16]), ALU.mult, ALU.add,
               [ST_b, sm3_b], [ST_b])
        op_act(k, PTs[:, :, :], ST[:, :, :], AF.Exp, [ST_b], [PTs_b])
        pO, pO_b = k.psn()
        for j in range(64):
            op_mm(k, pO[0:16, 0:257], PTs[:, j, :], KP[:, j, 64:321], j == 0, False, [PTs_b, KP_b.sub(j)], [pO_b])
        op_mm(k, pO[0:16, 0:257], PTs[0:NS, 64, :], ml.ckvN[0][:, 0:257], False, True, [PTs_b, ml.ckvN[1]], [pO_b])
        P.op('dve', lambda e, pO=pO: e.reciprocal(out=sm2[0:16, 1:2], in_=pO[0:16, 256:257]), [pO_b, sm2_b], [sm2_b])
        olat, olat_b, _ = ml.olat
        op_ts(k, 'dve', olat[:, :], pO[0:16, 0:256], sm2[0:16, 1:2], None, ALU.mult, None, [pO_b, sm2_b], [olat_b])
        for cc in range(2):
            op_tr(k, psT[:, cc * 16:(cc + 1) * 16], olat[:, cc * 128:(cc + 1) * 128], k.identB[0:16, 0:16], [olat_b, k.identB_b], [psT_b])
        op_act(k, ml.OLs[0][:, :, :, s], psT[:, 0:32].rearrange("p (c h) -> p c h", h=16), AF.Copy, [psT_b], [ml.OLs[1]])
        P.chk("s_one")
    for h in range(16):
        po, po_b = k.psn()
        for cc in range(2):
            op_mm(k, po[:, 0:NS], ml.wuv[0][:, cc, h * 128:(h + 1) * 128], ml.OLs[0][:, cc, h, :], cc == 0, cc == 1,
                  [ml.wuv[1], ml.OLs[1]], [po_b])
        op_tt(k, 'dve', k.GT[:, h, T:T + NS], po[:, 0:NS], ml.ZSs[0][:, h, :], ALU.mult, [po_b, ml.ZSs[1]], [k.GT_b.sub(h).sub(4)])

S5C = 64
S5NCH = T // S5C


def s5_setup(k):
    P, dr, nc, sb = k.P, k.dr, k.nc, k.sb
    s5 = K()
    k.s5 = s5
    s5.cst = sb("s5_cst", [128, 4], F32)
    s5.P64 = {n: sb("s5_p_" + n, [128, 64], F32) for n in
              ["lr", "li", "dt", "m", "th", "are", "aim", "fre", "fim", "t0", "t1", "lnm", "m64", "ph"]}
    s5.LB = [sb(f"s5_LB{i}", [128, 16, 128], BF16) for i in range(2)]
    s5.LC = [sb(f"s5_LC{i}", [128, 16, 128], BF16) for i in range(2)]
    s5.LC3 = [sb(f"s5_LC3{i}", [128, 16, 64], BF16) for i in range(2)]
    s5.Uz = sb("s5_Uz", [128, TWA], BF16)
    s5.mask4 = sb("s5_mask4", [128, 128], F32)
    s5.par = {n: sb("s5_par_" + n, [128, 16], F32) for n in ["d", "bg"]}
    s5.m01 = sb("s5_m01", [128, T], BF16)
    s5.U = sb("s5_U", [128, TWA], BF16)
    s5.wt = sb("s5_wt", [128, 8, 128], BF16)
    s5.tmpt = {n: sb("s5_tab_" + n, [128, 64], F32) for n in ["ang", "mg", "a", "b", "c"]}
    s5.pack = [sb(f"s5_pack{i}", [128, 6, 64], F32) for i in range(2)]
    s5.tabs = []
    for i in range(2):
        d_ = dict(s5.tmpt)
        for j_, n in enumerate(["Fc", "Fs", "Bc", "Bs", "Gc", "Gs"]):
            d_[n] = (s5.pack[i][0][:, j_, :], s5.pack[i][1].sub(n), 0)
        s5.tabs.append(d_)
    s5.tab = s5.tabs[0]
    s5.big = [sb(f"s5_big{i}", [128, T], F32) for i in range(5)]
    s5.sbf = [sb(f"s5_sbf{i}", [128, TWA], BF16) for i in range(2)]
    s5.ec_ = {n: sb("s5_e_" + n, [128, 64], F32) for n in ["er", "ei", "hr", "hi", "Er", "Ei", "cr", "ci"]}
    s5.fin = [sb(f"s5_fin{i}", [128, 64], F32) for i in range(2)]
    s5.s0T = [sb(f"s5_s0T{i}", [128, 64, NS], F32) for i in range(2)]
    s5.tmpn = [sb(f"s5_tmpn{i}", [128, NS], F32) for i in range(2)]
    s5.g = [sb(f"s5_g{i}", [128, 512], F32) for i in range(3)]
    s5.gb = sb("s5_gb", [128, 512], BF16)
    al = lambda name, shape, dt, o: (nc.alloc_sbuf_tensor_at(name, shape, dt, offset=o), Buf(name), o)
    s5.srow = al("s5_srow", [NS, 2048], F32, s5.big[0][2])
    s5.wg = al("s5_wg", [128, 16, 128], BF16, s5.big[1][2])
    s5.XX = [al("s5_XX0", [128, 64, 32], F32, s5.big[3][2]), al("s5_XX1", [128, 64, 32], F32, s5.big[4][2])]
    s5.bre = al("s5_bre", [128, 64, 16], F32, s5.sbf[0][2])
    s5.bim = al("s5_bim", [128, 64, 16], F32, s5.sbf[1][2])
    s5.CN = [al("s5_CN0", [128, 16, 64], F32, s5.big[2][2] + 4096), al("s5_CN1", [128, 16, 64], F32, s5.big[1][2] + 4096)]


def s5_trig(k, out_ap, out_b, ang_ap, ang_b, shift, w):
    P, s5 = k.P, k.s5
    a0, a0_b = s5.tmpt["a"][0][:, 0:w], s5.tmpt["a"][1]
    a1, a1_b = s5.tmpt["b"][0][:, 0:w], s5.tmpt["b"][1]
    a1i = s5.tmpt["b"][0][:].bitcast(I32)[:, 0:w]
    TWO_PI = 2 * PI_
    op_ts(k, 'dve', a0, ang_ap, 1.0 / TWO_PI, shift / TWO_PI, ALU.mult, ALU.add, [ang_b], [a0_b])
    P.op('dve', lambda e: e.tensor_copy(out=a1i, in_=a0), [a0_b], [a1_b])
    P.op('dve', lambda e: e.tensor_copy(out=a0, in_=a1i), [a1_b], [a0_b])
    op_stt(k, 'dve', a0, a0, -TWO_PI, ang_ap, ALU.mult, ALU.add, [a0_b, ang_b], [a0_b])
    op_ts(k, 'dve', a1, a0, shift, 0.0, ALU.add, ALU.is_lt, [a0_b], [a1_b])
    op_stt(k, 'dve', a0, a1, TWO_PI, a0, ALU.mult, ALU.add, [a0_b, a1_b], [a0_b])
    col = 1 if abs(shift - PI_) < 1e-9 else 2
    op_act(k, out_ap, a0, AF.Sin, [a0_b, s5.cst[1]], [out_b], bias=s5.cst[0][:, col:col + 1])


def s5_load(k):
    P, dr, nc, s5 = k.P, k.dr, k.nc, k.s5
    cst, cst_b, _ = s5.cst
    P.op('dve', lambda e: e.memset(cst[:, 0:1], -PI_), writes=[cst_b])
    P.op('dve', lambda e: e.memset(cst[:, 1:2], 0.0), writes=[cst_b])
    P.op('dve', lambda e: e.memset(cst[:, 2:3], 0.5 * PI_), writes=[cst_b])
    p = s5.P64
    for n, src in (("lr", "d_lambda_re"), ("li", "d_lambda_im")):
        for g2 in range(2):
            op_dma(k, 'sp', p[n][0][g2 * 64:(g2 + 1) * 64, :], dr[src].rearrange("(c g2) p -> g2 p c", g2=2)[g2],
                   [k.dbuf[src]], [p[n][1]], slow=True)
    for g2 in range(2):
        op_dma(k, 'sp', p["dt"][0][g2 * 64:(g2 + 1) * 64, :],
               dr["d_log_dt"].rearrange("(c g2) -> g2 c", g2=2)[g2:g2 + 1, :].partition_broadcast(64), [k.dbuf["d_log_dt"]], [p["dt"][1]],
               slow=True)
    A = lambda n: p[n][0][:, :]
    Bf = lambda n: p[n][1]
    op_act(k, A("dt"), A("dt"), AF.Exp, [Bf("dt")], [Bf("dt")])
    op_tt(k, 'dve', A("lnm"), A("lr"), A("dt"), ALU.mult, [Bf("lr"), Bf("dt")], [Bf("lnm")])
    op_act(k, A("m"), A("lnm"), AF.Exp, [Bf("lnm")], [Bf("m")])
    op_act(k, A("m64"), A("lnm"), AF.Exp, [Bf("lnm")], [Bf("m64")], scale=float(S5C))
    op_tt(k, 'dve', A("th"), A("li"), A("dt"), ALU.mult, [Bf("li"), Bf("dt")], [Bf("th")])
    op_ts(k, 'dve', A("ph"), A("th"), float(S5C), None, ALU.mult, None, [Bf("th")], [Bf("ph")])
    s5_trig(k, A("aim"), Bf("aim"), A("th"), Bf("th"), PI_, 64)
    s5_trig(k, A("are"), Bf("are"), A("th"), Bf("th"), 1.5 * PI_, 64)
    op_tt(k, 'dve', A("are"), A("are"), A("m"), ALU.mult, [Bf("are"), Bf("m")], [Bf("are")])
    op_tt(k, 'dve', A("aim"), A("aim"), A("m"), ALU.mult, [Bf("aim"), Bf("m")], [Bf("aim")])
    op_tt(k, 'dve', A("t0"), A("lr"), A("lr"), ALU.mult, [Bf("lr")], [Bf("t0")])
    op_tt(k, 'dve', A("t1"), A("li"), A("li"), ALU.mult, [Bf("li")], [Bf("t1")])
    op_tt(k, 'dve', A("t0"), A("t0"), A("t1"), ALU.add, [Bf("t0"), Bf("t1")], [Bf("t0")])
    P.op('dve', lambda e: e.reciprocal(out=A("t0"), in_=A("t0")), [Bf("t0")], [Bf("t0")])
    op_ts(k, 'dve', A("t1"), A("are"), -1.0, None, ALU.add, None, [Bf("are")], [Bf("t1")])
    op_tt(k, 'dve', A("fre"), A("t1"), A("lr"), ALU.mult, [Bf("t1"), Bf("lr")], [Bf("fre")])
    op_tt(k, 'dve', A("fim"), A("aim"), A("li"), ALU.mult, [Bf("aim"), Bf("li")], [Bf("fim")])
    op_tt(k, 'dve', A("fre"), A("fre"), A("fim"), ALU.add, [Bf("fre"), Bf("fim")], [Bf("fre")])
    op_tt(k, 'dve', A("fim"), A("aim"), A("lr"), ALU.mult, [Bf("aim"), Bf("lr")], [Bf("fim")])
    op_tt(k, 'dve', A("t1"), A("t1"), A("li"), ALU.mult, [Bf("t1"), Bf("li")], [Bf("t1")])
    op_tt(k, 'dve', A("fim"), A("fim"), A("t1"), ALU.subtract, [Bf("fim"), Bf("t1")], [Bf("fim")])
    op_tt(k, 'dve', A("fre"), A("fre"), A("t0"), ALU.mult, [Bf("fre"), Bf("t0")], [Bf("fre")])
    op_tt(k, 'dve', A("fim"), A("fim"), A("t0"), ALU.mult, [Bf("fim"), Bf("t0")], [Bf("fim")])
    for t_, src in ((s5.bre, "d_b_re"), (s5.bim, "d_b_im")):
        for g2 in range(2):
            op_dma(k, 'sp', t_[0][g2 * 64:(g2 + 1) * 64, :, :], dr[src].rearrange("(c g2) p q -> g2 p c q", g2=2)[g2],
                   [k.dbuf[src]], [t_[1]])
    for i in range(2):
        P.op('pool', lambda e, i=i: e.memset(s5.XX[i][0][:], 0.0), writes=[s5.XX[i][1]])
    big0, big0_b, _ = s5.big[0]
    big1, big1_b, _ = s5.big[1]
    v1 = lambda t: t[:, 0:1024].rearrange("p (c q) -> p c q", q=16)
    frb = A("fre").unsqueeze(2).to_broadcast([128, 64, 16])
    fib = A("fim").unsqueeze(2).to_broadcast([128, 64, 16])
    op_tt(k, 'dve', v1(big0), s5.bre[0][:], frb, ALU.mult, [s5.bre[1], Bf("fre")], [big0_b])
    op_tt(k, 'dve', v1(big1), s5.bim[0][:], fib, ALU.mult, [s5.bim[1], Bf("fim")], [big1_b])
    op_tt(k, 'dve', v1(big0), v1(big0), v1(big1), ALU.subtract, [big0_b, big1_b], [big0_b])
    for g2 in range(2):
        hP = slice(g2 * 64, g2 * 64 + 64)
        P.op('dve', lambda e, g2=g2, hP=hP: e.tensor_copy(out=s5.XX[0][0][hP, :, g2 * 16:(g2 + 1) * 16], in_=v1(big0)[hP]),
             [big0_b], [s5.XX[0][1]])
    op_tt(k, 'dve', v1(big0), s5.bim[0][:], frb, ALU.mult, [s5.bim[1], Bf("fre")], [big0_b])
    op_tt(k, 'dve', v1(big1), s5.bre[0][:], fib, ALU.mult, [s5.bre[1], Bf("fim")], [big1_b])
    op_tt(k, 'dve', v1(big0), v1(big0), v1(big1), ALU.add, [big0_b, big1_b], [big0_b])
    for g2 in range(2):
        hP = slice(g2 * 64, g2 * 64 + 64)
        P.op('dve', lambda e, g2=g2, hP=hP: e.tensor_copy(out=s5.XX[1][0][hP, :, g2 * 16:(g2 + 1) * 16], in_=v1(big0)[hP]),
             [big0_b], [s5.XX[1][1]])
    for i in range(2):
        for ec in range(16):
            pt, pt_b = k.psn()
            op_tr(k, pt[:, 0:128], s5.XX[i][0][:, 4 * ec:4 * ec + 4, :].rearrange("p a b -> p (a b)"), k.identF[:],
                  [s5.XX[i][1], k.identF_b], [pt_b])
            op_act(k, s5.LB[i][0][:, ec, :], pt[:, 0:128], AF.Copy, [pt_b], [s5.LB[i][1]])
    m4, m4_b, _ = s5.mask4
    P.op('dve', lambda e: e.memset(m4[:], 0.0), writes=[m4_b])
    big2, big2_b, _ = s5.big[2]
    P.op('dve', lambda e: e.tensor_reduce(out=big2[:, 4:6], in_=k.identF[:, :].rearrange("p (q g c) -> p g q c", q=4, g=2),
                                          axis=AX.XY, op=ALU.add), [k.identF_b], [big2_b])
    P.op('dve', lambda e: e.memset(m4[:], 1.0), writes=[m4_b])
    op_ts(k, 'dve', m4[:, 0:64], m4[:, 0:64], big2[:, 4:5], None, ALU.mult, None, [m4_b, big2_b], [m4_b])
    op_ts(k, 'dve', m4[:, 64:128], m4[:, 64:128], big2[:, 5:6], None, ALU.mult, None, [m4_b, big2_b], [m4_b])
    for i, src in ((0, "d_c_re"), (1, "d_c_im")):
        op_dma(k, 'sp', s5.CN[i][0][:, :, :], dr[src].rearrange("(e a) k p -> (a k) e p", a=8), [k.dbuf[src]], [s5.CN[i][1]])
        for ec in range(16):
            y4, y4_b, _ = s5.g[0]
            op_tt(k, 'dve', y4[:, 0:128].rearrange("p (a b) -> p a b", b=64), s5.CN[i][0][:, ec:ec + 1, :].to_broadcast([128, 2, 64]),
                  m4[:, :].rearrange("p (a b) -> p a b", b=64), ALU.mult, [s5.CN[i][1], m4_b], [y4_b])
            pt, pt_b = k.psn()
            op_tr(k, pt[:, 0:128], y4[:, 0:128], k.identF[:], [y4_b, k.identF_b], [pt_b])
            if i == 0:
                op_act(k, s5.LC[i][0][:, ec, :], pt[:, 0:128], AF.Copy, [pt_b], [s5.LC[i][1]])
            else:
                op_act(k, s5.LC[i][0][:, ec, :], pt[:, 0:128], AF.Copy, [pt_b], [s5.LC[i][1]], scale=-1.0)
    for i in range(2):
        P.op('dve', lambda e, i=i: e.tensor_copy(out=s5.LC3[i][0][:, :, :], in_=s5.LC[i][0][:, :, 64:128]), [s5.LC[i][1]], [s5.LC3[i][1]])
        P.op('dve', lambda e, i=i: e.memset(s5.LC3[i][0][:, :, 0:32], 0.0), writes=[s5.LC3[i][1]])
    for n, src in (("d", "d_d"), ("bg", "d_b_glu")):
        op_dma(k, 'sp', s5.par[n][0][:], dr[src].rearrange("(c p) -> p c", p=128), [k.dbuf[src]], [s5.par[n][1]], slow=True)
    m01, m01_b, _ = s5.m01
    P.op('pool', lambda e: e.memset(m01[:], 1.0), writes=[m01_b])
    P.op('pool', lambda e: e.memset(m01[:].rearrange("p (c t) -> p c t", t=S5C)[:, :, 0:1], 0.0), writes=[m01_b])


def s5_tables(k, tab, col):
    P, s5 = k.P, k.s5
    p = s5.P64
    A = lambda n: p[n][0]
    Bf = lambda n: p[n][1]
    TA = lambda n: tab[n][0][:, :]
    TB = lambda n: tab[n][1]
    P.op('pool', lambda e: e.iota(tab["c"][0][:].bitcast(I32)[:, 0:64], pattern=[[1, 64]], base=0, channel_multiplier=0),
         writes=[TB("c")])
    P.op('dve', lambda e: e.tensor_copy(out=TA("c"), in_=tab["c"][0][:].bitcast(I32)[:, 0:64]), [TB("c")], [TB("c")])
    op_ts(k, 'dve', TA("ang"), TA("c"), A("th")[:, col], None, ALU.mult, None, [TB("c"), Bf("th")], [TB("ang")])
    op_ts(k, 'dve', TA("mg"), TA("c"), A("lnm")[:, col], None, ALU.mult, None, [TB("c"), Bf("lnm")], [TB("mg")])
    s5_trig(k, TA("Fs"), TB("Fs"), TA("ang"), TB("ang"), PI_, 64)
    s5_trig(k, TA("Fc"), TB("Fc"), TA("ang"), TB("ang"), 1.5 * PI_, 64)
    op_act(k, TA("c"), TA("mg"), AF.Exp, [TB("mg")], [TB("c")])
    op_tt(k, 'dve', TA("Bc"), TA("Fc"), TA("c"), ALU.mult, [TB("Fc"), TB("c")], [TB("Bc")])
    op_tt(k, 'dve', TA("Bs"), TA("Fs"), TA("c"), ALU.mult, [TB("Fs"), TB("c")], [TB("Bs")])
    op_act(k, TA("c"), TA("mg"), AF.Exp, [TB("mg")], [TB("c")], scale=-1.0)
    op_tt(k, 'dve', TA("Fc"), TA("Fc"), TA("c"), ALU.mult, [TB("Fc"), TB("c")], [TB("Fc")])
    op_tt(k, 'dve', TA("Fs"), TA("Fs"), TA("c"), ALU.mult, [TB("Fs"), TB("c")], [TB("Fs")])
    P.op('pool', lambda e: e.iota(tab["c"][0][:].bitcast(I32)[:, 0:32], pattern=[[1, 32]], base=0, channel_multiplier=0),
         writes=[TB("c")])
    P.op('dve', lambda e: e.tensor_copy(out=tab["c"][0][:, 0:32], in_=tab["c"][0][:].bitcast(I32)[:, 0:32]), [TB("c")], [TB("c")])
    op_ts(k, 'dve', tab["ang"][0][:, 0:32], tab["c"][0][:, 0:32], A("ph")[:, col], None, ALU.mult, None, [TB("c"), Bf("ph")], [TB("ang")])
    s5_trig(k, tab["Gs"][0][:, 0:32], TB("Gs"), tab["ang"][0][:, 0:32], TB("ang"), PI_, 32)
    s5_trig(k, tab["Gc"][0][:, 0:32], TB("Gc"), tab["ang"][0][:, 0:32], TB("ang"), 1.5 * PI_, 32)


def layer_s5(k, ps_):
    P, dr, nc = k.P, k.dr, k.nc
    if not hasattr(k, "s5"):
        s5_setup(k)
    s5 = k.s5
    s5_load(k)
    P.barrier()
    P.chk("s5_load")
    tiles = ntiles(ps_)
    p = s5.P64
    A = lambda n: p[n][0]
    Bf = lambda n: p[n][1]
    d_w_in, d_w_in_b = dr["d_w_in"], k.dbuf["d_w_in"]
    U, U_b, _ = s5.U
    if ps_ == 0:
        for i, src in ((0, "state_ssm_re"), (1, "state_ssm_im")):
            for q4 in range(4):
                sr, sr_b, _ = s5.srow
                op_dma(k, 'sp', sr[:, :], dr[src].rearrange("s g p -> s (g p)")[:, q4 * 2048:(q4 + 1) * 2048], [k.dbuf[src]], [sr_b])
                for j in range(16):
                    sc = q4 * 16 + j
                    if j % 8 == 0:
                        pt, pt_b = k.psn()
                    op_tr(k, pt[:, (j % 8) * NS:(j % 8 + 1) * NS], sr[0:NS, j * 128:(j + 1) * 128], k.identF[0:NS, 0:NS],
                          [sr_b, k.identF_b], [pt_b])
                    if j % 8 == 7:
                        P.op('dve', lambda e, i=i, sc=sc, pt=pt: e.tensor_copy(
                            out=s5.s0T[i][0][:, sc - 7:sc + 1, :], in_=pt[:, 0:8 * NS].rearrange("p (a s) -> p a s", s=NS)),
                            [pt_b], [s5.s0T[i][1]])
    big = s5.big
    P.barrier()

    def ps56():
        i = 5 + k.ps_i[0] % 2
        k.ps_i[0] += 1
        return k.ps[i]
    for ec in range(16):
        load_w_cols(k, s5.wt[0], s5.wt[1], d_w_in, d_w_in_b, ec * 128)
        for ti, (c0, w) in enumerate(tiles):
            pu, pu_b = k.psn()
            for kk in range(8):
                op_mm(k, pu[:, 0:w], s5.wt[0][:, kk, :], k.hT[:, kk, c0 + 1:c0 + 1 + w], kk == 0, kk == 7,
                      [s5.wt[1]] + hT_reads(k, c0, w), [pu_b])
            op_act(k, U[:, c0:c0 + w], pu[:, 0:w], AF.Copy, [pu_b], [U_b.sub(ti)])
        Uz, Uz_b, _ = s5.Uz
        P.op('dve', lambda e: e.tensor_copy(out=Uz[64:128, :], in_=U[64:128, :]), [U_b], [Uz_b])
        P.op('dve', lambda e: e.memset(Uz[64:96, :], 0.0), writes=[Uz_b])
        P.chk("s5_u")
        py = {}
        for q in (0, 1, 3, 2):
            sc = ec * 4 + q
            col = slice(sc, sc + 1)
            qP = slice(q * 32, q * 32 + 32)
            tab = s5.tabs[sc % 2]
            TA = lambda n, tab=tab: tab[n][0][:, :]
            TB = lambda n, tab=tab: tab[n][1]
            if ps_ == 1:
                op_dma(k, 'sp', s5.pack[sc % 2][0][:, :, :], dr["tabscr"][sc], [k.dbuf["tabscr"]], [s5.pack[sc % 2][1]])
            if ps_ == 0:
                s5_tables(k, tab, col)
                op_dma(k, 'sp', dr["tabscr"][sc], s5.pack[sc % 2][0][:, :, :], [s5.pack[sc % 2][1]], [k.dbuf["tabscr"]])
            XR, XI, T1, T2 = [b_[0] for b_ in big[0:4]]
            XR_b, XI_b, T1_b, T2_b = [b_[1] for b_ in big[0:4]]
            QR_, QI_, QR_b, QI_b = XR, XI, XR_b, XI_b
            v3 = lambda t: t[:, :].rearrange("p (c t) -> p c t", t=S5C)
            bc = lambda n: tab[n][0][:, :].unsqueeze(1).to_broadcast([128, S5NCH, S5C])
            for ti, (c0, w) in enumerate(tiles):
                pbr, pbr_b = ps56()
                pbi, pbi_b = ps56()
                if q < 3:
                    op_mm(k, pbr[:, 0:w], s5.LB[0][0][qP, ec, :], U[qP, c0:c0 + w], True, True, [s5.LB[0][1], U_b.sub(ti)], [pbr_b])
                    op_mm(k, pbi[:, 0:w], s5.LB[1][0][qP, ec, :], U[qP, c0:c0 + w], True, True, [s5.LB[1][1], U_b.sub(ti)], [pbi_b])
                else:
                    op_mm(k, pbr[:, 0:w], s5.LB[0][0][64:128, ec, :], Uz[64:128, c0:c0 + w], True, True, [s5.LB[0][1], Uz_b], [pbr_b])
                    op_mm(k, pbi[:, 0:w], s5.LB[1][0][64:128, ec, :], Uz[64:128, c0:c0 + w], True, True, [s5.LB[1][1], Uz_b], [pbi_b])
                if c0 < T:
                    P.op('act', lambda e, c0=c0, pbr=pbr: e.activation(out=XR[:, c0:c0 + 512], in_=pbr[:, :], func=AF.Copy), [pbr_b], [XR_b])
                    P.op('act', lambda e, c0=c0, pbi=pbi: e.activation(out=XI[:, c0:c0 + 512], in_=pbi[:, :], func=AF.Copy), [pbi_b], [XI_b])
                else:
                    s0r, s0i = s5.s0T[0], s5.s0T[1]
                    tr_, ti__ = s5.tmpn[0], s5.tmpn[1]
                    op_stt(k, 'dve', tr_[0][:, :], s0r[0][:, sc, :], A("are")[:, col], pbr[:, 0:NS], ALU.mult, ALU.add,
                           [s0r[1], Bf("are"), pbr_b], [tr_[1]])
                    op_ts(k, 'dve', s5.g[1][0][:, 0:NS], s0i[0][:, sc, :], A("aim")[:, col], None, ALU.mult, None, [s0i[1], Bf("aim")], [s5.g[1][1]])
                    op_tt(k, 'dve', tr_[0][:, :], tr_[0][:, :], s5.g[1][0][:, 0:NS], ALU.subtract, [tr_[1], s5.g[1][1]], [tr_[1]])
                    op_stt(k, 'dve', ti__[0][:, :], s0i[0][:, sc, :], A("are")[:, col], pbi[:, 0:NS], ALU.mult, ALU.add,
                           [s0i[1], Bf("are"), pbi_b], [ti__[1]])
                    op_stt(k, 'dve', ti__[0][:, :], s0r[0][:, sc, :], A("aim")[:, col], ti__[0][:, :], ALU.mult, ALU.add,
                           [s0r[1], Bf("aim"), ti__[1]], [ti__[1]])
                    P.op('dve', lambda e, sc=sc: e.tensor_copy(out=s0r[0][:, sc, :], in_=tr_[0][:, :]), [tr_[1]], [s0r[1]])
                    P.op('dve', lambda e, sc=sc: e.tensor_copy(out=s0i[0][:, sc, :], in_=ti__[0][:, :]), [ti__[1]], [s0i[1]])
                    op_act(k, s5.sbf[0][0][:, T:T + NS], tr_[0][:, :], AF.Copy, [tr_[1]], [s5.sbf[0][1].sub(4)])
                    op_act(k, s5.sbf[1][0][:, T:T + NS], ti__[0][:, :], AF.Copy, [ti__[1]], [s5.sbf[1][1].sub(4)])
            op_tt(k, 'dve', v3(T1), v3(XR), bc("Fc"), ALU.mult, [XR_b, TB("Fc")], [T1_b])
            op_tt(k, 'pool', v3(T2), v3(XI), bc("Fs"), ALU.mult, [XI_b, TB("Fs")], [T2_b])
            op_tt(k, 'dve', v3(T1), v3(T1), v3(T2), ALU.add, [T1_b, T2_b], [T1_b])
            op_tt(k, 'pool', v3(T2), v3(XI), bc("Fc"), ALU.mult, [XI_b, TB("Fc")], [T2_b])
            op_tt(k, 'dve', v3(XI), v3(XR), bc("Fs"), ALU.mult, [XR_b, TB("Fs")], [XI_b])
            op_tt(k, 'pool', v3(T2), v3(T2), v3(XI), ALU.subtract, [T2_b, XI_b], [T2_b])
            m01 = s5.m01
            P.op('dve', lambda e: e.tensor_tensor_scan(out=QR_[:], data0=m01[0][:], data1=T1[:], initial=0.0, op0=ALU.mult, op1=ALU.add),
                 [m01[1], T1_b], [QR_b])
            P.op('dve', lambda e: e.tensor_tensor_scan(out=QI_[:], data0=m01[0][:], data1=T2[:], initial=0.0, op0=ALU.mult, op1=ALU.add),
                 [m01[1], T2_b], [QI_b])
            EE = s5.ec_
            e = lambda n: EE[n][0][:, 0:S5NCH]
            eb = lambda n: EE[n][1]
            qr_end = v3(QR_)[:, :, S5C - 1]
            qi_end = v3(QI_)[:, :, S5C - 1]
            bc63 = tab["Bc"][0][:, S5C - 1:S5C]
            bs63 = tab["Bs"][0][:, S5C - 1:S5C]
            op_ts(k, 'dve', e("er"), qr_end, bc63, None, ALU.mult, None, [QR_b, TB("Bc")], [eb("er")])
            op_ts(k, 'dve', e("hr"), qi_end, bs63, None, ALU.mult, None, [QI_b, TB("Bs")], [eb("hr")])
            op_tt(k, 'dve', e("er"), e("er"), e("hr"), ALU.subtract, [eb("er"), eb("hr")], [eb("er")])
            op_ts(k, 'dve', e("ei"), qr_end, bs63, None, ALU.mult, None, [QR_b, TB("Bs")], [eb("ei")])
            op_ts(k, 'dve', e("hr"), qi_end, bc63, None, ALU.mult, None, [QI_b, TB("Bc")], [eb("hr")])
            op_tt(k, 'dve', e("ei"), e("ei"), e("hr"), ALU.add, [eb("ei"), eb("hr")], [eb("ei")])
            Gc = tab["Gc"][0][:, 0:S5NCH]
            Gs = tab["Gs"][0][:, 0:S5NCH]
            op_tt(k, 'dve', e("hr"), e("er"), Gc, ALU.mult, [eb("er"), TB("Gc")], [eb("hr")])
            op_tt(k, 'dve', e("hi"), e("ei"), Gs, ALU.mult, [eb("ei"), TB("Gs")], [eb("hi")])
            op_tt(k, 'dve', e("hr"), e("hr"), e("hi"), ALU.add, [eb("hr"), eb("hi")], [eb("hr")])
            op_tt(k, 'dve', e("hi"), e("ei"), Gc, ALU.mult, [eb("ei"), TB("Gc")], [eb("hi")])
            op_tt(k, 'dve', e("cr"), e("er"), Gs, ALU.mult, [eb("er"), TB("Gs")], [eb("cr")])
            op_tt(k, 'dve', e("hi"), e("hi"), e("cr"), ALU.subtract, [eb("hi"), eb("cr")], [eb("hi")])
            op_ts(k, 'dve', e("ci"), Gc, 0.0, A("m64")[:, col], ALU.mult, ALU.add, [TB("Gc"), Bf("m64")], [eb("ci")])
            P.op('dve', lambda e_: e_.tensor_tensor_scan(out=e("Er"), data0=e("ci"), data1=e("hr"), initial=0.0, op0=ALU.mult, op1=ALU.add),
                 [eb("ci"), eb("hr")], [eb("Er")])
            P.op('dve', lambda e_: e_.tensor_tensor_scan(out=e("Ei"), data0=e("ci"), data1=e("hi"), initial=0.0, op0=ALU.mult, op1=ALU.add),
                 [eb("ci"), eb("hi")], [eb("Ei")])
            op_tt(k, 'dve', e("hr"), e("Er"), Gc, ALU.mult, [eb("Er"), TB("Gc")], [eb("hr")])
            op_tt(k, 'dve', e("cr"), e("Ei"), Gs, ALU.mult, [eb("Ei"), TB("Gs")], [eb("cr")])
            op_tt(k, 'dve', e("hr"), e("hr"), e("cr"), ALU.subtract, [eb("hr"), eb("cr")], [eb("hr")])
            op_tt(k, 'dve', e("hi"), e("Er"), Gs, ALU.mult, [eb("Er"), TB("Gs")], [eb("hi")])
            op_tt(k, 'dve', e("cr"), e("Ei"), Gc, ALU.mult, [eb("Ei"), TB("Gc")], [eb("cr")])
            op_tt(k, 'dve', e("hi"), e("hi"), e("cr"), ALU.add, [eb("hi"), eb("cr")], [eb("hi")])
            P.op('dve', lambda e_, sc=sc: e_.tensor_copy(out=s5.fin[0][0][:, sc:sc + 1], in_=EE["hr"][0][:, S5NCH - 1:S5NCH]), [eb("hr")], [s5.fin[0][1]])
            P.op('dve', lambda e_, sc=sc: e_.tensor_copy(out=s5.fin[1][0][:, sc:sc + 1], in_=EE["hi"][0][:, S5NCH - 1:S5NCH]), [eb("hi")], [s5.fin[1][1]])
            n1 = S5NCH - 1
            op_ts(k, 'dve', EE["cr"][0][:, 0:n1], EE["hr"][0][:, 0:n1], A("are")[:, col], None, ALU.mult, None, [eb("hr"), Bf("are")], [eb("cr")])
            op_ts(k, 'dve', EE["ci"][0][:, 0:n1], EE["hi"][0][:, 0:n1], A("aim")[:, col], None, ALU.mult, None, [eb("hi"), Bf("aim")], [eb("ci")])
            op_tt(k, 'dve', EE["cr"][0][:, 0:n1], EE["cr"][0][:, 0:n1], EE["ci"][0][:, 0:n1], ALU.subtract, [eb("cr"), eb("ci")], [eb("cr")])
            op_ts(k, 'dve', EE["ci"][0][:, 0:n1], EE["hr"][0][:, 0:n1], A("aim")[:, col], None, ALU.mult, None, [eb("hr"), Bf("aim")], [eb("ci")])
            op_ts(k, 'dve', EE["er"][0][:, 0:n1], EE["hi"][0][:, 0:n1], A("are")[:, col], None, ALU.mult, None, [eb("hi"), Bf("are")], [eb("er")])
            op_tt(k, 'dve', EE["ci"][0][:, 0:n1], EE["ci"][0][:, 0:n1], EE["er"][0][:, 0:n1], ALU.add, [eb("ci"), eb("er")], [eb("ci")])
            op_tt(k, 'dve', v3(T1)[:, 1:S5NCH, 0], v3(T1)[:, 1:S5NCH, 0], EE["cr"][0][:, 0:n1], ALU.add, [T1_b, eb("cr")], [T1_b])
            op_tt(k, 'dve', v3(T2)[:, 1:S5NCH, 0], v3(T2)[:, 1:S5NCH, 0], EE["ci"][0][:, 0:n1], ALU.add, [T2_b, eb("ci")], [T2_b])
            P.op('dve', lambda e_: e_.tensor_tensor_scan(out=QR_[:], data0=m01[0][:], data1=T1[:], initial=0.0, op0=ALU.mult, op1=ALU.add),
                 [m01[1], T1_b], [QR_b])
            P.op('dve', lambda e_: e_.tensor_tensor_scan(out=QI_[:], data0=m01[0][:], data1=T2[:], initial=0.0, op0=ALU.mult, op1=ALU.add),
                 [m01[1], T2_b], [QI_b])
            op_tt(k, 'dve', v3(T1), v3(QR_), bc("Bc"), ALU.mult, [QR_b, TB("Bc")], [T1_b])
            op_tt(k, 'pool', v3(T2), v3(QI_), bc("Bs"), ALU.mult, [QI_b, TB("Bs")], [T2_b])
            op_tt(k, 'dve', s5.sbf[0][0][:, 0:T].rearrange("p (c t) -> p c t", t=S5C), v3(T1), v3(T2), ALU.subtract, [T1_b, T2_b],
                  [s5.sbf[0][1].sub(0)])
            op_tt(k, 'pool', v3(T1), v3(QR_), bc("Bs"), ALU.mult, [QR_b, TB("Bs")], [T1_b])
            op_tt(k, 'dve', v3(T2), v3(QI_), bc("Bc"), ALU.mult, [QI_b, TB("Bc")], [T2_b])
            op_tt(k, 'pool', s5.sbf[1][0][:, 0:T].rearrange("p (c t) -> p c t", t=S5C), v3(T1), v3(T2), ALU.add, [T1_b, T2_b],
                  [s5.sbf[1][1].sub(0)])
            for ti, (c0, w) in enumerate(tiles):
                if q == 0:
                    py[ti] = k.ps[ti]
                pyt, pyt_b = py[ti]
                rs_ = [s5.sbf[0][1].sub(0 if c0 < T else 4), s5.sbf[1][1].sub(0 if c0 < T else 4)]
                if q < 2:
                    op_mm(k, pyt[qP, 0:w], s5.LC[0][0][:, ec, qP], s5.sbf[0][0][:, c0:c0 + w], True, False, [s5.LC[0][1]] + rs_, [pyt_b])
                    op_mm(k, pyt[qP, 0:w], s5.LC[1][0][:, ec, qP], s5.sbf[1][0][:, c0:c0 + w], False, True, [s5.LC[1][1]] + rs_, [pyt_b])
                elif q == 3:
                    op_mm(k, pyt[64:128, 0:w], s5.LC3[0][0][:, ec, :], s5.sbf[0][0][:, c0:c0 + w], True, False, [s5.LC3[0][1]] + rs_, [pyt_b])
                    op_mm(k, pyt[64:128, 0:w], s5.LC3[1][0][:, ec, :], s5.sbf[1][0][:, c0:c0 + w], False, False, [s5.LC3[1][1]] + rs_, [pyt_b])
                else:
                    op_mm(k, pyt[64:96, 0:w], s5.LC[0][0][:, ec, 64:96], s5.sbf[0][0][:, c0:c0 + w], False, False, [s5.LC[0][1]] + rs_, [pyt_b])
                    op_mm(k, pyt[64:96, 0:w], s5.LC[1][0][:, ec, 64:96], s5.sbf[1][0][:, c0:c0 + w], False, True, [s5.LC[1][1]] + rs_, [pyt_b])
            P.chk("s5_sc")
        for ti, (c0, w) in enumerate(tiles):
            pyt, pyt_b = py[ti]
            y_, y_b, _ = s5.g[0]
            t_, t_b, _ = s5.g[1]
            s_, s_b, _ = s5.g[2]
            op_stt(k, 'dve', y_[:, 0:w], U[:, c0:c0 + w], s5.par["d"][0][:, ec:ec + 1], pyt[:, 0:w], ALU.mult, ALU.add,
                   [U_b.sub(ti), s5.par["d"][1], pyt_b], [y_b])
            op_act(k, t_[:, 0:w], y_[:, 0:w], AF.Square, [y_b], [t_b])
            op_ts(k, 'dve', t_[:, 0:w], t_[:, 0:w], 0.044715, 1.0, ALU.mult, ALU.add, [t_b], [t_b])
            op_tt(k, 'dve', t_[:, 0:w], t_[:, 0:w], y_[:, 0:w], ALU.mult, [t_b, y_b], [t_b])
            op_act(k, s_[:, 0:w], t_[:, 0:w], AF.Sigmoid, [t_b], [s_b], scale=1.5957691216057308)
            op_tt(k, 'dve', k.GT[:, ec, c0:c0 + w], y_[:, 0:w], s_[:, 0:w], ALU.mult, [y_b, s_b], [k.GT_b.sub(ec).sub(ti)])
        P.chk("s5_ec")
    P.barrier()
    for i, nm in ((0, "sre_p"), (1, "sim_p")):
        for g2 in range(2):
            op_dma(k, 'sp', dr[nm][ps_].rearrange("(c g2) p -> g2 p c", g2=2)[g2], s5.fin[i][0][g2 * 64:(g2 + 1) * 64, :],
                   [s5.fin[i][1]], [k.dbuf[nm]], slow=True)
    if ps_ == 0:
        for i, nm in ((0, "sre_s"), (1, "sim_s")):
            for q4 in range(4):
                sr, sr_b, _ = s5.srow
                for j in range(16):
                    sc = q4 * 16 + j
                    if j % 4 == 0:
                        pt, pt_b = k.psn()
                    op_tr(k, pt[0:NS, (j % 4) * 128:(j % 4 + 1) * 128], s5.s0T[i][0][:, sc, :], k.identF[:], [s5.s0T[i][1], k.identF_b], [pt_b])
                    if j % 4 == 3:
                        P.op('dve', lambda e, j=j, pt=pt: e.tensor_copy(out=sr[0:NS, (j - 3) * 128:(j + 1) * 128], in_=pt[0:NS, 0:512]),
                             [pt_b], [sr_b])
                op_dma(k, 'sp', dr[nm].rearrange("s g p -> s (g p)")[:, q4 * 2048:(q4 + 1) * 2048], sr[0:NS, :], [sr_b], [k.dbuf[nm]])
    P.barrier()
    gscr, gscr_b = dr["gscr"], k.dbuf["gscr"]
    for e2 in range(16):
        wg, wg_b, _ = s5.wg
        op_dma(k, 'pool', wg[:, :, :], dr["d_w_glu"][:, e2 * 128:(e2 + 1) * 128].rearrange("(c p) e -> p c e", p=128), [k.dbuf["d_w_glu"]], [wg_b])
        load_w_cols(k, s5.wt[0], s5.wt[1], d_w_in, d_w_in_b, E + e2 * 128)
        for ti, (c0, w) in enumerate(tiles):
            pg, pg_b = k.psn()
            for ec in range(16):
                op_mm(k, pg[:, 0:w], wg[:, ec, :], k.GT[:, ec, c0:c0 + w], ec == 0, ec == 15, [wg_b, k.GT_b.sub(ec).sub(ti)], [pg_b])
            pz, pz_b = k.psn()
            for kk in range(8):
                op_mm(k, pz[:, 0:w], s5.wt[0][:, kk, :], k.hT[:, kk, c0 + 1:c0 + 1 + w], kk == 0, kk == 7,
                      [s5.wt[1]] + hT_reads(k, c0, w), [pz_b])
            s_, s_b, _ = s5.g[0]
            z_, z_b, _ = s5.g[1]
            op_act(k, s_[:, 0:w], pg[:, 0:w], AF.Sigmoid, [pg_b, s5.par["bg"][1]], [s_b], bias=s5.par["bg"][0][:, e2:e2 + 1])
            op_act(k, z_[:, 0:w], pz[:, 0:w], AF.Silu, [pz_b], [z_b])
            op_tt(k, 'dve', s_[:, 0:w], s_[:, 0:w], k.GT[:, e2, c0:c0 + w], ALU.mult, [s_b, k.GT_b.sub(e2).sub(ti)], [s_b])
            gb, gb_b, _ = s5.gb
            op_tt(k, 'dve', gb[:, 0:w], s_[:, 0:w], z_[:, 0:w], ALU.mult, [s_b, z_b], [gb_b])
            op_dma(k, 'sp', gscr[e2, :, c0:c0 + w], gb[:, 0:w], [gb_b], [gscr_b])
    P.barrier()
    TW = TWA if ps_ == 0 else T
    for e2 in range(16):
        op_dma(k, 'sp', k.GT[:, e2, 0:TW], gscr[e2, :, 0:TW], [gscr_b], [k.GT_b])
N_LAYERS = 4

_CACHE = {}


def kernel(**inp):
    n_layers = N_LAYERS
    N_IN = N_IN_BY_LAYER[n_layers]
    if "nc" not in _CACHE:
        _CACHE["nc"] = build(n_layers)
    nc, k = _CACHE["nc"]
    f = lambda a: np.ascontiguousarray(np.asarray(a))
    shared = {}
    for name, shp, dt in IN_SHAPES[:N_IN]:
        if name in ("xp", "xs", "state_conv", "state_shift", "state_wkv", "state_ssm_re", "state_ssm_im", "page_table"):
            continue
        if name == "cache_cat":
            shared[name] = np.concatenate([np.asarray(inp["cache_krope"]), np.asarray(inp["cache_ckv"])], axis=-1)
            continue
        shared[name] = f(inp[name])
    in_maps = []
    for c in range(NCORES):
        m = dict(shared)
        m["xp"] = f(inp["x_prompt"][2 * c:2 * c + 2])
        m["xs"] = f(inp["x_sample"][NS * c:NS * (c + 1), 0, :])
        for nm in ("state_conv", "state_shift", "state_wkv", "state_ssm_re", "state_ssm_im", "page_table"):
            if any(nm == x[0] for x in IN_SHAPES[:N_IN]):
                m[nm] = f(inp[nm][NS * c:NS * (c + 1)])
        in_maps.append(m)
    res = run_bass_kernel_spmd(nc, in_maps, core_ids=list(range(NCORES)))
    R = res.results
    cat = lambda nm: np.concatenate([np.asarray(R[c][nm]) for c in range(NCORES)], axis=0)
    y_p = cat("y_p")
    y_s = cat("y_s").reshape(128, 1, D)
    outs = (y_p, y_s, cat("conv_p"), cat("conv_s"), cat("shift_p"), cat("shift_s"), cat("wkv_p"), cat("wkv_s"),
            cat("ckv_p"), cat("ckv_s").reshape(128, 1, 256), cat("kr_p"), cat("kr_s").reshape(128, 1, 64),
            cat("sre_p"), cat("sre_s"), cat("sim_p"), cat("sim_s"))
    return tuple(np.ascontiguousarray(o, dtype=np.float32) for o in outs)
```

```python
from concourse.bass_utils import run_bass_kernel_spmd
import numpy as np
import concourse.bass as bass
import concourse.mybir as mybir

F32 = mybir.dt.float32
BF16 = mybir.dt.bfloat16
I32 = mybir.dt.int32
AF = mybir.ActivationFunctionType
ALU = mybir.AluOpType
AX = mybir.AxisListType

NDS = 6
ENG = ['pe', 'act', 'dve', 'pool', 'sp']
BLK = {'pe': 'tensor', 'act': 'scalar', 'dve': 'vector', 'pool': 'gpsimd', 'sp': 'sync'}


class Buf:
    __slots__ = ('name', 'w', 'r', 'parent', 'kids')

    def __init__(s, name, parent=None):
        s.name = name
        s.w = None
        s.r = []
        s.parent = parent
        s.kids = {}

    def sub(s, key):
        k = s.kids.get(key)
        if k is None:
            k = Buf(f'{s.name}.{key}', s)
            s.kids[key] = k
        return k

    def family(s):
        out = [s]
        p = s.parent
        while p is not None:
            out.append(p)
            p = p.parent
        if s.kids:
            st = list(s.kids.values())
            while st:
                k = st.pop()
                out.append(k)
                if k.kids:
                    st.extend(k.kids.values())
        return out


class Op:
    __slots__ = ('eng', 'fn', 'waits', 'idx', 'inc', 'dma', 'semk', 'val', 'seq')


class Prog:
    def __init__(s):
        s.q = {e: [] for e in ENG}
        s.seen = {e: {} for e in ENG}
        s.rr = {e: 0 for e in ENG}
        s.dlast = {}
        s.dcount = {}
        s.alldma = []

    def op(s, eng, fn, reads=(), writes=(), dma=False):
        if getattr(s, "dead", False):
            return None
        o = Op()
        o.eng = eng
        o.fn = fn
        o.dma = dma
        o.inc = bool(dma)
        o.waits = []
        o.idx = len(s.q[eng])
        o.semk = None
        o.val = None
        o.seq = None
        deps = {}
        for b in reads:
            for f in b.family():
                if f.w is not None:
                    deps[id(f.w)] = f.w
        for b in writes:
            for f in b.family():
                if f.w is not None:
                    deps[id(f.w)] = f.w
                for r in f.r:
                    deps[id(r)] = r
        if dma:
            k = s.rr[eng] % NDS
            s.rr[eng] += 1
            o.semk = k
            prev = s.dlast.get((eng, k))
            if prev is not None:
                deps[id(prev)] = prev
            s.dlast[(eng, k)] = o
            o.seq = s.dcount.get((eng, k), 0) + 1
            s.dcount[(eng, k)] = o.seq
            s.alldma.append(o)
        seen = s.seen[eng]
        for d in deps.values():
            if d.dma:
                key = (d.eng, d.semk)
                v = d.seq
            else:
                if d.eng == eng and eng == 'pe':
                    continue
                key = d.eng
                v = d.idx
            if seen.get(key, -1) >= v:
                continue
            seen[key] = v
            o.waits.append(d)
            d.inc = True
        for b in reads:
            b.r.append(o)
        for b in writes:
            b.w = o
            b.r = []
        s.q[eng].append(o)
        return o

    def chk(s, name):
        import os
        if os.environ.get("KSTOP", "") == name:
            s.dead = True

    def barrier(s):
        if getattr(s, "dead", False):
            return
        lasts = []
        for e in ENG:
            for o in reversed(s.q[e]):
                if o.fn is not None and not o.dma:
                    lasts.append(o)
                    break
        dl = list(s.dlast.values())
        for e in ENG:
            o = Op()
            o.eng = e
            o.fn = None
            o.dma = False
            o.inc = False
            o.waits = []
            o.idx = len(s.q[e])
            o.semk = None
            o.val = None
            o.seq = None
            seen = s.seen[e]
            for d in lasts + dl:
                if d.dma:
                    key = (d.eng, d.semk)
                    v = d.seq
                else:
                    if d.eng == e:
                        continue
                    key = d.eng
                    v = d.idx
                if seen.get(key, -1) >= v:
                    continue
                seen[key] = v
                o.waits.append(d)
                d.inc = True
            s.q[e].append(o)

    def finish(s, eng='sp'):
        o = Op()
        o.eng = eng
        o.fn = None
        o.dma = False
        o.inc = False
        o.waits = list(s.dlast.values())
        o.idx = len(s.q[eng])
        s.q[eng].append(o)

    def emit(s, nc):
        sems = {}
        for e in ENG:
            sems[e] = nc.alloc_semaphore(f'S_{e}')
            for k in range(NDS):
                sems[(e, k)] = nc.alloc_semaphore(f'D_{e}_{k}')
        for e in ENG:
            c = 0
            for o in s.q[e]:
                if o.dma:
                    o.val = 16 * o.seq
                elif o.inc:
                    c += 1
                    o.val = c
        n_ins = {e: 0 for e in ENG}
        with nc.Block() as block:
            for e in ENG:
                def body(eng, e=e):
                    for o in s.q[e]:
                        for d in o.waits:
                            sm = sems[(d.eng, d.semk)] if d.dma else sems[d.eng]
                            eng.wait_ge(sm, d.val)
                            n_ins[e] += 1
                        if o.fn is None:
                            continue
                        ins = o.fn(eng)
                        n_ins[e] += 1
                        if o.dma:
                            ins.then_inc(sems[(e, o.semk)], 16)
                        elif o.inc:
                            ins.then_inc(sems[e], 1)
                getattr(block, BLK[e])(body)
        return n_ins

import math
NCORES = 8
T = 2048
D = 1024
E = 2048
NS = 16
TWA = T + NS
SB0 = 16640
SBMAX = 229376

OUT_SHAPES = [
    ("y_p", [2, T, D]), ("y_s", [NS, D]),
    ("conv_p", [2, 30, E]), ("conv_s", [NS, 30, E]),
    ("shift_p", [2, D]), ("shift_s", [NS, D]),
    ("wkv_p", [2, 32, 64, 64]), ("wkv_s", [NS, 32, 64, 64]),
    ("ckv_p", [2, T, 256]), ("ckv_s", [NS, 256]),
    ("kr_p", [2, T, 64]), ("kr_s", [NS, 64]),
    ("sre_p", [2, 128, 64]), ("sre_s", [NS, 128, 64]),
    ("sim_p", [2, 128, 64]), ("sim_s", [NS, 128, 64]),
]

IN_SHAPES = [
    ("xp", [2, T, D], F32), ("xs", [NS, D], F32),
    ("state_conv", [NS, 30, E], F32), ("state_shift", [NS, D], F32),
    ("norm_pre", [4, D], F32), ("norm_post", [4, D], F32),
    ("a_w_in", [D, 3 * E], F32), ("a_b_in", [3 * E], F32), ("a_conv_w", [31, E], F32),
    ("a_conv_b", [E], F32), ("a_ln_g", [E], F32), ("a_ln_b", [E], F32), ("a_w_out", [E, D], F32),
    ("state_wkv", [NS, 32, 64, 64], F32),
    ("b_mu", [6, D], F32), ("b_w_rkvz", [4, D, E], F32), ("b_w0", [E], F32), ("b_w1", [D, 64], F32), ("b_w2", [64, E], F32),
    ("b_a0", [E], F32), ("b_a1", [D, 64], F32), ("b_a2", [64, E], F32), ("b_k_k", [E], F32), ("b_k_a", [E], F32),
    ("b_r_k", [32, 64], F32), ("b_ln_g", [E], F32), ("b_ln_b", [E], F32), ("b_w_out", [E, D], F32),
    ("cache_cat", [10240, 128, 320], F32), ("page_table", [NS, 64], I32),
    ("c_w_in", [D, 2752], F32), ("c_q_norm", [384], F32), ("c_kv_norm", [256], F32), ("c_w_uq", [384, 16, 192], F32),
    ("c_w_uk", [256, 16, 128], F32), ("c_w_uv", [256, 16, 128], F32), ("c_w_out", [E, D], F32),
    ("state_ssm_re", [NS, 128, 64], F32), ("state_ssm_im", [NS, 128, 64], F32),
    ("d_w_in", [D, 2 * E], F32), ("d_lambda_re", [128, 64], F32), ("d_lambda_im", [128, 64], F32), ("d_log_dt", [128], F32),
    ("d_b_re", [128, 64, 16], F32), ("d_b_im", [128, 64, 16], F32), ("d_c_re", [128, 16, 64], F32), ("d_c_im", [128, 16, 64], F32),
    ("d_d", [E], F32), ("d_w_glu", [E, E], F32), ("d_b_glu", [E], F32), ("d_w_out", [E, D], F32),
]


N_IN_BY_LAYER = {1: 13, 2: 28, 3: 37, 4: 51}


class K:
    pass


def build(n_layers=1):
    nc = bass.Bass("TRN2", target_bir_lowering=False)
    P = Prog()
    k = K()
    k.nc = nc
    k.P = P
    dr = {}
    for name, shp, dt in IN_SHAPES[:N_IN_BY_LAYER[n_layers]]:
        dr[name] = nc.dram_tensor(name, shp, dt, kind="ExternalInput").ap()
    for name, shp in OUT_SHAPES:
        dr[name] = nc.dram_tensor(name, shp, F32, kind="ExternalOutput").ap()
    dr["xscr"] = nc.dram_tensor("xscr", [2, T, D], F32, kind="Internal").ap()
    dr["xscr_s"] = nc.dram_tensor("xscr_s", [NS, D], F32, kind="Internal").ap()
    dr["xscr2"] = nc.dram_tensor("xscr2", [2, T, D], F32, kind="Internal").ap()
    dr["xscr2_s"] = nc.dram_tensor("xscr2_s", [NS, D], F32, kind="Internal").ap()
    dr["gscr"] = nc.dram_tensor("gscr", [16, 128, TWA], BF16, kind="Internal").ap()
    dr["tabscr"] = nc.dram_tensor("tabscr", [64, 128, 6, 64], F32, kind="Internal").ap()
    k.dr = dr
    k.dbuf = {n: Buf("dram_" + n) for n in dr}

    off = [SB0]

    def sb(name, shape, dt, at=None):
        esz = 2 if dt == BF16 else 4
        nbytes = int(np.prod(shape[1:])) * esz
        if at is None:
            o = off[0]
            off[0] += (nbytes + 63) // 64 * 64
            assert off[0] <= SBMAX, (name, off[0])
        else:
            o = at
        t = nc.alloc_sbuf_tensor_at(name, shape, dt, offset=o)
        return t, Buf(name), o

    k.sb = sb
    k.ps = []
    for i in range(7):
        k.ps.append((nc.alloc_psum_tensor(f"ps{i}", [128, 512], F32), Buf(f"ps{i}")))
    k.psT = (nc.alloc_psum_tensor("psT", [128, 1024], BF16), Buf("psT"))
    k.ps_i = [0]

    def psn():
        i = k.ps_i[0] % 7
        k.ps_i[0] += 1
        return k.ps[i]
    k.psn = psn

    k.identF, k.identF_b, _ = sb("identF", [128, 128], F32)
    k.identB, k.identB_b, _ = sb("identB", [128, 128], BF16)
    k.onesB, k.onesB_b, _ = sb("onesB", [128, 128], BF16)
    k.epsr, k.epsr_b, _ = sb("epsr", [128, 4], F32)
    P.op('pool', lambda e: e.memset(k.identF[:], 0.0), writes=[k.identF_b])
    P.op('pool', lambda e: e.affine_select(out=k.identF[:], in_=k.identF[:], pattern=[[-1, 128]],
                                           compare_op=ALU.not_equal, fill=1.0, base=0, channel_multiplier=1),
         reads=[k.identF_b], writes=[k.identF_b])
    P.op('dve', lambda e: e.tensor_copy(out=k.identB[:], in_=k.identF[:]), reads=[k.identF_b], writes=[k.identB_b])
    P.op('dve', lambda e: e.memset(k.onesB[:], 1.0), writes=[k.onesB_b])
    P.op('dve', lambda e: e.memset(k.epsr[:, 0:1], 1e-6), writes=[k.epsr_b])
    P.op('dve', lambda e: e.memset(k.epsr[:, 1:2], 1e-5), writes=[k.epsr_b])
    P.op('dve', lambda e: e.memset(k.epsr[:, 2:3], 64e-5), writes=[k.epsr_b])
    P.op('dve', lambda e: e.memset(k.epsr[:, 3:4], 0.0), writes=[k.epsr_b])

    k.hT, k.hT_b, k.hT_off = sb("hT", [128, 8, TWA + 1], BF16)
    k.GT, k.GT_b, _ = sb("GT", [128, 16, TWA], BF16)
    k.wo = (nc.alloc_sbuf_tensor_at("wo", [128, 16, D], BF16, offset=k.hT_off), k.hT_b)
    k.layer_base = off[0]
    k.gpre, k.gpre_b, _ = sb("gpre", [128, D], F32)
    k.gpost, k.gpost_b = k.gpre, k.gpre_b
    k.xt = [sb(f"xt{i}", [128, D], F32) for i in range(2)]
    hn_par = Buf("hnpar")
    k.hn = []
    for i in range(2):
        t_, _, o_ = sb(f"hn{i}", [128, D], BF16)
        k.hn.append((t_, hn_par.sub(i), o_))
    k.of = nc.alloc_sbuf_tensor_at("of", [128, D], F32, offset=k.hn[0][2])
    k.of_b = hn_par
    k.sqf = lambda np_, h: k.of[0:np_, h * 512:(h + 1) * 512]
    k.sq = sb("sqjunk", [128, D], BF16)
    k.st = [sb(f"stat{i}", [128, 8], F32) for i in range(4)]
    k.st_i = [0]
    k.hf = sb("hf32", [128, D], F32)
    k.off = off

    x_src = (dr["xp"], dr["xs"], k.dbuf["xp"], k.dbuf["xs"])
    wouts = ["a_w_out", "b_w_out", "c_w_out", "d_w_out"]
    for l in range(n_layers):
        x_dst = (dr["y_p"], dr["y_s"], k.dbuf["y_p"], k.dbuf["y_s"]) if (l == n_layers - 1) else \
            ((dr["xscr"], dr["xscr_s"], k.dbuf["xscr"], k.dbuf["xscr_s"]) if l % 2 == 0 else
             (dr["xscr2"], dr["xscr2_s"], k.dbuf["xscr2"], k.dbuf["xscr2_s"]))
        for ps_ in range(2):
            stage1(k, l, ps_, x_src)
            P.barrier()
            off[0] = k.layer_base
            if l == 0:
                layer_conv(k, ps_)
            elif l == 1:
                layer_rwkv(k, ps_)
            elif l == 2:
                layer_mla(k, ps_)
            elif l == 3:
                layer_s5(k, ps_)
            P.barrier()
            stage3(k, l, ps_, x_src, x_dst, dr[wouts[l]], k.dbuf[wouts[l]])
            P.barrier()
        x_src = x_dst
    P.finish('sp')
    k.n_ins = P.emit(nc)
    return nc, k


def ntiles(ps_):
    tl = [(i * 512, 512) for i in range(4)]
    if ps_ == 0:
        tl.append((T, NS))
    return tl


def rstd_from_ss(k, ss_ap, ss_b, out_ap, out_b, npart, inv_n, eps_col):
    P = k.P
    P.op('act', lambda e: e.activation(out=out_ap, in_=ss_ap, func=AF.Sqrt, scale=inv_n,
                                       bias=k.epsr[0:npart, eps_col:eps_col + 1]),
         reads=[ss_b, k.epsr_b], writes=[out_b])
    P.op('dve', lambda e: e.reciprocal(out=out_ap, in_=out_ap), reads=[out_b], writes=[out_b])


def stage1(k, l, ps_, x_src):
    P, dr = k.P, k.dr
    xp, xs, xp_b, xs_b = x_src
    P.op('dve', lambda e: e.memset(k.hT[:, :, 0:1], 0.0), writes=[k.hT_b])
    P.op('sp', lambda e: e.dma_start(out=k.gpre[:], in_=dr["norm_pre"][l:l + 1, :].partition_broadcast(128)),
         writes=[k.gpre_b], dma=True)
    tiles = [(xp[ps_, tt * 128:(tt + 1) * 128, :], 128, tt * 128, xp_b) for tt in range(16)]
    if ps_ == 0:
        tiles.append((xs[:, :], NS, T, xs_b))
    for i, (src, np_, c0, src_b) in enumerate(tiles):
        xt, xt_b, _ = k.xt[i % 2]
        hn, hn_b, _ = k.hn[i % 2]
        sq, sq_b, _ = k.sq
        st, st_b, _ = k.st[k.st_i[0] % 4]
        k.st_i[0] += 1
        P.op('sp', lambda e, xt=xt, src=src, np_=np_: e.dma_start(out=xt[0:np_, :], in_=src),
             reads=[src_b], writes=[xt_b], dma=True)
        P.op('act', lambda e, xt=xt, sq=sq, st=st, np_=np_: e.activation(out=sq[0:np_, :], in_=xt[0:np_, :], func=AF.Square,
                                                                         accum_out=st[0:np_, 0:1]),
             reads=[xt_b], writes=[sq_b, st_b])
        rstd_from_ss(k, st[0:np_, 0:1], st_b, st[0:np_, 1:2], st_b, np_, 1.0 / D, 0)
        P.op('dve', lambda e, hn=hn, xt=xt, st=st, np_=np_: e.scalar_tensor_tensor(
            out=hn[0:np_, :], in0=xt[0:np_, :], scalar=st[0:np_, 1:2], in1=k.gpre[0:np_, :], op0=ALU.mult, op1=ALU.mult),
            reads=[xt_b, st_b, k.gpre_b], writes=[hn_b])
        psT, psT_b = k.psT
        for kk in range(8):
            P.op('pe', lambda e, hn=hn, kk=kk, np_=np_: e.transpose(out=psT[:, kk * 128:kk * 128 + np_],
                                                                    in_=hn[0:np_, kk * 128:(kk + 1) * 128],
                                                                    identity=k.identB[0:np_, 0:np_]),
                 reads=[hn_b, k.identB_b], writes=[psT_b])
        P.op('act', lambda e, c0=c0, np_=np_: e.activation(
            out=k.hT[:, :, c0 + 1:c0 + 1 + np_], in_=psT[:].rearrange("p (k t) -> p k t", k=8)[:, :, 0:np_], func=AF.Copy),
            reads=[psT_b], writes=[k.hT_b.sub(c0 // 128)])
        if l == 1 and (i == 15 or np_ == NS):
            hf, hf_b, _ = k.hf
            P.op('dve', lambda e, hf=hf, xt=xt, st=st, np_=np_: e.scalar_tensor_tensor(
                out=hf[0:np_, :], in0=xt[0:np_, :], scalar=st[0:np_, 1:2], in1=k.gpre[0:np_, :], op0=ALU.mult, op1=ALU.mult),
                reads=[xt_b, st_b, k.gpre_b], writes=[hf_b])
            if np_ == NS:
                P.op('sp', lambda e, hf=hf: e.dma_start(out=dr["shift_s"][:, :], in_=hf[0:NS, :]), reads=[hf_b],
                     writes=[k.dbuf["shift_s"]], dma=True)
            else:
                P.op('sp', lambda e, hf=hf: e.dma_start(out=dr["shift_p"][ps_:ps_ + 1, :], in_=hf[127:128, :]), reads=[hf_b],
                     writes=[k.dbuf["shift_p"]], dma=True)


def hT_reads(k, c0, w):
    return [k.hT_b.sub(c) for c in range(c0 // 128, (c0 + w + 127) // 128)]


def load_w_cols(k, wt, wt_b, w_dram, w_dram_b, col0, ncols=128, nk=8):
    src = w_dram[:, col0:col0 + ncols].rearrange("(k p) e -> p k e", p=128)
    k.P.op('pool', lambda e: e.dma_start(out=wt[:, 0:nk, 0:ncols], in_=src), reads=[w_dram_b], writes=[wt_b], dma=True)


def layer_conv(k, ps_):
    P, dr, nc, sb = k.P, k.dr, k.nc, k.sb
    TW = TWA if ps_ == 0 else T
    if not hasattr(k, "cv"):
        cv = K()
        k.cv = cv
        cv.wt = [[sb(f"cvw{j}_{i}", [128, 8, 128], BF16) for i in range(2)] for j in range(2)]
        cv.wt.append(cv.wt[0])
        cv.cw = sb("cv_cw", [128, 16, 31], F32)
        cv.bin = sb("cv_bin", [128, 48], F32)
        cv.cb = sb("cv_cb", [128, 16], F32)
        cv.lg = sb("cv_lg", [128, 16], F32)
        cv.lb = sb("cv_lb", [128, 16], F32)
        cv.dg = sb("cv_dg", [128, 31, 128], BF16)
        cv.u = sb("cv_u", [128, 30 + T], BF16)
        cv.us = sb("cv_us", [128, NS], F32)
        cv.uf = [sb(f"cv_uf{i}", [128, 512], F32) for i in range(2)]
        cv.sg = [sb(f"cv_sg{i}", [128, 512], F32) for i in range(2)]
        cv.mean = sb("cv_mean", [128, TWA], F32)
        cv.rstd = sb("cv_rstd", [128, TWA], F32)
        cv.tmp = [cv.uf[0], cv.sg[0], cv.uf[1]]
        cv.csq = [(nc.alloc_sbuf_tensor_at(f"cv_csq{i}", [128, 512], BF16, offset=cv.sg[1][2] + i * 1024), cv.sg[1][1].sub(i), 0)
                  for i in range(2)]
        cv.cpt = (nc.alloc_sbuf_tensor_at("cv_cpt", [32, E], F32, offset=cv.mean[2]), cv.mean[1], 0)
        cv.ust = (nc.alloc_sbuf_tensor_at("cv_ust", [16, E], F32, offset=cv.rstd[2]), cv.rstd[1], 0)
        cv.strow = [sb(f"cv_strow{i}", [128, 128], F32) for i in range(4)]
        cv.stT = sb("cv_stT", [128, 480], F32)
        cv.prod = sb("cv_prod", [128, 480], F32)
        cv.cs = sb("cv_cs", [128, NS], F32)
    cv = k.cv
    if True:
        for c_ in range(16):
            P.op('sp', lambda e, c_=c_: e.dma_start(out=cv.cw[0][:, c_, :],
                                                    in_=dr["a_conv_w"][:, c_ * 128:(c_ + 1) * 128].rearrange("k p -> p k"),
                                                    allow_slow_non_contiguous=True), writes=[cv.cw[1]], dma=True)
        P.op('sp', lambda e: e.dma_start(out=cv.bin[0][:], in_=dr["a_b_in"].rearrange("(c p) -> p c", p=128),
                                         allow_slow_non_contiguous=True), writes=[cv.bin[1]], dma=True)
        for t_, nm in ((cv.cb, "a_conv_b"), (cv.lg, "a_ln_g"), (cv.lb, "a_ln_b")):
            P.op('sp', lambda e, t_=t_, nm=nm: e.dma_start(out=t_[0][:], in_=dr[nm].rearrange("(c p) -> p c", p=128),
                                                           allow_slow_non_contiguous=True), writes=[t_[1]], dma=True)
        P.op('dve', lambda e: e.memset(cv.u[0][:, 0:30], 0.0), writes=[cv.u[1].sub('pad')])
    cv = k.cv
    tiles = ntiles(ps_)
    u, u_b, _ = cv.u
    a_w_in, a_w_in_b = dr["a_w_in"], k.dbuf["a_w_in"]
    for ec in range(16):
        wa, wa_b, _ = cv.wt[0][ec % 2]
        wb, wb_b, _ = cv.wt[1][ec % 2]
        load_w_cols(k, wa, wa_b, a_w_in, a_w_in_b, ec * 128)
        load_w_cols(k, wb, wb_b, a_w_in, a_w_in_b, E + ec * 128)
        dg, dg_b, _ = cv.dg
        for kk in range(31):
            P.op('dve', lambda e, kk=kk, ec=ec: e.tensor_scalar(out=dg[:, kk, :], in0=k.identF[:], scalar1=cv.cw[0][:, ec, kk:kk + 1],
                                                                scalar2=None, op0=ALU.mult),
                 reads=[k.identF_b, cv.cw[1]], writes=[dg_b.sub(kk)])
        for ti, (c0, w) in enumerate(tiles):
            pa, pa_b = k.psn()
            pb, pb_b = k.psn()
            for kk in range(8):
                P.op('pe', lambda e, kk=kk, c0=c0, w=w, pa=pa, wa=wa: e.matmul(pa[:, 0:w], lhsT=wa[:, kk, :], rhs=k.hT[:, kk, c0 + 1:c0 + 1 + w],
                                                                              start=(kk == 0), stop=(kk == 7)),
                     reads=[wa_b] + hT_reads(k, c0, w), writes=[pa_b])
            for kk in range(8):
                P.op('pe', lambda e, kk=kk, c0=c0, w=w, pb=pb, wb=wb: e.matmul(pb[:, 0:w], lhsT=wb[:, kk, :], rhs=k.hT[:, kk, c0 + 1:c0 + 1 + w],
                                                                              start=(kk == 0), stop=(kk == 7)),
                     reads=[wb_b] + hT_reads(k, c0, w), writes=[pb_b])
            sg, sg_b, _ = cv.sg[ti % 2]
            uf, uf_b, _ = cv.uf[ti % 2]
            P.op('act', lambda e, sg=sg, pb=pb, w=w, ec=ec: e.activation(out=sg[:, 0:w], in_=pb[:, 0:w], func=AF.Sigmoid,
                                                                        bias=cv.bin[0][:, 16 + ec:17 + ec]),
                 reads=[pb_b, cv.bin[1]], writes=[sg_b])
            if w == 512:
                P.op('dve', lambda e, uf=uf, pa=pa, sg=sg, ec=ec: e.scalar_tensor_tensor(
                    out=uf[:], in0=pa[:], scalar=cv.bin[0][:, ec:ec + 1], in1=sg[:], op0=ALU.add, op1=ALU.mult),
                    reads=[pa_b, sg_b, cv.bin[1]], writes=[uf_b])
                P.op('act', lambda e, uf=uf, c0=c0: e.activation(out=u[:, 30 + c0:30 + c0 + 512], in_=uf[:], func=AF.Copy),
                     reads=[uf_b], writes=[u_b.sub(ti)])
                if ti == 3:
                    pt, pt_b = k.psn()
                    P.op('pe', lambda e, uf=uf, pt=pt: e.transpose(out=pt[0:30, 0:128], in_=uf[:, 482:512], identity=k.identF[:]),
                         reads=[uf_b, k.identF_b], writes=[pt_b])
                    P.op('dve', lambda e, pt=pt, ec=ec: e.tensor_copy(out=cv.cpt[0][0:30, ec * 128:(ec + 1) * 128], in_=pt[0:30, 0:128]),
                         reads=[pt_b], writes=[cv.cpt[1].sub(ec)])
            else:
                us, us_b, _ = cv.us
                P.op('dve', lambda e, pa=pa, sg=sg, ec=ec: e.scalar_tensor_tensor(
                    out=us[:, :], in0=pa[:, 0:NS], scalar=cv.bin[0][:, ec:ec + 1], in1=sg[:, 0:NS], op0=ALU.add, op1=ALU.mult),
                    reads=[pa_b, sg_b, cv.bin[1]], writes=[us_b])
        for ti in range(4):
            c0 = ti * 512
            pc, pc_b = k.psn()
            rd = [u_b.sub(ti), u_b.sub('pad')] + ([u_b.sub(ti - 1)] if ti > 0 else [])
            for kk in range(31):
                P.op('pe', lambda e, kk=kk, c0=c0, pc=pc: e.matmul(pc[:, :], lhsT=dg[:, kk, :], rhs=u[:, c0 + kk:c0 + kk + 512],
                                                                   start=(kk == 0), stop=(kk == 30)),
                     reads=[dg_b.sub(kk)] + rd, writes=[pc_b])
            P.op('act', lambda e, pc=pc, c0=c0, ec=ec: e.activation(out=k.GT[:, ec, c0:c0 + 512], in_=pc[:, :], func=AF.Identity,
                                                                   bias=cv.cb[0][:, ec:ec + 1]),
                 reads=[pc_b, cv.cb[1]], writes=[k.GT_b.sub(ec).sub(ti)])
        if ps_ == 0:
            conv_sample(k, ec)
    P.op('sp', lambda e: e.dma_start(out=dr["conv_p"][ps_, :, :], in_=cv.cpt[0][0:30, :]), reads=[cv.cpt[1]],
         writes=[k.dbuf["conv_p"]], dma=True)
    if ps_ == 0:
        P.op('sp', lambda e: e.dma_start(out=dr["conv_s"][:, 29, :], in_=cv.ust[0][:, :]), reads=[cv.ust[1]],
             writes=[k.dbuf["conv_s"]], dma=True)
        P.op('sp', lambda e: e.dma_start(out=dr["conv_s"][:, 0:29, :], in_=dr["state_conv"][:, 1:30, :]),
             reads=[k.dbuf["state_conv"]], writes=[k.dbuf["conv_s"]], dma=True)
    mean, mean_b, _ = cv.mean
    rstd, rstd_b, _ = cv.rstd
    for ti, (c0, w) in enumerate(tiles):
        p1, p1_b = k.psn()
        p2, p2_b = k.psn()
        for ec in range(16):
            P.op('pe', lambda e, ec=ec, c0=c0, w=w, p1=p1: e.matmul(p1[:, 0:w], lhsT=k.onesB[:], rhs=k.GT[:, ec, c0:c0 + w],
                                                                   start=(ec == 0), stop=(ec == 15)),
                 reads=[k.onesB_b, k.GT_b.sub(ec).sub(ti)], writes=[p1_b])
        for ec in range(16):
            cq, cq_b, _ = cv.csq[ec % 2]
            P.op('act', lambda e, ec=ec, c0=c0, w=w, cq=cq: e.activation(out=cq[:, 0:w], in_=k.GT[:, ec, c0:c0 + w], func=AF.Square),
                 reads=[k.GT_b.sub(ec).sub(ti)], writes=[cq_b])
            P.op('pe', lambda e, ec=ec, w=w, p2=p2, cq=cq: e.matmul(p2[:, 0:w], lhsT=k.onesB[:], rhs=cq[:, 0:w],
                                                                   start=(ec == 0), stop=(ec == 15)),
                 reads=[k.onesB_b, cq_b], writes=[p2_b])
        tm, tm_b, _ = cv.tmp[0]
        P.op('act', lambda e, c0=c0, w=w, p1=p1: e.activation(out=mean[:, c0:c0 + w], in_=p1[:, 0:w], func=AF.Copy, scale=1.0 / E),
             reads=[p1_b], writes=[mean_b.sub(ti)])
        P.op('act', lambda e, w=w, p1=p1: e.activation(out=tm[:, 0:w], in_=p1[:, 0:w], func=AF.Square, scale=1.0 / E),
             reads=[p1_b], writes=[tm_b])
        P.op('dve', lambda e, c0=c0, w=w, p2=p2: e.scalar_tensor_tensor(out=rstd[:, c0:c0 + w], in0=p2[:, 0:w], scalar=1.0 / E,
                                                                        in1=tm[:, 0:w], op0=ALU.mult, op1=ALU.subtract),
             reads=[p2_b, tm_b], writes=[rstd_b.sub(ti)])
        P.op('act', lambda e, c0=c0, w=w: e.activation(out=rstd[:, c0:c0 + w], in_=rstd[:, c0:c0 + w], func=AF.Sqrt,
                                                       bias=k.epsr[:, 1:2]),
             reads=[rstd_b.sub(ti), k.epsr_b], writes=[rstd_b.sub(ti)])
        P.op('dve', lambda e, c0=c0, w=w: e.reciprocal(out=rstd[:, c0:c0 + w], in_=rstd[:, c0:c0 + w]),
             reads=[rstd_b.sub(ti)], writes=[rstd_b.sub(ti)])
    for ec in range(16):
        wz, wz_b, _ = cv.wt[2][ec % 2]
        load_w_cols(k, wz, wz_b, a_w_in, a_w_in_b, 2 * E + ec * 128)
        for ti, (c0, w) in enumerate(tiles):
            pz, pz_b = k.psn()
            for kk in range(8):
                P.op('pe', lambda e, kk=kk, c0=c0, w=w, pz=pz, wz=wz: e.matmul(pz[:, 0:w], lhsT=wz[:, kk, :], rhs=k.hT[:, kk, c0 + 1:c0 + 1 + w],
                                                                              start=(kk == 0), stop=(kk == 7)),
                     reads=[wz_b] + hT_reads(k, c0, w), writes=[pz_b])
            t0, t0_b, _ = cv.tmp[1]
            t1, t1_b, _ = cv.tmp[2]
            gb = k.GT_b.sub(ec).sub(ti)
            P.op('dve', lambda e, ec=ec, c0=c0, w=w: e.tensor_tensor(out=t0[:, 0:w], in0=k.GT[:, ec, c0:c0 + w], in1=mean[:, c0:c0 + w],
                                                                    op=ALU.subtract),
                 reads=[gb, mean_b.sub(ti)], writes=[t0_b])
            P.op('dve', lambda e, c0=c0, w=w: e.tensor_tensor(out=t0[:, 0:w], in0=t0[:, 0:w], in1=rstd[:, c0:c0 + w], op=ALU.mult),
                 reads=[t0_b, rstd_b.sub(ti)], writes=[t0_b])
            P.op('act', lambda e, ec=ec, w=w: e.activation(out=t0[:, 0:w], in_=t0[:, 0:w], func=AF.Silu,
                                                           scale=cv.lg[0][:, ec:ec + 1], bias=cv.lb[0][:, ec:ec + 1]),
                 reads=[t0_b, cv.lg[1], cv.lb[1]], writes=[t0_b])
            P.op('act', lambda e, ec=ec, w=w, pz=pz: e.activation(out=t1[:, 0:w], in_=pz[:, 0:w], func=AF.Silu,
                                                                  bias=cv.bin[0][:, 32 + ec:33 + ec]),
                 reads=[pz_b, cv.bin[1]], writes=[t1_b])
            P.op('dve', lambda e, ec=ec, c0=c0, w=w: e.tensor_tensor(out=k.GT[:, ec, c0:c0 + w], in0=t0[:, 0:w], in1=t1[:, 0:w], op=ALU.mult),
                 reads=[t0_b, t1_b], writes=[gb])


def conv_sample(k, ec):
    P, dr, cv = k.P, k.dr, k.cv
    us, us_b, _ = cv.us
    stT, stT_b, _ = cv.stT
    pt, pt_b = k.psn()
    for r in range(4):
        nr = 128 if r < 3 else 96
        sr, sr_b, _ = cv.strow[r]
        src = dr["state_conv"].rearrange("s k e -> (s k) e")[r * 128:r * 128 + nr, ec * 128:(ec + 1) * 128]
        P.op('sp', lambda e, sr=sr, src=src, nr=nr: e.dma_start(out=sr[0:nr, :], in_=src), reads=[k.dbuf["state_conv"]],
             writes=[sr_b], dma=True)
        P.op('pe', lambda e, sr=sr, nr=nr, r=r, pt=pt: e.transpose(out=pt[:, r * 128:r * 128 + nr], in_=sr[0:nr, :],
                                                                  identity=k.identF[0:nr, 0:nr]),
             reads=[sr_b, k.identF_b], writes=[pt_b])
    P.op('dve', lambda e, pt=pt: e.tensor_copy(out=stT[:, :], in_=pt[:, 0:480]), reads=[pt_b], writes=[stT_b])
    prod, prod_b, _ = cv.prod
    cs, cs_b, _ = cv.cs
    P.op('dve', lambda e, ec=ec: e.tensor_tensor(out=prod[:, :].rearrange("p (s k) -> p s k", k=30),
                                                 in0=stT[:, :].rearrange("p (s k) -> p s k", k=30),
                                                 in1=cv.cw[0][:, ec:ec + 1, 0:30].to_broadcast([128, NS, 30]), op=ALU.mult),
         reads=[stT_b, cv.cw[1]], writes=[prod_b])
    P.op('dve', lambda e: e.tensor_reduce(out=cs[:, :], in_=prod[:, :].rearrange("p (s k) -> p s k", k=30), axis=AX.X, op=ALU.add),
         reads=[prod_b], writes=[cs_b])
    P.op('dve', lambda e, ec=ec: e.scalar_tensor_tensor(out=cs[:, :], in0=us[:, :], scalar=cv.cw[0][:, ec, 30:31], in1=cs[:, :],
                                                        op0=ALU.mult, op1=ALU.add),
         reads=[us_b, cs_b, cv.cw[1]], writes=[cs_b])
    P.op('act', lambda e, ec=ec: e.activation(out=k.GT[:, ec, T:T + NS], in_=cs[:, :], func=AF.Identity, bias=cv.cb[0][:, ec:ec + 1]),
         reads=[cs_b, cv.cb[1]], writes=[k.GT_b.sub(ec).sub(4)])
    p2, p2_b = k.psn()
    P.op('pe', lambda e, p2=p2: e.transpose(out=p2[0:NS, 0:128], in_=us[:, :], identity=k.identF[:]),
         reads=[us_b, k.identF_b], writes=[p2_b])
    P.op('dve', lambda e, p2=p2, ec=ec: e.tensor_copy(out=cv.ust[0][:, ec * 128:(ec + 1) * 128], in_=p2[0:NS, 0:128]),
         reads=[p2_b], writes=[cv.ust[1].sub(ec)])


def stage3(k, l, ps_, x_src, x_dst, w_out, w_out_b):
    P, dr = k.P, k.dr
    xp, xs, xp_b, xs_b = x_src
    yp, ys, yp_b, ys_b = x_dst
    wo, wo_b = k.wo
    P.op('sp', lambda e: e.dma_start(out=k.gpost[:], in_=dr["norm_post"][l:l + 1, :].partition_broadcast(128)),
         writes=[k.gpost_b], dma=True)
    for ec in range(16):
        P.op('pool', lambda e, ec=ec: e.dma_start(out=wo[:, ec, :], in_=w_out[ec * 128:(ec + 1) * 128, :]),
             reads=[w_out_b], writes=[wo_b], dma=True)
    tiles = [(xp[ps_, tt * 128:(tt + 1) * 128, :], yp[ps_, tt * 128:(tt + 1) * 128, :], 128, tt * 128, xp_b, yp_b) for tt in range(16)]
    if ps_ == 0:
        tiles.append((xs[:, :], ys[:, :], NS, T, xs_b, ys_b))
    for i, (src, dst, np_, c0, src_b, dst_b) in enumerate(tiles):
        xt, xt_b, _ = k.xt[i % 2]
        hn, hn_b, _ = k.hn[i % 2]
        sq, sq_b, _ = k.sq
        st, st_b, _ = k.st[k.st_i[0] % 4]
        k.st_i[0] += 1
        P.op('sp', lambda e, xt=xt, src=src, np_=np_: e.dma_start(out=xt[0:np_, :], in_=src),
             reads=[src_b], writes=[xt_b], dma=True)
        pp = [k.psn(), k.psn()]
        gtr = [k.GT_b.sub(ec).sub(min(c0 // 512, 4)) for ec in range(16)]
        for h in range(2):
            ph, ph_b = pp[h]
            for ec in range(16):
                P.op('pe', lambda e, ec=ec, h=h, ph=ph, c0=c0, np_=np_: e.matmul(ph[0:np_, :], lhsT=k.GT[:, ec, c0:c0 + np_],
                                                                                rhs=wo[:, ec, h * 512:(h + 1) * 512],
                                                                                start=(ec == 0), stop=(ec == 15)),
                     reads=[wo_b, gtr[ec]], writes=[ph_b])
            P.op('act', lambda e, h=h, ph=ph, st=st, np_=np_: e.activation(out=sq[0:np_, 0:512], in_=ph[0:np_, :], func=AF.Square,
                                                                          accum_out=st[0:np_, 2 + h:3 + h]),
                 reads=[ph_b], writes=[sq_b, st_b])
        P.op('dve', lambda e, st=st, np_=np_: e.tensor_tensor(out=st[0:np_, 4:5], in0=st[0:np_, 2:3], in1=st[0:np_, 3:4], op=ALU.add),
             reads=[st_b], writes=[st_b])
        rstd_from_ss(k, st[0:np_, 4:5], st_b, st[0:np_, 5:6], st_b, np_, 1.0 / D, 0)
        for h in range(2):
            ph, ph_b = pp[h]
            P.op('dve', lambda e, h=h, ph=ph, st=st, np_=np_, hn=hn: e.scalar_tensor_tensor(
                out=k.sqf(np_, h), in0=ph[0:np_, :], scalar=st[0:np_, 5:6], in1=k.gpost[0:np_, h * 512:(h + 1) * 512],
                op0=ALU.mult, op1=ALU.mult),
                reads=[ph_b, st_b, k.gpost_b], writes=[k.of_b])
        P.op('dve', lambda e, xt=xt, np_=np_: e.tensor_tensor(out=xt[0:np_, :], in0=xt[0:np_, :], in1=k.of[0:np_, :], op=ALU.add),
             reads=[xt_b, k.of_b], writes=[xt_b])
        P.op('sp', lambda e, xt=xt, dst=dst, np_=np_: e.dma_start(out=dst, in_=xt[0:np_, :]),
             reads=[xt_b], writes=[dst_b], dma=True)

def op_tt(k, eng, out, in0, in1, op, reads, writes):
    return k.P.op(eng, lambda e: e.tensor_tensor(out=out, in0=in0, in1=in1, op=op), reads, writes)


def op_ts(k, eng, out, in0, s1, s2, op0, op1, reads, writes):
    if s2 is None:
        return k.P.op(eng, lambda e: e.tensor_scalar(out=out, in0=in0, scalar1=s1, scalar2=None, op0=op0), reads, writes)
    return k.P.op(eng, lambda e: e.tensor_scalar(out=out, in0=in0, scalar1=s1, scalar2=s2, op0=op0, op1=op1), reads, writes)


def op_stt(k, eng, out, in0, scalar, in1, op0, op1, reads, writes):
    return k.P.op(eng, lambda e: e.scalar_tensor_tensor(out=out, in0=in0, scalar=scalar, in1=in1, op0=op0, op1=op1), reads, writes)


def op_act(k, out, in_, func, reads, writes, scale=None, bias=None):
    kw = {}
    if scale is not None:
        kw['scale'] = scale
    if bias is not None:
        kw['bias'] = bias
    return k.P.op('act', lambda e: e.activation(out=out, in_=in_, func=func, **kw), reads, writes)


def op_mm(k, out, lhsT, rhs, start, stop, reads, writes):
    return k.P.op('pe', lambda e: e.matmul(out, lhsT=lhsT, rhs=rhs, start=start, stop=stop), reads, writes)


def op_tr(k, out, in_, ident, reads, writes):
    return k.P.op('pe', lambda e: e.transpose(out=out, in_=in_, identity=ident), reads, writes)


def op_dma(k, eng, out, in_, reads, writes, slow=False):
    if slow:
        return k.P.op(eng, lambda e: e.dma_start(out=out, in_=in_, allow_slow_non_contiguous=True), reads, writes, dma=True)
    return k.P.op(eng, lambda e: e.dma_start(out=out, in_=in_), reads, writes, dma=True)


def hT_reads_prev(k, c0, w):
    out = []
    if c0 == 0:
        out.append(k.hT_b.sub('pad'))
    lo = max(c0 - 1, 0) // 128
    hi = (c0 + w - 2) // 128
    for c in range(lo, hi + 1):
        out.append(k.hT_b.sub(c))
    return out


DECAY_C = 0.6065306597126334


def rwkv_setup(k):
    P, dr, nc, sb = k.P, k.dr, k.nc, k.sb
    rw = K()
    k.rw = rw
    rw.W = [[sb(f"rwW{j}{ab}", [128, 8, 128], BF16) for ab in range(2)] for j in range(4)]
    rw.wraw = sb("rw_wraw", [128, 8, 128], BF16)
    rw.mu = sb("rw_mu", [128, 6, 8], F32)
    rw.omu = sb("rw_omu", [128, 6, 8], F32)
    rw.par = {n: sb("rw_p_" + n, [128, 16], F32) for n in ["w0", "a0", "k_k", "k_a", "omk_a", "r_k", "ln_g", "ln_b"]}
    rw.lw = [sb(f"rw_lw{i}", [128, 8, 128], BF16) for i in range(2)]
    rw.w2a2 = sb("rw_w2a2", [128, E], BF16)
    rw.lora = sb("rw_lora", [128, TWA], BF16)
    rw.mscan = sb("rw_mscan", [128, 512], F32)
    rw.mk = {n: sb("rw_mk_" + n, [128, 512], BF16) for n in ["nUs", "nLs", "Us", "Ui"]}
    rw.irep = sb("rw_irep", [128, 64], F32)
    rw.boB = sb("rw_boB", [128, 128], BF16)
    rw.boF = sb("rw_boF", [128, 128], F32)
    rw.f = [sb(f"rw_f{i}", [128, 512], F32) for i in range(13)]
    rw.h16 = {n: sb("rw_h_" + n, [128, 512], BF16) for n in ["KT", "RT", "BT", "KKT", "VT", "ZS", "YB", "YQ"]}
    rw.tok = {n: sb("rw_tok_" + n, [128, 8, 64], BF16) for n in ["B", "K", "V"]}
    rw.g = {n: sb("rw_g_" + n, [128, 512], BF16) for n in ["Inv", "AkT", "BrT", "KrT"]}
    rw.Pp = [sb(f"rw_P{i}", [128, 512], BF16) for i in range(2)]
    rw.Qp = [sb(f"rw_Q{i}", [128, 512], BF16) for i in range(2)]
    rw.S = sb("rw_S", [128, 512], F32)
    rw.Sbf = sb("rw_Sbf", [128, 512], BF16)
    rw.tokKap = sb("rw_tokKap", [128, 8, 64], BF16)
    rw.M1Tn = sb("rw_M1Tn", [128, 512], BF16)
    rw.AVsb = sb("rw_AVsb", [128, 8, 64], BF16)
    rw.Wn = sb("rw_Wn", [128, 8, 64], BF16)
    rw.Xsb = sb("rw_Xsb", [128, 64], BF16)
    rw.Usb = sb("rw_Usb", [128, 64], BF16)
    rw.H = sb("rw_H", [128, 64], F32)
    rw.Hbf = sb("rw_Hbf", [128, 64], BF16)
    rw.TH = sb("rw_TH", [128, 64], F32)
    rw.shT = sb("rw_shT", [128, 8, NS], BF16)
    rw.shrow = sb("rw_shrow", [NS, D], F32)
    rw.small = sb("rw_small", [128, 64], F32)


def rwkv_load(k):
    P, dr, nc, rw = k.P, k.dr, k.nc, k.rw
    for j in range(6):
        op_dma(k, 'sp', rw.mu[0][:, j, :], dr["b_mu"][j].rearrange("(k p) -> p k", p=128), [k.dbuf["b_mu"]], [rw.mu[1]], slow=True)
    op_ts(k, 'dve', rw.omu[0][:], rw.mu[0][:], -1.0, 1.0, ALU.mult, ALU.add, [rw.mu[1]], [rw.omu[1]])
    for n, src in [("w0", "b_w0"), ("a0", "b_a0"), ("k_k", "b_k_k"), ("k_a", "b_k_a"), ("ln_g", "b_ln_g"), ("ln_b", "b_ln_b")]:
        op_dma(k, 'sp', rw.par[n][0][:], dr[src].rearrange("(c p) -> p c", p=128), [k.dbuf[src]], [rw.par[n][1]], slow=True)
    op_dma(k, 'sp', rw.par["r_k"][0][:], dr["b_r_k"].rearrange("(c h2) j -> (h2 j) c", h2=2), [k.dbuf["b_r_k"]],
           [rw.par["r_k"][1]], slow=True)
    op_ts(k, 'dve', rw.par["omk_a"][0][:], rw.par["k_a"][0][:], -1.0, 1.0, ALU.mult, ALU.add, [rw.par["k_a"][1]], [rw.par["omk_a"][1]])
    wr, wr_b, _ = rw.wraw
    op_dma(k, 'pool', wr[:, :, 0:64], dr["b_w1"].rearrange("(k p) e -> p k e", p=128), [k.dbuf["b_w1"]], [wr_b])
    op_dma(k, 'pool', wr[:, :, 64:128], dr["b_a1"].rearrange("(k p) e -> p k e", p=128), [k.dbuf["b_a1"]], [wr_b])
    for half, j in ((0, 4), (1, 5)):
        cs_ = slice(half * 64, half * 64 + 64)
        op_tt(k, 'dve', rw.lw[0][0][:, :, cs_], wr[:, :, cs_], rw.omu[0][:, j, :].unsqueeze(2).to_broadcast([128, 8, 64]), ALU.mult,
              [wr_b, rw.omu[1]], [rw.lw[0][1]])
        op_tt(k, 'dve', rw.lw[1][0][:, :, cs_], wr[:, :, cs_], rw.mu[0][:, j, :].unsqueeze(2).to_broadcast([128, 8, 64]), ALU.mult,
              [wr_b, rw.mu[1]], [rw.lw[1][1]])
    op_dma(k, 'pool', rw.w2a2[0][0:64, :], dr["b_w2"], [k.dbuf["b_w2"]], [rw.w2a2[1]])
    op_dma(k, 'pool', rw.w2a2[0][64:128, :], dr["b_a2"], [k.dbuf["b_a2"]], [rw.w2a2[1]])
    ms, ms_b, _ = rw.mscan
    P.op('dve', lambda e: e.memset(ms[:], 1.0), writes=[ms_b])
    P.op('dve', lambda e: e.memset(ms[:].rearrange("p (c t) -> p c t", t=64)[:, :, 0:1], 0.0), writes=[ms_b])
    for n, val, cm, pat, cmp_ in [("nUs", -1.0, -1, 1, ALU.is_gt), ("Us", 1.0, -1, 1, ALU.is_gt), ("Ui", 1.0, -1, 1, ALU.is_ge),
                                  ("nLs", -1.0, 1, -1, ALU.is_gt)]:
        t_, b_, _ = rw.mk[n]
        tf, tf_b, _ = rw.f[0]
        P.op('pool', lambda e, tf=tf, val=val: e.memset(tf[0:64, :], val), writes=[tf_b])
        P.op('pool', lambda e, tf=tf, cm=cm, pat=pat, cmp_=cmp_: e.affine_select(
            out=tf[0:64, :].rearrange("p (c t) -> p c t", t=64), in_=tf[0:64, :].rearrange("p (c t) -> p c t", t=64),
            pattern=[[0, 8], [pat, 64]], compare_op=cmp_, fill=0.0, base=0, channel_multiplier=cm),
            reads=[tf_b], writes=[tf_b])
        P.op('dve', lambda e, t_=t_, tf=tf: e.tensor_copy(out=t_[0:64, :], in_=tf[0:64, :]), reads=[tf_b], writes=[b_])
        op_dma(k, 'sp', t_[64:128, :], t_[0:64, :], [b_], [b_])
    op_tt(k, 'dve', rw.irep[0][:], k.identF[:, 0:64], k.identF[:, 64:128], ALU.add, [k.identF_b], [rw.irep[1]])
    for t_, b_, _ in (rw.boB, rw.boF):
        P.op('dve', lambda e, t_=t_: e.memset(t_[:], 0.0), writes=[b_])
        P.op('dve', lambda e, t_=t_: e.memset(t_[0:64, 0:64], 1.0), writes=[b_])
        P.op('dve', lambda e, t_=t_: e.memset(t_[64:128, 64:128], 1.0), writes=[b_])


def layer_rwkv(k, ps_):
    P, dr, nc = k.P, k.dr, k.nc
    if not hasattr(k, "rw"):
        rwkv_setup(k)
    rwkv_load(k)
    rw = k.rw
    P.chk("setup")
    tiles = ntiles(ps_)
    lora, lora_b, _ = rw.lora
    if ps_ == 0:
        sr, sr_b, _ = rw.shrow
        op_dma(k, 'sp', sr[:, :], dr["state_shift"], [k.dbuf["state_shift"]], [sr_b])
        pt, pt_b = k.psn()
        for kk in range(8):
            op_tr(k, pt[:, kk * NS:(kk + 1) * NS], sr[0:NS, kk * 128:(kk + 1) * 128], k.identF[0:NS, 0:NS], [sr_b, k.identF_b], [pt_b])
        op_act(k, rw.shT[0][:, :, :], pt[:, 0:8 * NS].rearrange("p (k s) -> p k s", s=NS), AF.Copy, [pt_b], [rw.shT[1]])

    def prev_rhs(kk, c0, w):
        if c0 >= T:
            return rw.shT[0][:, kk, :], [rw.shT[1]]
        return k.hT[:, kk, c0:c0 + w], hT_reads_prev(k, c0, w)

    for ti, (c0, w) in enumerate(tiles):
        pl, pl_b = k.psn()
        for kk in range(8):
            op_mm(k, pl[:, 0:w], rw.lw[0][0][:, kk, :], k.hT[:, kk, c0 + 1:c0 + 1 + w], kk == 0, False,
                  [rw.lw[0][1]] + hT_reads(k, c0, w), [pl_b])
        for kk in range(8):
            r_, rb_ = prev_rhs(kk, c0, w)
            op_mm(k, pl[:, 0:w], rw.lw[1][0][:, kk, :], r_, False, kk == 7, [rw.lw[1][1]] + rb_, [pl_b])
        op_act(k, lora[0:64, c0:c0 + w], pl[0:64, 0:w], AF.Tanh, [pl_b], [lora_b.sub(ti)])
        op_act(k, lora[64:128, c0:c0 + w], pl[64:128, 0:w], AF.Copy, [pl_b], [lora_b.sub(ti)])

    P.chk("lora")
    F = rw.f
    for hp in range(16):
        hc = slice(hp * 128, (hp + 1) * 128)
        wr, wr_b, _ = rw.wraw
        for j in range(4):
            op_dma(k, 'pool', wr[:, :, :], dr["b_w_rkvz"][j][:, hc].rearrange("(k p) e -> p k e", p=128), [k.dbuf["b_w_rkvz"]], [wr_b])
            op_tt(k, 'dve', rw.W[j][0][0][:], wr[:], rw.omu[0][:, j, :].unsqueeze(2).to_broadcast([128, 8, 128]), ALU.mult,
                  [wr_b, rw.omu[1]], [rw.W[j][0][1]])
            op_tt(k, 'pool', rw.W[j][1][0][:], wr[:], rw.mu[0][:, j, :].unsqueeze(2).to_broadcast([128, 8, 128]), ALU.mult,
                  [wr_b, rw.mu[1]], [rw.W[j][1][1]])
        P.chk("w")
        H, H_b, _ = rw.H
        Hbf, Hbf_b, _ = rw.Hbf
        P.op('dve', lambda e: e.memset(H[:], 0.0), writes=[H_b])
        P.op('dve', lambda e: e.memset(Hbf[:], 0.0), writes=[Hbf_b])
        par = lambda n: rw.par[n][0][:, hp:hp + 1]
        parb = lambda n: rw.par[n][1]
        for ti, (c0, w) in enumerate(tiles):
            sample = c0 >= T
            pj = []
            for j in range(4):
                pp, pp_b = k.psn()
                for kk in range(8):
                    op_mm(k, pp[:, 0:w], rw.W[j][0][0][:, kk, :], k.hT[:, kk, c0 + 1:c0 + 1 + w], kk == 0, False,
                          [rw.W[j][0][1]] + hT_reads(k, c0, w), [pp_b])
                for kk in range(8):
                    r_, rb_ = prev_rhs(kk, c0, w)
                    op_mm(k, pp[:, 0:w], rw.W[j][1][0][:, kk, :], r_, False, kk == 7, [rw.W[j][1][1]] + rb_, [pp_b])
                pj.append((pp, pp_b))
            (pr, pr_b), (pk, pk_b), (pv, pv_b), (pz, pz_b) = pj
            P.chk("proj")
            pw, pw_b = k.psn()
            op_mm(k, pw[:, 0:w], rw.w2a2[0][0:64, hc], lora[0:64, c0:c0 + w], True, True, [rw.w2a2[1], lora_b.sub(ti)], [pw_b])
            pa, pa_b = k.psn()
            op_mm(k, pa[:, 0:w], rw.w2a2[0][64:128, hc], lora[64:128, c0:c0 + w], True, True, [rw.w2a2[1], lora_b.sub(ti)], [pa_b])
            P.chk("pwpa")
            W_ = slice(0, w)
            A, SG, TMP, KP, KKF, SQ, Bt, R, V, RK, BON, PP, YN = [F[i] for i in range(13)]
            op_act(k, A[0][:, W_], pa[:, W_], AF.Sigmoid, [pa_b, parb("a0")], [A[1]], bias=par("a0"))
            op_act(k, SG[0][:, W_], pw[:, W_], AF.Sigmoid, [pw_b, parb("w0")], [SG[1]], bias=par("w0"))
            P.chk("e0a")
            op_act(k, TMP[0][:, W_], A[0][:, W_], AF.Identity, [A[1], parb("k_a"), parb("omk_a")], [TMP[1]], scale=par("k_a"), bias=par("omk_a"))
            P.chk("e0b")
            op_tt(k, 'dve', KP[0][:, W_], pk[:, W_], TMP[0][:, W_], ALU.mult, [pk_b, TMP[1]], [KP[1]])
            P.chk("e0c")
            op_ts(k, 'dve', KKF[0][:, W_], pk[:, W_], par("k_k"), None, ALU.mult, None, [pk_b, parb("k_k")], [KKF[1]])
            P.chk("e0d")
            op_act(k, SQ[0][:, W_], KKF[0][:, W_], AF.Square, [KKF[1]], [SQ[1]])
            P.chk("e1")
            pn, pn_b = k.psn()
            op_mm(k, pn[:, W_], rw.boF[0][:], SQ[0][:, W_], True, True, [rw.boF[1], SQ[1]], [pn_b])
            op_act(k, SQ[0][:, W_], pn[:, W_], AF.Sqrt, [pn_b], [SQ[1]])
            op_ts(k, 'dve', SQ[0][:, W_], SQ[0][:, W_], 1e-12, None, ALU.max, None, [SQ[1]], [SQ[1]])
            P.op('dve', lambda e, W_=W_: e.reciprocal(out=SQ[0][:, W_], in_=SQ[0][:, W_]), [SQ[1]], [SQ[1]])
            op_tt(k, 'dve', KKF[0][:, W_], KKF[0][:, W_], SQ[0][:, W_], ALU.mult, [KKF[1], SQ[1]], [KKF[1]])
            op_tt(k, 'dve', Bt[0][:, W_], KKF[0][:, W_], A[0][:, W_], ALU.mult, [KKF[1], A[1]], [Bt[1]])
            P.chk("e2")
            op_act(k, R[0][:, W_], pr[:, W_], AF.Copy, [pr_b], [R[1]])
            op_act(k, V[0][:, W_], pv[:, W_], AF.Copy, [pv_b], [V[1]])
            op_stt(k, 'dve', RK[0][:, W_], R[0][:, W_], par("r_k"), KP[0][:, W_], ALU.mult, ALU.mult, [R[1], KP[1], parb("r_k")], [RK[1]])
            pb, pb_b = k.psn()
            op_mm(k, pb[:, W_], rw.boF[0][:], RK[0][:, W_], True, True, [rw.boF[1], RK[1]], [pb_b])
            op_tt(k, 'dve', BON[0][:, W_], pb[:, W_], V[0][:, W_], ALU.mult, [pb_b, V[1]], [BON[1]])
            ZS = rw.h16["ZS"]
            op_act(k, ZS[0][:, W_], pz[:, W_], AF.Silu, [pz_b], [ZS[1]])
            P.chk("elem")
            if not sample:
                ysrc, ysrc_b = rwkv_chunks(k, hp, ti, c0, A, SG, TMP, KP, KKF, Bt, R, V, PP, pv, pv_b)
            else:
                ysrc, ysrc_b = rwkv_sample(k, hp, SG, KP, KKF, Bt, R, V)
            P.chk("chunks")
            YB, YQ = rw.h16["YB"], rw.h16["YQ"]
            op_act(k, YB[0][:, W_], ysrc[:, W_], AF.Copy, [ysrc_b], [YB[1]])
            op_act(k, YQ[0][:, W_], ysrc[:, W_], AF.Square, [ysrc_b], [YQ[1]])
            pm, pm_b = k.psn()
            pq, pq_b = k.psn()
            op_mm(k, pm[:, W_], rw.boB[0][:], YB[0][:, W_], True, True, [rw.boB[1], YB[1]], [pm_b])
            op_mm(k, pq[:, W_], rw.boB[0][:], YQ[0][:, W_], True, True, [rw.boB[1], YQ[1]], [pq_b])
            MEAN, MSQ, RS = A, SG, TMP
            op_act(k, MEAN[0][:, W_], pm[:, W_], AF.Copy, [pm_b], [MEAN[1]], scale=1.0 / 64)
            op_act(k, MSQ[0][:, W_], pm[:, W_], AF.Square, [pm_b], [MSQ[1]], scale=1.0 / 64)
            op_stt(k, 'dve', RS[0][:, W_], pq[:, W_], 1.0 / 64, MSQ[0][:, W_], ALU.mult, ALU.subtract, [pq_b, MSQ[1]], [RS[1]])
            op_act(k, RS[0][:, W_], RS[0][:, W_], AF.Sqrt, [RS[1], k.epsr_b], [RS[1]], bias=k.epsr[:, 2:3])
            P.op('dve', lambda e, W_=W_, RS=RS: e.reciprocal(out=RS[0][:, W_], in_=RS[0][:, W_]), [RS[1]], [RS[1]])
            op_tt(k, 'dve', YN[0][:, W_], ysrc[:, W_], MEAN[0][:, W_], ALU.subtract, [ysrc_b, MEAN[1]], [YN[1]])
            op_tt(k, 'dve', YN[0][:, W_], YN[0][:, W_], RS[0][:, W_], ALU.mult, [YN[1], RS[1]], [YN[1]])
            op_act(k, YN[0][:, W_], YN[0][:, W_], AF.Identity, [YN[1], parb("ln_g"), parb("ln_b")], [YN[1]], scale=par("ln_g"), bias=par("ln_b"))
            op_tt(k, 'dve', YN[0][:, W_], YN[0][:, W_], BON[0][:, W_], ALU.add, [YN[1], BON[1]], [YN[1]])
            op_tt(k, 'dve', k.GT[:, hp, c0:c0 + w], YN[0][:, W_], ZS[0][:, W_], ALU.mult, [YN[1], ZS[1]], [k.GT_b.sub(hp).sub(ti)])
            P.chk("gn")
            if ti == 3:
                pt, pt_b = k.psn()
                for h in range(2):
                    hP = slice(h * 64, h * 64 + 64)
                    op_mm(k, pt[hP, 0:64], H[hP, :], k.identF[hP, hP], True, True, [H_b, k.identF_b], [pt_b])
                sm, sm_b, _ = rw.small
                P.op('dve', lambda e, pt=pt: e.tensor_copy(out=sm[:, :], in_=pt[:, 0:64]), [pt_b], [sm_b])
                op_dma(k, 'sp', dr["wkv_p"][ps_, 2 * hp:2 * hp + 2, :, :].rearrange("h i j -> (h i) j"), sm[:, :], [sm_b], [k.dbuf["wkv_p"]])


def rwkv_chunks(k, hp, ti, c0, A, SG, TMP, KP, KK, Bt, R, V, PP, pv, pv_b):
    P, rw = k.P, k.rw
    H, H_b, _ = rw.H
    Hbf, Hbf_b, _ = rw.Hbf
    KT, RT, BT, KKT, VT = [rw.h16[n] for n in ["KT", "RT", "BT", "KKT", "VT"]]
    ms = rw.mscan
    CS = TMP
    P.op('dve', lambda e: e.tensor_tensor_scan(out=CS[0][:], data0=ms[0][:], data1=SG[0][:], initial=0.0, op0=ALU.mult, op1=ALU.add),
         [ms[1], SG[1]], [CS[1]])
    op_tt(k, 'dve', SG[0][:], CS[0][:], SG[0][:], ALU.subtract, [CS[1], SG[1]], [SG[1]])
    op_act(k, PP[0][:], CS[0][:], AF.Exp, [CS[1]], [PP[1]], scale=-DECAY_C)
    op_act(k, CS[0][:], CS[0][:], AF.Exp, [CS[1]], [CS[1]], scale=DECAY_C)
    op_act(k, SG[0][:], SG[0][:], AF.Exp, [SG[1]], [SG[1]], scale=-DECAY_C)
    op_tt(k, 'dve', KT[0][:], KK[0][:], SG[0][:], ALU.mult, [KK[1], SG[1]], [KT[1]])
    op_tt(k, 'dve', RT[0][:], R[0][:], PP[0][:], ALU.mult, [R[1], PP[1]], [RT[1]])
    op_tt(k, 'dve', BT[0][:], Bt[0][:], CS[0][:], ALU.mult, [Bt[1], CS[1]], [BT[1]])
    op_tt(k, 'dve', KKT[0][:], KP[0][:], CS[0][:], ALU.mult, [KP[1], CS[1]], [KKT[1]])
    op_act(k, VT[0][:], pv[:, :], AF.Copy, [pv_b], [VT[1]])
    rw.tok["Kap"] = rw.tokKap
    for n, X in (("B", BT), ("K", KKT), ("V", VT), ("Kap", KT)):
        pt, pt_b = k.psn()
        for c in range(8):
            for h in range(2):
                hP = slice(h * 64, h * 64 + 64)
                op_mm(k, pt[hP, c * 64:(c + 1) * 64], X[0][hP, c * 64:(c + 1) * 64], k.identB[hP, hP], True, True,
                      [X[1], k.identB_b], [pt_b])
        op_act(k, rw.tok[n][0][:, :, :], pt[:, 0:512].rearrange("p (c t) -> p c t", t=64), AF.Copy, [pt_b], [rw.tok[n][1]])
    Btok, Ktok, Vtok = rw.tok["B"], rw.tok["K"], rw.tok["V"]
    P.chk("tok")
    grams = [("AbT", BT, KT), ("Ab", KT, BT), ("AkT", KKT, KT), ("BrT", BT, RT), ("KrT", KKT, RT)]
    gps = {}
    for n, L, Rr in grams:
        pg, pg_b = k.psn()
        for c in range(8):
            cs_ = slice(c * 64, (c + 1) * 64)
            for h in range(2):
                hP = slice(h * 64, h * 64 + 64)
                op_mm(k, pg[hP, cs_], L[0][hP, cs_], Rr[0][hP, cs_], True, True, [L[1], Rr[1]], [pg_b])
        gps[n] = (pg, pg_b)
    Pc, Qc = rw.Pp[0], rw.Qp[0]
    S, Sbf = rw.S, rw.Sbf
    op_tt(k, 'dve', Qc[0][:], gps["AbT"][0][:, :], rw.mk["nUs"][0][:], ALU.mult, [gps["AbT"][1], rw.mk["nUs"][1]], [Qc[1]])
    op_tt(k, 'dve', Pc[0][:], gps["Ab"][0][:, :], rw.mk["nLs"][0][:], ALU.mult, [gps["Ab"][1], rw.mk["nLs"][1]], [Pc[1]])
    op_tt(k, 'dve', rw.g["AkT"][0][:], gps["AkT"][0][:, :], rw.mk["Us"][0][:], ALU.mult, [gps["AkT"][1], rw.mk["Us"][1]], [rw.g["AkT"][1]])
    op_tt(k, 'dve', rw.g["BrT"][0][:], gps["BrT"][0][:, :], rw.mk["Ui"][0][:], ALU.mult, [gps["BrT"][1], rw.mk["Ui"][1]], [rw.g["BrT"][1]])
    op_tt(k, 'dve', rw.g["KrT"][0][:], gps["KrT"][0][:, :], rw.mk["Ui"][0][:], ALU.mult, [gps["KrT"][1], rw.mk["Ui"][1]], [rw.g["KrT"][1]])
    op_tt(k, 'dve', S[0][:].rearrange("p (c t) -> p c t", t=64), Qc[0][:].rearrange("p (c t) -> p c t", t=64),
          rw.irep[0][:, :].unsqueeze(1).to_broadcast([128, 8, 64]), ALU.add, [Qc[1], rw.irep[1]], [S[1]])
    op_act(k, Sbf[0][:], S[0][:], AF.Copy, [S[1]], [Sbf[1]])
    for it in range(1, 6):
        Pn, Qn = rw.Pp[it % 2], rw.Qp[it % 2]
        pP, pP_b = k.psn()
        for c in range(8):
            cs_ = slice(c * 64, (c + 1) * 64)
            for h in range(2):
                hP = slice(h * 64, h * 64 + 64)
                op_mm(k, pP[hP, cs_], Qc[0][hP, cs_], Pc[0][hP, cs_], True, True, [Qc[1], Pc[1]], [pP_b])
        if it < 5:
            pQ, pQ_b = k.psn()
            for c in range(8):
                cs_ = slice(c * 64, (c + 1) * 64)
                for h in range(2):
                    hP = slice(h * 64, h * 64 + 64)
                    op_mm(k, pQ[hP, cs_], Pc[0][hP, cs_], Qc[0][hP, cs_], True, True, [Qc[1], Pc[1]], [pQ_b])
        op_act(k, Pn[0][:], pP[:, :], AF.Copy, [pP_b], [Pn[1]])
        if it < 5:
            P.op('dve', lambda e, Qn=Qn, pQ=pQ: e.tensor_copy(out=Qn[0][:], in_=pQ[:, :]), [pQ_b], [Qn[1]])
        pS, pS_b = k.psn()
        for c in range(8):
            cs_ = slice(c * 64, (c + 1) * 64)
            for h in range(2):
                hP = slice(h * 64, h * 64 + 64)
                op_mm(k, pS[hP, cs_], Pn[0][hP, cs_], Sbf[0][hP, cs_], True, True, [Pn[1], Sbf[1]], [pS_b])
        op_tt(k, 'dve', S[0][:], S[0][:], pS[:, :], ALU.add, [S[1], pS_b], [S[1]])
        dst = Sbf if it < 5 else rw.g["Inv"]
        op_act(k, dst[0][:], S[0][:], AF.Copy, [S[1]], [dst[1]])
        Pc, Qc = Pn, Qn
    Inv, AkT, BrT, KrT = [rw.g[n] for n in ["Inv", "AkT", "BrT", "KrT"]]
    tokKap, M1Tn, AVsb, Wn = rw.tokKap, rw.M1Tn, rw.AVsb, rw.Wn
    pm1, pm1_b = k.psn()
    pav, pav_b = k.psn()
    for c in range(8):
        cs_ = slice(c * 64, (c + 1) * 64)
        for h in range(2):
            hP = slice(h * 64, h * 64 + 64)
            op_mm(k, pm1[hP, cs_], tokKap[0][hP, c, :], Inv[0][hP, cs_], True, True, [tokKap[1], Inv[1]], [pm1_b])
            op_mm(k, pav[hP, cs_], AkT[0][hP, cs_], Vtok[0][hP, c, :], True, True, [AkT[1], Vtok[1]], [pav_b])
    op_act(k, M1Tn[0][:, :], pm1[:, :], AF.Copy, [pm1_b], [M1Tn[1]], scale=-1.0)
    P.op('dve', lambda e: e.tensor_copy(out=AVsb[0][:, :, :], in_=pav[:, :].rearrange("p (c i) -> p c i", i=64)), [pav_b], [AVsb[1]])
    pw2, pw2_b = k.psn()
    for c in range(8):
        cs_ = slice(c * 64, (c + 1) * 64)
        for h in range(2):
            hP = slice(h * 64, h * 64 + 64)
            op_mm(k, pw2[hP, cs_], Inv[0][hP, cs_], AVsb[0][hP, c, :], True, True, [Inv[1], AVsb[1]], [pw2_b])
    op_act(k, Wn[0][:, :, :], pw2[:, :].rearrange("p (c i) -> p c i", i=64), AF.Copy, [pw2_b], [Wn[1]], scale=-1.0)
    P.chk("inv")
    py, py_b = k.ps[6]

    def ps6():
        i = k.ps_i[0] % 6
        k.ps_i[0] += 1
        return k.ps[i]
    Xsb, Usb, TH = rw.Xsb, rw.Usb, rw.TH
    op_ts(k, 'dve', TH[0][:, :], H[:, :], PP[0][:, 63:64], None, ALU.mult, None, [H_b, PP[1]], [TH[1]])
    for c in range(8):
        cs_ = slice(c * 64, (c + 1) * 64)
        pu, pu_b = ps6()
        for h in range(2):
            hP = slice(h * 64, h * 64 + 64)
            op_mm(k, pu[hP, 0:64], M1Tn[0][hP, cs_], Hbf[hP, :], True, True, [M1Tn[1], Hbf_b], [pu_b])
        op_tt(k, 'dve', Usb[0][:, :], pu[:, 0:64], Wn[0][:, c, :], ALU.add, [pu_b, Wn[1]], [Usb[1]])
        for h in range(2):
            hP = slice(h * 64, h * 64 + 64)
            op_mm(k, py[hP, cs_], Hbf[hP, :], RT[0][hP, cs_], True, False, [Hbf_b, RT[1]], [py_b])
            op_mm(k, py[hP, cs_], Usb[0][hP, :], BrT[0][hP, cs_], False, False, [Usb[1], BrT[1]], [py_b])
            op_mm(k, py[hP, cs_], Vtok[0][hP, c, :], KrT[0][hP, cs_], False, True, [Vtok[1], KrT[1]], [py_b])
        ph, ph_b = ps6()
        for h in range(2):
            hP = slice(h * 64, h * 64 + 64)
            op_mm(k, ph[hP, 0:64], Btok[0][hP, c, :], Usb[0][hP, :], True, False, [Btok[1], Usb[1]], [ph_b])
            op_mm(k, ph[hP, 0:64], Ktok[0][hP, c, :], Vtok[0][hP, c, :], False, True, [Ktok[1], Vtok[1]], [ph_b])
        pc_ap = PP[0][:, c * 64 + 63:c * 64 + 64]
        op_stt(k, 'dve', Hbf[:, :], ph[:, 0:64], pc_ap, TH[0][:, :], ALU.mult, ALU.add, [ph_b, PP[1], TH[1]], [Hbf_b])
        op_stt(k, 'dve', H[:, :], ph[:, 0:64], pc_ap, TH[0][:, :], ALU.mult, ALU.add, [ph_b, PP[1], TH[1]], [H_b])
        if c < 7:
            op_ts(k, 'dve', TH[0][:, :], H[:, :], PP[0][:, (c + 1) * 64 + 63:(c + 1) * 64 + 64], None, ALU.mult, None, [H_b, PP[1]], [TH[1]])
    Ysb = rw.f[5]
    op_act(k, Ysb[0][:, :], py[:, :], AF.Copy, [py_b], [Ysb[1]])
    return Ysb[0], Ysb[1]


def rwkv_sample(k, hp, SG, KP, KK, Bt, R, V):
    P, rw, dr = k.P, k.rw, k.dr
    F = rw.f
    Wd = F[2]
    op_act(k, Wd[0][:, 0:NS], SG[0][:, 0:NS], AF.Exp, [SG[1]], [Wd[1]], scale=-DECAY_C)
    ysm, ysm_b, _ = rw.small
    Sst_t, Sn_t, T1_t, RX_t = F[5], F[9], F[11], rw.S
    sa_t = F[12]
    for half in range(2):
        s0 = half * 8
        ss_ = slice(s0, s0 + 8)
        v3 = lambda t: t[0][:, :].rearrange("p (s j) -> p s j", j=64)
        op_dma(k, 'sp', v3(Sst_t), dr["state_wkv"][s0:s0 + 8, 2 * hp:2 * hp + 2, :, :].rearrange("s h i j -> (h i) s j"),
               [k.dbuf["state_wkv"]], [Sst_t[1]])

        def bcast(X):
            op_tt(k, 'dve', v3(RX_t), rw.irep[0][:, :].unsqueeze(1).to_broadcast([128, 8, 64]),
                  X[0][:, ss_].unsqueeze(2).to_broadcast([128, 8, 64]), ALU.mult, [rw.irep[1], X[1]], [RX_t[1]])
            pb, pb_b = k.psn()
            op_mm(k, pb[:, :], rw.boF[0][:], RX_t[0][:, :], True, True, [rw.boF[1], RX_t[1]], [pb_b])
            return pb[:, :].rearrange("p (s j) -> p s j", j=64), pb_b

        kkb, kkb_b = bcast(KK)
        op_tt(k, 'dve', v3(T1_t), v3(Sst_t), kkb, ALU.mult, [Sst_t[1], kkb_b], [T1_t[1]])
        P.op('dve', lambda e, ss_=ss_: e.tensor_reduce(out=sa_t[0][:, ss_], in_=v3(T1_t), axis=AX.X, op=ALU.add, negate=True),
             [T1_t[1]], [sa_t[1]])
        wb_, wb_b = bcast(Wd)
        op_tt(k, 'dve', v3(Sn_t), v3(Sst_t), wb_, ALU.mult, [Sst_t[1], wb_b], [Sn_t[1]])
        bb_, bb_b = bcast(Bt)
        op_tt(k, 'dve', v3(T1_t), bb_, sa_t[0][:, ss_].unsqueeze(2).to_broadcast([128, 8, 64]), ALU.mult, [bb_b, sa_t[1]], [T1_t[1]])
        op_tt(k, 'dve', v3(Sn_t), v3(Sn_t), v3(T1_t), ALU.add, [Sn_t[1], T1_t[1]], [Sn_t[1]])
        kb_, kb_b = bcast(KP)
        op_tt(k, 'dve', v3(T1_t), kb_, V[0][:, ss_].unsqueeze(2).to_broadcast([128, 8, 64]), ALU.mult, [kb_b, V[1]], [T1_t[1]])
        op_tt(k, 'dve', v3(Sn_t), v3(Sn_t), v3(T1_t), ALU.add, [Sn_t[1], T1_t[1]], [Sn_t[1]])
        rb_, rb_b = bcast(R)
        op_tt(k, 'dve', v3(T1_t), v3(Sn_t), rb_, ALU.mult, [Sn_t[1], rb_b], [T1_t[1]])
        P.op('dve', lambda e, ss_=ss_: e.tensor_reduce(out=ysm[:, ss_], in_=v3(T1_t), axis=AX.X, op=ALU.add), [T1_t[1]], [ysm_b])
        op_dma(k, 'sp', dr["wkv_s"][s0:s0 + 8, 2 * hp:2 * hp + 2, :, :].rearrange("s h i j -> (h i) s j"), v3(Sn_t),
               [Sn_t[1]], [k.dbuf["wkv_s"]])
    return ysm, ysm_b

MLA_SCALE_ = 192.0 ** -0.5
PI_ = 3.141592653589793
NPOOL = 10240


def mla_setup(k):
    P, dr, nc, sb = k.P, k.dr, k.nc, k.sb
    ml = K()
    k.ml = ml
    ml.cst = sb("ml_cst", [128, 4], F32)
    ml.Qs = sb("ml_Qs", [128, 2, NS, 16], BF16)
    ml.QRs = sb("ml_QRs", [64, NS, 16], BF16)
    ml.OLs = sb("ml_OLs", [128, 2, 16, NS], BF16)
    ml.ZSs = sb("ml_ZSs", [128, 16, NS], BF16)
    ml.KsT = sb("ml_KsT", [128, 3, NS], BF16)
    ml.ckvN = sb("ml_ckvN", [NS, 258], BF16)
    ml.wuv = sb("ml_wuv", [128, 2, E], BF16)
    ml.mskc = sb("ml_mskc", [NS, NS], F32)
    ml.ones16 = sb("ml_ones16", [NS, 128], F32)
    ml.maskB = sb("ml_maskB", [128, 128], BF16)
    ml.qg = sb("ml_qg", [128, 3], F32)
    ml.big_base = k.off[0]
    ml.cqn = sb("ml_cqn", [128, 3, TWA], BF16)
    ml.KTc = sb("ml_KTc", [128, 2, TWA], BF16)
    ml.KTr = sb("ml_KTr", [64, TWA], BF16)
    ml.ctok = sb("ml_ctok", [128, 17, 256], BF16)
    ml.cos2 = sb("ml_cos2", [64, TWA], BF16)
    ml.sin2 = sb("ml_sin2", [64, TWA], BF16)
    ml.t = [sb(f"ml_t{i}", [128, 576], F32) for i in range(3)]
    ml.sqb = sb("ml_sqb", [128, 512], BF16)
    ml.rA = sb("ml_rA", [128, 64], F32)
    ml.rB = sb("ml_rB", [128, 64], F32)
    ml.st = [sb(f"ml_st{i}", [128, 16], F32) for i in range(4)]
    ml.st_i = [0]
    early = k.off[0]
    ml.wkv = sb("ml_wkv", [128, 8, 320], BF16)
    ml.wq = sb("ml_wq", [128, 8, 384], BF16)
    ml.kvg = sb("ml_kvg", [128, 256], F32)
    ml.costk = sb("ml_costk", [128, 17, 32], F32)
    ml.sintk = sb("ml_sintk", [128, 17, 32], F32)
    ml.CK = sb("ml_CK", [128, 256], F32)
    ml.KRf = sb("ml_KRf", [128, 64], F32)
    ml.KRb = sb("ml_KRb", [128, 64], BF16)
    end_early = k.off[0]
    k.off[0] = early
    ml.QL = sb("ml_QL", [128, 2, TWA], BF16)
    ml.QR = sb("ml_QR", [64, TWA], BF16)
    ml.Pbf = sb("ml_Pbf", [128, T], BF16)
    ml.PT = sb("ml_PT", [128, 16, 128], BF16)
    ml.OLT = sb("ml_OLT", [128, 2, 512], BF16)
    ml.ZS = sb("ml_ZS", [128, 4, 512], BF16)
    ml.QN = sb("ml_QN", [128, 512], BF16)
    ml.wuq = sb("ml_wuq", [128, 3, 192], BF16)
    ml.wsw = sb("ml_wsw", [128, 3, 64], BF16)
    ml.wukr = sb("ml_wukr", [128, 2, 128], F32)
    ml.wukT = sb("ml_wukT", [128, 256], BF16)
    ml.wz = sb("ml_wz", [128, 8, 128], BF16)
    ml.Dg = sb("ml_Dg", [128, 128], BF16)
    k.off[0] = max(k.off[0], end_early)
    ml.end_a = k.off[0]
    k.off[0] = ml.big_base
    ml.KPs = [sb(f"ml_KP{i}", [128, 64, 322], BF16) for i in range(2)]
    ml.ST = sb("ml_ST", [128, 65, 16], F32)
    ml.PTs = sb("ml_PTs", [128, 65, 16], BF16)
    ml.KTp = sb("ml_KTp", [128, 2, 384], BF16)
    ml.pti = sb("ml_pti", [128, NS * 64], I32)
    ml.sm = [sb(f"ml_sm{i}", [128, 32], F32) for i in range(4)]
    ml.olat = sb("ml_olat", [NS, 256], BF16)
    k.off[0] = max(k.off[0], ml.end_a)


def mla_trig(k, out_ap, out_b, ang_ap, shift, np_, w):
    ml = k.ml
    P = k.P
    t0, t0_b, _ = ml.t[0]
    t1, t1_b, _ = ml.t[1]
    a0 = t0[0:np_, 0:w]
    a1 = t1[0:np_, 0:w]
    a1i = t1[:].bitcast(I32)[0:np_, 0:w]
    TWO_PI = 2 * PI_
    rd = [ml.t[2][1]]
    op_ts(k, 'dve', a0, ang_ap, 1.0 / TWO_PI, shift / TWO_PI, ALU.mult, ALU.add, rd, [t0_b])
    P.op('dve', lambda e: e.tensor_copy(out=a1i, in_=a0), [t0_b], [t1_b])
    P.op('dve', lambda e: e.tensor_copy(out=a0, in_=a1i), [t1_b], [t0_b])
    op_stt(k, 'dve', a0, a0, -TWO_PI, ang_ap, ALU.mult, ALU.add, [t0_b] + rd, [t0_b])
    op_ts(k, 'dve', a1, a0, shift, 0.0, ALU.add, ALU.is_lt, [t0_b], [t1_b])
    op_stt(k, 'dve', a0, a1, TWO_PI, a0, ALU.mult, ALU.add, [t0_b, t1_b], [t0_b])
    col = 1 if abs(shift - PI_) < 1e-9 else 2
    return op_act(k, out_ap, a0, AF.Sin, [t0_b, ml.cst[1]], [out_b], bias=ml.cst[0][0:np_, col:col + 1])


def layer_mla(k, ps_):
    P, dr, nc = k.P, k.dr, k.nc
    if not hasattr(k, "ml"):
        mla_setup(k)
    ml = k.ml
    TW = TWA if ps_ == 0 else T
    tiles = ntiles(ps_)
    c_w_in, c_w_in_b = dr["c_w_in"], k.dbuf["c_w_in"]

    def nst():
        s_ = ml.st[ml.st_i[0] % 4]
        ml.st_i[0] += 1
        return s_

    def ps5():
        i = k.ps_i[0] % 5
        k.ps_i[0] += 1
        return k.ps[i]

    cst, cst_b, _ = ml.cst
    P.op('dve', lambda e: e.memset(cst[:, 0:1], -PI_), writes=[cst_b])
    P.op('dve', lambda e: e.memset(cst[:, 1:2], 0.0), writes=[cst_b])
    P.op('dve', lambda e: e.memset(cst[:, 2:3], 0.5 * PI_), writes=[cst_b])
    load_w_cols(k, ml.wkv[0], ml.wkv[1], c_w_in, c_w_in_b, 384, ncols=320)
    load_w_cols(k, ml.wq[0], ml.wq[1], c_w_in, c_w_in_b, 0, ncols=384)
    op_dma(k, 'pool', ml.wuv[0][:, :, :], dr["c_w_uv"].rearrange("(c p) h v -> p c (h v)", p=128), [k.dbuf["c_w_uv"]], [ml.wuv[1]])
    op_dma(k, 'sp', ml.kvg[0][:], dr["c_kv_norm"].rearrange("(o c) -> o c", o=1).partition_broadcast(128), [k.dbuf["c_kv_norm"]], [ml.kvg[1]])
    op_dma(k, 'sp', ml.qg[0][:], dr["c_q_norm"].rearrange("(c p) -> p c", p=128), [k.dbuf["c_q_norm"]], [ml.qg[1]], slow=True)
    tf, tf_b, _ = ml.t[0]
    P.op('pool', lambda e: e.memset(tf[:, 0:128], 0.0), writes=[tf_b])
    P.op('pool', lambda e: e.affine_select(out=tf[:, 0:128], in_=tf[:, 0:128], pattern=[[-1, 128]], compare_op=ALU.is_ge, fill=-1e9,
                                           base=0, channel_multiplier=1), reads=[tf_b], writes=[tf_b])
    P.op('dve', lambda e: e.tensor_copy(out=ml.maskB[0][:], in_=tf[:, 0:128]), reads=[tf_b], writes=[ml.maskB[1]])
    P.op('pool', lambda e: e.memset(ml.mskc[0][:], 0.0), writes=[ml.mskc[1]])
    P.op('pool', lambda e: e.affine_select(out=ml.mskc[0][:], in_=ml.mskc[0][:], pattern=[[-1, NS]], compare_op=ALU.is_equal, fill=-1e9,
                                           base=0, channel_multiplier=1), reads=[ml.mskc[1]], writes=[ml.mskc[1]])
    P.op('dve', lambda e: e.memset(ml.ones16[0][:], 1.0), writes=[ml.ones16[1]])
    ti_, ti_b, _ = ml.t[1]
    tii = ti_[:].bitcast(I32)
    P.op('pool', lambda e: e.iota(tii[:, 0:32], pattern=[[1, 32]], base=0, channel_multiplier=0), writes=[ti_b])
    invf, invf_b, _ = ml.rA
    P.op('dve', lambda e: e.tensor_copy(out=invf[:, 0:32], in_=tii[:, 0:32]), reads=[ti_b], writes=[invf_b])
    op_act(k, invf[:, 0:32], invf[:, 0:32], AF.Exp, [invf_b], [invf_b], scale=-math.log(10000.0) / 32.0)
    P.op('pool', lambda e: e.iota(tii[:, 64:80], pattern=[[128, 16]], base=0, channel_multiplier=1), writes=[ti_b])
    posf, posf_b, _ = ml.rB
    P.op('dve', lambda e: e.tensor_copy(out=posf[:, 0:16], in_=tii[:, 64:80]), reads=[ti_b], writes=[posf_b])
    P.op('dve', lambda e: e.memset(posf[:, 16:17], 8192.0), writes=[posf_b])
    ang, ang_b, _ = ml.t[2]
    angv = ang[:, 0:17 * 32].rearrange("p (a i) -> p a i", i=32)
    op_tt(k, 'dve', angv, posf[:, 0:17].unsqueeze(2).to_broadcast([128, 17, 32]), invf[:, 0:32].unsqueeze(1).to_broadcast([128, 17, 32]),
          ALU.mult, [posf_b, invf_b], [ang_b])
    tmp, tmp_b, _ = ml.t[0]
    mla_trig(k, ml.sintk[0][:, :, :].rearrange("p a i -> p (a i)"), ml.sintk[1], ang[:, 0:544], PI_, 128, 544)
    mla_trig(k, ml.costk[0][:, :, :].rearrange("p a i -> p (a i)"), ml.costk[1], ang[:, 0:544], 1.5 * PI_, 128, 544)
    P.op('pool', lambda e: e.iota(tii[0:32, 0:1], pattern=[[1, 1]], base=0, channel_multiplier=1), writes=[ti_b])
    P.op('pool', lambda e: e.iota(tii[32:64, 0:1], pattern=[[1, 1]], base=0, channel_multiplier=1), writes=[ti_b])
    P.op('dve', lambda e: e.tensor_copy(out=invf[0:64, 32:33], in_=tii[0:64, 0:1]), reads=[ti_b], writes=[invf_b])
    op_act(k, invf[0:64, 33:34], invf[0:64, 32:33], AF.Exp, [invf_b], [invf_b], scale=-math.log(10000.0) / 32.0)
    P.op('dve', lambda e: e.memset(invf[0:32, 34:35], -1.0), writes=[invf_b])
    P.op('dve', lambda e: e.memset(invf[32:64, 34:35], 1.0), writes=[invf_b])
    for ti, (c0, w) in enumerate(tiles):
        if c0 < T:
            P.op('pool', lambda e, c0=c0: e.iota(tii[0:64, 0:512], pattern=[[1, 512]], base=c0, channel_multiplier=0), writes=[ti_b])
            P.op('dve', lambda e: e.tensor_copy(out=ang[0:64, 0:512], in_=tii[0:64, 0:512]), reads=[ti_b], writes=[ang_b])
        else:
            P.op('dve', lambda e: e.memset(ang[0:64, 0:NS], 8192.0), writes=[ang_b])
        op_ts(k, 'dve', ang[0:64, 0:w], ang[0:64, 0:w], invf[0:64, 33:34], None, ALU.mult, None, [ang_b, invf_b], [ang_b])
        mla_trig(k, ml.cos2[0][:, c0:c0 + w], ml.cos2[1], ang[0:64, 0:w], 1.5 * PI_, 64, w)
        mla_trig(k, ti_[0:64, 0:w], ti_b, ang[0:64, 0:w], PI_, 64, w)
        op_ts(k, 'dve', ml.sin2[0][:, c0:c0 + w], ti_[0:64, 0:w], invf[0:64, 34:35], None, ALU.mult, None, [ti_b, invf_b], [ml.sin2[1]])
    P.chk("m_tab")
    ttiles = [(tt * 128, 128, tt) for tt in range(16)]
    if ps_ == 0:
        ttiles.append((T, NS, 16))
    ctok, ctok_b, _ = ml.ctok
    for (c0, np_, tt) in ttiles:
        pkv, pkv_b = k.psn()
        for kk in range(8):
            op_mm(k, pkv[0:np_, 0:320], k.hT[:, kk, c0 + 1:c0 + 1 + np_], ml.wkv[0][:, kk, :], kk == 0, kk == 7,
                  [ml.wkv[1], k.hT_b.sub(c0 // 128)], [pkv_b])
        st, st_b, _ = nst()
        P.op('act', lambda e, np_=np_, pkv=pkv, st=st: e.activation(out=ml.sqb[0][0:np_, 0:256], in_=pkv[0:np_, 0:256],
                                                                    func=AF.Square, accum_out=st[0:np_, 0:1]),
             [pkv_b], [ml.sqb[1], st_b])
        rstd_from_ss(k, st[0:np_, 0:1], st_b, st[0:np_, 1:2], st_b, np_, 1.0 / 256, 0)
        CK, CK_b, _ = ml.CK
        op_stt(k, 'dve', CK[0:np_, :], pkv[0:np_, 0:256], st[0:np_, 1:2], ml.kvg[0][0:np_, :], ALU.mult, ALU.mult,
               [pkv_b, st_b, ml.kvg[1]], [CK_b])
        if c0 < T:
            op_dma(k, 'sp', dr["ckv_p"][ps_, c0:c0 + 128, :], CK[:, :], [CK_b], [k.dbuf["ckv_p"]])
        else:
            op_dma(k, 'sp', dr["ckv_s"][:, :], CK[0:NS, :], [CK_b], [k.dbuf["ckv_s"]])
            op_act(k, ml.ckvN[0][:, 0:256], CK[0:NS, :], AF.Copy, [CK_b], [ml.ckvN[1]])
            P.op('dve', lambda e: e.memset(ml.ckvN[0][:, 256:258], 1.0), writes=[ml.ckvN[1]])
        op_act(k, ctok[0:np_, tt, :], CK[0:np_, :], AF.Copy, [CK_b], [ctok_b.sub(tt)])
        rA, rA_b, _ = ml.rA
        rB, rB_b, _ = ml.rB
        KRf, KRf_b, _ = ml.KRf
        kr3 = pkv[0:np_, 256:320].rearrange("p (a i) -> p a i", i=32)
        op_tt(k, 'dve', rA[0:np_, 0:64].rearrange("p (a i) -> p a i", i=32), kr3,
              ml.costk[0][0:np_, tt:tt + 1, :].to_broadcast([np_, 2, 32]), ALU.mult, [pkv_b, ml.costk[1]], [rA_b])
        op_tt(k, 'dve', rB[0:np_, 0:64].rearrange("p (a i) -> p a i", i=32), kr3,
              ml.sintk[0][0:np_, tt:tt + 1, :].to_broadcast([np_, 2, 32]), ALU.mult, [pkv_b, ml.sintk[1]], [rB_b])
        op_tt(k, 'dve', KRf[0:np_, 0:32], rA[0:np_, 0:32], rB[0:np_, 32:64], ALU.subtract, [rA_b, rB_b], [KRf_b])
        op_tt(k, 'dve', KRf[0:np_, 32:64], rB[0:np_, 0:32], rA[0:np_, 32:64], ALU.add, [rA_b, rB_b], [KRf_b])
        if c0 < T:
            op_dma(k, 'sp', dr["kr_p"][ps_, c0:c0 + 128, :], KRf[:, :], [KRf_b], [k.dbuf["kr_p"]])
        else:
            op_dma(k, 'sp', dr["kr_s"][:, :], KRf[0:NS, :], [KRf_b], [k.dbuf["kr_s"]])
        KRb, KRb_b, _ = ml.KRb
        op_act(k, KRb[0:np_, :], KRf[0:np_, :], AF.Copy, [KRf_b], [KRb_b])
        psT, psT_b = k.psT
        for j in range(2):
            op_tr(k, psT[:, j * 128:j * 128 + np_], ctok[0:np_, tt, j * 128:(j + 1) * 128], k.identB[0:np_, 0:np_],
                  [ctok_b.sub(tt), k.identB_b], [psT_b])
        op_tr(k, psT[0:64, 256:256 + np_], KRb[0:np_, :], k.identB[0:np_, 0:np_], [KRb_b, k.identB_b], [psT_b])
        op_act(k, ml.KTc[0][:, :, c0:c0 + np_], psT[:, 0:256].rearrange("p (j t) -> p j t", t=128)[:, :, 0:np_], AF.Copy,
               [psT_b], [ml.KTc[1].sub(tt)])
        P.op('dve', lambda e, c0=c0, np_=np_: e.tensor_copy(out=ml.KTr[0][:, c0:c0 + np_], in_=psT[0:64, 256:256 + np_]),
             [psT_b], [ml.KTr[1].sub(tt)])
    if ps_ == 0:
        op_act(k, ml.KsT[0][:, 0:2, :], ml.KTc[0][:, :, T:T + NS], AF.Copy, [ml.KTc[1].sub(16)], [ml.KsT[1]])
        op_act(k, ml.KsT[0][0:64, 2, :], ml.KTr[0][:, T:T + NS], AF.Copy, [ml.KTr[1].sub(16)], [ml.KsT[1]])
    P.chk("m_1")
    for ti, (c0, w) in enumerate(tiles):
        pqs = []
        for qc in range(3):
            pq, pq_b = k.psn()
            for kk in range(8):
                op_mm(k, pq[:, 0:w], ml.wq[0][:, kk, qc * 128:(qc + 1) * 128], k.hT[:, kk, c0 + 1:c0 + 1 + w], kk == 0, kk == 7,
                      [ml.wq[1]] + hT_reads(k, c0, w), [pq_b])
            pqs.append((pq, pq_b))
        pss, pss_b = k.psn()
        for qc in range(3):
            sqb, sqb_b, _ = ml.sqb
            op_act(k, sqb[:, 0:w], pqs[qc][0][:, 0:w], AF.Square, [pqs[qc][1]], [sqb_b])
            op_mm(k, pss[:, 0:w], k.onesB[:], sqb[:, 0:w], qc == 0, qc == 2, [k.onesB_b, sqb_b], [pss_b])
        rst, rst_b, _ = ml.t[0]
        op_act(k, rst[:, 0:w], pss[:, 0:w], AF.Sqrt, [pss_b, k.epsr_b], [rst_b], scale=1.0 / 384, bias=k.epsr[:, 0:1])
        P.op('dve', lambda e, w=w: e.reciprocal(out=rst[:, 0:w], in_=rst[:, 0:w]), [rst_b], [rst_b])
        for qc in range(3):
            op_stt(k, 'dve', ml.cqn[0][:, qc, c0:c0 + w], pqs[qc][0][:, 0:w], ml.qg[0][:, qc:qc + 1], rst[:, 0:w], ALU.mult, ALU.mult,
                   [pqs[qc][1], ml.qg[1], rst_b], [ml.cqn[1].sub(ti)])
    P.chk("m_2")
    P.barrier()
    QL, QL_b, _ = ml.QL
    QR, QR_b, _ = ml.QR
    for h in range(16):
        wuq, wuq_b, _ = ml.wuq
        op_dma(k, 'pool', wuq[:, :, :], dr["c_w_uq"][:, h, :].rearrange("(c p) e -> p c e", p=128), [k.dbuf["c_w_uq"]], [wuq_b])
        wsw, wsw_b, _ = ml.wsw
        P.op('dve', lambda e: e.tensor_copy(out=wsw[:, :, 0:32], in_=wuq[:, :, 160:192]), [wuq_b], [wsw_b])
        P.op('dve', lambda e: e.tensor_copy(out=wsw[:, :, 32:64], in_=wuq[:, :, 128:160]), [wuq_b], [wsw_b])
        wukr, wukr_b, _ = ml.wukr
        op_dma(k, 'sp', wukr[:, :, :], dr["c_w_uk"][:, h, :].rearrange("(c p) n -> p c n", p=128), [k.dbuf["c_w_uk"]], [wukr_b])
        pw_, pw_b = k.psn()
        for cc in range(2):
            op_tr(k, pw_[:, cc * 128:(cc + 1) * 128], wukr[:, cc, :], k.identF[:], [wukr_b, k.identF_b], [pw_b])
        op_act(k, ml.wukT[0][:, :], pw_[:, 0:256], AF.Copy, [pw_b], [ml.wukT[1]])
        load_w_cols(k, ml.wz[0], ml.wz[1], c_w_in, c_w_in_b, 704 + h * 128)
        for ti, (c0, w) in enumerate(tiles):
            rq = [ml.cqn[1].sub(ti)]
            pqn, pqn_b = k.psn()
            for qc in range(3):
                op_mm(k, pqn[:, 0:w], wuq[:, qc, 0:128], ml.cqn[0][:, qc, c0:c0 + w], qc == 0, qc == 2, [wuq_b] + rq, [pqn_b])
            pra, pra_b = k.psn()
            for qc in range(3):
                op_mm(k, pra[0:64, 0:w], wuq[:, qc, 128:192], ml.cqn[0][:, qc, c0:c0 + w], qc == 0, qc == 2, [wuq_b] + rq, [pra_b])
            prb, prb_b = k.psn()
            for qc in range(3):
                op_mm(k, prb[0:64, 0:w], wsw[:, qc, :], ml.cqn[0][:, qc, c0:c0 + w], qc == 0, qc == 2, [wsw_b] + rq, [prb_b])
            QN, QN_b, _ = ml.QN
            op_act(k, QN[:, 0:w], pqn[:, 0:w], AF.Copy, [pqn_b], [QN_b])
            t1, t1_b, _ = ml.t[1]
            t2, t2_b, _ = ml.t[2]
            op_tt(k, 'dve', t1[0:64, 0:w], pra[0:64, 0:w], ml.cos2[0][:, c0:c0 + w], ALU.mult, [pra_b, ml.cos2[1]], [t1_b])
            op_tt(k, 'dve', t2[0:64, 0:w], prb[0:64, 0:w], ml.sin2[0][:, c0:c0 + w], ALU.mult, [prb_b, ml.sin2[1]], [t2_b])
            op_tt(k, 'dve', QR[:, c0:c0 + w], t1[0:64, 0:w], t2[0:64, 0:w], ALU.add, [t1_b, t2_b], [QR_b.sub(ti)])
            for cc in range(2):
                pql, pql_b = k.psn()
                op_mm(k, pql[:, 0:w], ml.wukT[0][:, cc * 128:(cc + 1) * 128], QN[:, 0:w], True, True, [ml.wukT[1], QN_b], [pql_b])
                op_act(k, QL[:, cc, c0:c0 + w], pql[:, 0:w], AF.Copy, [pql_b], [QL_b.sub(ti)])
            pz, pz_b = k.psn()
            for kk in range(8):
                op_mm(k, pz[:, 0:w], ml.wz[0][:, kk, :], k.hT[:, kk, c0 + 1:c0 + 1 + w], kk == 0, kk == 7,
                      [ml.wz[1]] + hT_reads(k, c0, w), [pz_b])
            if c0 < T:
                op_act(k, ml.ZS[0][:, ti, :], pz[:, :], AF.Silu, [pz_b], [ml.ZS[1].sub(ti)])
            else:
                op_act(k, ml.ZSs[0][:, h, :], pz[:, 0:NS], AF.Silu, [pz_b], [ml.ZSs[1]])
                P.op('dve', lambda e, h=h: e.tensor_copy(out=ml.Qs[0][:, :, :, h], in_=QL[:, :, T:T + NS]), [QL_b.sub(ti)], [ml.Qs[1]])
                P.op('dve', lambda e, h=h: e.tensor_copy(out=ml.QRs[0][:, :, h], in_=QR[:, T:T + NS]), [QR_b.sub(ti)], [ml.QRs[1]])
        P.chk("m_q")
        Pbf, Pbf_b, _ = ml.Pbf
        PT, PT_b, _ = ml.PT
        pov = [k.ps[5], k.ps[6]]
        for qb in range(16):
            t0 = qb * 128
            L = t0 + 128
            nb = (L + 511) // 512
            tq = qb // 4
            banks = [ps5() for _ in range(nb)]
            kt_r = [ml.KTc[1].sub(x) for x in range(qb + 1)] + [ml.KTr[1].sub(x) for x in range(qb + 1)]
            for b in range(nb):
                l0 = b * 512
                lw = min(512, L - l0)
                bk, bk_b = banks[b]
                last = (b == nb - 1)
                op_mm(k, bk[:, 0:lw], QL[:, 0, t0:t0 + 128], ml.KTc[0][:, 0, l0:l0 + lw], True, False, [QL_b.sub(tq)] + kt_r, [bk_b])
                op_mm(k, bk[:, 0:lw], QL[:, 1, t0:t0 + 128], ml.KTc[0][:, 1, l0:l0 + lw], False, False, [QL_b.sub(tq)] + kt_r, [bk_b])
                op_mm(k, bk[:, 0:lw], QR[:, t0:t0 + 128], ml.KTr[0][:, l0:l0 + lw], False, not last, [QR_b.sub(tq)] + kt_r, [bk_b])
                if last:
                    dcol = t0 - l0
                    op_mm(k, bk[:, dcol:dcol + 128], k.identB[:], ml.maskB[0][:], False, True, [k.identB_b, ml.maskB[1]], [bk_b])
            st, st_b, _ = nst()
            for b in range(nb):
                lw = min(512, L - b * 512)
                bk, bk_b = banks[b]
                P.op('dve', lambda e, b=b, lw=lw, bk=bk, st=st: e.tensor_reduce(out=st[:, b:b + 1], in_=bk[:, 0:lw], axis=AX.X, op=ALU.max),
                     [bk_b], [st_b])
            if nb > 1:
                P.op('dve', lambda e, nb=nb, st=st: e.tensor_reduce(out=st[:, 4:5], in_=st[:, 0:nb], axis=AX.X, op=ALU.max), [st_b], [st_b])
                mcol = st[:, 4:5]
            else:
                mcol = st[:, 0:1]
            op_ts(k, 'dve', st[:, 5:6], mcol, -MLA_SCALE_, None, ALU.mult, None, [st_b], [st_b])
            for b in range(nb):
                l0 = b * 512
                lw = min(512, L - l0)
                bk, bk_b = banks[b]
                P.op('act', lambda e, b=b, l0=l0, lw=lw, bk=bk, st=st: e.activation(out=Pbf[:, l0:l0 + lw], in_=bk[:, 0:lw], func=AF.Exp,
                                                                                   scale=MLA_SCALE_, bias=st[:, 5:6],
                                                                                   accum_out=st[:, 8 + b:9 + b]),
                     [bk_b, st_b], [Pbf_b.sub(b), st_b])
            if nb > 1:
                P.op('dve', lambda e, nb=nb, st=st: e.tensor_reduce(out=st[:, 6:7], in_=st[:, 8:8 + nb], axis=AX.X, op=ALU.add), [st_b], [st_b])
                scol = st[:, 6:7]
            else:
                scol = st[:, 8:9]
            P.op('dve', lambda e, st=st, scol=scol: e.reciprocal(out=st[:, 7:8], in_=scol), [st_b], [st_b])
            Dg, Dg_b, _ = ml.Dg
            op_ts(k, 'dve', Dg[:, :], k.identF[:, :], st[:, 7:8], None, ALU.mult, None, [k.identF_b, st_b], [Dg_b])
            for g0 in range(0, qb + 1, 4):
                g1 = min(qb + 1, g0 + 4)
                pt, pt_b = ps5()
                for kb in range(g0, g1):
                    op_mm(k, pt[:, (kb - g0) * 128:(kb - g0 + 1) * 128], Pbf[:, kb * 128:(kb + 1) * 128], Dg[:, :], True, True,
                          [Pbf_b.sub(kb // 4), Dg_b], [pt_b])
                n_ = g1 - g0
                if (g0 // 4) % 2 == 0:
                    op_act(k, PT[:, g0:g1, :], pt[:, 0:n_ * 128].rearrange("p (g t) -> p g t", t=128), AF.Copy, [pt_b], [PT_b.sub(g0 // 4)])
                else:
                    P.op('dve', lambda e, g0=g0, g1=g1, n_=n_, pt=pt: e.tensor_copy(
                        out=PT[:, g0:g1, :], in_=pt[:, 0:n_ * 128].rearrange("p (g t) -> p g t", t=128)), [pt_b], [PT_b.sub(g0 // 4)])
            qc_ = slice((qb % 4) * 128, (qb % 4 + 1) * 128)
            for cc in range(2):
                pv_, pv_b = pov[cc]
                for kb in range(qb + 1):
                    op_mm(k, pv_[:, qc_], ctok[:, kb, cc * 128:(cc + 1) * 128], PT[:, kb, :], kb == 0, kb == qb,
                          [ctok_b.sub(kb), PT_b.sub(kb // 4)], [pv_b])
            if qb % 4 == 3:
                OLT, OLT_b, _ = ml.OLT
                op_act(k, OLT[:, 0, :], pov[0][0][:, :], AF.Copy, [pov[0][1]], [OLT_b])
                P.op('dve', lambda e: e.tensor_copy(out=OLT[:, 1, :], in_=pov[1][0][:, :]), [pov[1][1]], [OLT_b])
                po, po_b = ps5()
                for cc in range(2):
                    op_mm(k, po[:, :], ml.wuv[0][:, cc, h * 128:(h + 1) * 128], OLT[:, cc, :], cc == 0, cc == 1, [ml.wuv[1], OLT_b], [po_b])
                op_tt(k, 'dve', k.GT[:, h, tq * 512:(tq + 1) * 512], po[:, :], ml.ZS[0][:, tq, :], ALU.mult, [po_b, ml.ZS[1].sub(tq)],
                      [k.GT_b.sub(h).sub(tq)])
            P.chk("m_att")
    if ps_ == 0:
        P.barrier()
        mla_sample(k)


def mla_sample(k):
    P, dr, nc, ml = k.P, k.dr, k.nc, k.ml
    ST, ST_b, _ = ml.ST
    PTs, PTs_b, _ = ml.PTs
    pti, pti_b, _ = ml.pti
    ptf, ptf_b = pti[:].bitcast(F32), pti_b
    idx, idx_b = pti, pti_b
    op_dma(k, 'sp', pti[:, :], dr["page_table"].rearrange("s j -> (s j)").rearrange("(o n) -> o n", o=1).partition_broadcast(128),
           [k.dbuf["page_table"]], [pti_b])
    P.op('dve', lambda e: e.tensor_copy(out=ptf[:, :], in_=pti[:, :]), [pti_b], [ptf_b])
    sm0, sm0_b, _ = ml.sm[0]
    smi = sm0[:].bitcast(I32)
    P.op('pool', lambda e: e.iota(smi[:, 0:1], pattern=[[1, 1]], base=0, channel_multiplier=1), writes=[sm0_b])
    P.op('dve', lambda e: e.tensor_copy(out=sm0[:, 1:2], in_=smi[:, 0:1]), [sm0_b], [sm0_b])
    op_ts(k, 'dve', ptf[:, :], ptf[:, :], 128.0, sm0[:, 1:2], ALU.mult, ALU.add, [ptf_b, sm0_b], [ptf_b])
    P.op('dve', lambda e: e.tensor_copy(out=idx[:, :], in_=ptf[:, :]), [ptf_b], [idx_b])
    for i_ in range(2):
        P.op('dve', lambda e, i_=i_: e.memset(ml.KPs[i_][0][:, :, 320:322], 1.0), writes=[ml.KPs[i_][1]])
    cat_rows = dr["cache_cat"].rearrange("n t c -> (n t) c")
    P.chk("s_idx")
    for s in range(NS):
        KP, KP_b, _ = ml.KPs[s % 2]
        for j in range(64):
            n = s * 64 + j
            P.op('pool', lambda e, j=j, n=n, KP=KP: e.indirect_dma_start(out=KP[:, j, 0:320], out_offset=None, in_=cat_rows,
                                                                        in_offset=bass.IndirectOffsetOnAxis(ap=idx[:, n:n + 1], axis=0)),
                 [idx_b, k.dbuf["cache_cat"]], [KP_b.sub(j)], dma=True)
        P.chk("s_gather")
        psT, psT_b = k.psT
        pS = [k.psn(), k.psn()]
        for j in range(64):
            o_ = (j % 2) * 384
            if True:
                op_tr(k, psT[0:64, o_:o_ + 128], KP[:, j, 0:64], k.identB[:], [KP_b.sub(j), k.identB_b], [psT_b])
                op_tr(k, psT[:, o_ + 128:o_ + 256], KP[:, j, 64:192], k.identB[:], [KP_b.sub(j), k.identB_b], [psT_b])
                op_tr(k, psT[:, o_ + 256:o_ + 384], KP[:, j, 192:320], k.identB[:], [KP_b.sub(j), k.identB_b], [psT_b])
            if j % 2 == 1:
                KTp, KTp_b, _ = ml.KTp
                op_act(k, KTp[:, :, :], psT[:, 0:768].rearrange("p (a c) -> p a c", c=384), AF.Copy, [psT_b], [KTp_b])
                for jj in (j - 1, j):
                    a = jj % 2
                    bk, bk_b = pS[jj // 32]
                    cs_ = slice((jj % 32) * 16, (jj % 32 + 1) * 16)
                    op_mm(k, bk[:, cs_], KTp[0:64, a, 0:128], ml.QRs[0][:, s, :], True, False, [KTp_b, ml.QRs[1]], [bk_b])
                    op_mm(k, bk[:, cs_], KTp[:, a, 128:256], ml.Qs[0][:, 0, s, :], False, False, [KTp_b, ml.Qs[1]], [bk_b])
                    op_mm(k, bk[:, cs_], KTp[:, a, 256:384], ml.Qs[0][:, 1, s, :], False, True, [KTp_b, ml.Qs[1]], [bk_b])
        for g in range(2):
            bk, bk_b = pS[g]
            P.op('dve', lambda e, g=g, bk=bk: e.tensor_copy(out=ST[:, g * 32:(g + 1) * 32, :], in_=bk[:, :].rearrange("p (j h) -> p j h", h=16)),
                 [bk_b], [ST_b])
        pN, pN_b = k.psn()
        op_mm(k, pN[0:NS, 0:16], ml.KsT[0][0:64, 2, :], ml.QRs[0][:, s, :], True, False, [ml.KsT[1], ml.QRs[1]], [pN_b])
        op_mm(k, pN[0:NS, 0:16], ml.KsT[0][:, 0, :], ml.Qs[0][:, 0, s, :], False, False, [ml.KsT[1], ml.Qs[1]], [pN_b])
        op_mm(k, pN[0:NS, 0:16], ml.KsT[0][:, 1, :], ml.Qs[0][:, 1, s, :], False, True, [ml.KsT[1], ml.Qs[1]], [pN_b])
        P.op('dve', lambda e: e.memset(ST[:, 64, :], -1e9), writes=[ST_b])
        op_ts(k, 'dve', ST[0:NS, 64, :], pN[0:NS, 0:16], ml.mskc[0][:, s:s + 1], None, ALU.add, None, [pN_b, ml.mskc[1]], [ST_b])
        sm1, sm1_b, _ = ml.sm[1]
        P.op('dve', lambda e: e.tensor_reduce(out=sm1[:, 0:16], in_=ST[:, :, :].rearrange("p j h -> p h j"), axis=AX.X, op=ALU.max),
             [ST_b], [sm1_b])
        pM, pM_b = k.psn()
        op_tr(k, pM[0:16, 0:128], sm1[:, 0:16], k.identF[:], [sm1_b, k.identF_b], [pM_b])
        sm2, sm2_b, _ = ml.sm[2]
        P.op('dve', lambda e, pM=pM: e.tensor_reduce(out=sm2[0:16, 0:1], in_=pM[0:16, 0:128], axis=AX.X, op=ALU.max), [pM_b], [sm2_b])
        op_ts(k, 'dve', sm2[0:16, 16:32], k.identF[0:16, 0:16], sm2[0:16, 0:1], None, ALU.mult, None, [k.identF_b, sm2_b], [sm2_b])
        pB, pB_b = k.psn()
        op_mm(k, pB[:, 0:16], ml.ones16[0][:, :], sm2[0:16, 16:32], True, True, [ml.ones16[1], sm2_b], [pB_b])
        sm3, sm3_b, _ = ml.sm[3]
        op_act(k, sm3[:, 0:16], pB[:, 0:16], AF.Copy, [pB_b], [sm3_b], scale=-MLA_SCALE_)
        op_stt(k, 'dve', ST[:, :, :], ST[:, :, :], MLA_SCALE_, sm3[:, 0:16].unsqueeze(1).to_broadcast([128, 65, 16]), ALU.mult, ALU.add,
               [ST_b, sm3_b], [ST_b])
        op_act(k, PTs[:, :, :], ST[:, :, :], AF.Exp, [ST_b], [PTs_b])
        pO, pO_b = k.psn()
        for j in range(64):
            op_mm(k, pO[0:16, 0:257], PTs[:, j, :], KP[:, j, 64:321], j == 0, False, [PTs_b, KP_b.sub(j)], [pO_b])
        op_mm(k, pO[0:16, 0:257], PTs[0:NS, 64, :], ml.ckvN[0][:, 0:257], False, True, [PTs_b, ml.ckvN[1]], [pO_b])
        P.op('dve', lambda e, pO=pO: e.reciprocal(out=sm2[0:16, 1:2], in_=pO[0:16, 256:257]), [pO_b, sm2_b], [sm2_b])
        olat, olat_b, _ = ml.olat
        op_ts(k, 'dve', olat[:, :], pO[0:16, 0:256], sm2[0:16, 1:2], None, ALU.mult, None, [pO_b, sm2_b], [olat_b])
        for cc in range(2):
            op_tr(k, psT[:, cc * 16:(cc + 1) * 16], olat[:, cc * 128:(cc + 1) * 128], k.identB[0:16, 0:16], [olat_b, k.identB_b], [psT_b])
        op_act(k, ml.OLs[0][:, :, :, s], psT[:, 0:32].rearrange("p (c h) -> p c h", h=16), AF.Copy, [psT_b], [ml.OLs[1]])
        P.chk("s_one")
    for h in range(16):
        po, po_b = k.psn()
        for cc in range(2):
            op_mm(k, po[:, 0:NS], ml.wuv[0][:, cc, h * 128:(h + 1) * 128], ml.OLs[0][:, cc, h, :], cc == 0, cc == 1,
                  [ml.wuv[1], ml.OLs[1]], [po_b])
        op_tt(k, 'dve', k.GT[:, h, T:T + NS], po[:, 0:NS], ml.ZSs[0][:, h, :], ALU.mult, [po_b, ml.ZSs[1]], [k.GT_b.sub(h).sub(4)])

S5C = 64
S5NCH = T // S5C


def s5_setup(k):
    P, dr, nc, sb = k.P, k.dr, k.nc, k.sb
    s5 = K()
    k.s5 = s5
    s5.cst = sb("s5_cst", [128, 4], F32)
    s5.P64 = {n: sb("s5_p_" + n, [128, 64], F32) for n in
              ["lr", "li", "dt", "m", "th", "are", "aim", "fre", "fim", "t0", "t1", "lnm", "m64", "ph"]}
    s5.LB = [sb(f"s5_LB{i}", [128, 16, 128], BF16) for i in range(2)]
    s5.LC = [sb(f"s5_LC{i}", [128, 16, 128], BF16) for i in range(2)]
    s5.LC3 = [sb(f"s5_LC3{i}", [128, 16, 64], BF16) for i in range(2)]
    s5.Uz = sb("s5_Uz", [128, TWA], BF16)
    s5.mask4 = sb("s5_mask4", [128, 128], F32)
    s5.par = {n: sb("s5_par_" + n, [128, 16], F32) for n in ["d", "bg"]}
    s5.m01 = sb("s5_m01", [128, T], BF16)
    s5.U = sb("s5_U", [128, TWA], BF16)
    s5.wt = sb("s5_wt", [128, 8, 128], BF16)
    s5.tmpt = {n: sb("s5_tab_" + n, [128, 64], F32) for n in ["ang", "mg", "a", "b", "c"]}
    s5.pack = [sb(f"s5_pack{i}", [128, 6, 64], F32) for i in range(2)]
    s5.tabs = []
    for i in range(2):
        d_ = dict(s5.tmpt)
        for j_, n in enumerate(["Fc", "Fs", "Bc", "Bs", "Gc", "Gs"]):
            d_[n] = (s5.pack[i][0][:, j_, :], s5.pack[i][1].sub(n), 0)
        s5.tabs.append(d_)
    s5.tab = s5.tabs[0]
    s5.big = [sb(f"s5_big{i}", [128, T], F32) for i in range(5)]
    s5.sbf = [sb(f"s5_sbf{i}", [128, TWA], BF16) for i in range(2)]
    s5.ec_ = {n: sb("s5_e_" + n, [128, 64], F32) for n in ["er", "ei", "hr", "hi", "Er", "Ei", "cr", "ci"]}
    s5.fin = [sb(f"s5_fin{i}", [128, 64], F32) for i in range(2)]
    s5.s0T = [sb(f"s5_s0T{i}", [128, 64, NS], F32) for i in range(2)]
    s5.tmpn = [sb(f"s5_tmpn{i}", [128, NS], F32) for i in range(2)]
    s5.g = [sb(f"s5_g{i}", [128, 512], F32) for i in range(3)]
    s5.gb = sb("s5_gb", [128, 512], BF16)
    al = lambda name, shape, dt, o: (nc.alloc_sbuf_tensor_at(name, shape, dt, offset=o), Buf(name), o)
    s5.srow = al("s5_srow", [NS, 2048], F32, s5.big[0][2])
    s5.wg = al("s5_wg", [128, 16, 128], BF16, s5.big[1][2])
    s5.XX = [al("s5_XX0", [128, 64, 32], F32, s5.big[3][2]), al("s5_XX1", [128, 64, 32], F32, s5.big[4][2])]
    s5.bre = al("s5_bre", [128, 64, 16], F32, s5.sbf[0][2])
    s5.bim = al("s5_bim", [128, 64, 16], F32, s5.sbf[1][2])
    s5.CN = [al("s5_CN0", [128, 16, 64], F32, s5.big[2][2] + 4096), al("s5_CN1", [128, 16, 64], F32, s5.big[1][2] + 4096)]


def s5_trig(k, out_ap, out_b, ang_ap, ang_b, shift, w):
    P, s5 = k.P, k.s5
    a0, a0_b = s5.tmpt["a"][0][:, 0:w], s5.tmpt["a"][1]
    a1, a1_b = s5.tmpt["b"][0][:, 0:w], s5.tmpt["b"][1]
    a1i = s5.tmpt["b"][0][:].bitcast(I32)[:, 0:w]
    TWO_PI = 2 * PI_
    op_ts(k, 'dve', a0, ang_ap, 1.0 / TWO_PI, shift / TWO_PI, ALU.mult, ALU.add, [ang_b], [a0_b])
    P.op('dve', lambda e: e.tensor_copy(out=a1i, in_=a0), [a0_b], [a1_b])
    P.op('dve', lambda e: e.tensor_copy(out=a0, in_=a1i), [a1_b], [a0_b])
    op_stt(k, 'dve', a0, a0, -TWO_PI, ang_ap, ALU.mult, ALU.add, [a0_b, ang_b], [a0_b])
    op_ts(k, 'dve', a1, a0, shift, 0.0, ALU.add, ALU.is_lt, [a0_b], [a1_b])
    op_stt(k, 'dve', a0, a1, TWO_PI, a0, ALU.mult, ALU.add, [a0_b, a1_b], [a0_b])
    col = 1 if abs(shift - PI_) < 1e-9 else 2
    op_act(k, out_ap, a0, AF.Sin, [a0_b, s5.cst[1]], [out_b], bias=s5.cst[0][:, col:col + 1])


def s5_load(k):
    P, dr, nc, s5 = k.P, k.dr, k.nc, k.s5
    cst, cst_b, _ = s5.cst
    P.op('dve', lambda e: e.memset(cst[:, 0:1], -PI_), writes=[cst_b])
    P.op('dve', lambda e: e.memset(cst[:, 1:2], 0.0), writes=[cst_b])
    P.op('dve', lambda e: e.memset(cst[:, 2:3], 0.5 * PI_), writes=[cst_b])
    p = s5.P64
    for n, src in (("lr", "d_lambda_re"), ("li", "d_lambda_im")):
        for g2 in range(2):
            op_dma(k, 'sp', p[n][0][g2 * 64:(g2 + 1) * 64, :], dr[src].rearrange("(c g2) p -> g2 p c", g2=2)[g2],
                   [k.dbuf[src]], [p[n][1]], slow=True)
    for g2 in range(2):
        op_dma(k, 'sp', p["dt"][0][g2 * 64:(g2 + 1) * 64, :],
               dr["d_log_dt"].rearrange("(c g2) -> g2 c", g2=2)[g2:g2 + 1, :].partition_broadcast(64), [k.dbuf["d_log_dt"]], [p["dt"][1]],
               slow=True)
    A = lambda n: p[n][0][:, :]
    Bf = lambda n: p[n][1]
    op_act(k, A("dt"), A("dt"), AF.Exp, [Bf("dt")], [Bf("dt")])
    op_tt(k, 'dve', A("lnm"), A("lr"), A("dt"), ALU.mult, [Bf("lr"), Bf("dt")], [Bf("lnm")])
    op_act(k, A("m"), A("lnm"), AF.Exp, [Bf("lnm")], [Bf("m")])
    op_act(k, A("m64"), A("lnm"), AF.Exp, [Bf("lnm")], [Bf("m64")], scale=float(S5C))
    op_tt(k, 'dve', A("th"), A("li"), A("dt"), ALU.mult, [Bf("li"), Bf("dt")], [Bf("th")])
    op_ts(k, 'dve', A("ph"), A("th"), float(S5C), None, ALU.mult, None, [Bf("th")], [Bf("ph")])
    s5_trig(k, A("aim"), Bf("aim"), A("th"), Bf("th"), PI_, 64)
    s5_trig(k, A("are"), Bf("are"), A("th"), Bf("th"), 1.5 * PI_, 64)
    op_tt(k, 'dve', A("are"), A("are"), A("m"), ALU.mult, [Bf("are"), Bf("m")], [Bf("are")])
    op_tt(k, 'dve', A("aim"), A("aim"), A("m"), ALU.mult, [Bf("aim"), Bf("m")], [Bf("aim")])
    op_tt(k, 'dve', A("t0"), A("lr"), A("lr"), ALU.mult, [Bf("lr")], [Bf("t0")])
    op_tt(k, 'dve', A("t1"), A("li"), A("li"), ALU.mult, [Bf("li")], [Bf("t1")])
    op_tt(k, 'dve', A("t0"), A("t0"), A("t1"), ALU.add, [Bf("t0"), Bf("t1")], [Bf("t0")])
    P.op('dve', lambda e: e.reciprocal(out=A("t0"), in_=A("t0")), [Bf("t0")], [Bf("t0")])
    op_ts(k, 'dve', A("t1"), A("are"), -1.0, None, ALU.add, None, [Bf("are")], [Bf("t1")])
    op_tt(k, 'dve', A("fre"), A("t1"), A("lr"), ALU.mult, [Bf("t1"), Bf("lr")], [Bf("fre")])
    op_tt(k, 'dve', A("fim"), A("aim"), A("li"), ALU.mult, [Bf("aim"), Bf("li")], [Bf("fim")])
    op_tt(k, 'dve', A("fre"), A("fre"), A("fim"), ALU.add, [Bf("fre"), Bf("fim")], [Bf("fre")])
    op_tt(k, 'dve', A("fim"), A("aim"), A("lr"), ALU.mult, [Bf("aim"), Bf("lr")], [Bf("fim")])
    op_tt(k, 'dve', A("t1"), A("t1"), A("li"), ALU.mult, [Bf("t1"), Bf("li")], [Bf("t1")])
    op_tt(k, 'dve', A("fim"), A("fim"), A("t1"), ALU.subtract, [Bf("fim"), Bf("t1")], [Bf("fim")])
    op_tt(k, 'dve', A("fre"), A("fre"), A("t0"), ALU.mult, [Bf("fre"), Bf("t0")], [Bf("fre")])
    op_tt(k, 'dve', A("fim"), A("fim"), A("t0"), ALU.mult, [Bf("fim"), Bf("t0")], [Bf("fim")])
    for t_, src in ((s5.bre, "d_b_re"), (s5.bim, "d_b_im")):
        for g2 in range(2):
            op_dma(k, 'sp', t_[0][g2 * 64:(g2 + 1) * 64, :, :], dr[src].rearrange("(c g2) p q -> g2 p c q", g2=2)[g2],
                   [k.dbuf[src]], [t_[1]])
    for i in range(2):
        P.op('pool', lambda e, i=i: e.memset(s5.XX[i][0][:], 0.0), writes=[s5.XX[i][1]])
    big0, big0_b, _ = s5.big[0]
    big1, big1_b, _ = s5.big[1]
    v1 = lambda t: t[:, 0:1024].rearrange("p (c q) -> p c q", q=16)
    frb = A("fre").unsqueeze(2).to_broadcast([128, 64, 16])
    fib = A("fim").unsqueeze(2).to_broadcast([128, 64, 16])
    op_tt(k, 'dve', v1(big0), s5.bre[0][:], frb, ALU.mult, [s5.bre[1], Bf("fre")], [big0_b])
    op_tt(k, 'dve', v1(big1), s5.bim[0][:], fib, ALU.mult, [s5.bim[1], Bf("fim")], [big1_b])
    op_tt(k, 'dve', v1(big0), v1(big0), v1(big1), ALU.subtract, [big0_b, big1_b], [big0_b])
    for g2 in range(2):
        hP = slice(g2 * 64, g2 * 64 + 64)
        P.op('dve', lambda e, g2=g2, hP=hP: e.tensor_copy(out=s5.XX[0][0][hP, :, g2 * 16:(g2 + 1) * 16], in_=v1(big0)[hP]),
             [big0_b], [s5.XX[0][1]])
    op_tt(k, 'dve', v1(big0), s5.bim[0][:], frb, ALU.mult, [s5.bim[1], Bf("fre")], [big0_b])
    op_tt(k, 'dve', v1(big1), s5.bre[0][:], fib, ALU.mult, [s5.bre[1], Bf("fim")], [big1_b])
    op_tt(k, 'dve', v1(big0), v1(big0), v1(big1), ALU.add, [big0_b, big1_b], [big0_b])
    for g2 in range(2):
        hP = slice(g2 * 64, g2 * 64 + 64)
        P.op('dve', lambda e, g2=g2, hP=hP: e.tensor_copy(out=s5.XX[1][0][hP, :, g2 * 16:(g2 + 1) * 16], in_=v1(big0)[hP]),
             [big0_b], [s5.XX[1][1]])
    for i in range(2):
        for ec in range(16):
            pt, pt_b = k.psn()
            op_tr(k, pt[:, 0:128], s5.XX[i][0][:, 4 * ec:4 * ec + 4, :].rearrange("p a b -> p (a b)"), k.identF[:],
                  [s5.XX[i][1], k.identF_b], [pt_b])
            op_act(k, s5.LB[i][0][:, ec, :], pt[:, 0:128], AF.Copy, [pt_b], [s5.LB[i][1]])
    m4, m4_b, _ = s5.mask4
    P.op('dve', lambda e: e.memset(m4[:], 0.0), writes=[m4_b])
    big2, big2_b, _ = s5.big[2]
    P.op('dve', lambda e: e.tensor_reduce(out=big2[:, 4:6], in_=k.identF[:, :].rearrange("p (q g c) -> p g q c", q=4, g=2),
                                          axis=AX.XY, op=ALU.add), [k.identF_b], [big2_b])
    P.op('dve', lambda e: e.memset(m4[:], 1.0), writes=[m4_b])
    op_ts(k, 'dve', m4[:, 0:64], m4[:, 0:64], big2[:, 4:5], None, ALU.mult, None, [m4_b, big2_b], [m4_b])
    op_ts(k, 'dve', m4[:, 64:128], m4[:, 64:128], big2[:, 5:6], None, ALU.mult, None, [m4_b, big2_b], [m4_b])
    for i, src in ((0, "d_c_re"), (1, "d_c_im")):
        op_dma(k, 'sp', s5.CN[i][0][:, :, :], dr[src].rearrange("(e a) k p -> (a k) e p", a=8), [k.dbuf[src]], [s5.CN[i][1]])
        for ec in range(16):
            y4, y4_b, _ = s5.g[0]
            op_tt(k, 'dve', y4[:, 0:128].rearrange("p (a b) -> p a b", b=64), s5.CN[i][0][:, ec:ec + 1, :].to_broadcast([128, 2, 64]),
                  m4[:, :].rearrange("p (a b) -> p a b", b=64), ALU.mult, [s5.CN[i][1], m4_b], [y4_b])
            pt, pt_b = k.psn()
            op_tr(k, pt[:, 0:128], y4[:, 0:128], k.identF[:], [y4_b, k.identF_b], [pt_b])
            if i == 0:
                op_act(k, s5.LC[i][0][:, ec, :], pt[:, 0:128], AF.Copy, [pt_b], [s5.LC[i][1]])
            else:
                op_act(k, s5.LC[i][0][:, ec, :], pt[:, 0:128], AF.Copy, [pt_b], [s5.LC[i][1]], scale=-1.0)
    for i in range(2):
        P.op('dve', lambda e, i=i: e.tensor_copy(out=s5.LC3[i][0][:, :, :], in_=s5.LC[i][0][:, :, 64:128]), [s5.LC[i][1]], [s5.LC3[i][1]])
        P.op('dve', lambda e, i=i: e.memset(s5.LC3[i][0][:, :, 0:32], 0.0), writes=[s5.LC3[i][1]])
    for n, src in (("d", "d_d"), ("bg", "d_b_glu")):
        op_dma(k, 'sp', s5.par[n][0][:], dr[src].rearrange("(c p) -> p c", p=128), [k.dbuf[src]], [s5.par[n][1]], slow=True)
    m01, m01_b, _ = s5.m01
    P.op('pool', lambda e: e.memset(m01[:], 1.0), writes=[m01_b])
    P.op('pool', lambda e: e.memset(m01[:].rearrange("p (c t) -> p c t", t=S5C)[:, :, 0:1], 0.0), writes=[m01_b])


def s5_tables(k, tab, col):
    P, s5 = k.P, k.s5
    p = s5.P64
    A = lambda n: p[n][0]
    Bf = lambda n: p[n][1]
    TA = lambda n: tab[n][0][:, :]
    TB = lambda n: tab[n][1]
    P.op('pool', lambda e: e.iota(tab["c"][0][:].bitcast(I32)[:, 0:64], pattern=[[1, 64]], base=0, channel_multiplier=0),
         writes=[TB("c")])
    P.op('dve', lambda e: e.tensor_copy(out=TA("c"), in_=tab["c"][0][:].bitcast(I32)[:, 0:64]), [TB("c")], [TB("c")])
    op_ts(k, 'dve', TA("ang"), TA("c"), A("th")[:, col], None, ALU.mult, None, [TB("c"), Bf("th")], [TB("ang")])
    op_ts(k, 'dve', TA("mg"), TA("c"), A("lnm")[:, col], None, ALU.mult, None, [TB("c"), Bf("lnm")], [TB("mg")])
    s5_trig(k, TA("Fs"), TB("Fs"), TA("ang"), TB("ang"), PI_, 64)
    s5_trig(k, TA("Fc"), TB("Fc"), TA("ang"), TB("ang"), 1.5 * PI_, 64)
    op_act(k, TA("c"), TA("mg"), AF.Exp, [TB("mg")], [TB("c")])
    op_tt(k, 'dve', TA("Bc"), TA("Fc"), TA("c"), ALU.mult, [TB("Fc"), TB("c")], [TB("Bc")])
    op_tt(k, 'dve', TA("Bs"), TA("Fs"), TA("c"), ALU.mult, [TB("Fs"), TB("c")], [TB("Bs")])
    op_act(k, TA("c"), TA("mg"), AF.Exp, [TB("mg")], [TB("c")], scale=-1.0)
    op_tt(k, 'dve', TA("Fc"), TA("Fc"), TA("c"), ALU.mult, [TB("Fc"), TB("c")], [TB("Fc")])
    op_tt(k, 'dve', TA("Fs"), TA("Fs"), TA("c"), ALU.mult, [TB("Fs"), TB("c")], [TB("Fs")])
    P.op('pool', lambda e: e.iota(tab["c"][0][:].bitcast(I32)[:, 0:32], pattern=[[1, 32]], base=0, channel_multiplier=0),
         writes=[TB("c")])
    P.op('dve', lambda e: e.tensor_copy(out=tab["c"][0][:, 0:32], in_=tab["c"][0][:].bitcast(I32)[:, 0:32]), [TB("c")], [TB("c")])
    op_ts(k, 'dve', tab["ang"][0][:, 0:32], tab["c"][0][:, 0:32], A("ph")[:, col], None, ALU.mult, None, [TB("c"), Bf("ph")], [TB("ang")])
    s5_trig(k, tab["Gs"][0][:, 0:32], TB("Gs"), tab["ang"][0][:, 0:32], TB("ang"), PI_, 32)
    s5_trig(k, tab["Gc"][0][:, 0:32], TB("Gc"), tab["ang"][0][:, 0:32], TB("ang"), 1.5 * PI_, 32)


def layer_s5(k, ps_):
    P, dr, nc = k.P, k.dr, k.nc
    if not hasattr(k, "s5"):
        s5_setup(k)
    s5 = k.s5
    s5_load(k)
    P.barrier()
    P.chk("s5_load")
    tiles = ntiles(ps_)
    p = s5.P64
    A = lambda n: p[n][0]
    Bf = lambda n: p[n][1]
    d_w_in, d_w_in_b = dr["d_w_in"], k.dbuf["d_w_in"]
    U, U_b, _ = s5.U
    if ps_ == 0:
        for i, src in ((0, "state_ssm_re"), (1, "state_ssm_im")):
            for q4 in range(4):
                sr, sr_b, _ = s5.srow
                op_dma(k, 'sp', sr[:, :], dr[src].rearrange("s g p -> s (g p)")[:, q4 * 2048:(q4 + 1) * 2048], [k.dbuf[src]], [sr_b])
                for j in range(16):
                    sc = q4 * 16 + j
                    if j % 8 == 0:
                        pt, pt_b = k.psn()
                    op_tr(k, pt[:, (j % 8) * NS:(j % 8 + 1) * NS], sr[0:NS, j * 128:(j + 1) * 128], k.identF[0:NS, 0:NS],
                          [sr_b, k.identF_b], [pt_b])
                    if j % 8 == 7:
                        P.op('dve', lambda e, i=i, sc=sc, pt=pt: e.tensor_copy(
                            out=s5.s0T[i][0][:, sc - 7:sc + 1, :], in_=pt[:, 0:8 * NS].rearrange("p (a s) -> p a s", s=NS)),
                            [pt_b], [s5.s0T[i][1]])
    big = s5.big
    P.barrier()

    def ps56():
        i = 5 + k.ps_i[0] % 2
        k.ps_i[0] += 1
        return k.ps[i]
    for ec in range(16):
        load_w_cols(k, s5.wt[0], s5.wt[1], d_w_in, d_w_in_b, ec * 128)
        for ti, (c0, w) in enumerate(tiles):
            pu, pu_b = k.psn()
            for kk in range(8):
                op_mm(k, pu[:, 0:w], s5.wt[0][:, kk, :], k.hT[:, kk, c0 + 1:c0 + 1 + w], kk == 0, kk == 7,
                      [s5.wt[1]] + hT_reads(k, c0, w), [pu_b])
            op_act(k, U[:, c0:c0 + w], pu[:, 0:w], AF.Copy, [pu_b], [U_b.sub(ti)])
        Uz, Uz_b, _ = s5.Uz
        P.op('dve', lambda e: e.tensor_copy(out=Uz[64:128, :], in_=U[64:128, :]), [U_b], [Uz_b])
        P.op('dve', lambda e: e.memset(Uz[64:96, :], 0.0), writes=[Uz_b])
        P.chk("s5_u")
        py = {}
        for q in (0, 1, 3, 2):
            sc = ec * 4 + q
            col = slice(sc, sc + 1)
            qP = slice(q * 32, q * 32 + 32)
            tab = s5.tabs[sc % 2]
            TA = lambda n, tab=tab: tab[n][0][:, :]
            TB = lambda n, tab=tab: tab[n][1]
            if ps_ == 1:
                op_dma(k, 'sp', s5.pack[sc % 2][0][:, :, :], dr["tabscr"][sc], [k.dbuf["tabscr"]], [s5.pack[sc % 2][1]])
            if ps_ == 0:
                s5_tables(k, tab, col)
                op_dma(k, 'sp', dr["tabscr"][sc], s5.pack[sc % 2][0][:, :, :], [s5.pack[sc % 2][1]], [k.dbuf["tabscr"]])
            XR, XI, T1, T2 = [b_[0] for b_ in big[0:4]]
            XR_b, XI_b, T1_b, T2_b = [b_[1] for b_ in big[0:4]]
            QR_, QI_, QR_b, QI_b = XR, XI, XR_b, XI_b
            v3 = lambda t: t[:, :].rearrange("p (c t) -> p c t", t=S5C)
            bc = lambda n: tab[n][0][:, :].unsqueeze(1).to_broadcast([128, S5NCH, S5C])
            for ti, (c0, w) in enumerate(tiles):
                pbr, pbr_b = ps56()
                pbi, pbi_b = ps56()
                if q < 3:
                    op_mm(k, pbr[:, 0:w], s5.LB[0][0][qP, ec, :], U[qP, c0:c0 + w], True, True, [s5.LB[0][1], U_b.sub(ti)], [pbr_b])
                    op_mm(k, pbi[:, 0:w], s5.LB[1][0][qP, ec, :], U[qP, c0:c0 + w], True, True, [s5.LB[1][1], U_b.sub(ti)], [pbi_b])
                else:
                    op_mm(k, pbr[:, 0:w], s5.LB[0][0][64:128, ec, :], Uz[64:128, c0:c0 + w], True, True, [s5.LB[0][1], Uz_b], [pbr_b])
                    op_mm(k, pbi[:, 0:w], s5.LB[1][0][64:128, ec, :], Uz[64:128, c0:c0 + w], True, True, [s5.LB[1][1], Uz_b], [pbi_b])
                if c0 < T:
                    P.op('act', lambda e, c0=c0, pbr=pbr: e.activation(out=XR[:, c0:c0 + 512], in_=pbr[:, :], func=AF.Copy), [pbr_b], [XR_b])
                    P.op('act', lambda e, c0=c0, pbi=pbi: e.activation(out=XI[:, c0:c0 + 512], in_=pbi[:, :], func=AF.Copy), [pbi_b], [XI_b])
                else:
                    s0r, s0i = s5.s0T[0], s5.s0T[1]
                    tr_, ti__ = s5.tmpn[0], s5.tmpn[1]
                    op_stt(k, 'dve', tr_[0][:, :], s0r[0][:, sc, :], A("are")[:, col], pbr[:, 0:NS], ALU.mult, ALU.add,
                           [s0r[1], Bf("are"), pbr_b], [tr_[1]])
                    op_ts(k, 'dve', s5.g[1][0][:, 0:NS], s0i[0][:, sc, :], A("aim")[:, col], None, ALU.mult, None, [s0i[1], Bf("aim")], [s5.g[1][1]])
                    op_tt(k, 'dve', tr_[0][:, :], tr_[0][:, :], s5.g[1][0][:, 0:NS], ALU.subtract, [tr_[1], s5.g[1][1]], [tr_[1]])
                    op_stt(k, 'dve', ti__[0][:, :], s0i[0][:, sc, :], A("are")[:, col], pbi[:, 0:NS], ALU.mult, ALU.add,
                           [s0i[1], Bf("are"), pbi_b], [ti__[1]])
                    op_stt(k, 'dve', ti__[0][:, :], s0r[0][:, sc, :], A("aim")[:, col], ti__[0][:, :], ALU.mult, ALU.add,
                           [s0r[1], Bf("aim"), ti__[1]], [ti__[1]])
                    P.op('dve', lambda e, sc=sc: e.tensor_copy(out=s0r[0][:, sc, :], in_=tr_[0][:, :]), [tr_[1]], [s0r[1]])
                    P.op('dve', lambda e, sc=sc: e.tensor_copy(out=s0i[0][:, sc, :], in_=ti__[0][:, :]), [ti__[1]], [s0i[1]])
                    op_act(k, s5.sbf[0][0][:, T:T + NS], tr_[0][:, :], AF.Copy, [tr_[1]], [s5.sbf[0][1].sub(4)])
                    op_act(k, s5.sbf[1][0][:, T:T + NS], ti__[0][:, :], AF.Copy, [ti__[1]], [s5.sbf[1][1].sub(4)])
            op_tt(k, 'dve', v3(T1), v3(XR), bc("Fc"), ALU.mult, [XR_b, TB("Fc")], [T1_b])
            op_tt(k, 'pool', v3(T2), v3(XI), bc("Fs"), ALU.mult, [XI_b, TB("Fs")], [T2_b])
            op_tt(k, 'dve', v3(T1), v3(T1), v3(T2), ALU.add, [T1_b, T2_b], [T1_b])
            op_tt(k, 'pool', v3(T2), v3(XI), bc("Fc"), ALU.mult, [XI_b, TB("Fc")], [T2_b])
            op_tt(k, 'dve', v3(XI), v3(XR), bc("Fs"), ALU.mult, [XR_b, TB("Fs")], [XI_b])
            op_tt(k, 'pool', v3(T2), v3(T2), v3(XI), ALU.subtract, [T2_b, XI_b], [T2_b])
            m01 = s5.m01
            P.op('dve', lambda e: e.tensor_tensor_scan(out=QR_[:], data0=m01[0][:], data1=T1[:], initial=0.0, op0=ALU.mult, op1=ALU.add),
                 [m01[1], T1_b], [QR_b])
            P.op('dve', lambda e: e.tensor_tensor_scan(out=QI_[:], data0=m01[0][:], data1=T2[:], initial=0.0, op0=ALU.mult, op1=ALU.add),
                 [m01[1], T2_b], [QI_b])
            EE = s5.ec_
            e = lambda n: EE[n][0][:, 0:S5NCH]
            eb = lambda n: EE[n][1]
            qr_end = v3(QR_)[:, :, S5C - 1]
            qi_end = v3(QI_)[:, :, S5C - 1]
            bc63 = tab["Bc"][0][:, S5C - 1:S5C]
            bs63 = tab["Bs"][0][:, S5C - 1:S5C]
            op_ts(k, 'dve', e("er"), qr_end, bc63, None, ALU.mult, None, [QR_b, TB("Bc")], [eb("er")])
            op_ts(k, 'dve', e("hr"), qi_end, bs63, None, ALU.mult, None, [QI_b, TB("Bs")], [eb("hr")])
            op_tt(k, 'dve', e("er"), e("er"), e("hr"), ALU.subtract, [eb("er"), eb("hr")], [eb("er")])
            op_ts(k, 'dve', e("ei"), qr_end, bs63, None, ALU.mult, None, [QR_b, TB("Bs")], [eb("ei")])
            op_ts(k, 'dve', e("hr"), qi_end, bc63, None, ALU.mult, None, [QI_b, TB("Bc")], [eb("hr")])
            op_tt(k, 'dve', e("ei"), e("ei"), e("hr"), ALU.add, [eb("ei"), eb("hr")], [eb("ei")])
            Gc = tab["Gc"][0][:, 0:S5NCH]
            Gs = tab["Gs"][0][:, 0:S5NCH]
            op_tt(k, 'dve', e("hr"), e("er"), Gc, ALU.mult, [eb("er"), TB("Gc")], [eb("hr")])
            op_tt(k, 'dve', e("hi"), e("ei"), Gs, ALU.mult, [eb("ei"), TB("Gs")], [eb("hi")])
            op_tt(k, 'dve', e("hr"), e("hr"), e("hi"), ALU.add, [eb("hr"), eb("hi")], [eb("hr")])
            op_tt(k, 'dve', e("hi"), e("ei"), Gc, ALU.mult, [eb("ei"), TB("Gc")], [eb("hi")])
            op_tt(k, 'dve', e("cr"), e("er"), Gs, ALU.mult, [eb("er"), TB("Gs")], [eb("cr")])
            op_tt(k, 'dve', e("hi"), e("hi"), e("cr"), ALU.subtract, [eb("hi"), eb("cr")], [eb("hi")])
            op_ts(k, 'dve', e("ci"), Gc, 0.0, A("m64")[:, col], ALU.mult, ALU.add, [TB("Gc"), Bf("m64")], [eb("ci")])
            P.op('dve', lambda e_: e_.tensor_tensor_scan(out=e("Er"), data0=e("ci"), data1=e("hr"), initial=0.0, op0=ALU.mult, op1=ALU.add),
                 [eb("ci"), eb("hr")], [eb("Er")])
            P.op('dve', lambda e_: e_.tensor_tensor_scan(out=e("Ei"), data0=e("ci"), data1=e("hi"), initial=0.0, op0=ALU.mult, op1=ALU.add),
                 [eb("ci"), eb("hi")], [eb("Ei")])
            op_tt(k, 'dve', e("hr"), e("Er"), Gc, ALU.mult, [eb("Er"), TB("Gc")], [eb("hr")])
            op_tt(k, 'dve', e("cr"), e("Ei"), Gs, ALU.mult, [eb("Ei"), TB("Gs")], [eb("cr")])
            op_tt(k, 'dve', e("hr"), e("hr"), e("cr"), ALU.subtract, [eb("hr"), eb("cr")], [eb("hr")])
            op_tt(k, 'dve', e("hi"), e("Er"), Gs, ALU.mult, [eb("Er"), TB("Gs")], [eb("hi")])
            op_tt(k, 'dve', e("cr"), e("Ei"), Gc, ALU.mult, [eb("Ei"), TB("Gc")], [eb("cr")])
            op_tt(k, 'dve', e("hi"), e("hi"), e("cr"), ALU.add, [eb("hi"), eb("cr")], [eb("hi")])
            P.op('dve', lambda e_, sc=sc: e_.tensor_copy(out=s5.fin[0][0][:, sc:sc + 1], in_=EE["hr"][0][:, S5NCH - 1:S5NCH]), [eb("hr")], [s5.fin[0][1]])
            P.op('dve', lambda e_, sc=sc: e_.tensor_copy(out=s5.fin[1][0][:, sc:sc + 1], in_=EE["hi"][0][:, S5NCH - 1:S5NCH]), [eb("hi")], [s5.fin[1][1]])
            n1 = S5NCH - 1
            op_ts(k, 'dve', EE["cr"][0][:, 0:n1], EE["hr"][0][:, 0:n1], A("are")[:, col], None, ALU.mult, None, [eb("hr"), Bf("are")], [eb("cr")])
            op_ts(k, 'dve', EE["ci"][0][:, 0:n1], EE["hi"][0][:, 0:n1], A("aim")[:, col], None, ALU.mult, None, [eb("hi"), Bf("aim")], [eb("ci")])
            op_tt(k, 'dve', EE["cr"][0][:, 0:n1], EE["cr"][0][:, 0:n1], EE["ci"][0][:, 0:n1], ALU.subtract, [eb("cr"), eb("ci")], [eb("cr")])
            op_ts(k, 'dve', EE["ci"][0][:, 0:n1], EE["hr"][0][:, 0:n1], A("aim")[:, col], None, ALU.mult, None, [eb("hr"), Bf("aim")], [eb("ci")])
            op_ts(k, 'dve', EE["er"][0][:, 0:n1], EE["hi"][0][:, 0:n1], A("are")[:, col], None, ALU.mult, None, [eb("hi"), Bf("are")], [eb("er")])
            op_tt(k, 'dve', EE["ci"][0][:, 0:n1], EE["ci"][0][:, 0:n1], EE["er"][0][:, 0:n1], ALU.add, [eb("ci"), eb("er")], [eb("ci")])
            op_tt(k, 'dve', v3(T1)[:, 1:S5NCH, 0], v3(T1)[:, 1:S5NCH, 0], EE["cr"][0][:, 0:n1], ALU.add, [T1_b, eb("cr")], [T1_b])
            op_tt(k, 'dve', v3(T2)[:, 1:S5NCH, 0], v3(T2)[:, 1:S5NCH, 0], EE["ci"][0][:, 0:n1], ALU.add, [T2_b, eb("ci")], [T2_b])
            P.op('dve', lambda e_: e_.tensor_tensor_scan(out=QR_[:], data0=m01[0][:], data1=T1[:], initial=0.0, op0=ALU.mult, op1=ALU.add),
                 [m01[1], T1_b], [QR_b])
            P.op('dve', lambda e_: e_.tensor_tensor_scan(out=QI_[:], data0=m01[0][:], data1=T2[:], initial=0.0, op0=ALU.mult, op1=ALU.add),
                 [m01[1], T2_b], [QI_b])
            op_tt(k, 'dve', v3(T1), v3(QR_), bc("Bc"), ALU.mult, [QR_b, TB("Bc")], [T1_b])
            op_tt(k, 'pool', v3(T2), v3(QI_), bc("Bs"), ALU.mult, [QI_b, TB("Bs")], [T2_b])
            op_tt(k, 'dve', s5.sbf[0][0][:, 0:T].rearrange("p (c t) -> p c t", t=S5C), v3(T1), v3(T2), ALU.subtract, [T1_b, T2_b],
                  [s5.sbf[0][1].sub(0)])
            op_tt(k, 'pool', v3(T1), v3(QR_), bc("Bs"), ALU.mult, [QR_b, TB("Bs")], [T1_b])
            op_tt(k, 'dve', v3(T2), v3(QI_), bc("Bc"), ALU.mult, [QI_b, TB("Bc")], [T2_b])
            op_tt(k, 'pool', s5.sbf[1][0][:, 0:T].rearrange("p (c t) -> p c t", t=S5C), v3(T1), v3(T2), ALU.add, [T1_b, T2_b],
                  [s5.sbf[1][1].sub(0)])
            for ti, (c0, w) in enumerate(tiles):
                if q == 0:
                    py[ti] = k.ps[ti]
                pyt, pyt_b = py[ti]
                rs_ = [s5.sbf[0][1].sub(0 if c0 < T else 4), s5.sbf[1][1].sub(0 if c0 < T else 4)]
                if q < 2:
                    op_mm(k, pyt[qP, 0:w], s5.LC[0][0][:, ec, qP], s5.sbf[0][0][:, c0:c0 + w], True, False, [s5.LC[0][1]] + rs_, [pyt_b])
                    op_mm(k, pyt[qP, 0:w], s5.LC[1][0][:, ec, qP], s5.sbf[1][0][:, c0:c0 + w], False, True, [s5.LC[1][1]] + rs_, [pyt_b])
                elif q == 3:
                    op_mm(k, pyt[64:128, 0:w], s5.LC3[0][0][:, ec, :], s5.sbf[0][0][:, c0:c0 + w], True, False, [s5.LC3[0][1]] + rs_, [pyt_b])
                    op_mm(k, pyt[64:128, 0:w], s5.LC3[1][0][:, ec, :], s5.sbf[1][0][:, c0:c0 + w], False, False, [s5.LC3[1][1]] + rs_, [pyt_b])
                else:
                    op_mm(k, pyt[64:96, 0:w], s5.LC[0][0][:, ec, 64:96], s5.sbf[0][0][:, c0:c0 + w], False, False, [s5.LC[0][1]] + rs_, [pyt_b])
                    op_mm(k, pyt[64:96, 0:w], s5.LC[1][0][:, ec, 64:96], s5.sbf[1][0][:, c0:c0 + w], False, True, [s5.LC[1][1]] + rs_, [pyt_b])
            P.chk("s5_sc")
        for ti, (c0, w) in enumerate(tiles):
            pyt, pyt_b = py[ti]
            y_, y_b, _ = s5.g[0]
            t_, t_b, _ = s5.g[1]
            s_, s_b, _ = s5.g[2]
            op_stt(k, 'dve', y_[:, 0:w], U[:, c0:c0 + w], s5.par["d"][0][:, ec:ec + 1], pyt[:, 0:w], ALU.mult, ALU.add,
                   [U_b.sub(ti), s5.par["d"][1], pyt_b], [y_b])
            op_act(k, t_[:, 0:w], y_[:, 0:w], AF.Square, [y_b], [t_b])
            op_ts(k, 'dve', t_[:, 0:w], t_[:, 0:w], 0.044715, 1.0, ALU.mult, ALU.add, [t_b], [t_b])
            op_tt(k, 'dve', t_[:, 0:w], t_[:, 0:w], y_[:, 0:w], ALU.mult, [t_b, y_b], [t_b])
            op_act(k, s_[:, 0:w], t_[:, 0:w], AF.Sigmoid, [t_b], [s_b], scale=1.5957691216057308)
            op_tt(k, 'dve', k.GT[:, ec, c0:c0 + w], y_[:, 0:w], s_[:, 0:w], ALU.mult, [y_b, s_b], [k.GT_b.sub(ec).sub(ti)])
        P.chk("s5_ec")
    P.barrier()
    for i, nm in ((0, "sre_p"), (1, "sim_p")):
        for g2 in range(2):
            op_dma(k, 'sp', dr[nm][ps_].rearrange("(c g2) p -> g2 p c", g2=2)[g2], s5.fin[i][0][g2 * 64:(g2 + 1) * 64, :],
                   [s5.fin[i][1]], [k.dbuf[nm]], slow=True)
    if ps_ == 0:
        for i, nm in ((0, "sre_s"), (1, "sim_s")):
            for q4 in range(4):
                sr, sr_b, _ = s5.srow
                for j in range(16):
                    sc = q4 * 16 + j
                    if j % 4 == 0:
                        pt, pt_b = k.psn()
                    op_tr(k, pt[0:NS, (j % 4) * 128:(j % 4 + 1) * 128], s5.s0T[i][0][:, sc, :], k.identF[:], [s5.s0T[i][1], k.identF_b], [pt_b])
                    if j % 4 == 3:
                        P.op('dve', lambda e, j=j, pt=pt: e.tensor_copy(out=sr[0:NS, (j - 3) * 128:(j + 1) * 128], in_=pt[0:NS, 0:512]),
                             [pt_b], [sr_b])
                op_dma(k, 'sp', dr[nm].rearrange("s g p -> s (g p)")[:, q4 * 2048:(q4 + 1) * 2048], sr[0:NS, :], [sr_b], [k.dbuf[nm]])
    P.barrier()
    gscr, gscr_b = dr["gscr"], k.dbuf["gscr"]
    for e2 in range(16):
        wg, wg_b, _ = s5.wg
        op_dma(k, 'pool', wg[:, :, :], dr["d_w_glu"][:, e2 * 128:(e2 + 1) * 128].rearrange("(c p) e -> p c e", p=128), [k.dbuf["d_w_glu"]], [wg_b])
        load_w_cols(k, s5.wt[0], s5.wt[1], d_w_in, d_w_in_b, E + e2 * 128)
        for ti, (c0, w) in enumerate(tiles):
            pg, pg_b = k.psn()
            for ec in range(16):
                op_mm(k, pg[:, 0:w], wg[:, ec, :], k.GT[:, ec, c0:c0 + w], ec == 0, ec == 15, [wg_b, k.GT_b.sub(ec).sub(ti)], [pg_b])
            pz, pz_b = k.psn()
            for kk in range(8):
                op_mm(k, pz[:, 0:w], s5.wt[0][:, kk, :], k.hT[:, kk, c0 + 1:c0 + 1 + w], kk == 0, kk == 7,
                      [s5.wt[1]] + hT_reads(k, c0, w), [pz_b])
            s_, s_b, _ = s5.g[0]
            z_, z_b, _ = s5.g[1]
            op_act(k, s_[:, 0:w], pg[:, 0:w], AF.Sigmoid, [pg_b, s5.par["bg"][1]], [s_b], bias=s5.par["bg"][0][:, e2:e2 + 1])
            op_act(k, z_[:, 0:w], pz[:, 0:w], AF.Silu, [pz_b], [z_b])
            op_tt(k, 'dve', s_[:, 0:w], s_[:, 0:w], k.GT[:, e2, c0:c0 + w], ALU.mult, [s_b, k.GT_b.sub(e2).sub(ti)], [s_b])
            gb, gb_b, _ = s5.gb
            op_tt(k, 'dve', gb[:, 0:w], s_[:, 0:w], z_[:, 0:w], ALU.mult, [s_b, z_b], [gb_b])
            op_dma(k, 'sp', gscr[e2, :, c0:c0 + w], gb[:, 0:w], [gb_b], [gscr_b])
    P.barrier()
    TW = TWA if ps_ == 0 else T
    for e2 in range(16):
        op_dma(k, 'sp', k.GT[:, e2, 0:TW], gscr[e2, :, 0:TW], [gscr_b], [k.GT_b])
N_LAYERS = 4

_CACHE = {}


def kernel(**inp):
    n_layers = N_LAYERS
    N_IN = N_IN_BY_LAYER[n_layers]
    if "nc" not in _CACHE:
        _CACHE["nc"] = build(n_layers)
    nc, k = _CACHE["nc"]
    f = lambda a: np.ascontiguousarray(np.asarray(a))
    shared = {}
    for name, shp, dt in IN_SHAPES[:N_IN]:
        if name in ("xp", "xs", "state_conv", "state_shift", "state_wkv", "state_ssm_re", "state_ssm_im", "page_table"):
            continue
        if name == "cache_cat":
            shared[name] = np.concatenate([np.asarray(inp["cache_krope"]), np.asarray(inp["cache_ckv"])], axis=-1)
            continue
        shared[name] = f(inp[name])
    in_maps = []
    for c in range(NCORES):
        m = dict(shared)
        m["xp"] = f(inp["x_prompt"][2 * c:2 * c + 2])
        m["xs"] = f(inp["x_sample"][NS * c:NS * (c + 1), 0, :])
        for nm in ("state_conv", "state_shift", "state_wkv", "state_ssm_re", "state_ssm_im", "page_table"):
            if any(nm == x[0] for x in IN_SHAPES[:N_IN]):
                m[nm] = f(inp[nm][NS * c:NS * (c + 1)])
        in_maps.append(m)
    res = run_bass_kernel_spmd(nc, in_maps, core_ids=list(range(NCORES)))
    R = res.results
    cat = lambda nm: np.concatenate([np.asarray(R[c][nm]) for c in range(NCORES)], axis=0)
    y_p = cat("y_p")
    y_s = cat("y_s").reshape(128, 1, D)
    outs = (y_p, y_s, cat("conv_p"), cat("conv_s"), cat("shift_p"), cat("shift_s"), cat("wkv_p"), cat("wkv_s"),
            cat("ckv_p"), cat("ckv_s").reshape(128, 1, 256), cat("kr_p"), cat("kr_s").reshape(128, 1, 64),
            cat("sre_p"), cat("sre_s"), cat("sim_p"), cat("sim_s"))
    return tuple(np.ascontiguousarray(o, dtype=np.float32) for o in outs)
```
